# Optimizing a Trainium2 kernel written in Bass

```python
import math
import jax, jax.numpy as jnp
from jax import lax
import numpy as np

D_MODEL = 1024
BATCH = 8
SEQ = 4096
DEPTH = 1

GRID_W = 64
CTX_LEN = 256
NORM_EPS = 1e-6

N_HEADS = 8
HEAD_DIM = 64
V_DIM = 2 * HEAD_DIM
ATTN_QK_W = N_HEADS * 2 * HEAD_DIM
ATTN_V_W = N_HEADS * V_DIM
ROPE_AXIS_DIM = HEAD_DIM // 2
ROPE_THETA = 10000.0
Q_BLOCK = 128

HYENA_W = 1024
HYENA_ORDER = 2
FILTER_EMB = 33
FILTER_BANDS = (FILTER_EMB - 1) // 2
FILTER_HIDDEN = 64
DECAY_TARGET = 1e-2
FAST_DECAY_PCT = 0.3
SLOW_DECAY_PCT = 1.5

K_OFF = 0
V_OFF = K_OFF + ATTN_QK_W
Q_OFF = V_OFF + ATTN_V_W
HY_OFF = Q_OFF + ATTN_QK_W
GATE_OFF = HY_OFF + 3 * HYENA_W
IN_W = GATE_OFF + 2 * D_MODEL

N_EXPERTS = 64
N_GROUPS = 8
TOPK_GROUPS = 4
TOP_K = 8
EXPERT_HIDDEN = 256
SHARED_HIDDEN = 256
ROUTED_SCALE = 2.5
MOE_BLOCK = 128

kernel_name = "hybrid_diffattn_hyena_moe_dit"


def rmsnorm(x, g):
    xf = x.astype(jnp.float32)
    y = xf * lax.rsqrt(jnp.mean(xf * xf, axis=-1, keepdims=True) + NORM_EPS)
    return (y * g.astype(jnp.float32)).astype(x.dtype)


def adaln(cond, w, b, n_chunks):
    m = jax.nn.silu(cond) @ w[:, :n_chunks * D_MODEL] + b[:n_chunks * D_MODEL]
    return jnp.split(m[..., None, :], n_chunks, axis=-1)


def modulate(h, shift, scale):
    return h * (1 + scale) + shift


def rope_2d(rows):
    f32 = jnp.float32
    row = jnp.repeat(jnp.arange(rows), GRID_W).astype(f32)
    col = jnp.tile(jnp.arange(GRID_W), rows).astype(f32)
    inv = ROPE_THETA ** (-jnp.arange(0, ROPE_AXIS_DIM, 2, dtype=f32) / ROPE_AXIS_DIM)
    ang = jnp.stack([row[:, None] * inv, col[:, None] * inv], axis=1)
    return jnp.cos(ang), jnp.sin(ang)


def apply_rope_2d(x, cos, sin):
    L = x.shape[1]
    xa = x.reshape(*x.shape[:-1], 2, 2, ROPE_AXIS_DIM // 2)
    x1, x2 = xa[..., 0, :], xa[..., 1, :]
    c = cos[None, :, None, None]
    s = sin[None, :, None, None]
    out = jnp.stack([x1 * c - x2 * s, x2 * c + x1 * s], axis=-2)
    return out.reshape(x.shape).astype(x.dtype)


def qk_heads(p):
    return p.reshape(*p.shape[:-1], N_HEADS, 2, HEAD_DIM)


def v_heads(p):
    return p.reshape(*p.shape[:-1], N_HEADS, V_DIM)


def diff_attend(q, k, v, lam):
    s = jnp.einsum('bqhmd,bkhmd->bhmqk', q, k).astype(jnp.float32) * (HEAD_DIM ** -0.5)
    p = jax.nn.softmax(s, axis=-1)
    a = p[:, :, 0] - lam * p[:, :, 1]
    return jnp.einsum('bhqk,bkhe->bqhe', a.astype(v.dtype), v)


def diff_attention_blocked(q, k_all, v_all, lam):
    B, L = q.shape[:2]
    nb = L // Q_BLOCK
    qb = jnp.moveaxis(q.reshape(B, nb, Q_BLOCK, *q.shape[2:]), 1, 0)
    o = lax.map(lambda qq: diff_attend(qq, k_all, v_all, lam), qb)
    return jnp.moveaxis(o, 0, 1).reshape(B, L, N_HEADS, V_DIM)


def diff_sub_norm(o, g, lam_init):
    o = rmsnorm(o, g) * (1 - lam_init)
    return o.reshape(*o.shape[:-2], N_HEADS * V_DIM)


def implicit_filters(n, w1, b1, freq, w2, b2, w3):
    f32 = jnp.float32
    pos = jnp.arange(n, dtype=f32)[:, None]
    t = jnp.linspace(0.0, 1.0, n, dtype=f32)[:, None]
    w = 2 * math.pi * pos / n
    bands = jnp.linspace(1e-4, FILTER_BANDS - 1, FILTER_BANDS, dtype=f32)
    z = jnp.concatenate([t, jnp.cos(bands * w), -jnp.sin(bands * w)], axis=-1)
    fr = freq.astype(f32)
    hdn = jnp.sin(fr * (z @ w1.astype(f32) + b1.astype(f32)))
    hdn = jnp.sin(fr * (hdn @ w2.astype(f32) + b2.astype(f32)))
    h = (hdn @ w3.astype(f32)).reshape(n, HYENA_ORDER, 2, HYENA_W)
    deltas = jnp.abs(jnp.linspace(math.log(DECAY_TARGET) / SLOW_DECAY_PCT,
                                  math.log(DECAY_TARGET) / FAST_DECAY_PCT, HYENA_W, dtype=f32))
    h = h * jnp.exp(-t[:, :, None, None] * deltas)
    kern = jnp.concatenate([h[:, :, 0], jnp.zeros((1, HYENA_ORDER, HYENA_W), f32), h[:0:-1, :, 1]], axis=0)
    return kern / jnp.sum(jnp.abs(kern), axis=0, keepdims=True)


def long_conv(z, kern, bias):
    n = z.shape[1]
    zf32 = z.astype(jnp.float32)
    zf = jnp.fft.rfft(zf32, n=2 * n, axis=1)
    kf = jnp.fft.rfft(kern, axis=0)
    y = jnp.fft.irfft(zf * kf[None], n=2 * n, axis=1)[:, :n]
    return (y + zf32 * bias.astype(jnp.float32)).astype(z.dtype)


def hyena(u, conv_w, conv_b, filt, hy_bias):
    n = u.shape[1]
    up = jnp.pad(u, ((0, 0), (1, 1), (0, 0)))
    u = up[:, :-2] * conv_w[0] + up[:, 1:-1] * conv_w[1] + up[:, 2:] * conv_w[2] + conv_b
    v, x1, x2 = jnp.split(u, 3, axis=-1)
    kern = implicit_filters(n, *filt)
    z = x1 * long_conv(v, kern[:, 0], hy_bias[0])
    return x2 * long_conv(z, kern[:, 1], hy_bias[1])


def merge_branches(attn, hy, gates, w_pa, w_ph, w_out):
    g_a, g_h = jnp.split(gates, 2, axis=-1)
    y = jax.nn.sigmoid(g_a) * (attn @ w_pa) + jax.nn.sigmoid(g_h) * (hy @ w_ph)
    return y @ w_out


def swiglu(t, w_g, w_u, w_d):
    return (jax.nn.silu(t @ w_g) * (t @ w_u)) @ w_d


def moe(h, router_w, router_bias, w_g, w_u, w_d, s_g, s_u, s_d):
    B_, L_, D = h.shape
    t = h.reshape(-1, D)
    N = t.shape[0]
    scores = jax.nn.sigmoid((t @ router_w).astype(jnp.float32))
    choice = scores + router_bias.astype(jnp.float32)
    grp = choice.reshape(N, N_GROUPS, N_EXPERTS // N_GROUPS)
    grp_score = lax.top_k(grp, 2)[0].sum(-1)
    _, gidx = lax.top_k(grp_score, TOPK_GROUPS)
    gmask = jax.nn.one_hot(gidx, N_GROUPS, dtype=jnp.float32).sum(1) > 0
    masked = jnp.where(jnp.repeat(gmask, N_EXPERTS // N_GROUPS, axis=1), choice, -jnp.inf)
    _, eidx = lax.top_k(masked, TOP_K)
    wsel = jnp.take_along_axis(scores, eidx, axis=1)
    wsel = wsel / jnp.sum(wsel, axis=-1, keepdims=True) * ROUTED_SCALE
    NK = N * TOP_K
    flat_e = eidx.reshape(-1)
    flat_tok = jnp.repeat(jnp.arange(N, dtype=jnp.int32), TOP_K)
    flat_w = wsel.reshape(-1)
    order = jnp.argsort(flat_e)
    sorted_e = flat_e[order]
    counts = jnp.zeros((N_EXPERTS,), jnp.int32).at[flat_e].add(1)
    padded = (counts + MOE_BLOCK - 1) // MOE_BLOCK * MOE_BLOCK
    pad_end = jnp.cumsum(padded)
    pad_start = pad_end - padded
    start = jnp.cumsum(counts) - counts
    dest = pad_start[sorted_e] + jnp.arange(NK, dtype=jnp.int32) - start[sorted_e]
    NB = -(-(NK + N_EXPERTS * (MOE_BLOCK - 1)) // MOE_BLOCK)
    P = NB * MOE_BLOCK
    row_tok = jnp.full((P,), N, jnp.int32).at[dest].set(flat_tok[order])
    row_w = jnp.zeros((P,), jnp.float32).at[dest].set(flat_w[order])
    blk_e = jnp.minimum(jnp.searchsorted(pad_end, jnp.arange(NB, dtype=jnp.int32) * MOE_BLOCK, side='right'),
                        N_EXPERTS - 1)
    t_pad = jnp.concatenate([t, jnp.zeros((1, D), t.dtype)], axis=0)

    def expert_block(args):
        tok, e = args
        xb = jnp.take(t_pad, tok, axis=0)
        return swiglu(xb, w_g[e], w_u[e], w_d[e])

    yb = lax.map(expert_block, (row_tok.reshape(NB, MOE_BLOCK), blk_e))
    routed = jnp.zeros((N + 1, D), jnp.float32).at[row_tok].add(
        yb.reshape(P, D).astype(jnp.float32) * row_w[:, None])[:N]
    out = routed.astype(t.dtype) + swiglu(t, s_g, s_u, s_d)
    return out.reshape(B_, L_, D)


def setup_inputs(seed: int = 0) -> dict:
    key = jax.random.key(seed)
    ks = jax.random.split(key, 40)
    f32 = jnp.float32
    D, Lr = D_MODEL, DEPTH

    def nrm(i, shape, scale):
        return jax.random.normal(ks[i], shape, f32) * scale

    return {
        "x": nrm(0, (BATCH, SEQ, D), 1.0),
        "c": nrm(1, (BATCH, D), 1.0),
        "ctx": nrm(2, (BATCH, CTX_LEN, D), 1.0),
        "c_ctx": nrm(3, (D,), 1.0),
        "ada_w": nrm(4, (Lr, D, 6 * D), 0.5 * D ** -0.5),
        "ada_b": nrm(5, (Lr, 6 * D), 0.01),
        "norm1_g": 1.0 + nrm(6, (Lr, D), 0.02),
        "norm2_g": 1.0 + nrm(7, (Lr, D), 0.02),
        "w_in": nrm(8, (Lr, D, IN_W), D ** -0.5),
        "lam_q1": nrm(9, (Lr, HEAD_DIM), 0.1),
        "lam_k1": nrm(10, (Lr, HEAD_DIM), 0.1),
        "lam_q2": nrm(11, (Lr, HEAD_DIM), 0.1),
        "lam_k2": nrm(12, (Lr, HEAD_DIM), 0.1),
        "subln_g": 1.0 + nrm(13, (Lr, V_DIM), 0.02),
        "hy_conv_w": nrm(14, (Lr, 3, 3 * HYENA_W), 3 ** -0.5),
        "hy_conv_b": nrm(15, (Lr, 3 * HYENA_W), 0.01),
        "filt_w1": nrm(16, (Lr, FILTER_EMB, FILTER_HIDDEN), FILTER_EMB ** -0.5),
        "filt_b1": nrm(17, (Lr, FILTER_HIDDEN), 0.1),
        "filt_freq": 1.0 + nrm(18, (Lr, FILTER_HIDDEN), 0.02),
        "filt_w2": nrm(19, (Lr, FILTER_HIDDEN, FILTER_HIDDEN), FILTER_HIDDEN ** -0.5),
        "filt_b2": nrm(20, (Lr, FILTER_HIDDEN), 0.1),
        "filt_w3": nrm(21, (Lr, FILTER_HIDDEN, HYENA_ORDER * 2 * HYENA_W), FILTER_HIDDEN ** -0.5),
        "hy_bias": nrm(22, (Lr, HYENA_ORDER, HYENA_W), 1.0),
        "w_branch_attn": nrm(23, (Lr, ATTN_V_W, D), ATTN_V_W ** -0.5),
        "w_branch_hyena": nrm(24, (Lr, HYENA_W, D), HYENA_W ** -0.5),
        "w_out": nrm(25, (Lr, D, D), D ** -0.5),
        "router_w": nrm(26, (Lr, D, N_EXPERTS), D ** -0.5),
        "router_bias": nrm(27, (Lr, N_EXPERTS), 0.01),
        "exp_w_gate": nrm(28, (Lr, N_EXPERTS, D, EXPERT_HIDDEN), D ** -0.5),
        "exp_w_up": nrm(29, (Lr, N_EXPERTS, D, EXPERT_HIDDEN), D ** -0.5),
        "exp_w_down": nrm(30, (Lr, N_EXPERTS, EXPERT_HIDDEN, D), EXPERT_HIDDEN ** -0.5),
        "shared_w_gate": nrm(31, (Lr, D, SHARED_HIDDEN), D ** -0.5),
        "shared_w_up": nrm(32, (Lr, D, SHARED_HIDDEN), D ** -0.5),
        "shared_w_down": nrm(33, (Lr, SHARED_HIDDEN, D), SHARED_HIDDEN ** -0.5),
        "final_norm_g": 1.0 + nrm(34, (D,), 0.02),
    }


def reference(x, c, ctx, c_ctx, ada_w, ada_b, norm1_g, norm2_g, w_in, lam_q1, lam_k1, lam_q2, lam_k2,
              subln_g, hy_conv_w, hy_conv_b, filt_w1, filt_b1, filt_freq, filt_w2, filt_b2, filt_w3, hy_bias,
              w_branch_attn, w_branch_hyena, w_out, router_w, router_bias, exp_w_gate, exp_w_up, exp_w_down,
              shared_w_gate, shared_w_up, shared_w_down, final_norm_g):
    f32 = jnp.float32
    ROWS = x.shape[1] // GRID_W
    cos, sin = rope_2d(ROWS)
    for layer in range(DEPTH):
        last = layer == DEPTH - 1
        lam_init = 0.8 - 0.6 * math.exp(-0.3 * layer)
        lam = (jnp.exp(jnp.sum(lam_q1[layer].astype(f32) * lam_k1[layer].astype(f32)))
               - jnp.exp(jnp.sum(lam_q2[layer].astype(f32) * lam_k2[layer].astype(f32))) + lam_init)
        filt = (filt_w1[layer], filt_b1[layer], filt_freq[layer], filt_w2[layer], filt_b2[layer], filt_w3[layer])
        sh1, sc1, g1, sh2, sc2, g2 = adaln(c, ada_w[layer], ada_b[layer], 6)
        cmod = adaln(c_ctx, ada_w[layer], ada_b[layer], 2 if last else 6)
        w = w_in[layer]

        hx = modulate(rmsnorm(x, norm1_g[layer]), sh1, sc1)
        hc = modulate(rmsnorm(ctx, norm1_g[layer]), cmod[0], cmod[1])
        px = hx @ w
        pc_kv = hc @ w[:, :Q_OFF]
        kc = qk_heads(pc_kv[..., K_OFF:V_OFF])
        vc = v_heads(pc_kv[..., V_OFF:Q_OFF])
        kx = apply_rope_2d(qk_heads(px[..., K_OFF:V_OFF]), cos, sin)
        vx = v_heads(px[..., V_OFF:Q_OFF])
        qx = apply_rope_2d(qk_heads(px[..., Q_OFF:HY_OFF]), cos, sin)
        k_all = jnp.concatenate([kx, kc], axis=1)
        v_all = jnp.concatenate([vx, vc], axis=1)
        ax = diff_sub_norm(diff_attention_blocked(qx, k_all, v_all, lam), subln_g[layer], lam_init)
        yx = hyena(px[..., HY_OFF:GATE_OFF], hy_conv_w[layer], hy_conv_b[layer], filt, hy_bias[layer])
        mix_x = merge_branches(ax, yx, px[..., GATE_OFF:IN_W], w_branch_attn[layer], w_branch_hyena[layer],
                               w_out[layer])
        if not last:
            pc_rest = hc @ w[:, Q_OFF:]
            qc = qk_heads(pc_rest[..., :HY_OFF - Q_OFF])
            ac = diff_sub_norm(diff_attend(qc, kc, vc, lam), subln_g[layer], lam_init)
            yc = hyena(pc_rest[..., HY_OFF - Q_OFF:GATE_OFF - Q_OFF], hy_conv_w[layer], hy_conv_b[layer], filt,
                       hy_bias[layer])
            ctx = ctx + cmod[2] * merge_branches(ac, yc, pc_rest[..., GATE_OFF - Q_OFF:], w_branch_attn[layer],
                                                 w_branch_hyena[layer], w_out[layer])
            hc2 = modulate(rmsnorm(ctx, norm2_g[layer]), cmod[3], cmod[4])
            ctx = ctx + cmod[5] * moe(hc2, router_w[layer], router_bias[layer], exp_w_gate[layer],
                                      exp_w_up[layer], exp_w_down[layer], shared_w_gate[layer],
                                      shared_w_up[layer], shared_w_down[layer])
        x = x + g1 * mix_x

        hx2 = modulate(rmsnorm(x, norm2_g[layer]), sh2, sc2)
        x = x + g2 * moe(hx2, router_w[layer], router_bias[layer], exp_w_gate[layer], exp_w_up[layer],
                         exp_w_down[layer], shared_w_gate[layer], shared_w_up[layer], shared_w_down[layer])
    return rmsnorm(x, final_norm_g)
```

```python
import math
from contextlib import ExitStack
import numpy as np
import concourse.bass as bass
import concourse.mybir as mybir
from concourse.bass_utils import run_bass_kernel_spmd

F32 = mybir.dt.float32; BF16 = mybir.dt.bfloat16; I32 = mybir.dt.int32
AF = mybir.ActivationFunctionType
ALU = mybir.AluOpType
AXL = mybir.AxisListType
DTSZ = {F32: 4, BF16: 2, I32: 4}

D = 1024; SEQ = 4096; CTX = 256; TOK = SEQ + CTX; NH = 8; INW = 8192
NBLK = 320; NSLOT = NBLK * 128; BIGK = 65536.0; BIGI = 1.0e6; NWB = 4
DEBUG = False
PHASES = 99


class Buf:
    __slots__ = ("name", "w", "r")

    def __init__(self, name=""):
        self.name = name; self.w = None; self.r = []


class Sem:
    def __init__(self, h):
        self.h = h; self.n = 0


class Prog:
    ENG = ("pe", "act", "dve", "pool", "sp")

    def __init__(self, nc, es):
        self.nc = nc; self.es = es
        self.q = {e: [] for e in self.ENG}
        self.esem = {e: Sem(es.enter_context(nc.semaphore("prog_" + e))) for e in self.ENG}
        self.known = {e: {} for e in self.ENG}
        self.dsems = []
        self.off = 16640; self.ntile = 0
        self.cap = nc.SBUF_PARTITION_SIZE_BYTES
        self.ninst = 0

    def tile(self, shape, dtype, name=None):
        nb = int(np.prod(shape[1:])) * DTSZ[dtype]
        nb = (nb + 63) // 64 * 64
        assert self.off + nb <= self.cap, f"SBUF overflow {self.off}+{nb} > {self.cap} ({name})"
        self.ntile += 1
        t = self.nc.alloc_sbuf_tensor_at(f"{name or 't'}_{self.ntile}", list(shape), dtype, offset=self.off)
        self.off += nb
        return t

    def mark(self):
        return self.off

    def reset(self, m):
        self.off = m

    def dsem(self, name="d"):
        s = Sem(self.es.enter_context(self.nc.semaphore(f"{name}_{len(self.dsems)}")))
        self.dsems.append(s); return s

    def _deps(self, eng, reads, writes):
        deps = []
        for b in reads:
            if b.w is not None: deps.append(b.w)
        for b in writes:
            if b.w is not None: deps.append(b.w)
            deps.extend(b.r)
        best = {}
        for (s, v) in deps:
            k = id(s)
            if k not in best or best[k][1] < v: best[k] = (s, v)
        waits = []
        kn = self.known[eng]
        for (s, v) in best.values():
            if eng == "pe" and s is self.esem["pe"]: continue
            if kn.get(id(s), 0) >= v: continue
            kn[id(s)] = v
            waits.append((s, v))
        return waits

    def op(self, eng, fn, reads=(), writes=(), sig=True):
        waits = self._deps(eng, reads, writes)
        S = self.esem[eng]
        if sig: S.n += 1
        tok = (S, S.n if sig else S.n + 1)
        self.q[eng].append((waits, fn, (S.h, 1) if sig else None))
        for b in reads: b.r.append(tok)
        for b in writes:
            b.w = tok; b.r = []
        self.ninst += 1
        return tok

    def dma(self, eng, out, in_, sem, reads=(), writes=(), **kw):
        waits = self._deps(eng, reads, writes)
        sem.n += 16
        tok = (sem, sem.n)
        self.q[eng].append((waits, lambda e: e.dma_start(out=out, in_=in_, **kw), (sem.h, 16)))
        for b in reads: b.r.append(tok)
        for b in writes:
            b.w = tok; b.r = []
        self.ninst += 1
        return tok

    def reg(self, e, v):
        if not hasattr(self, "_regs"): self._regs = {}
        if v not in self._regs: self._regs[v] = e.to_reg(v)
        return self._regs[v]

    def barrier(self):
        allsem = [self.esem[e] for e in self.ENG] + self.dsems
        for e in self.ENG:
            waits = []
            kn = self.known[e]
            for s in allsem:
                if s.n > 0 and kn.get(id(s), 0) < s.n and not (s is self.esem[e]):
                    kn[id(s)] = s.n; waits.append((s, s.n))
            if waits: self.q[e].append((waits, None, None))

    def emit(self):
        nc = self.nc
        self.barrier()
        with nc.Block() as block:
            def run(engname):
                def f(e):
                    for (waits, fn, inc) in self.q[engname]:
                        for (s, v) in waits: e.wait_ge(s.h, v)
                        if fn is None: continue
                        ins = fn(e)
                        if inc is not None: ins.then_inc(inc[0], inc[1])
                return f
            block.tensor(run("pe")); block.scalar(run("act")); block.vector(run("dve"))
            block.gpsimd(run("pool")); block.sync(run("sp"))


class Ring:
    def __init__(self, P, n, shape, dtype, name, sem=True):
        self.t = [P.tile(shape, dtype, f"{name}{i}") for i in range(n)]
        self.b = [Buf(f"{name}{i}") for i in range(n)]
        self.s = [P.dsem(name) for i in range(n)] if sem else [None] * n
        self.i = -1; self.n = n

    def next(self):
        self.i = (self.i + 1) % self.n
        return self.t[self.i], self.b[self.i], self.s[self.i]


def rope_tables():
    p = np.arange(128); d = p % 64
    axis = d // 32; half = (d % 32) // 16; fr = d % 16
    inv = (10000.0 ** (-(np.arange(0, 32, 2, dtype=np.float32)) / 32.0)).astype(np.float32)
    t = np.arange(SEQ)
    row = (t // 64).astype(np.float32); col = (t % 64).astype(np.float32)
    pos = np.where(axis[:, None] == 0, row[None, :], col[None, :]).astype(np.float32)
    ang = (pos * inv[fr][:, None]).astype(np.float32)
    C = np.cos(ang).astype(np.float32)
    S = np.sin(ang).astype(np.float32) * np.where(half == 0, -1.0, 1.0)[:, None].astype(np.float32)
    return np.ascontiguousarray(C), np.ascontiguousarray(S.astype(np.float32))


def hyena_tables():
    n = SEQ; N = 2 * SEQ
    tp = np.arange(N)
    pos = np.where(tp < n, tp, N - tp).astype(np.float32)
    pos[n] = 0.0
    tt = (pos / np.float32(n - 1)).astype(np.float32)
    w = (np.float32(2 * math.pi) * pos / np.float32(n)).astype(np.float32)
    bands = np.linspace(1e-4, 15.0, 16, dtype=np.float32)
    bw = (bands[None, :] * w[:, None]).astype(np.float32)
    z = np.concatenate([tt[:, None], np.cos(bw), -np.sin(bw)], axis=1).astype(np.float32)
    ttx = tt.copy(); ttx[n] = 1.0e4
    deltas = np.abs(np.linspace(math.log(1e-2) / 1.5, math.log(1e-2) / 0.3, D, dtype=np.float32)).astype(np.float32)
    T = {}
    T["zextT"] = np.ascontiguousarray(z.T)
    T["ttx"] = np.ascontiguousarray(ttx[None, :])
    T["negdelta"] = np.ascontiguousarray((-deltas).reshape(8, 128).T)
    s1 = np.arange(64)[:, None].astype(np.float64); f1 = np.arange(33)[None, :].astype(np.float64)
    a = 2 * np.pi * s1 * f1 / 64
    T["E1"] = np.concatenate([np.cos(a), -np.sin(a)], axis=1).astype(np.float32)
    s2 = np.arange(128)[:, None].astype(np.float64)
    a = 2 * np.pi * s2 * f1 / N
    Tr, Ti = np.cos(a), -np.sin(a)
    T["TW1"] = np.stack([np.stack([Tr, Ti], 1), np.stack([Ti, Tr], 1)], 1).astype(np.float32)
    f2 = np.arange(128)[None, :].astype(np.float64)
    a = 2 * np.pi * s2 * f2 / 128
    T["E2"] = np.stack([np.cos(a), -np.sin(a), np.sin(a)], 1).astype(np.float32)
    T["Ginv"] = np.stack([np.cos(a.T), np.sin(a.T)], 1).astype(np.float32)
    f1c = np.arange(33)[:, None].astype(np.float64); s2r = np.arange(128)[None, :].astype(np.float64)
    a = 2 * np.pi * f1c * s2r / N
    Tc, Ts = np.cos(a), np.sin(a)
    W1 = np.concatenate([np.stack([Tc, -Ts], 1), np.stack([-Ts, -Tc], 1)], 0)
    W2 = np.concatenate([np.stack([Ts, Tc], 1), np.stack([Tc, -Ts], 1)], 0)
    T["TW2"] = np.stack([W1, W2], 1).astype(np.float32)
    wt = np.full(33, 2.0); wt[0] = 1.0; wt[32] = 1.0
    s1r = np.arange(32)[None, :].astype(np.float64)
    a = 2 * np.pi * f1c * s1r / 64
    la = wt[:, None] * np.cos(a) / N; lb = -wt[:, None] * np.sin(a) / N
    T["LAB"] = np.stack([np.concatenate([la, la], 0), np.concatenate([lb, lb], 0)], 1).astype(np.float32)
    return {k: np.ascontiguousarray(v) for k, v in T.items()}


def build(debug=False, phases=99):
    nc = bass.Bass("TRN2", target_bir_lowering=False)
    skind = "ExternalOutput" if debug else "Internal"

    def din(name, shape, dt=F32):
        return nc.dram_tensor(name, list(shape), dt, kind="ExternalInput").ap()

    def dscr(name, shape, dt):
        return nc.dram_tensor(name, list(shape), dt, kind=skind).ap()

    xT = din("xT", [D, TOK]); cc = din("cc", [128, 8, 2]); ada_w = din("ada_w", [D, 6 * D])
    ada_bT = din("ada_bT", [128, 48]); gvec = din("gvec", [128, 24]); lamv = din("lamv", [1, 256])
    w_in = din("w_in", [D, INW]); ropeC = din("ropeC", [128, SEQ]); ropeS = din("ropeS", [128, SEQ])
    convw = din("convw", [128, 3, 24]); convb = din("convb", [128, 24]); subln = din("subln", [1, 128]); sublnT = din("sublnT", [128, 1])
    zextT = din("zextT", [33, 2 * SEQ]); ttx = din("ttx", [1, 2 * SEQ]); negdelta = din("negdelta", [128, 8])
    E1d = din("E1", [64, 66]); TW1d = din("TW1", [128, 2, 2, 33]); E2d = din("E2", [128, 3, 128]); Ginvd = din("Ginv", [128, 2, 128])
    TW2d = din("TW2", [66, 2, 2, 128]); LABd = din("LAB", [66, 2, 32])
    w_pa = din("w_pa", [D, D]); w_ph = din("w_ph", [D, D]); w_o = din("w_o", [D, D]); rw = din("rw", [D, 64]); rbias = din("rbias", [1, 64])
    ewg = din("ewg", [65 * 128, 2048]); ewu = din("ewu", [65 * 128, 2048]); ewd = din("ewd", [65 * 128, 2048])
    bvals = din("bvals", [1, NBLK]); pcol = din("pcol", [128, 1])
    fw1 = din("fw1", [33, 64]); fw2 = din("fw2", [64, 64]); fw3 = din("fw3", [64, 4096]); fvec = din("fvec", [64, 3]); hyb = din("hyb", [1, 2048])
    outT = nc.dram_tensor("outT", [D, SEQ], F32, kind="ExternalOutput").ap()

    QT = dscr("QT", [NH, 128, SEQ], BF16); KT = dscr("KT", [NH, 128, TOK], BF16)
    AXs = dscr("AXs", [D, SEQ], BF16); KERN = dscr("KERN", [2, D, 2 * SEQ], BF16)
    KF = dscr("KF", [2, 128, D, 2, 33], F32); HY = dscr("HY", [D, SEQ], BF16)
    X1 = dscr("X1", [D, SEQ], F32); HX2 = dscr("HX2", [D, SEQ], BF16); HX2tok = dscr("HX2tok", [SEQ, D], BF16)
    XG = dscr("XG", [NSLOT, D], BF16); YG = dscr("YG", [NSLOT, D], F32); WB = dscr("WB", [65 * 128, 6144], BF16)
    VV = dscr("VV", [NH, 128, 34, 128], BF16); UU = dscr("UU", [3, D, SEQ], F32); GG = dscr("GG", [2 * D, SEQ], BF16)
    dbg = {}
    if debug:
        dbg["hx"] = nc.dram_tensor("dbg_hx", [128, 8, TOK], BF16, kind="ExternalOutput").ap()
        dbg["vec"] = nc.dram_tensor("dbg_vec", [128, 80], F32, kind="ExternalOutput").ap()
        dbg["slot"] = nc.dram_tensor("dbg_slot", [128, 32, 8], I32, kind="ExternalOutput").ap()
        dbg["gk"] = nc.dram_tensor("dbg_gk", [128, 32, 8], F32, kind="ExternalOutput").ap()
        dbg["blk"] = nc.dram_tensor("dbg_blk", [128, NBLK], I32, kind="ExternalOutput").ap()
        dbg["acc"] = nc.dram_tensor("dbg_acc", [2, 128, 8, 2048], F32, kind="ExternalOutput").ap()

    with ExitStack() as es:
        P = Prog(nc, es)
        psb = [nc.alloc_psum_tensor(f"psb{i}", [128, 512], F32) for i in range(8)]
        psB = [Buf(f"ps{i}") for i in range(8)]
        pi = [0]

        def ps_next():
            pi[0] = (pi[0] + 1) % 8
            return psb[pi[0]], psB[pi[0]]

        vec = P.tile([128, 80], F32, "vec"); vecB = Buf("vec")
        ones = P.tile([128, 128], F32, "ones"); onesB = Buf("ones")
        ident = P.tile([128, 128], F32, "ident"); identB = Buf("ident")
        P.op("pool", lambda e: e.memset(ones[:], 1.0), writes=[onesB])
        P.op("pool", lambda e: e.memset(ident[:], 0.0), writes=[identB])
        P.op("pool", lambda e: e.affine_select(out=ident[:], in_=ident[:], pattern=[[-1, 128]], compare_op=ALU.not_equal,
                                               fill=1.0, base=0, channel_multiplier=1), reads=[identB], writes=[identB])
        g08 = P.tile([128, 128], F32, "g08"); g08B = Buf("g08")
        m_persist = P.mark()
        hxT = P.tile([128, 8, TOK], BF16, "hxT"); hxB = [Buf(f"hx{i}") for i in range(9)]
        m_hx = P.mark()

        cct = P.tile([128, 8, 2], F32, "cct"); sil = P.tile([128, 8, 2], F32, "sil"); cB = Buf(); silB = Buf()
        adab = P.tile([128, 48], F32, "adab"); gv = P.tile([128, 24], F32, "gv"); lamt = P.tile([128, 256], F32, "lamt")
        modT = P.tile([128, 48, 2], F32, "modT"); modB = Buf()
        smB = Buf(); s_small = P.dsem("small"); s_cc = P.dsem("cc")
        P.dma("sp", cct[:], cc[:, :, :], s_cc, writes=[cB])
        P.dma("sp", adab[:], ada_bT[:, :], s_small, writes=[smB])
        P.dma("sp", gv[:], gvec[:, :], s_small, writes=[smB])
        P.dma("sp", lamt[:], lamv.partition_broadcast(128), s_small, writes=[smB])
        P.op("act", lambda e: e.activation(out=sil[:], in_=cct[:], func=AF.Sigmoid), reads=[cB], writes=[silB])
        P.op("dve", lambda e: e.tensor_tensor(out=sil[:], in0=sil[:], in1=cct[:], op=ALU.mult), reads=[cB, silB], writes=[silB])
        aw = Ring(P, 2, [128, 8, 1024], F32, "adaw")
        psm, psmB = ps_next()
        adaw_v = ada_w.rearrange("(k p) f -> p k f", p=128)
        for g in range(6):
            wt, wB, ws = aw.next()
            P.dma("sp" if g % 2 == 0 else "act", wt[:], adaw_v[:, :, g * 1024:(g + 1) * 1024], ws, writes=[wB])
            for fc in range(8):
                f = g * 8 + fc
                for k in range(8):
                    P.op("pe", lambda e, wt=wt, k=k, fc=fc, f=f: e.matmul(psm[:, 2 * f:2 * f + 2], wt[:, k, fc * 128:(fc + 1) * 128],
                                                                         sil[:, k, :], start=(k == 0), stop=(k == 7)),
                         reads=[wB, silB], writes=[psmB], sig=(k == 7))
        P.op("dve", lambda e: e.tensor_tensor(out=modT[:], in0=psm[:, 0:96].rearrange("p (f j) -> p f j", j=2),
                                              in1=adab[:].unsqueeze(2).to_broadcast([128, 48, 2]), op=ALU.add),
             reads=[psmB, smB], writes=[modB])
        def vop(fn, rd=(modB, smB)):
            P.op("dve", fn, reads=list(rd) + [vecB], writes=[vecB])
        vop(lambda e: e.scalar_tensor_tensor(out=vec[:, 0:8], in0=modT[:, 8:16, 0], scalar=1.0, in1=gv[:, 0:8], op0=ALU.add, op1=ALU.mult))
        vop(lambda e: e.tensor_copy(out=vec[:, 8:16], in_=modT[:, 0:8, 0]))
        vop(lambda e: e.scalar_tensor_tensor(out=vec[:, 16:24], in0=modT[:, 8:16, 1], scalar=1.0, in1=gv[:, 0:8], op0=ALU.add, op1=ALU.mult))
        vop(lambda e: e.tensor_copy(out=vec[:, 24:32], in_=modT[:, 0:8, 1]))
        vop(lambda e: e.scalar_tensor_tensor(out=vec[:, 32:40], in0=modT[:, 32:40, 0], scalar=1.0, in1=gv[:, 8:16], op0=ALU.add, op1=ALU.mult))
        vop(lambda e: e.tensor_copy(out=vec[:, 40:48], in_=modT[:, 24:32, 0]))
        vop(lambda e: e.tensor_copy(out=vec[:, 48:56], in_=modT[:, 16:24, 0]))
        vop(lambda e: e.tensor_copy(out=vec[:, 56:64], in_=modT[:, 40:48, 0]))
        vop(lambda e: e.tensor_copy(out=vec[:, 68:76], in_=gv[:, 16:24]))
        lt = P.tile([128, 128], F32, "lt"); ltB = Buf()
        P.op("dve", lambda e: e.tensor_tensor(out=lt[:].rearrange("p (a b) -> p a b", a=2),
                                              in0=lamt[:].rearrange("p (a c b) -> p a c b", a=2, c=2)[:, :, 0, :],
                                              in1=lamt[:].rearrange("p (a c b) -> p a c b", a=2, c=2)[:, :, 1, :], op=ALU.mult),
             reads=[smB], writes=[ltB])
        P.op("dve", lambda e: e.tensor_reduce(out=vec[:, 66:68], in_=lt[:].rearrange("p (a b) -> p a b", a=2), axis=AXL.X, op=ALU.add),
             reads=[ltB, vecB], writes=[vecB])
        P.op("act", lambda e: e.activation(out=vec[:, 66:68], in_=vec[:, 66:68], func=AF.Exp), reads=[vecB], writes=[vecB])
        P.op("dve", lambda e: e.scalar_tensor_tensor(out=vec[:, 64:65], in0=vec[:, 67:68], scalar=-0.2, in1=vec[:, 66:67],
                                                     op0=ALU.add, op1=ALU.subtract), reads=[vecB], writes=[vecB])
        if debug:
            P.dma("sp", dbg["vec"][:, :], vec[:], s_small, reads=[vecB])

        xr = Ring(P, 2, [128, 8, 512], F32, "xt"); sq = P.tile([128, 8, 512], F32, "sq"); sqB = Buf()
        rstd = P.tile([128, 512], F32, "rstd"); rsB = Buf()
        xT_v = xT.rearrange("(k p) t -> p k t", p=128)

        def rms_rstd(src_t, src_b, n, sq=sq, sqB=sqB, rstd=rstd, rsB=rsB, eps=1e-6, dim=1024.0):
            P.op("act", lambda e: e.activation(out=sq[:, :, 0:n], in_=src_t[:, :, 0:n], func=AF.Square), reads=[src_b], writes=[sqB])
            pt, pB = ps_next()
            for k in range(8):
                P.op("pe", lambda e, k=k: e.matmul(pt[:, 0:n], ones[:], sq[:, k, 0:n], start=(k == 0), stop=(k == 7)),
                     reads=[sqB, onesB], writes=[pB], sig=(k == 7))
            P.op("dve", lambda e: e.tensor_scalar(out=rstd[:, 0:n], in0=pt[:, 0:n], scalar1=1.0 / dim, scalar2=eps, op0=ALU.mult, op1=ALU.add),
                 reads=[pB], writes=[rsB])
            P.op("act", lambda e: e.activation(out=rstd[:, 0:n], in_=rstd[:, 0:n], func=AF.Sqrt), reads=[rsB], writes=[rsB])
            P.op("dve", lambda e: e.reciprocal(out=rstd[:, 0:n], in_=rstd[:, 0:n]), reads=[rsB], writes=[rsB])

        for i in range(9):
            t0 = i * 512; n = 512 if i < 8 else 256
            xt, xB, xs = xr.next()
            P.dma("sp", xt[:, :, 0:n], xT_v[:, :, t0:t0 + n], xs, writes=[xB])
            rms_rstd(xt, xB, n)
            P.op("dve", lambda e, xt=xt, n=n: e.tensor_tensor(out=sq[:, :, 0:n], in0=xt[:, :, 0:n],
                                                              in1=rstd[:, 0:n].unsqueeze(1).to_broadcast([128, 8, n]), op=ALU.mult),
                 reads=[xB, rsB, sqB], writes=[sqB])
            ao = 0 if i < 8 else 16
            for k in range(8):
                P.op("act", lambda e, k=k, t0=t0, n=n, ao=ao: e.activation(out=hxT[:, k, t0:t0 + n], in_=sq[:, k, 0:n], func=AF.Identity,
                                                                             scale=vec[:, ao + k:ao + k + 1], bias=vec[:, ao + 8 + k:ao + 9 + k]),
                     reads=[sqB, vecB], writes=[hxB[i]])
        if debug:
            P.dma("sp", dbg["hx"][:, :, :], hxT[:], s_small, reads=hxB)
        P.barrier()
        P.reset(m_hx)
        if phases < 2:
            P.emit(); return nc

        m2 = P.mark()
        wr = Ring(P, 2, [128, 8, 1024], BF16, "wg"); wperm = P.tile([128, 8, 1024], BF16, "wperm"); wpB = Buf()
        cw = P.tile([128, 3, 24], F32, "cw"); cb = P.tile([128, 24], F32, "cb"); cwB = Buf()
        s_cw = P.dsem("cw"); s_rope = P.dsem("rope")
        P.dma("act", cw[:], convw[:, :, :], s_cw, writes=[cwB])
        P.dma("act", cb[:], convb[:, :], s_cw, writes=[cwB])
        ob = Ring(P, 4, [128, 512], BF16, "ob")
        vst = Ring(P, 2, [128, 1024], BF16, "vst")
        m2b = P.mark()
        rC = P.tile([128, SEQ], F32, "rC"); rS = P.tile([128, SEQ], F32, "rS"); ropB = Buf()
        P.dma("act", rC[:], ropeC[:, :], s_rope, writes=[ropB])
        P.dma("act", rS[:], ropeS[:, :], s_rope, writes=[ropB])
        t1r = Ring(P, 4, [128, 512], F32, "t1", sem=False)
        win_v = w_in.rearrange("(k p) n -> p k n", p=128)
        alt = [0]
        for g in (0, 2, 1, 3, 4, 5, 6, 7):
            if g == 1:
                P.barrier(); P.reset(m2b)
                pbuf = P.tile([128, SEQ + 2], F32, "pbuf"); pbB = Buf()
                ur = Ring(P, 2, [128, SEQ], F32, "ubuf")
                P.op("pool", lambda e: e.memset(pbuf[:, 0:1], 0.0), writes=[pbB])
                P.op("pool", lambda e: e.memset(pbuf[:, SEQ + 1:SEQ + 2], 0.0), writes=[pbB])
            wt, wB, ws = wr.next()
            P.dma("pool", wt[:], win_v[:, :, g * 1024:(g + 1) * 1024], ws, writes=[wB])
            if g in (0, 2):
                wv = wt[:].rearrange("p k (b h j) -> p k b h j", h=2, j=16)
                pv = wperm[:].rearrange("p k (b h j) -> p k b h j", h=2, j=16)
                for k in range(8):
                    P.op("pool", lambda e, wv=wv, k=k: e.tensor_copy(out=pv[:, k, :, 0, :], in_=wv[:, k, :, 1, :]), reads=[wB], writes=[wpB])
                    P.op("pool", lambda e, wv=wv, k=k: e.tensor_copy(out=pv[:, k, :, 1, :], in_=wv[:, k, :, 0, :]), reads=[wB], writes=[wpB])
                dst = KT if g == 0 else QT
                for h in range(8):
                    for i in range(9 if g == 0 else 8):
                        t0 = i * 512; n = 512 if i < 8 else 256
                        pa, paB = ps_next()
                        for k in range(8):
                            P.op("pe", lambda e, pa=pa, wt=wt, k=k, h=h, t0=t0, n=n: e.matmul(pa[:, 0:n], wt[:, k, h * 128:(h + 1) * 128],
                                                                                               hxT[:, k, t0:t0 + n], start=(k == 0), stop=(k == 7)),
                                 reads=[wB, hxB[i]], writes=[paB], sig=(k == 7))
                        o, oB, osem = ob.next()
                        if i < 8:
                            pb_, pbB_ = ps_next()
                            for k in range(8):
                                P.op("pe", lambda e, pb_=pb_, k=k, h=h, t0=t0, n=n: e.matmul(pb_[:, 0:n], wperm[:, k, h * 128:(h + 1) * 128],
                                                                                              hxT[:, k, t0:t0 + n], start=(k == 0), stop=(k == 7)),
                                     reads=[wpB, hxB[i]], writes=[pbB_], sig=(k == 7))
                            ta, taB, _ = t1r.next(); tb, tbB, _ = t1r.next()
                            P.op("dve", lambda e, ta=ta, pa=pa, t0=t0: e.tensor_tensor(out=ta[:], in0=pa[:], in1=rC[:, t0:t0 + 512], op=ALU.mult),
                                 reads=[paB, ropB], writes=[taB])
                            P.op("dve", lambda e, tb=tb, pb_=pb_, t0=t0: e.tensor_tensor(out=tb[:], in0=pb_[:], in1=rS[:, t0:t0 + 512], op=ALU.mult),
                                 reads=[pbB_, ropB], writes=[tbB])
                            P.op("pool", lambda e, o=o, ta=ta, tb=tb: e.tensor_tensor(out=o[:], in0=ta[:], in1=tb[:], op=ALU.add),
                                 reads=[taB, tbB], writes=[oB])
                        else:
                            P.op("act", lambda e, o=o, pa=pa, n=n: e.copy(out=o[:, 0:n], in_=pa[:, 0:n]), reads=[paB], writes=[oB])
                        P.dma("sp", dst[h, :, t0:t0 + n], o[:, 0:n], osem, reads=[oB])
            elif g == 1:
                for j in range(34):
                    vt, vB, vs = vst.next()
                    for hf in range(2):
                        pa, paB = ps_next()
                        for k in range(8):
                            P.op("pe", lambda e, pa=pa, wt=wt, k=k, j=j, hf=hf: e.matmul(pa[:], hxT[:, k, j * 128:(j + 1) * 128],
                                                                                         wt[:, k, hf * 512:(hf + 1) * 512], start=(k == 0), stop=(k == 7)),
                                 reads=[wB, hxB[j // 4]], writes=[paB], sig=(k == 7))
                        if hf == 0:
                            P.op("act", lambda e, vt=vt, pa=pa: e.copy(out=vt[:, 0:512], in_=pa[:]), reads=[paB], writes=[vB])
                        else:
                            P.op("dve", lambda e, vt=vt, pa=pa: e.tensor_copy(out=vt[:, 512:1024], in_=pa[:]), reads=[paB], writes=[vB])
                    P.dma("sp", VV[:, :, j, :].rearrange("h p e -> p h e"), vt[:].rearrange("p (h e) -> p h e", h=NH), vs, reads=[vB])
            elif g in (3, 4, 5):
                for c in range(8):
                    ch = (g - 3) * 8 + c
                    for i in range(8):
                        t0 = i * 512
                        pa, paB = ps_next()
                        for k in range(8):
                            P.op("pe", lambda e, pa=pa, wt=wt, k=k, c=c, t0=t0: e.matmul(pa[:], wt[:, k, c * 128:(c + 1) * 128],
                                                                                         hxT[:, k, t0:t0 + 512], start=(k == 0), stop=(k == 7)),
                                 reads=[wB, hxB[i]], writes=[paB], sig=(k == 7))
                        P.op("act", lambda e, pa=pa, t0=t0: e.copy(out=pbuf[:, 1 + t0:1 + t0 + 512], in_=pa[:]), reads=[paB], writes=[pbB])
                    u, uB, us = ur.next()
                    eng = "dve"
                    P.op(eng, lambda e, u=u, ch=ch: e.tensor_scalar(out=u[:], in0=pbuf[:, 0:SEQ], scalar1=cw[:, 0, ch:ch + 1], scalar2=cb[:, ch:ch + 1],
                                                                    op0=ALU.mult, op1=ALU.add), reads=[pbB, cwB], writes=[uB])
                    P.op(eng, lambda e, u=u, ch=ch: e.scalar_tensor_tensor(out=u[:], in0=pbuf[:, 1:SEQ + 1], scalar=cw[:, 1, ch:ch + 1], in1=u[:],
                                                                           op0=ALU.mult, op1=ALU.add), reads=[pbB, cwB, uB], writes=[uB])
                    P.op(eng, lambda e, u=u, ch=ch: e.scalar_tensor_tensor(out=u[:], in0=pbuf[:, 2:SEQ + 2], scalar=cw[:, 2, ch:ch + 1], in1=u[:],
                                                                           op0=ALU.mult, op1=ALU.add), reads=[pbB, cwB, uB], writes=[uB])
                    P.dma("sp", UU[g - 3, c * 128:(c + 1) * 128, :], u[:], us, reads=[uB])
            else:
                for c in range(8):
                    for i in range(8):
                        t0 = i * 512
                        pa, paB = ps_next()
                        for k in range(8):
                            P.op("pe", lambda e, pa=pa, wt=wt, k=k, c=c, t0=t0: e.matmul(pa[:], wt[:, k, c * 128:(c + 1) * 128],
                                                                                         hxT[:, k, t0:t0 + 512], start=(k == 0), stop=(k == 7)),
                                 reads=[wB, hxB[i]], writes=[paB], sig=(k == 7))
                        o, oB, osem = ob.next()
                        P.op("act", lambda e, o=o, pa=pa: e.activation(out=o[:], in_=pa[:], func=AF.Sigmoid), reads=[paB], writes=[oB])
                        P.dma("sp", GG[(g - 6) * 1024 + c * 128:(g - 6) * 1024 + (c + 1) * 128, t0:t0 + 512], o[:], osem, reads=[oB])
        P.barrier()
        P.reset(m_persist)
        if phases < 3:
            P.emit(); return nc

        s_g = P.dsem("g08")
        g08c = P.tile([128, 1], F32, "g08c")
        P.dma("sp", g08c[:], sublnT[:, :], s_g, writes=[g08B])
        P.op("dve", lambda e: e.tensor_scalar(out=g08c[:], in0=g08c[:], scalar1=0.8, scalar2=None, op0=ALU.mult), reads=[g08B], writes=[g08B])
        qzr = [Ring(P, 2, [128, SEQ], BF16, f"qz{m}") for m in range(2)]
        kr = Ring(P, 2, [128, TOK], BF16, "kT"); vr = Ring(P, 2, [128, 34, 128], BF16, "vh")
        for m in range(2):
            for bi_ in range(2):
                t_ = qzr[m].t[bi_]
                P.op("pool", lambda e, t_=t_, m=m: e.memset(t_[64 * (1 - m):64 * (1 - m) + 64, :], 0.0), writes=[qzr[m].b[bi_]])
        onesb = P.tile([128, 128], BF16, "onesb"); onesbB = Buf("onesb")
        P.op("pool", lambda e: e.memset(onesb[:], 1.0), writes=[onesbB])
        ptr = Ring(P, 3, [128, 34, 512], BF16, "pT", sem=False)
        zfl = P.tile([128, 1024], BF16, "zfl"); ztB = Buf("zfl"); xgB = Buf("XG"); s_z = P.dsem("zfill")
        P.op("pool", lambda e, zfl=zfl: e.memset(zfl[:], 0.0), writes=[ztB])
        XG_z = XG.rearrange("(p a) d -> p (a d)", p=128)
        zwin = [Buf(f"zw{i}") for i in range(4)]; zsem = [s_z] + [P.dsem("zfill") for _ in range(3)]
        zctr = [0]
        def zero_fill(nz):
            for _ in range(nz):
                zi = zctr[0]
                if zi >= NBLK * D // 1024: return
                zctr[0] += 1
                P.dma("sp", XG_z[:, zi * 1024:(zi + 1) * 1024], zfl[:], zsem[zi % 4], reads=[ztB], writes=[zwin[zi % 4]])
        wstg = Ring(P, 1, [128, 6144], BF16, "wstg"); s_wb = P.dsem("wbst"); wbB = Buf("WB")
        def precast(e_):
            st_t, st_B, st_s = wstg.next()
            P.dma("pool", st_t[:, 0:2048], ewg[e_ * 128:(e_ + 1) * 128, :], st_s, writes=[st_B])
            P.dma("pool", st_t[:, 2048:4096], ewu[e_ * 128:(e_ + 1) * 128, :], st_s, reads=[], writes=[])
            P.dma("pool", st_t[:, 4096:6144], ewd[e_ * 128:(e_ + 1) * 128, :], st_s, reads=[], writes=[])
            st_B.w = (st_s, st_s.n)
            P.dma("pool", WB[e_ * 128:(e_ + 1) * 128, :], st_t[:], s_wb, reads=[st_B], writes=([wbB] if e_ == 64 else []))
        tq = P.tile([128, 512], F32, "tq"); tqB = Buf("tq")
        csr = Ring(P, 2, [128, 2, 512], F32, "csum", sem=False); cpr = Ring(P, 1, [128, 2, 512], F32, "cpool", sem=False)
        rr = Ring(P, 1, [128, 512], F32, "rrec", sem=False)
        oo = P.tile([128, 512], F32, "oo"); ooB = Buf("oo"); o2 = P.tile([128, 512], F32, "o2"); o2B = Buf("o2")
        axo = Ring(P, 2, [128, 512], BF16, "axo")
        SB = [1, 2, 3]; TB = 0; NDV = 24
        OB = {0: (4, 5), 1: (6, 7)}
        sidx = [0]

        def load_head(h):
            k, kB, ks_ = kr.next(); v, vB, vs_ = vr.next()
            qs = []
            for m in range(2):
                q, qB, qs_ = qzr[m].next()
                P.dma("sp" if m == 0 else "act", q[64 * m:64 * m + 64, :], QT[h, 64 * m:64 * m + 64, :], qs_, writes=[qB])
                qs.append((q, qB))
            P.dma("sp", k[:], KT[h, :, :], ks_, writes=[kB])
            P.dma("act", v[:], VV[h, :, :, :], vs_, writes=[vB])
            return (qs, k, kB, v, vB)

        def qk_exp_steps(hd, i, m, pt, ptB):
            qs, k, kB, v, vB = hd
            q, qB = qs[m]
            steps = []
            for j in range(34):
                def st(j=j):
                    b = SB[sidx[0] % 3]; sidx[0] += 1
                    P.op("pe", lambda e: e.matmul(psb[b][:], k[:, j * 128:(j + 1) * 128], q[:, i * 512:(i + 1) * 512], start=True, stop=True),
                         reads=[kB, qB], writes=[psB[b]])
                    P.op("act", lambda e: e.activation(out=pt[:, j, :], in_=psb[b][:], func=AF.Exp, scale=0.125),
                         reads=[psB[b]], writes=[ptB])
                steps.append(st)
            return steps

        def av_steps(hd, m, pt, ptB):
            qs, k, kB, v, vB = hd
            ob_, sb_ = OB[m]
            cs, csB, _ = csr.next(); cp, cpB, _ = cpr.next()
            steps = []
            for j in range(34):
                def st(j=j):
                    P.op("pe", lambda e: e.matmul(psb[ob_][:], v[:, j, :], pt[:, j, :], start=(j == 0), stop=(j == 33)),
                         reads=[ptB, vB], writes=[psB[ob_]], sig=(j == 33))
                    if j == 1:
                        P.op("dve", lambda e: e.tensor_copy(out=cs[:], in_=pt[:, 0:2, :]), reads=[ptB], writes=[csB])
                    elif j % 2 == 1 and j < NDV:
                        P.op("dve", lambda e: e.tensor_tensor(out=cs[:], in0=pt[:, j - 1:j + 1, :], in1=cs[:], op=ALU.add), reads=[ptB, csB], writes=[csB])
                    elif j == NDV + 1:
                        P.op("pool", lambda e: e.tensor_copy(out=cp[:], in_=pt[:, NDV:NDV + 2, :]), reads=[ptB], writes=[cpB])
                    elif j % 2 == 1 and j > NDV:
                        P.op("pool", lambda e: e.tensor_tensor(out=cp[:], in0=pt[:, j - 1:j + 1, :], in1=cp[:], op=ALU.add), reads=[ptB, cpB], writes=[cpB])
                    if j == NDV - 1:
                        P.op("dve", lambda e: e.tensor_tensor(out=cs[:, 0, :], in0=cs[:, 0, :], in1=cs[:, 1, :], op=ALU.add), reads=[csB], writes=[csB])
                    if j == 33:
                        P.op("pool", lambda e: e.tensor_tensor(out=cp[:, 0, :], in0=cp[:, 0, :], in1=cp[:, 1, :], op=ALU.add), reads=[cpB], writes=[cpB])
                        P.op("pe", lambda e: e.matmul(psb[sb_][:], ones[:], cs[:, 0, :], start=True, stop=False), reads=[csB, onesB], writes=[psB[sb_]], sig=False)
                        P.op("pe", lambda e: e.matmul(psb[sb_][:], ones[:], cp[:, 0, :], start=False, stop=True), reads=[cpB, onesB], writes=[psB[sb_]])
                steps.append(st)
            return steps

        def combine_a():
            ob_, sb_ = OB[0]
            r, rB, _ = rr.next()
            P.op("dve", lambda e: e.reciprocal(out=r[:], in_=psb[sb_][:]), reads=[psB[sb_]], writes=[rB])
            P.op("dve", lambda e: e.tensor_tensor(out=tq[:], in0=psb[ob_][:], in1=r[:], op=ALU.mult), reads=[psB[ob_], rB, tqB], writes=[tqB])

        def combine_b(h, i):
            ob_, sb_ = OB[1]
            r, rB, _ = rr.next()
            P.op("dve", lambda e: e.reciprocal(out=r[:], in_=psb[sb_][:]), reads=[psB[sb_]], writes=[rB])
            P.op("dve", lambda e: e.tensor_tensor(out=r[:], in0=psb[ob_][:], in1=r[:], op=ALU.mult), reads=[psB[ob_], rB], writes=[rB])
            P.op("dve", lambda e: e.scalar_tensor_tensor(out=oo[:], in0=r[:], scalar=vec[:, 64:65], in1=tq[:], op0=ALU.mult, op1=ALU.add),
                 reads=[rB, vecB, tqB, ooB], writes=[ooB])
            P.op("pool", lambda e: e.tensor_tensor(out=o2[:], in0=oo[:], in1=oo[:], op=ALU.mult), reads=[ooB, o2B], writes=[o2B])
            P.op("pe", lambda e: e.matmul(psb[TB][:], ones[:], o2[:], start=True, stop=True), reads=[o2B, onesB], writes=[psB[TB]])
            P.op("dve", lambda e: e.tensor_scalar(out=o2[:], in0=psb[TB][:], scalar1=1.0 / 128, scalar2=1e-6, op0=ALU.mult, op1=ALU.add),
                 reads=[psB[TB], o2B], writes=[o2B])
            P.op("act", lambda e: e.activation(out=o2[:], in_=o2[:], func=AF.Ln), reads=[o2B], writes=[o2B])
            P.op("act", lambda e: e.activation(out=o2[:], in_=o2[:], func=AF.Exp, scale=-0.5), reads=[o2B], writes=[o2B])
            ao, aoB, aos = axo.next()
            P.op("dve", lambda e: e.scalar_tensor_tensor(out=ao[:], in0=oo[:], scalar=g08c[:, 0:1], in1=o2[:], op0=ALU.mult, op1=ALU.mult),
                 reads=[ooB, o2B, g08B], writes=[aoB])
            P.dma("sp", AXs[h * 128:(h + 1) * 128, i * 512:(i + 1) * 512], ao[:], aos, reads=[aoB])

        stages = [(h, i, m) for h in range(NH) for i in range(8) for m in range(2)]
        heads = {0: load_head(0)}
        prev = None
        for si, (h, i, m) in enumerate(stages):
            if i == 1 and m == 0 and h + 1 < NH:
                heads[h + 1] = load_head(h + 1)
            if si % 2 == 0:
                precast(si // 2)
            zero_fill(3)
            pt, ptB, _ = ptr.next()
            qk = qk_exp_steps(heads[h], i, m, pt, ptB)
            av_prev, post_prev = prev if prev is not None else ([], None)
            for j in range(34):
                qk[j]()
                if av_prev: av_prev[j]()
            if post_prev is not None: post_prev()
            post = (lambda: combine_a()) if m == 0 else (lambda h=h, i=i: combine_b(h, i))
            prev = (av_steps(heads[h], m, pt, ptB), post)
        for st in prev[0]: st()
        prev[1]()
        precast(64)
        zero_fill(NBLK)
        P.barrier()
        P.reset(m_persist)
        if phases < 4:
            P.emit(); return nc

        TWO_PI = 2.0 * math.pi
        s_t = P.dsem("tabs"); tabB = Buf("tabs")
        E1 = P.tile([64, 66], BF16, "E1"); TW1 = P.tile([128, 2, 2, 33], F32, "TW1"); E2 = P.tile([128, 3, 128], BF16, "E2")
        Ginv = P.tile([128, 2, 128], BF16, "Ginv"); TW2 = P.tile([66, 2, 2, 128], F32, "TW2"); LAB = P.tile([66, 2, 32], BF16, "LAB")
        hb = P.tile([128, 2048], F32, "hb"); ndl = P.tile([128, 8], F32, "ndl")
        P.dma("pool", E1[:], E1d[:, :], s_t, writes=[tabB]); P.dma("sp", TW1[:], TW1d[:, :, :, :], s_t, writes=[tabB])
        P.dma("pool", E2[:], E2d[:, :, :], s_t, writes=[tabB]); P.dma("pool", Ginv[:], Ginvd[:, :, :], s_t, writes=[tabB])
        P.dma("sp", TW2[:], TW2d[:, :, :, :], s_t, writes=[tabB]); P.dma("pool", LAB[:], LABd[:, :, :], s_t, writes=[tabB])
        P.dma("sp", hb[:], hyb.partition_broadcast(128), s_t, writes=[tabB]); P.dma("sp", ndl[:], negdelta[:, :], s_t, writes=[tabB])
        m4 = P.mark()

        hd2T = P.tile([64, 2 * SEQ], BF16, "hd2T"); hd2B = Buf("hd2")
        w3t = P.tile([64, 4096], BF16, "w3t"); w3B = Buf("w3")
        s_w3 = P.dsem("w3")
        P.dma("pool", w3t[:], fw3[:, :], s_w3, writes=[w3B])
        m4a = P.mark()
        zt = P.tile([33, 2 * SEQ], F32, "zt"); w1t = P.tile([33, 64], F32, "w1t"); w2t = P.tile([64, 64], F32, "w2t"); fv = P.tile([64, 3], F32, "fv")
        fB = Buf("filt_in")
        s_f = P.dsem("filt")
        P.dma("sp", zt[:], zextT[:, :], s_f, writes=[fB]); P.dma("sp", w1t[:], fw1[:, :], s_f, writes=[fB])
        P.dma("sp", w2t[:], fw2[:, :], s_f, writes=[fB]); P.dma("sp", fv[:], fvec[:, :], s_f, writes=[fB])
        ar = Ring(P, 2, [64, 512], F32, "marg", sem=False); kir = Ring(P, 2, [64, 512], I32, "mki", sem=False)
        kfr = Ring(P, 2, [64, 512], F32, "mkf", sem=False); h1r = Ring(P, 2, [64, 512], F32, "mh1", sem=False)

        def sin_layer(ps_, psB_, bcol, out_ap, outB):
            a, aB, _ = ar.next(); ki, kiB, _ = kir.next(); kf_, kfB, _ = kfr.next()
            P.op("dve", lambda e: e.tensor_scalar(out=a[:], in0=ps_[0:64, :], scalar1=fv[:, bcol:bcol + 1], scalar2=fv[:, 2:3], op0=ALU.add, op1=ALU.mult),
                 reads=[psB_, fB], writes=[aB])
            P.op("dve", lambda e: e.tensor_scalar(out=ki[:], in0=a[:], scalar1=1.0 / TWO_PI, scalar2=None, op0=ALU.mult), reads=[aB], writes=[kiB])
            P.op("dve", lambda e: e.tensor_copy(out=kf_[:], in_=ki[:]), reads=[kiB], writes=[kfB])
            P.op("dve", lambda e: e.scalar_tensor_tensor(out=a[:], in0=kf_[:], scalar=-TWO_PI, in1=a[:], op0=ALU.mult, op1=ALU.add),
                 reads=[kfB, aB], writes=[aB])
            P.op("dve", lambda e: e.tensor_scalar(out=a[:], in0=a[:], scalar1=math.pi, scalar2=-math.pi, op0=ALU.min, op1=ALU.max), reads=[aB], writes=[aB])
            P.op("act", lambda e: e.activation(out=out_ap, in_=a[:], func=AF.Sin), reads=[aB], writes=[outB])

        for tt in range(16):
            pa, paB = ps_next()
            P.op("pe", lambda e, pa=pa, tt=tt: e.matmul(pa[0:64, :], w1t[:], zt[:, tt * 512:(tt + 1) * 512], start=True, stop=True),
                 reads=[fB], writes=[paB])
            h1, h1B, _ = h1r.next()
            sin_layer(pa, paB, 0, h1[:], h1B)
            pb_, pbB_ = ps_next()
            P.op("pe", lambda e, pb_=pb_, h1=h1: e.matmul(pb_[0:64, :], w2t[:], h1[:], start=True, stop=True), reads=[fB, h1B], writes=[pbB_])
            sin_layer(pb_, pbB_, 1, hd2T[:, tt * 512:(tt + 1) * 512], hd2B)
        P.barrier(); P.reset(m4a)

        ttb = P.tile([128, 2 * SEQ], F32, "ttb"); ttB = Buf("ttb")
        s_tt = P.dsem("ttb")
        P.dma("sp", ttb[:], ttx.partition_broadcast(128), s_tt, writes=[ttB])
        kur = Ring(P, 2, [128, 2 * SEQ], F32, "ku", sem=False); kbr = Ring(P, 2, [128, 2 * SEQ], BF16, "kub")
        dkr = Ring(P, 3, [128, 512], F32, "dk", sem=False); abr = Ring(P, 3, [128, 512], F32, "kab", sem=False)
        asum = P.tile([128, 32], F32, "asum"); asB = Buf("asum")
        asum2 = P.tile([128, 2, 32], F32, "asum2")
        for cc in range(8):
            kus = [kur.next() for _ in range(2)]
            for tt in range(16):
                dr = 0 if tt < 8 else 1
                dk, dkB, _ = dkr.next()
                P.op("act", lambda e, dk=dk, tt=tt, cc=cc: e.activation(out=dk[:], in_=ttb[:, tt * 512:(tt + 1) * 512], func=AF.Exp, scale=ndl[:, cc:cc + 1]),
                     reads=[ttB, tabB], writes=[dkB])
                for o in range(2):
                    ku, kuB, _ = kus[o]
                    col0 = o * 2048 + dr * 1024 + cc * 128
                    pa, paB = ps_next()
                    P.op("pe", lambda e, pa=pa, col0=col0, tt=tt: e.matmul(pa[:], w3t[:, col0:col0 + 128], hd2T[:, tt * 512:(tt + 1) * 512], start=True, stop=True),
                         reads=[w3B, hd2B], writes=[paB])
                    P.op("dve", lambda e, ku=ku, pa=pa, dk=dk, tt=tt: e.tensor_tensor(out=ku[:, tt * 512:(tt + 1) * 512], in0=pa[:], in1=dk[:], op=ALU.mult),
                         reads=[paB, dkB], writes=[kuB])
                    ab, abB, _ = abr.next()
                    P.op("act", lambda e, ab=ab, ku=ku, tt=tt: e.activation(out=ab[:], in_=ku[:, tt * 512:(tt + 1) * 512], func=AF.Abs), reads=[kuB], writes=[abB])
                    P.op("dve", lambda e, ab=ab, tt=tt, o=o: e.tensor_reduce(out=asum2[:, o, tt:tt + 1], in_=ab[:], axis=AXL.X, op=ALU.add), reads=[abB, asB], writes=[asB])
            for o in range(2):
                ku, kuB, _ = kus[o]
                P.op("dve", lambda e, o=o: e.tensor_reduce(out=asum2[:, o, 16:17], in_=asum2[:, o, 0:16], axis=AXL.X, op=ALU.add), reads=[asB], writes=[asB])
                P.op("dve", lambda e, o=o: e.reciprocal(out=asum2[:, o, 17:18], in_=asum2[:, o, 16:17]), reads=[asB], writes=[asB])
                kb, kbB, kbs = kbr.next()
                P.op("act", lambda e, kb=kb, ku=ku, o=o: e.activation(out=kb[:], in_=ku[:], func=AF.Copy, scale=asum2[:, o, 17:18]),
                     reads=[kuB, asB], writes=[kbB])
                P.dma("sp", KERN[o, cc * 128:(cc + 1) * 128, :], kb[:], kbs, reads=[kbB])
        P.barrier(); P.reset(m4)
        if phases < 5:
            P.emit(); return nc

        tmr = Ring(P, 4, [128, 7, 2, 33], F32, "twtmp", sem=False)

        def fft_fwd(Z, ZB, K, nch, Bt, BtB):
            c = 0
            while c < nch:
                g = min(7, nch - c)
                pa, paB = ps_next()
                for u in range(g):
                    P.op("pe", lambda e, pa=pa, u=u, c=c: e.matmul(pa[:, u * 66:(u + 1) * 66], Z[0:K, c + u, :], E1[0:K, :], start=True, stop=True),
                         reads=[ZB, tabB], writes=[paB], sig=(u == g - 1))
                A = pa[:, 0:g * 66].rearrange("p (g r f) -> p g r f", r=2, f=33)
                t1, t1B, _ = tmr.next(); t2, t2B, _ = tmr.next()
                P.op("dve", lambda e, A=A, t1=t1, g=g: e.tensor_tensor(out=t1[:, 0:g], in0=A, in1=TW1[:, 0].unsqueeze(1).to_broadcast([128, g, 2, 33]), op=ALU.mult),
                     reads=[paB, tabB], writes=[t1B])
                P.op("dve", lambda e, A=A, t2=t2, g=g: e.tensor_tensor(out=t2[:, 0:g], in0=A, in1=TW1[:, 1].unsqueeze(1).to_broadcast([128, g, 2, 33]), op=ALU.mult),
                     reads=[paB, tabB], writes=[t2B])
                P.op("pool", lambda e, t1=t1, g=g, c=c: e.tensor_tensor(out=Bt[:, 0, c:c + g, :], in0=t1[:, 0:g, 0, :], in1=t1[:, 0:g, 1, :], op=ALU.subtract),
                     reads=[t1B], writes=[BtB])
                P.op("pool", lambda e, t2=t2, g=g, c=c: e.tensor_tensor(out=Bt[:, 1, c:c + g, :], in0=t2[:, 0:g, 0, :], in1=t2[:, 0:g, 1, :], op=ALU.add),
                     reads=[t2B], writes=[BtB])
                c += g

        def fft_stage2(Bt, BtB, c0, n):
            xr_, xrB = ps_next(); xi_, xiB = ps_next()
            br = Bt[:, 0, c0:c0 + n, :].rearrange("p c f -> p (c f)"); bi = Bt[:, 1, c0:c0 + n, :].rearrange("p c f -> p (c f)")
            w = n * 33
            P.op("pe", lambda e: e.matmul(xr_[:, 0:w], E2[:, 0, :], br, start=True, stop=False), reads=[BtB, tabB], writes=[xrB], sig=False)
            P.op("pe", lambda e: e.matmul(xr_[:, 0:w], E2[:, 2, :], bi, start=False, stop=True), reads=[BtB, tabB], writes=[xrB])
            P.op("pe", lambda e: e.matmul(xi_[:, 0:w], E2[:, 0, :], bi, start=True, stop=False), reads=[BtB, tabB], writes=[xiB], sig=False)
            P.op("pe", lambda e: e.matmul(xi_[:, 0:w], E2[:, 1, :], br, start=False, stop=True), reads=[BtB, tabB], writes=[xiB])
            return xr_, xrB, xi_, xiB

        def fft_fwd_g(Z, ZB, K, nch, Bt, BtB, tring):
            c = 0
            while c < nch:
                g = min(7, nch - c)
                pa, paB = ps_next()
                for u in range(g):
                    P.op("pe", lambda e, pa=pa, u=u, c=c: e.matmul(pa[:, u * 66:(u + 1) * 66], Z[0:K, c + u, :], E1[0:K, :], start=True, stop=True),
                         reads=[ZB, tabB], writes=[paB], sig=(u == g - 1))
                A = pa[:, 0:g * 66].rearrange("p (g r f) -> p g r f", r=2, f=33)
                t1, t1B, _ = tring.next(); t2, t2B, _ = tring.next()
                P.op("dve", lambda e, A=A, t1=t1, g=g: e.tensor_tensor(out=t1[:, 0:g], in0=A, in1=TW1[:, 0].unsqueeze(1).to_broadcast([128, g, 2, 33]), op=ALU.mult),
                     reads=[paB, tabB], writes=[t1B])
                P.op("dve", lambda e, A=A, t2=t2, g=g: e.tensor_tensor(out=t2[:, 0:g], in0=A, in1=TW1[:, 1].unsqueeze(1).to_broadcast([128, g, 2, 33]), op=ALU.mult),
                     reads=[paB, tabB], writes=[t2B])
                P.op("pool", lambda e, t1=t1, g=g, c=c: e.tensor_tensor(out=Bt[:, 0, c:c + g, :], in0=t1[:, 0:g, 0, :], in1=t1[:, 0:g, 1, :], op=ALU.subtract),
                     reads=[t1B], writes=[BtB])
                P.op("pool", lambda e, t2=t2, g=g, c=c: e.tensor_tensor(out=Bt[:, 1, c:c + g, :], in0=t2[:, 0:g, 0, :], in1=t2[:, 0:g, 1, :], op=ALU.add),
                     reads=[t2B], writes=[BtB])
                c += g
                yield

        zkr = Ring(P, 2, [64, 64, 128], BF16, "zk"); btr = Ring(P, 2, [128, 2, 64, 33], BF16, "bt", sem=False)
        kfo = Ring(P, 2, [128, 64, 2, 33], F32, "kfo")

        def kf_chain(o, hc):
            zk, zkB, zks = zkr.next()
            for q4 in range(2):
                P.dma("sp", zk[:, q4 * 32:(q4 + 1) * 32, :],
                      KERN[o, hc * 64 + q4 * 32:hc * 64 + (q4 + 1) * 32, :].rearrange("c (a b) -> a c b", b=128), zks, writes=[zkB])
            bt, btB, _ = btr.next()
            yield
            yield from fft_fwd_g(zk, zkB, 64, 64, bt, btB, tmr)
            ko, koB, kos = kfo.next()
            c0 = 0
            while c0 < 64:
                n = min(15, 64 - c0)
                xr_, xrB, xi_, xiB = fft_stage2(bt, btB, c0, n)
                P.op("dve", lambda e, ko=ko, xr_=xr_, c0=c0, n=n, o=o, hc=hc: e.tensor_tensor(
                    out=ko[:, c0:c0 + n, 0, :], in0=xr_[:, 0:n * 33].rearrange("p (c f) -> p c f", f=33),
                    in1=hb[:, o * 1024 + hc * 64 + c0:o * 1024 + hc * 64 + c0 + n].unsqueeze(2).to_broadcast([128, n, 33]), op=ALU.add),
                    reads=[xrB, tabB], writes=[koB])
                P.op("act", lambda e, ko=ko, xi_=xi_, c0=c0, n=n: e.copy(out=ko[:, c0:c0 + n, 1, :], in_=xi_[:, 0:n * 33].rearrange("p (c f) -> p c f", f=33)),
                     reads=[xiB], writes=[koB])
                c0 += n
                yield
            P.dma("act", KF[o, :, hc * 64:(hc + 1) * 64, :, :], ko[:], kos, reads=[koB])

        for hc in range(16):
            gens = [kf_chain(0, hc), kf_chain(1, hc)]
            alive = [True, True]
            while any(alive):
                for gi_ in range(2):
                    if alive[gi_]:
                        try:
                            next(gens[gi_])
                        except StopIteration:
                            alive[gi_] = False
        P.barrier(); P.reset(m4)
        if phases < 6:
            P.emit(); return nc

        NCG = 32
        def chain_bufs(p):
            d = {}
            d["zv"] = Ring(P, 1, [32, NCG, 128], BF16, f"zv{p}"); d["x1"] = Ring(P, 1, [32, NCG, 128], BF16, f"x1t{p}"); d["x2"] = Ring(P, 1, [32, NCG, 128], BF16, f"x2t{p}")
            d["k0"] = Ring(P, 1, [128, NCG, 2, 33], F32, f"kf0{p}"); d["k1"] = Ring(P, 1, [128, NCG, 2, 33], F32, f"kf1{p}")
            d["bt"] = Ring(P, 1, [128, 2, NCG, 33], BF16, f"btd{p}", sem=False); d["xp"] = Ring(P, 1, [128, NCG, 2, 33], BF16, f"xpt{p}", sem=False)
            d["zz"] = Ring(P, 1, [32, NCG, 128], BF16, f"zz{p}", sem=False); d["hy"] = Ring(P, 1, [32, NCG, 128], BF16, f"hyo{p}")
            return d
        CB = [chain_bufs(0), chain_bufs(1)]
        Rr = Ring(P, 3, [66, 2, 16, 128], BF16, "Rinv", sem=False)
        pwr = Ring(P, 6, [128, 15, 33], F32, "pwt", sem=False); sar = Ring(P, 6, [66, 2, 2, 128], F32, "sat", sem=False)
        tmr2 = Ring(P, 6, [128, 7, 2, 33], F32, "twtmp2", sem=False)

        def conv_g(cb, Zin, ZinB, kf_, kfB, gate, gateB, out_t, outB):
            bt, btB, _ = cb["bt"].next(); xp, xpB, _ = cb["xp"].next()
            yield from fft_fwd_g(Zin, ZinB, 32, NCG, bt, btB, tmr2)
            c0 = 0
            while c0 < NCG:
                n = min(15, NCG - c0)
                xr_, xrB, xi_, xiB = fft_stage2(bt, btB, c0, n)
                XR = xr_[:, 0:n * 33].rearrange("p (c f) -> p c f", f=33); XI = xi_[:, 0:n * 33].rearrange("p (c f) -> p c f", f=33)
                kr_ = kf_[:, c0:c0 + n, 0, :]; ki_ = kf_[:, c0:c0 + n, 1, :]
                ts = [pwr.next() for _ in range(4)]
                for (tb_, src, kk) in ((ts[0], XR, kr_), (ts[1], XI, ki_), (ts[2], XR, ki_), (ts[3], XI, kr_)):
                    P.op("dve", lambda e, tb_=tb_, src=src, kk=kk, n=n: e.tensor_tensor(out=tb_[0][:, 0:n, :], in0=src, in1=kk, op=ALU.mult),
                         reads=[xrB, xiB, kfB], writes=[tb_[1]])
                P.op("pool", lambda e, xp=xp, c0=c0, n=n, ts=ts: e.tensor_tensor(out=xp[:, c0:c0 + n, 0, :], in0=ts[0][0][:, 0:n, :], in1=ts[1][0][:, 0:n, :], op=ALU.subtract),
                     reads=[ts[0][1], ts[1][1]], writes=[xpB])
                P.op("pool", lambda e, xp=xp, c0=c0, n=n, ts=ts: e.tensor_tensor(out=xp[:, c0:c0 + n, 1, :], in0=ts[2][0][:, 0:n, :], in1=ts[3][0][:, 0:n, :], op=ALU.add),
                     reads=[ts[2][1], ts[3][1]], writes=[xpB])
                c0 += n
                yield
            for c16 in range(NCG // 16):
                R, RB, _ = Rr.next()
                for cp in range(8):
                    c = c16 * 16 + cp * 2
                    pa, paB = ps_next()
                    for u in range(2):
                        P.op("pe", lambda e, pa=pa, u=u, c=c: e.matmul(pa[0:66, u * 256:(u + 1) * 256], xp[:, c + u, :, :].rearrange("p r f -> p (r f)"),
                                                                     Ginv[:].rearrange("p r s -> p (r s)"), start=True, stop=True),
                             reads=[xpB, tabB], writes=[paB], sig=(u == 1))
                    Pv = pa[0:66, :].rearrange("p (g h s) -> p g h s", g=2, h=2)
                    q1 = sar.next(); q2 = sar.next()
                    P.op("dve", lambda e, Pv=Pv, q1=q1: e.tensor_tensor(out=q1[0][:], in0=Pv, in1=TW2[:, 0].unsqueeze(1).to_broadcast([66, 2, 2, 128]), op=ALU.mult),
                         reads=[paB, tabB], writes=[q1[1]])
                    P.op("dve", lambda e, Pv=Pv, q2=q2: e.tensor_tensor(out=q2[0][:], in0=Pv, in1=TW2[:, 1].unsqueeze(1).to_broadcast([66, 2, 2, 128]), op=ALU.mult),
                         reads=[paB, tabB], writes=[q2[1]])
                    P.op("pool", lambda e, R=R, q1=q1, cp=cp: e.tensor_tensor(out=R[:, 0, cp * 2:cp * 2 + 2, :], in0=q1[0][:, :, 0, :], in1=q1[0][:, :, 1, :], op=ALU.add),
                         reads=[q1[1]], writes=[RB])
                    P.op("pool", lambda e, R=R, q2=q2, cp=cp: e.tensor_tensor(out=R[:, 1, cp * 2:cp * 2 + 2, :], in0=q2[0][:, :, 0, :], in1=q2[0][:, :, 1, :], op=ALU.add),
                         reads=[q2[1]], writes=[RB])
                    yield
                for c4 in range(4):
                    c = c16 * 16 + c4 * 4
                    pa, paB = ps_next()
                    P.op("pe", lambda e, pa=pa, R=R, c4=c4: e.matmul(pa[0:32, :], LAB[:, 0, :], R[:, 0, c4 * 4:c4 * 4 + 4, :].rearrange("p c s -> p (c s)"), start=True, stop=False),
                         reads=[RB, tabB], writes=[paB], sig=False)
                    P.op("pe", lambda e, pa=pa, R=R, c4=c4: e.matmul(pa[0:32, :], LAB[:, 1, :], R[:, 1, c4 * 4:c4 * 4 + 4, :].rearrange("p c s -> p (c s)"), start=False, stop=True),
                         reads=[RB, tabB], writes=[paB])
                    P.op("dve", lambda e, pa=pa, c=c: e.tensor_tensor(out=out_t[:, c:c + 4, :], in0=pa[0:32, :].rearrange("p (c s) -> p c s", s=128),
                                                                      in1=gate[:, c:c + 4, :], op=ALU.mult),
                         reads=[paB, gateB], writes=[outB])
                    yield

        def chain_g(p, hc):
            cb = CB[p]; cb0 = hc * NCG
            zv, zvB, zvs = cb["zv"].next(); x1t, x1B, x1s = cb["x1"].next(); x2t, x2B, x2s = cb["x2"].next()
            k0, k0B, k0s = cb["k0"].next(); k1, k1B, k1s = cb["k1"].next()
            for (dst, dB, dsm, src) in ((zv, zvB, zvs, UU[0, cb0:cb0 + NCG, :]), (x1t, x1B, x1s, UU[1, cb0:cb0 + NCG, :]), (x2t, x2B, x2s, UU[2, cb0:cb0 + NCG, :])):
                P.dma("pool", dst[:], src.rearrange("c (a b) -> a c b", b=128), dsm, writes=[dB])
            P.dma("sp", k0[:], KF[0, :, cb0:cb0 + NCG, :, :], k0s, writes=[k0B])
            P.dma("sp", k1[:], KF[1, :, cb0:cb0 + NCG, :, :], k1s, writes=[k1B])
            zz, zzB, _ = cb["zz"].next(); hy, hyB, hys = cb["hy"].next()
            yield
            yield from conv_g(cb, zv, zvB, k0, k0B, x1t, x1B, zz, zzB)
            yield from conv_g(cb, zz, zzB, k1, k1B, x2t, x2B, hy, hyB)
            P.dma("act", HY[cb0:cb0 + NCG, :].rearrange("c (a b) -> a c b", b=128), hy[:], hys, reads=[hyB])

        for pr_ in range(D // NCG // 2):
            gens = [chain_g(0, 2 * pr_), chain_g(1, 2 * pr_ + 1)]
            alive = [True, True]
            while any(alive):
                for gi_ in range(2):
                    if alive[gi_]:
                        try:
                            next(gens[gi_])
                        except StopIteration:
                            alive[gi_] = False
        P.barrier(); P.reset(m_persist)
        if phases < 7:
            P.emit(); return nc

        posA = P.tile([128, 32, 64], F32, "posA"); gatA = P.tile([128, 32, 64], F32, "gatA"); pgB = Buf("posgate")
        slotI = P.tile([128, 256], I32, "slotI"); gkA = P.tile([128, 32, 8], F32, "gkA"); blkI = P.tile([128, NBLK], I32, "blkI"); metaB = Buf("meta")
        m_persist = P.mark()
        s_w = P.dsem("w5"); w5B = Buf("w5")
        wpa = P.tile([128, 8, D], BF16, "wpa"); wph = P.tile([128, 8, D], BF16, "wph"); wo = P.tile([128, 8, D], BF16, "wo")
        rwt = P.tile([128, 8, 64], F32, "rwt"); rbt = P.tile([128, 64], F32, "rbt")
        P.dma("pool", wpa[:], w_pa.rearrange("(k p) n -> p k n", p=128), s_w, writes=[w5B])
        P.dma("pool", wph[:], w_ph.rearrange("(k p) n -> p k n", p=128), s_w, writes=[w5B])
        P.dma("pool", wo[:], w_o.rearrange("(k p) n -> p k n", p=128), s_w, writes=[w5B])
        P.dma("sp", rwt[:], rw.rearrange("(k p) n -> p k n", p=128), s_w, writes=[w5B])
        P.dma("sp", rbt[:], rbias.partition_broadcast(128), s_w, writes=[w5B])
        selb = P.tile([128, 64], BF16, "selb"); ltri = P.tile([128, 128], BF16, "ltri"); onesb5 = P.tile([128, 128], BF16, "onesb5"); triB = Buf("tri")
        carry = P.tile([128, 64], F32, "carry"); carB = Buf("carry")
        P.op("pool", lambda e: e.memset(carry[:], 0.0), writes=[carB])
        P.op("pool", lambda e: e.memset(onesb5[:], 1.0), writes=[triB])
        P.op("pool", lambda e: e.memset(ltri[:], 1.0), writes=[triB])
        P.op("pool", lambda e: e.affine_select(out=ltri[:], in_=ltri[:], pattern=[[1, 128]], compare_op=ALU.is_gt, fill=0.0, base=0, channel_multiplier=-1),
             reads=[triB], writes=[triB])
        m5b = P.mark()
        htkr = Ring(P, 2, [128, D], BF16, "htk")
        axr = Ring(P, 1, [128, 8, 512], BF16, "axT"); hyr5 = Ring(P, 1, [128, 8, 512], BF16, "hyT"); gtr = Ring(P, 1, [128, 16, 512], BF16, "gt")
        xr5 = Ring(P, 1, [128, 8, 512], F32, "xt5")
        yT = P.tile([128, 8, 512], BF16, "yT"); yB = Buf("yT")
        x1r5 = Ring(P, 1, [128, 8, 512], F32, "x1T")
        sq5 = P.tile([128, 8, 512], F32, "sq5"); sq5B = Buf("sq5"); rstd5 = P.tile([128, 512], F32, "rstd5"); rs5B = Buf("rs5")
        hx2f = P.tile([128, 8, 512], F32, "hx2f"); hx2fB = Buf("hx2f")
        hx2b = Ring(P, 1, [128, 8, 512], BF16, "hx2b")
        y1r = Ring(P, 4, [128, 512], F32, "y1", sem=False)
        rt = [P.tile([128, 64], F32, f"rt{i}") for i in range(6)]; rtB = Buf("rt")
        rs_ = P.tile([128, 40], F32, "rsm")
        AX_v = AXs.rearrange("(k p) t -> p k t", p=128); HY_v = HY.rearrange("(k p) t -> p k t", p=128)
        GG_v = GG.rearrange("(k p) t -> p k t", p=128); X1_v = X1.rearrange("(k p) t -> p k t", p=128); HX2_v = HX2.rearrange("(k p) t -> p k t", p=128)
        for i in range(8):
            t0 = i * 512
            ax_, axB, axs = axr.next(); hy_, hyB5, hys5 = hyr5.next(); gt, gtB, gts = gtr.next(); xt5, xB5, xs5 = xr5.next()
            P.dma("sp", ax_[:], AX_v[:, :, t0:t0 + 512], axs, writes=[axB])
            P.dma("act", hy_[:], HY_v[:, :, t0:t0 + 512], hys5, writes=[hyB5])
            P.dma("sp", gt[:, 0:8, :], GG_v[:, 0:8, t0:t0 + 512], gts, writes=[gtB])
            P.dma("act", gt[:, 8:16, :], GG_v[:, 8:16, t0:t0 + 512], gts, writes=[gtB])
            P.dma("sp", xt5[:], xT_v[:, :, t0:t0 + 512], xs5, writes=[xB5])
            for dc in range(8):
                pa, paB = ps_next(); ph_, phB = ps_next()
                for k in range(8):
                    P.op("pe", lambda e, pa=pa, k=k, dc=dc, ax_=ax_: e.matmul(pa[:], wpa[:, k, dc * 128:(dc + 1) * 128], ax_[:, k, :], start=(k == 0), stop=(k == 7)),
                         reads=[w5B, axB], writes=[paB], sig=(k == 7))
                for k in range(8):
                    P.op("pe", lambda e, ph_=ph_, k=k, dc=dc, hy_=hy_: e.matmul(ph_[:], wph[:, k, dc * 128:(dc + 1) * 128], hy_[:, k, :], start=(k == 0), stop=(k == 7)),
                         reads=[w5B, hyB5], writes=[phB], sig=(k == 7))
                ya, yaB, _ = y1r.next(); yb_, ybB, _ = y1r.next()
                P.op("dve", lambda e, ya=ya, pa=pa, gt=gt, dc=dc: e.tensor_tensor(out=ya[:], in0=pa[:], in1=gt[:, dc, :], op=ALU.mult), reads=[paB, gtB], writes=[yaB])
                P.op("dve", lambda e, yb_=yb_, ph_=ph_, gt=gt, dc=dc: e.tensor_tensor(out=yb_[:], in0=ph_[:], in1=gt[:, 8 + dc, :], op=ALU.mult), reads=[phB, gtB], writes=[ybB])
                P.op("pool", lambda e, ya=ya, yb_=yb_, dc=dc: e.tensor_tensor(out=yT[:, dc, :], in0=ya[:], in1=yb_[:], op=ALU.add), reads=[yaB, ybB], writes=[yB])
            x1T, x1B5, x1s5 = x1r5.next()
            for dc in range(8):
                pm, pmB = ps_next()
                for k in range(8):
                    P.op("pe", lambda e, pm=pm, k=k, dc=dc: e.matmul(pm[:], wo[:, k, dc * 128:(dc + 1) * 128], yT[:, k, :], start=(k == 0), stop=(k == 7)),
                         reads=[w5B, yB], writes=[pmB], sig=(k == 7))
                P.op("dve", lambda e, pm=pm, dc=dc, x1T=x1T, xt5=xt5: e.scalar_tensor_tensor(out=x1T[:, dc, :], in0=pm[:], scalar=vec[:, 48 + dc:49 + dc], in1=xt5[:, dc, :],
                                                                                          op0=ALU.mult, op1=ALU.add), reads=[pmB, vecB, xB5], writes=[x1B5])
            P.dma("sp", X1_v[:, :, t0:t0 + 512], x1T[:], x1s5, reads=[x1B5])
            rms_rstd(x1T, x1B5, 512, sq=sq5, sqB=sq5B, rstd=rstd5, rsB=rs5B)
            P.op("dve", lambda e, x1T=x1T: e.tensor_tensor(out=sq5[:], in0=x1T[:], in1=rstd5[:].unsqueeze(1).to_broadcast([128, 8, 512]), op=ALU.mult),
                 reads=[x1B5, rs5B, sq5B], writes=[sq5B])
            hb2, hb2B, hb2s = hx2b.next()
            for k in range(8):
                P.op("act", lambda e, k=k: e.activation(out=hx2f[:, k, :], in_=sq5[:, k, :], func=AF.Identity, scale=vec[:, 32 + k:33 + k], bias=vec[:, 40 + k:41 + k]),
                     reads=[sq5B, vecB], writes=[hx2fB])
            P.op("pool", lambda e, hb2=hb2: e.tensor_copy(out=hb2[:], in_=hx2f[:]), reads=[hx2fB], writes=[hb2B])
            P.dma("act", HX2_v[:, :, t0:t0 + 512], hb2[:], hb2s, reads=[hb2B])
            for sb_ in range(4):
                st_ = i * 4 + sb_
                pr, prB = ps_next()
                for k in range(8):
                    P.op("pe", lambda e, pr=pr, k=k, sb_=sb_: e.matmul(pr[:, 0:64], hx2f[:, k, sb_ * 128:(sb_ + 1) * 128], rwt[:, k, :], start=(k == 0), stop=(k == 7)),
                         reads=[hx2fB, w5B], writes=[prB], sig=(k == 7))
                for hf in range(2):
                    ptk, ptkB = ps_next()
                    for kk in range(4):
                        k = hf * 4 + kk
                        P.op("pe", lambda e, ptk=ptk, k=k, kk=kk, sb_=sb_: e.transpose(out=ptk[:, kk * 128:(kk + 1) * 128], in_=hx2f[:, k, sb_ * 128:(sb_ + 1) * 128], identity=ident[:]),
                             reads=[hx2fB, identB], writes=[ptkB], sig=(kk == 3))
                    if hf == 0:
                        htk, htkB, htks = htkr.next()
                        P.op("act", lambda e, htk=htk, ptk=ptk: e.copy(out=htk[:, 0:512], in_=ptk[:]), reads=[ptkB], writes=[htkB])
                    else:
                        P.op("dve", lambda e, htk=htk, ptk=ptk: e.tensor_copy(out=htk[:, 512:1024], in_=ptk[:]), reads=[ptkB], writes=[htkB])
                P.dma("act", HX2tok[st_ * 128:(st_ + 1) * 128, :], htk[:], htks, reads=[htkB])
                sc_, ch_, c2_, msk, wse, gte = rt
                def R_(fn, eng="dve", rd=(), wr=()):
                    P.op(eng, fn, reads=[rtB] + list(rd), writes=[rtB] + list(wr))
                R_(lambda e, pr=pr: e.activation(out=sc_[:], in_=pr[:, 0:64], func=AF.Sigmoid), eng="act", rd=[prB])
                R_(lambda e: e.tensor_tensor(out=ch_[:], in0=sc_[:], in1=rbt[:], op=ALU.add), rd=[w5B])
                R_(lambda e: e.tensor_reduce(out=rs_[:, 0:8], in_=ch_[:].rearrange("p (g j) -> p g j", j=8), axis=AXL.X, op=ALU.max))
                R_(lambda e: e.tensor_tensor(out=c2_[:].rearrange("p (g j) -> p g j", j=8), in0=ch_[:].rearrange("p (g j) -> p g j", j=8),
                                             in1=rs_[:, 0:8].unsqueeze(2).to_broadcast([128, 8, 8]), op=ALU.is_equal))
                R_(lambda e: e.scalar_tensor_tensor(out=c2_[:], in0=c2_[:], scalar=-1.0e9, in1=ch_[:], op0=ALU.mult, op1=ALU.add))
                R_(lambda e: e.tensor_reduce(out=rs_[:, 8:16], in_=c2_[:].rearrange("p (g j) -> p g j", j=8), axis=AXL.X, op=ALU.max))
                R_(lambda e: e.tensor_tensor(out=rs_[:, 0:8], in0=rs_[:, 0:8], in1=rs_[:, 8:16], op=ALU.add))
                R_(lambda e: e.max(out=rs_[:, 16:24], in_=rs_[:, 0:8]))
                R_(lambda e: e.tensor_scalar(out=rs_[:, 8:16], in0=rs_[:, 0:8], scalar1=rs_[:, 19:20], scalar2=None, op0=ALU.is_ge))
                R_(lambda e: e.tensor_scalar(out=rs_[:, 8:16], in0=rs_[:, 8:16], scalar1=-1.0, scalar2=1.0e9, op0=ALU.add, op1=ALU.mult))
                R_(lambda e: e.tensor_tensor(out=msk[:].rearrange("p (g j) -> p g j", j=8), in0=ch_[:].rearrange("p (g j) -> p g j", j=8),
                                             in1=rs_[:, 8:16].unsqueeze(2).to_broadcast([128, 8, 8]), op=ALU.add))
                R_(lambda e: e.max(out=rs_[:, 24:32], in_=msk[:]))
                R_(lambda e: e.tensor_scalar(out=wse[:], in0=msk[:], scalar1=rs_[:, 31:32], scalar2=None, op0=ALU.is_ge))
                R_(lambda e: e.tensor_copy(out=selb[:], in_=wse[:]))
                R_(lambda e: e.tensor_tensor(out=wse[:], in0=wse[:], in1=sc_[:], op=ALU.mult))
                R_(lambda e: e.tensor_reduce(out=rs_[:, 32:33], in_=wse[:], axis=AXL.X, op=ALU.add))
                R_(lambda e: e.reciprocal(out=rs_[:, 33:34], in_=rs_[:, 32:33]))
                R_(lambda e, st_=st_: e.tensor_scalar(out=gatA[:, st_, :], in0=wse[:], scalar1=rs_[:, 33:34], scalar2=2.5, op0=ALU.mult, op1=ALU.mult), wr=[pgB])
                pp, ppB = ps_next()
                P.op("pe", lambda e, pp=pp: e.matmul(pp[:, 0:64], ltri[:], selb[:], start=True, stop=True), reads=[rtB, triB], writes=[ppB])
                P.op("pe", lambda e, pp=pp: e.matmul(pp[:, 64:128], onesb5[:], selb[:], start=True, stop=True), reads=[rtB, triB], writes=[ppB])
                P.op("dve", lambda e, pp=pp, st_=st_: e.tensor_tensor(out=posA[:, st_, :], in0=pp[:, 0:64], in1=carry[:], op=ALU.add), reads=[ppB, carB, pgB], writes=[pgB])
                P.op("dve", lambda e, pp=pp: e.tensor_tensor(out=carry[:], in0=pp[:, 64:128], in1=carry[:], op=ALU.add), reads=[ppB, carB], writes=[carB])
        P.barrier(); P.reset(m5b)
        W2K = 32 * 64
        slotf = P.tile([128, 32, 64], F32, "slotf"); keyt = P.tile([128, 32, 64], F32, "keyt"); eqt = P.tile([128, 32, 64], F32, "eqt"); m2B = Buf("meta2")
        rowA = P.tile([128, 64], F32, "rowA"); rowB = P.tile([128, 64], F32, "rowB"); rowI = P.tile([128, 64], I32, "rowI"); padd = P.tile([128, 64], F32, "padd")
        k8 = P.tile([128, 32, 8], F32, "k8"); skf = P.tile([128, 32, 8], F32, "skf")
        bvt = P.tile([128, NBLK], F32, "bvt"); blkf = P.tile([128, NBLK], F32, "blkf"); chg = P.tile([128, NBLK], F32, "chg"); pct = P.tile([128, 1], F32, "pct")
        cmpt = P.tile([128, 80, 64], F32, "cmpt")
        s_m = P.dsem("meta")
        P.dma("sp", bvt[:], bvals.partition_broadcast(128), s_m, writes=[m2B]); P.dma("sp", pct[:], pcol[:, :], s_m, writes=[m2B])
        def M_(fn, eng="dve", rd=(), wr=()):
            P.op(eng, fn, reads=[m2B] + list(rd), writes=[m2B] + list(wr))
        M_(lambda e: e.tensor_scalar(out=rowA[:], in0=carry[:], scalar1=1.0 / 128, scalar2=127.0 / 128 - 0.5 + 1.0 / 256, op0=ALU.mult, op1=ALU.add), rd=[carB])
        M_(lambda e: e.tensor_copy(out=rowI[:], in_=rowA[:]))
        M_(lambda e: e.tensor_copy(out=rowA[:], in_=rowI[:]))
        M_(lambda e: e.tensor_scalar(out=padd[:], in0=rowA[:], scalar1=128.0, scalar2=None, op0=ALU.mult))
        M_(lambda e: e.tensor_copy(out=rowA[:], in_=padd[:]))
        cur, oth = rowA, rowB
        for sh in (1, 2, 4, 8, 16, 32):
            M_(lambda e, cur=cur, oth=oth, sh=sh: e.tensor_copy(out=oth[:, 0:sh], in_=cur[:, 0:sh]))
            M_(lambda e, cur=cur, oth=oth, sh=sh: e.tensor_tensor(out=oth[:, sh:64], in0=cur[:, sh:64], in1=cur[:, 0:64 - sh], op=ALU.add))
            cur, oth = oth, cur
        pend = cur; pstart = oth
        M_(lambda e: e.tensor_tensor(out=pstart[:], in0=pend[:], in1=padd[:], op=ALU.subtract))
        M_(lambda e: e.tensor_tensor(out=slotf[:], in0=posA[:], in1=pstart[:].unsqueeze(1).to_broadcast([128, 32, 64]), op=ALU.add), rd=[pgB])
        M_(lambda e: e.tensor_scalar(out=keyt[:], in0=slotf[:], scalar1=-1.0, scalar2=BIGK + 1.0, op0=ALU.mult, op1=ALU.add))
        M_(lambda e: e.tensor_single_scalar(out=eqt[:], in_=gatA[:], scalar=0.0, op=ALU.is_gt), rd=[pgB])
        M_(lambda e: e.tensor_tensor(out=keyt[:], in0=keyt[:], in1=eqt[:], op=ALU.mult))
        M_(lambda e: e.tensor_scalar(out=keyt[:], in0=keyt[:], scalar1=-1.0, scalar2=None, op0=ALU.add))
        for st_ in range(32):
            M_(lambda e, st_=st_: e.max(out=k8[:, st_, :], in_=keyt[:, st_, :]))
        M_(lambda e: e.tensor_scalar(out=skf[:], in0=k8[:], scalar1=-1.0, scalar2=BIGK, op0=ALU.mult, op1=ALU.add))
        M_(lambda e: e.tensor_copy(out=slotI[:], in_=skf[:].rearrange("p a b -> p (a b)")), wr=[metaB])
        for k in range(8):
            M_(lambda e, k=k: e.tensor_tensor(out=eqt[:], in0=slotf[:], in1=skf[:, :, k:k + 1].to_broadcast([128, 32, 64]), op=ALU.is_equal))
            M_(lambda e: e.tensor_tensor(out=eqt[:], in0=eqt[:], in1=gatA[:], op=ALU.mult), rd=[pgB])
            M_(lambda e, k=k: e.tensor_reduce(out=gkA[:, :, k], in_=eqt[:], axis=AXL.X, op=ALU.add), wr=[metaB])
        for cq in range(NBLK // 80):
            M_(lambda e, cq=cq: e.tensor_tensor(out=cmpt[:], in0=pend[:].unsqueeze(1).to_broadcast([128, 80, 64]),
                                                in1=bvt[:, cq * 80:(cq + 1) * 80].unsqueeze(2).to_broadcast([128, 80, 64]), op=ALU.is_le))
            M_(lambda e, cq=cq: e.tensor_reduce(out=blkf[:, cq * 80:(cq + 1) * 80], in_=cmpt[:], axis=AXL.X, op=ALU.add))
        M_(lambda e: e.tensor_scalar(out=blkf[:], in0=blkf[:], scalar1=63.0, scalar2=128.0, op0=ALU.min, op1=ALU.mult))
        M_(lambda e: e.memset(chg[:, 0:NWB], 1.0))
        M_(lambda e: e.tensor_tensor(out=chg[:, NWB:NBLK], in0=blkf[:, NWB:NBLK], in1=blkf[:, 0:NBLK - NWB], op=ALU.not_equal))
        M_(lambda e: e.tensor_scalar(out=blkf[:], in0=blkf[:], scalar1=pct[:, 0:1], scalar2=-BIGI, op0=ALU.add, op1=ALU.add))
        M_(lambda e: e.tensor_tensor(out=blkf[:], in0=blkf[:], in1=chg[:], op=ALU.mult))
        M_(lambda e: e.tensor_scalar(out=blkf[:], in0=blkf[:], scalar1=BIGI, scalar2=None, op0=ALU.add))
        M_(lambda e: e.tensor_copy(out=blkI[:], in_=blkf[:]), wr=[metaB])
        dtr = Ring(P, 3, [128, D], BF16, "dtok")
        dsc = [P.dsem("scat") for _ in range(3)]
        for st_ in range(32):
            dt_, dtB, dts = dtr.next()
            P.dma("sp", dt_[:], HX2tok[st_ * 128:(st_ + 1) * 128, :], dts, writes=[dtB])
            for k in range(8):
                waits = P._deps("pool", [dtB, metaB] + zwin, [])
                ssem = dsc[dtr.i]
                ssem.n += 16
                P.q["pool"].append((waits, (lambda e, dt_=dt_, st_=st_, k=k: e.indirect_dma_start(
                    out=XG[:, :], out_offset=bass.IndirectOffsetOnAxis(ap=slotI[:, st_ * 8 + k:st_ * 8 + k + 1], axis=0), in_=dt_[:, :], in_offset=None,
                    bounds_check=P.reg(e, NSLOT - 1), oob_is_err=False)), (ssem.h, 16)))
                dtB.r.append((ssem, ssem.n))
        xgBs = [Buf(f"xg{i}") for i in range(3)]
        for i_, b_ in enumerate(xgBs):
            b_.w = (dsc[i_], dsc[i_].n)
        if debug:
            s_dbg = P.dsem("dbg")
            P.dma("sp", dbg["slot"].rearrange("p a b -> p (a b)"), slotI[:], s_dbg, reads=[metaB]); P.dma("sp", dbg["gk"][:, :, :], gkA[:], s_dbg, reads=[metaB])
            P.dma("sp", dbg["blk"][:, :], blkI[:], s_dbg, reads=[metaB])
        P.barrier(); P.reset(m_persist)
        if phases < 8:
            P.emit(); return nc

        wbuf = [(P.tile([128, 6144], BF16, f"wall{i}"), Buf(f"bw{i}"), P.dsem("bw")) for i in range(NWB)]
        for (a_, wB_, _) in wbuf:
            P.op("pool", lambda e, a_=a_: e.memset(a_[:], 0.0), writes=[wB_])
        xbr = Ring(P, 4, [128, D], BF16, "xb"); xgtr = Ring(P, 2, [128, 8, 128], BF16, "xgT", sem=False)
        sgr6 = Ring(P, 2, [128, 256], F32, "sg6", sem=False); atkr = Ring(P, 2, [128, 256], BF16, "atk", sem=False)
        atr = Ring(P, 2, [128, 2, 128], BF16, "aT", sem=False); ybr = Ring(P, 3, [128, D], F32, "yb")
        ygB = Buf("YG")
        identb = P.tile([128, 128], BF16, "identb"); identbB = Buf("identb")
        P.op("dve", lambda e: e.tensor_copy(out=identb[:], in_=ident[:]), reads=[identB], writes=[identbB])
        def block_g(b):
            wall, wB_, wsem = wbuf[b % NWB]
            wg_ = wall[:, 0:2048].rearrange("p (k h) -> p k h", k=8); wu_ = wall[:, 2048:4096].rearrange("p (k h) -> p k h", k=8)
            wd_ = wall[:, 4096:6144].rearrange("p (k d) -> p k d", k=2)
            waits = P._deps("pool", [metaB, wbB], [wB_])
            wsem.n += 16
            P.q["pool"].append((waits, (lambda e, wall=wall, b=b: e.indirect_dma_start(
                out=wall[:, :], out_offset=None, in_=WB[:, :], in_offset=bass.IndirectOffsetOnAxis(ap=blkI[:, b:b + 1], axis=0),
                bounds_check=P.reg(e, 65 * 128 - 1), oob_is_err=False)), (wsem.h, 16)))
            wB_.w = (wsem, wsem.n); wB_.r = []
            xb, xbB, xbs = xbr.next()
            P.dma("sp", xb[:], XG[b * 128:(b + 1) * 128, :], xbs, reads=xgBs, writes=[xbB])
            ptb, ptbB = ps_next()
            ptv = ptb[:].bitcast(BF16)
            for k in range(8):
                P.op("pe", lambda e, ptv=ptv, xb=xb, k=k: e.transpose(out=ptv[:, k * 128:(k + 1) * 128], in_=xb[:, k * 128:(k + 1) * 128], identity=identb[:]),
                     reads=[xbB, identbB], writes=[ptbB], sig=(k == 7))
            xgT, xgTB, _ = xgtr.next()
            P.op("dve", lambda e, xgT=xgT, ptv=ptv: e.tensor_copy(out=xgT[:].rearrange("p k s -> p (k s)"), in_=ptv), reads=[ptbB], writes=[xgTB])
            yield
            pg, pgB_ = ps_next(); pu, puB = ps_next()
            for k in range(8):
                P.op("pe", lambda e, pg=pg, wg_=wg_, k=k, xgT=xgT: e.matmul(pg[:, 0:256], xgT[:, k, :], wg_[:, k, :], start=(k == 0), stop=(k == 7)),
                     reads=[wB_, xgTB], writes=[pgB_], sig=(k == 7))
                P.op("pe", lambda e, pu=pu, wu_=wu_, k=k, xgT=xgT: e.matmul(pu[:, 0:256], xgT[:, k, :], wu_[:, k, :], start=(k == 0), stop=(k == 7)),
                     reads=[wB_, xgTB], writes=[puB], sig=(k == 7))
            sg, sgB, _ = sgr6.next(); atk, atkB, _ = atkr.next(); aT, aTB, _ = atr.next()
            P.op("act", lambda e, sg=sg, pg=pg: e.activation(out=sg[:], in_=pg[:, 0:256], func=AF.Silu), reads=[pgB_], writes=[sgB])
            P.op("dve", lambda e, atk=atk, sg=sg, pu=pu: e.tensor_tensor(out=atk[:], in0=pu[:, 0:256], in1=sg[:], op=ALU.mult), reads=[puB, sgB], writes=[atkB])
            yield
            pat, patB = ps_next()
            patv = pat[:].bitcast(BF16)
            for hh in range(2):
                P.op("pe", lambda e, patv=patv, atk=atk, hh=hh: e.transpose(out=patv[:, hh * 128:(hh + 1) * 128], in_=atk[:, hh * 128:(hh + 1) * 128], identity=identb[:]),
                     reads=[atkB, identbB], writes=[patB], sig=(hh == 1))
            P.op("act", lambda e, aT=aT, patv=patv: e.copy(out=aT[:].rearrange("p h s -> p (h s)"), in_=patv[:, 0:256]), reads=[patB], writes=[aTB])
            yield
            yb, ybB, ybs = ybr.next()
            for hf in range(2):
                py, pyB = ps_next()
                for hh in range(2):
                    P.op("pe", lambda e, py=py, aT=aT, wd_=wd_, hh=hh, hf=hf: e.matmul(py[:], aT[:, hh, :], wd_[:, hh, hf * 512:(hf + 1) * 512], start=(hh == 0), stop=(hh == 1)),
                         reads=[aTB, wB_], writes=[pyB], sig=(hh == 1))
                if hf == 0:
                    P.op("act", lambda e, yb=yb, py=py: e.copy(out=yb[:, 0:512], in_=py[:]), reads=[pyB], writes=[ybB])
                else:
                    P.op("dve", lambda e, yb=yb, py=py: e.tensor_copy(out=yb[:, 512:1024], in_=py[:]), reads=[pyB], writes=[ybB])
            P.dma("act", YG[b * 128:(b + 1) * 128, :], yb[:], ybs, reads=[ybB])

        live = []
        for b in range(NBLK + 4):
            if b < NBLK:
                live.append(block_g(b))
            nxt = []
            for g_ in live:
                try:
                    next(g_); nxt.append(g_)
                except StopIteration:
                    pass
            live = nxt
        P.barrier(); P.reset(m_persist)
        if debug:
            pass
        if phases < 9:
            P.emit(); return nc

        swg = P.tile([128, 8, 256], BF16, "swg"); swu = P.tile([128, 8, 256], BF16, "swu"); swd = P.tile([128, 2, D], BF16, "swd"); swB = Buf("sw"); s_sw = P.dsem("sw")
        P.dma("sp", swg[:].rearrange("p a b -> p (a b)"), WB[64 * 128:65 * 128, 0:2048], s_sw, reads=[wbB], writes=[swB])
        P.dma("sp", swu[:].rearrange("p a b -> p (a b)"), WB[64 * 128:65 * 128, 2048:4096], s_sw, reads=[wbB], writes=[swB])
        P.dma("sp", swd[:].rearrange("p a b -> p (a b)"), WB[64 * 128:65 * 128, 4096:6144], s_sw, reads=[wbB], writes=[swB])
        hxr = Ring(P, 2, [128, 8, 512], BF16, "hx6"); sg6 = Ring(P, 2, [128, 512], F32, "sgs", sem=False); aa6 = Ring(P, 2, [128, 512], BF16, "aas", sem=False)
        accS = P.tile([128, 8, 512], F32, "accS"); accSB = Buf("accS")
        ykr = Ring(P, 4, [128, D], F32, "yk"); atok = P.tile([128, D], F32, "atok"); atokB = Buf("atok")
        for t_ in ykr.t:
            P.op("pool", lambda e, t_=t_: e.memset(t_[:], 0.0), writes=[ykr.b[ykr.t.index(t_)]])
        x1l = Ring(P, 1, [128, 8, 512], F32, "x1l"); sq6 = P.tile([128, 8, 512], F32, "sq6"); sq6B = Buf("sq6")
        rstd6 = P.tile([128, 512], F32, "rstd6"); rs6B = Buf("rs6"); otr = Ring(P, 2, [128, 8, 512], F32, "outt")
        outT_v = outT.rearrange("(k p) t -> p k t", p=128)
        for i in range(8):
            t0 = i * 512
            hx_, hxB_, hxs_ = hxr.next()
            P.dma("sp", hx_[:], HX2_v[:, :, t0:t0 + 512], hxs_, writes=[hxB_])
            aas = []
            for hh in range(2):
                pg, pgB_ = ps_next(); pu, puB = ps_next()
                for k in range(8):
                    P.op("pe", lambda e, pg=pg, k=k, hh=hh, hx_=hx_: e.matmul(pg[:], swg[:, k, hh * 128:(hh + 1) * 128], hx_[:, k, :], start=(k == 0), stop=(k == 7)),
                         reads=[swB, hxB_], writes=[pgB_], sig=(k == 7))
                for k in range(8):
                    P.op("pe", lambda e, pu=pu, k=k, hh=hh, hx_=hx_: e.matmul(pu[:], swu[:, k, hh * 128:(hh + 1) * 128], hx_[:, k, :], start=(k == 0), stop=(k == 7)),
                         reads=[swB, hxB_], writes=[puB], sig=(k == 7))
                sg, sgB, _ = sg6.next(); aa, aaB, _ = aa6.next()
                P.op("act", lambda e, sg=sg, pg=pg: e.activation(out=sg[:], in_=pg[:], func=AF.Silu), reads=[pgB_], writes=[sgB])
                P.op("dve", lambda e, aa=aa, pu=pu, sg=sg: e.tensor_tensor(out=aa[:], in0=pu[:], in1=sg[:], op=ALU.mult), reads=[puB, sgB], writes=[aaB])
                aas.append((aa, aaB))
            for dc in range(8):
                pd, pdB = ps_next()
                for hh in range(2):
                    P.op("pe", lambda e, pd=pd, hh=hh, dc=dc, aa=aas[hh][0]: e.matmul(pd[:], swd[:, hh, dc * 128:(dc + 1) * 128], aa[:], start=(hh == 0), stop=(hh == 1)),
                         reads=[swB, aas[hh][1]], writes=[pdB], sig=(hh == 1))
                P.op("act", lambda e, pd=pd, dc=dc: e.copy(out=accS[:, dc, :], in_=pd[:]), reads=[pdB], writes=[accSB])
            for sb_ in range(4):
                st_ = i * 4 + sb_
                for k in range(8):
                    yk, ykB, yks = ykr.next()
                    waits = P._deps("pool", [metaB, ygB], [ykB])
                    yks.n += 16
                    P.q["pool"].append((waits, (lambda e, yk=yk, st_=st_, k=k: e.indirect_dma_start(
                        out=yk[:, :], out_offset=None, in_=YG[:, :], in_offset=bass.IndirectOffsetOnAxis(ap=slotI[:, st_ * 8 + k:st_ * 8 + k + 1], axis=0),
                        bounds_check=P.reg(e, NSLOT - 1), oob_is_err=False)), (yks.h, 16)))
                    ykB.w = (yks, yks.n); ykB.r = []
                    if k == 0:
                        P.op("dve", lambda e, yk=yk, st_=st_: e.tensor_scalar(out=atok[:], in0=yk[:], scalar1=gkA[:, st_, 0:1], scalar2=None, op0=ALU.mult),
                             reads=[ykB, metaB, atokB], writes=[atokB])
                    else:
                        P.op("dve", lambda e, yk=yk, st_=st_, k=k: e.scalar_tensor_tensor(out=atok[:], in0=yk[:], scalar=gkA[:, st_, k:k + 1], in1=atok[:], op0=ALU.mult, op1=ALU.add),
                             reads=[ykB, metaB, atokB], writes=[atokB])
                for kg in range(2):
                    ptk, ptkB = ps_next()
                    for kk in range(4):
                        k = kg * 4 + kk
                        P.op("pe", lambda e, ptk=ptk, k=k, kk=kk: e.transpose(out=ptk[:, kk * 128:(kk + 1) * 128], in_=atok[:, k * 128:(k + 1) * 128], identity=ident[:]),
                             reads=[atokB, identB], writes=[ptkB], sig=(kk == 3))
                    P.op("dve", lambda e, ptk=ptk, kg=kg, sb_=sb_: e.tensor_tensor(out=accS[:, kg * 4:(kg + 1) * 4, sb_ * 128:(sb_ + 1) * 128],
                                                                                in0=ptk[:].rearrange("p (k t) -> p k t", k=4),
                                                                                in1=accS[:, kg * 4:(kg + 1) * 4, sb_ * 128:(sb_ + 1) * 128], op=ALU.add),
                         reads=[ptkB, accSB], writes=[accSB])
            x1t_, x1B_, x1s_ = x1l.next()
            P.dma("sp", x1t_[:], X1_v[:, :, t0:t0 + 512], x1s_, writes=[x1B_])
            for k in range(8):
                P.op("dve", lambda e, k=k, x1t_=x1t_: e.scalar_tensor_tensor(out=x1t_[:, k, :], in0=accS[:, k, :], scalar=vec[:, 56 + k:57 + k], in1=x1t_[:, k, :],
                                                                           op0=ALU.mult, op1=ALU.add), reads=[accSB, vecB, x1B_], writes=[x1B_])
            rms_rstd(x1t_, x1B_, 512, sq=sq6, sqB=sq6B, rstd=rstd6, rsB=rs6B)
            P.op("dve", lambda e, x1t_=x1t_: e.tensor_tensor(out=sq6[:], in0=x1t_[:], in1=rstd6[:].unsqueeze(1).to_broadcast([128, 8, 512]), op=ALU.mult),
                 reads=[x1B_, rs6B, sq6B], writes=[sq6B])
            ot, otB, ots = otr.next()
            for k in range(8):
                P.op("act", lambda e, k=k, ot=ot: e.activation(out=ot[:, k, :], in_=sq6[:, k, :], func=AF.Copy, scale=vec[:, 68 + k:69 + k]),
                     reads=[sq6B, vecB], writes=[otB])
            P.dma("act", outT_v[:, :, t0:t0 + 512], ot[:], ots, reads=[otB])
        P.emit()
        nc._n_dsems = len(P.dsems)
    return nc


def prep_inputs(inputs):
    f = lambda a: np.ascontiguousarray(np.asarray(a, dtype=np.float32))
    x = f(inputs["x"]); ctx = f(inputs["ctx"]); c = f(inputs["c"]); c_ctx = f(inputs["c_ctx"])
    C, S = rope_tables()
    shared = {
        "ada_w": f(inputs["ada_w"][0]),
        "ada_bT": f(inputs["ada_b"][0].reshape(48, 128).T),
        "gvec": f(np.concatenate([np.asarray(inputs[k]).reshape(8, 128).T for k in ("norm1_g", "norm2_g", "final_norm_g")], axis=1)),
        "lamv": f(np.concatenate([np.asarray(inputs[k]).reshape(-1) for k in ("lam_q1", "lam_k1", "lam_q2", "lam_k2")])[None, :]),
        "w_in": f(inputs["w_in"][0]),
        "ropeC": C, "ropeS": S,
        "convw": f(np.asarray(inputs["hy_conv_w"][0]).reshape(3, 24, 128).transpose(2, 0, 1)),
        "convb": f(np.asarray(inputs["hy_conv_b"][0]).reshape(24, 128).T),
        "subln": f(np.asarray(inputs["subln_g"][0]).reshape(1, 128)),
        "sublnT": f(np.asarray(inputs["subln_g"][0]).reshape(128, 1)),
        "fw1": f(inputs["filt_w1"][0]), "fw2": f(inputs["filt_w2"][0]), "fw3": f(inputs["filt_w3"][0]),
        "fvec": f(np.stack([np.asarray(inputs[k][0]) for k in ("filt_b1", "filt_b2", "filt_freq")], axis=1)),
        "hyb": f(np.asarray(inputs["hy_bias"][0]).reshape(1, 2048)),
        "w_pa": f(inputs["w_branch_attn"][0]), "w_ph": f(inputs["w_branch_hyena"][0]), "w_o": f(inputs["w_out"][0]),
        "rw": f(inputs["router_w"][0]), "rbias": f(np.asarray(inputs["router_bias"][0]).reshape(1, 64)),
        "ewg": f(np.concatenate([inputs["exp_w_gate"][0], inputs["shared_w_gate"]], axis=0).reshape(65, 8, 128, 256).transpose(0, 2, 1, 3).reshape(65 * 128, 2048)),
        "ewu": f(np.concatenate([inputs["exp_w_up"][0], inputs["shared_w_up"]], axis=0).reshape(65, 8, 128, 256).transpose(0, 2, 1, 3).reshape(65 * 128, 2048)),
        "ewd": f(np.concatenate([inputs["exp_w_down"][0], inputs["shared_w_down"]], axis=0).reshape(65, 2, 128, 1024).transpose(0, 2, 1, 3).reshape(65 * 128, 2048)),
        "bvals": f((np.arange(NBLK) * 128.0).reshape(1, NBLK)), "pcol": f(np.arange(128.0).reshape(128, 1)),
    }
    shared.update(hyena_tables())
    maps = []
    for b in range(8):
        m = dict(shared)
        m["xT"] = np.ascontiguousarray(np.concatenate([x[b].T, ctx[b].T], axis=1))
        m["cc"] = np.ascontiguousarray(np.stack([c[b].reshape(8, 128).T, c_ctx.reshape(8, 128).T], axis=-1))
        maps.append(m)
    return maps


def kernel(**inputs):
    nc = build(DEBUG, PHASES)
    maps = prep_inputs(inputs)
    res = run_bass_kernel_spmd(nc, maps, core_ids=list(range(8)))
    out = np.stack([np.asarray(r["outT"]).T for r in res.results], axis=0)
    return np.ascontiguousarray(out.astype(np.float32))
```

```python
import math
from contextlib import ExitStack
import numpy as np
import concourse.bass as bass
import concourse.mybir as mybir
from concourse.bass_utils import run_bass_kernel_spmd

F32 = mybir.dt.float32; BF16 = mybir.dt.bfloat16; I32 = mybir.dt.int32
AF = mybir.ActivationFunctionType
ALU = mybir.AluOpType
AXL = mybir.AxisListType
DTSZ = {F32: 4, BF16: 2, I32: 4}

D = 1024; SEQ = 4096; CTX = 256; TOK = SEQ + CTX; NH = 8; INW = 8192
NBLK = 320; NSLOT = NBLK * 128; BIGK = 65536.0; BIGI = 1.0e6; NWB = 4
DEBUG = False
PHASES = 99


class Buf:
    __slots__ = ("name", "w", "r")

    def __init__(self, name=""):
        self.name = name; self.w = None; self.r = []


class Sem:
    def __init__(self, h):
        self.h = h; self.n = 0


class Prog:
    ENG = ("pe", "act", "dve", "pool", "sp")

    def __init__(self, nc, es):
        self.nc = nc; self.es = es
        self.q = {e: [] for e in self.ENG}
        self.esem = {e: Sem(es.enter_context(nc.semaphore("prog_" + e))) for e in self.ENG}
        self.known = {e: {} for e in self.ENG}
        self.dsems = []
        self.off = 16640; self.ntile = 0
        self.cap = nc.SBUF_PARTITION_SIZE_BYTES
        self.ninst = 0

    def tile(self, shape, dtype, name=None):
        nb = int(np.prod(shape[1:])) * DTSZ[dtype]
        nb = (nb + 63) // 64 * 64
        assert self.off + nb <= self.cap, f"SBUF overflow {self.off}+{nb} > {self.cap} ({name})"
        self.ntile += 1
        t = self.nc.alloc_sbuf_tensor_at(f"{name or 't'}_{self.ntile}", list(shape), dtype, offset=self.off)
        self.off += nb
        return t

    def mark(self):
        return self.off

    def reset(self, m):
        self.off = m

    def dsem(self, name="d"):
        s = Sem(self.es.enter_context(self.nc.semaphore(f"{name}_{len(self.dsems)}")))
        self.dsems.append(s); return s

    def _deps(self, eng, reads, writes):
        deps = []
        for b in reads:
            if b.w is not None: deps.append(b.w)
        for b in writes:
            if b.w is not None: deps.append(b.w)
            deps.extend(b.r)
        best = {}
        for (s, v) in deps:
            k = id(s)
            if k not in best or best[k][1] < v: best[k] = (s, v)
        waits = []
        kn = self.known[eng]
        for (s, v) in best.values():
            if eng == "pe" and s is self.esem["pe"]: continue
            if kn.get(id(s), 0) >= v: continue
            kn[id(s)] = v
            waits.append((s, v))
        return waits

    def op(self, eng, fn, reads=(), writes=(), sig=True):
        waits = self._deps(eng, reads, writes)
        S = self.esem[eng]
        if sig: S.n += 1
        tok = (S, S.n if sig else S.n + 1)
        self.q[eng].append((waits, fn, (S.h, 1) if sig else None))
        for b in reads: b.r.append(tok)
        for b in writes:
            b.w = tok; b.r = []
        self.ninst += 1
        return tok

    def dma(self, eng, out, in_, sem, reads=(), writes=(), **kw):
        waits = self._deps(eng, reads, writes)
        sem.n += 16
        tok = (sem, sem.n)
        self.q[eng].append((waits, lambda e: e.dma_start(out=out, in_=in_, **kw), (sem.h, 16)))
        for b in reads: b.r.append(tok)
        for b in writes:
            b.w = tok; b.r = []
        self.ninst += 1
        return tok

    def reg(self, e, v):
        if not hasattr(self, "_regs"): self._regs = {}
        if v not in self._regs: self._regs[v] = e.to_reg(v)
        return self._regs[v]

    def barrier(self):
        allsem = [self.esem[e] for e in self.ENG] + self.dsems
        for e in self.ENG:
            waits = []
            kn = self.known[e]
            for s in allsem:
                if s.n > 0 and kn.get(id(s), 0) < s.n and not (s is self.esem[e]):
                    kn[id(s)] = s.n; waits.append((s, s.n))
            if waits: self.q[e].append((waits, None, None))

    def emit(self):
        nc = self.nc
        self.barrier()
        with nc.Block() as block:
            def run(engname):
                def f(e):
                    for (waits, fn, inc) in self.q[engname]:
                        for (s, v) in waits: e.wait_ge(s.h, v)
                        if fn is None: continue
                        ins = fn(e)
                        if inc is not None: ins.then_inc(inc[0], inc[1])
                return f
            block.tensor(run("pe")); block.scalar(run("act")); block.vector(run("dve"))
            block.gpsimd(run("pool")); block.sync(run("sp"))


class Ring:
    def __init__(self, P, n, shape, dtype, name, sem=True):
        self.t = [P.tile(shape, dtype, f"{name}{i}") for i in range(n)]
        self.b = [Buf(f"{name}{i}") for i in range(n)]
        self.s = [P.dsem(name) for i in range(n)] if sem else [None] * n
        self.i = -1; self.n = n

    def next(self):
        self.i = (self.i + 1) % self.n
        return self.t[self.i], self.b[self.i], self.s[self.i]


def rope_tables():
    p = np.arange(128); d = p % 64
    axis = d // 32; half = (d % 32) // 16; fr = d % 16
    inv = (10000.0 ** (-(np.arange(0, 32, 2, dtype=np.float32)) / 32.0)).astype(np.float32)
    t = np.arange(SEQ)
    row = (t // 64).astype(np.float32); col = (t % 64).astype(np.float32)
    pos = np.where(axis[:, None] == 0, row[None, :], col[None, :]).astype(np.float32)
    ang = (pos * inv[fr][:, None]).astype(np.float32)
    C = np.cos(ang).astype(np.float32)
    S = np.sin(ang).astype(np.float32) * np.where(half == 0, -1.0, 1.0)[:, None].astype(np.float32)
    return np.ascontiguousarray(C), np.ascontiguousarray(S.astype(np.float32))


def hyena_tables():
    n = SEQ; N = 2 * SEQ
    tp = np.arange(N)
    pos = np.where(tp < n, tp, N - tp).astype(np.float32)
    pos[n] = 0.0
    tt = (pos / np.float32(n - 1)).astype(np.float32)
    w = (np.float32(2 * math.pi) * pos / np.float32(n)).astype(np.float32)
    bands = np.linspace(1e-4, 15.0, 16, dtype=np.float32)
    bw = (bands[None, :] * w[:, None]).astype(np.float32)
    z = np.concatenate([tt[:, None], np.cos(bw), -np.sin(bw)], axis=1).astype(np.float32)
    ttx = tt.copy(); ttx[n] = 1.0e4
    deltas = np.abs(np.linspace(math.log(1e-2) / 1.5, math.log(1e-2) / 0.3, D, dtype=np.float32)).astype(np.float32)
    T = {}
    T["zextT"] = np.ascontiguousarray(z.T)
    T["ttx"] = np.ascontiguousarray(ttx[None, :])
    T["negdelta"] = np.ascontiguousarray((-deltas).reshape(8, 128).T)
    s1 = np.arange(64)[:, None].astype(np.float64); f1 = np.arange(33)[None, :].astype(np.float64)
    a = 2 * np.pi * s1 * f1 / 64
    T["E1"] = np.concatenate([np.cos(a), -np.sin(a)], axis=1).astype(np.float32)
    s2 = np.arange(128)[:, None].astype(np.float64)
    a = 2 * np.pi * s2 * f1 / N
    Tr, Ti = np.cos(a), -np.sin(a)
    T["TW1"] = np.stack([np.stack([Tr, Ti], 1), np.stack([Ti, Tr], 1)], 1).astype(np.float32)
    f2 = np.arange(128)[None, :].astype(np.float64)
    a = 2 * np.pi * s2 * f2 / 128
    T["E2"] = np.stack([np.cos(a), -np.sin(a), np.sin(a)], 1).astype(np.float32)
    T["Ginv"] = np.stack([np.cos(a.T), np.sin(a.T)], 1).astype(np.float32)
    f1c = np.arange(33)[:, None].astype(np.float64); s2r = np.arange(128)[None, :].astype(np.float64)
    a = 2 * np.pi * f1c * s2r / N
    Tc, Ts = np.cos(a), np.sin(a)
    W1 = np.concatenate([np.stack([Tc, -Ts], 1), np.stack([-Ts, -Tc], 1)], 0)
    W2 = np.concatenate([np.stack([Ts, Tc], 1), np.stack([Tc, -Ts], 1)], 0)
    T["TW2"] = np.stack([W1, W2], 1).astype(np.float32)
    wt = np.full(33, 2.0); wt[0] = 1.0; wt[32] = 1.0
    s1r = np.arange(32)[None, :].astype(np.float64)
    a = 2 * np.pi * f1c * s1r / 64
    la = wt[:, None] * np.cos(a) / N; lb = -wt[:, None] * np.sin(a) / N
    T["LAB"] = np.stack([np.concatenate([la, la], 0), np.concatenate([lb, lb], 0)], 1).astype(np.float32)
    return {k: np.ascontiguousarray(v) for k, v in T.items()}


def build(debug=False, phases=99):
    nc = bass.Bass("TRN2", target_bir_lowering=False)
    skind = "ExternalOutput" if debug else "Internal"

    def din(name, shape, dt=F32):
        return nc.dram_tensor(name, list(shape), dt, kind="ExternalInput").ap()

    def dscr(name, shape, dt):
        return nc.dram_tensor(name, list(shape), dt, kind=skind).ap()

    xT = din("xT", [D, TOK]); cc = din("cc", [128, 8, 2]); ada_w = din("ada_w", [D, 6 * D])
    ada_bT = din("ada_bT", [128, 48]); gvec = din("gvec", [128, 24]); lamv = din("lamv", [1, 256])
    w_in = din("w_in", [D, INW]); ropeC = din("ropeC", [128, SEQ]); ropeS = din("ropeS", [128, SEQ])
    convw = din("convw", [128, 3, 24]); convb = din("convb", [128, 24]); subln = din("subln", [1, 128]); sublnT = din("sublnT", [128, 1])
    zextT = din("zextT", [33, 2 * SEQ]); ttx = din("ttx", [1, 2 * SEQ]); negdelta = din("negdelta", [128, 8])
    E1d = din("E1", [64, 66]); TW1d = din("TW1", [128, 2, 2, 33]); E2d = din("E2", [128, 3, 128]); Ginvd = din("Ginv", [128, 2, 128])
    TW2d = din("TW2", [66, 2, 2, 128]); LABd = din("LAB", [66, 2, 32])
    w_pa = din("w_pa", [D, D]); w_ph = din("w_ph", [D, D]); w_o = din("w_o", [D, D]); rw = din("rw", [D, 64]); rbias = din("rbias", [1, 64])
    ewg = din("ewg", [65 * 128, 2048]); ewu = din("ewu", [65 * 128, 2048]); ewd = din("ewd", [65 * 128, 2048])
    bvals = din("bvals", [1, NBLK]); pcol = din("pcol", [128, 1])
    fw1 = din("fw1", [33, 64]); fw2 = din("fw2", [64, 64]); fw3 = din("fw3", [64, 4096]); fvec = din("fvec", [64, 3]); hyb = din("hyb", [1, 2048])
    outT = nc.dram_tensor("outT", [D, SEQ], F32, kind="ExternalOutput").ap()

    QT = dscr("QT", [NH, 128, SEQ], BF16); KT = dscr("KT", [NH, 128, TOK], BF16)
    AXs = dscr("AXs", [D, SEQ], BF16); KERN = dscr("KERN", [2, D, 2 * SEQ], BF16)
    KF = dscr("KF", [2, 128, D, 2, 33], F32); HY = dscr("HY", [D, SEQ], BF16)
    X1 = dscr("X1", [D, SEQ], F32); HX2 = dscr("HX2", [D, SEQ], BF16); HX2tok = dscr("HX2tok", [SEQ, D], BF16)
    XG = dscr("XG", [NSLOT, D], BF16); YG = dscr("YG", [NSLOT, D], F32); WB = dscr("WB", [65 * 128, 6144], BF16)
    VV = dscr("VV", [NH, 128, 34, 128], BF16); UU = dscr("UU", [3, D, SEQ], F32); GG = dscr("GG", [2 * D, SEQ], BF16)
    dbg = {}
    if debug:
        dbg["hx"] = nc.dram_tensor("dbg_hx", [128, 8, TOK], BF16, kind="ExternalOutput").ap()
        dbg["vec"] = nc.dram_tensor("dbg_vec", [128, 80], F32, kind="ExternalOutput").ap()
        dbg["slot"] = nc.dram_tensor("dbg_slot", [128, 32, 8], I32, kind="ExternalOutput").ap()
        dbg["gk"] = nc.dram_tensor("dbg_gk", [128, 32, 8], F32, kind="ExternalOutput").ap()
        dbg["blk"] = nc.dram_tensor("dbg_blk", [128, NBLK], I32, kind="ExternalOutput").ap()
        dbg["acc"] = nc.dram_tensor("dbg_acc", [2, 128, 8, 2048], F32, kind="ExternalOutput").ap()

    with ExitStack() as es:
        P = Prog(nc, es)
        psb = [nc.alloc_psum_tensor(f"psb{i}", [128, 512], F32) for i in range(8)]
        psB = [Buf(f"ps{i}") for i in range(8)]
        pi = [0]

        def ps_next():
            pi[0] = (pi[0] + 1) % 8
            return psb[pi[0]], psB[pi[0]]

        vec = P.tile([128, 80], F32, "vec"); vecB = Buf("vec")
        ones = P.tile([128, 128], F32, "ones"); onesB = Buf("ones")
        ident = P.tile([128, 128], F32, "ident"); identB = Buf("ident")
        P.op("pool", lambda e: e.memset(ones[:], 1.0), writes=[onesB])
        P.op("pool", lambda e: e.memset(ident[:], 0.0), writes=[identB])
        P.op("pool", lambda e: e.affine_select(out=ident[:], in_=ident[:], pattern=[[-1, 128]], compare_op=ALU.not_equal,
                                               fill=1.0, base=0, channel_multiplier=1), reads=[identB], writes=[identB])
        g08 = P.tile([128, 128], F32, "g08"); g08B = Buf("g08")
        m_persist = P.mark()
        hxT = P.tile([128, 8, TOK], BF16, "hxT"); hxB = [Buf(f"hx{i}") for i in range(9)]
        m_hx = P.mark()

        cct = P.tile([128, 8, 2], F32, "cct"); sil = P.tile([128, 8, 2], F32, "sil"); cB = Buf(); silB = Buf()
        adab = P.tile([128, 48], F32, "adab"); gv = P.tile([128, 24], F32, "gv"); lamt = P.tile([128, 256], F32, "lamt")
        modT = P.tile([128, 48, 2], F32, "modT"); modB = Buf()
        smB = Buf(); s_small = P.dsem("small"); s_cc = P.dsem("cc")
        P.dma("sp", cct[:], cc[:, :, :], s_cc, writes=[cB])
        P.dma("sp", adab[:], ada_bT[:, :], s_small, writes=[smB])
        P.dma("sp", gv[:], gvec[:, :], s_small, writes=[smB])
        P.dma("sp", lamt[:], lamv.partition_broadcast(128), s_small, writes=[smB])
        P.op("act", lambda e: e.activation(out=sil[:], in_=cct[:], func=AF.Sigmoid), reads=[cB], writes=[silB])
        P.op("dve", lambda e: e.tensor_tensor(out=sil[:], in0=sil[:], in1=cct[:], op=ALU.mult), reads=[cB, silB], writes=[silB])
        aw = Ring(P, 2, [128, 8, 1024], F32, "adaw")
        psm, psmB = ps_next()
        adaw_v = ada_w.rearrange("(k p) f -> p k f", p=128)
        for g in range(6):
            wt, wB, ws = aw.next()
            P.dma("sp" if g % 2 == 0 else "act", wt[:], adaw_v[:, :, g * 1024:(g + 1) * 1024], ws, writes=[wB])
            for fc in range(8):
                f = g * 8 + fc
                for k in range(8):
                    P.op("pe", lambda e, wt=wt, k=k, fc=fc, f=f: e.matmul(psm[:, 2 * f:2 * f + 2], wt[:, k, fc * 128:(fc + 1) * 128],
                                                                         sil[:, k, :], start=(k == 0), stop=(k == 7)),
                         reads=[wB, silB], writes=[psmB], sig=(k == 7))
        P.op("dve", lambda e: e.tensor_tensor(out=modT[:], in0=psm[:, 0:96].rearrange("p (f j) -> p f j", j=2),
                                              in1=adab[:].unsqueeze(2).to_broadcast([128, 48, 2]), op=ALU.add),
             reads=[psmB, smB], writes=[modB])
        def vop(fn, rd=(modB, smB)):
            P.op("dve", fn, reads=list(rd) + [vecB], writes=[vecB])
        vop(lambda e: e.scalar_tensor_tensor(out=vec[:, 0:8], in0=modT[:, 8:16, 0], scalar=1.0, in1=gv[:, 0:8], op0=ALU.add, op1=ALU.mult))
        vop(lambda e: e.tensor_copy(out=vec[:, 8:16], in_=modT[:, 0:8, 0]))
        vop(lambda e: e.scalar_tensor_tensor(out=vec[:, 16:24], in0=modT[:, 8:16, 1], scalar=1.0, in1=gv[:, 0:8], op0=ALU.add, op1=ALU.mult))
        vop(lambda e: e.tensor_copy(out=vec[:, 24:32], in_=modT[:, 0:8, 1]))
        vop(lambda e: e.scalar_tensor_tensor(out=vec[:, 32:40], in0=modT[:, 32:40, 0], scalar=1.0, in1=gv[:, 8:16], op0=ALU.add, op1=ALU.mult))
        vop(lambda e: e.tensor_copy(out=vec[:, 40:48], in_=modT[:, 24:32, 0]))
        vop(lambda e: e.tensor_copy(out=vec[:, 48:56], in_=modT[:, 16:24, 0]))
        vop(lambda e: e.tensor_copy(out=vec[:, 56:64], in_=modT[:, 40:48, 0]))
        vop(lambda e: e.tensor_copy(out=vec[:, 68:76], in_=gv[:, 16:24]))
        lt = P.tile([128, 128], F32, "lt"); ltB = Buf()
        P.op("dve", lambda e: e.tensor_tensor(out=lt[:].rearrange("p (a b) -> p a b", a=2),
                                              in0=lamt[:].rearrange("p (a c b) -> p a c b", a=2, c=2)[:, :, 0, :],
                                              in1=lamt[:].rearrange("p (a c b) -> p a c b", a=2, c=2)[:, :, 1, :], op=ALU.mult),
             reads=[smB], writes=[ltB])
        P.op("dve", lambda e: e.tensor_reduce(out=vec[:, 66:68], in_=lt[:].rearrange("p (a b) -> p a b", a=2), axis=AXL.X, op=ALU.add),
             reads=[ltB, vecB], writes=[vecB])
        P.op("act", lambda e: e.activation(out=vec[:, 66:68], in_=vec[:, 66:68], func=AF.Exp), reads=[vecB], writes=[vecB])
        P.op("dve", lambda e: e.scalar_tensor_tensor(out=vec[:, 64:65], in0=vec[:, 67:68], scalar=-0.2, in1=vec[:, 66:67],
                                                     op0=ALU.add, op1=ALU.subtract), reads=[vecB], writes=[vecB])
        if debug:
            P.dma("sp", dbg["vec"][:, :], vec[:], s_small, reads=[vecB])

        xr = Ring(P, 2, [128, 8, 512], F32, "xt"); sq = P.tile([128, 8, 512], F32, "sq"); sqB = Buf()
        rstd = P.tile([128, 512], F32, "rstd"); rsB = Buf()
        xT_v = xT.rearrange("(k p) t -> p k t", p=128)

        def rms_rstd(src_t, src_b, n, sq=sq, sqB=sqB, rstd=rstd, rsB=rsB, eps=1e-6, dim=1024.0):
            P.op("act", lambda e: e.activation(out=sq[:, :, 0:n], in_=src_t[:, :, 0:n], func=AF.Square), reads=[src_b], writes=[sqB])
            pt, pB = ps_next()
            for k in range(8):
                P.op("pe", lambda e, k=k: e.matmul(pt[:, 0:n], ones[:], sq[:, k, 0:n], start=(k == 0), stop=(k == 7)),
                     reads=[sqB, onesB], writes=[pB], sig=(k == 7))
            P.op("dve", lambda e: e.tensor_scalar(out=rstd[:, 0:n], in0=pt[:, 0:n], scalar1=1.0 / dim, scalar2=eps, op0=ALU.mult, op1=ALU.add),
                 reads=[pB], writes=[rsB])
            P.op("act", lambda e: e.activation(out=rstd[:, 0:n], in_=rstd[:, 0:n], func=AF.Sqrt), reads=[rsB], writes=[rsB])
            P.op("dve", lambda e: e.reciprocal(out=rstd[:, 0:n], in_=rstd[:, 0:n]), reads=[rsB], writes=[rsB])

        for i in range(9):
            t0 = i * 512; n = 512 if i < 8 else 256
            xt, xB, xs = xr.next()
            P.dma("sp", xt[:, :, 0:n], xT_v[:, :, t0:t0 + n], xs, writes=[xB])
            rms_rstd(xt, xB, n)
            P.op("dve", lambda e, xt=xt, n=n: e.tensor_tensor(out=sq[:, :, 0:n], in0=xt[:, :, 0:n],
                                                              in1=rstd[:, 0:n].unsqueeze(1).to_broadcast([128, 8, n]), op=ALU.mult),
                 reads=[xB, rsB, sqB], writes=[sqB])
            ao = 0 if i < 8 else 16
            for k in range(8):
                P.op("act", lambda e, k=k, t0=t0, n=n, ao=ao: e.activation(out=hxT[:, k, t0:t0 + n], in_=sq[:, k, 0:n], func=AF.Identity,
                                                                             scale=vec[:, ao + k:ao + k + 1], bias=vec[:, ao + 8 + k:ao + 9 + k]),
                     reads=[sqB, vecB], writes=[hxB[i]])
        if debug:
            P.dma("sp", dbg["hx"][:, :, :], hxT[:], s_small, reads=hxB)
        P.barrier()
        P.reset(m_hx)
        if phases < 2:
            P.emit(); return nc

        m2 = P.mark()
        wr = Ring(P, 2, [128, 8, 1024], BF16, "wg"); wperm = P.tile([128, 8, 1024], BF16, "wperm"); wpB = Buf()
        cw = P.tile([128, 3, 24], F32, "cw"); cb = P.tile([128, 24], F32, "cb"); cwB = Buf()
        s_cw = P.dsem("cw"); s_rope = P.dsem("rope")
        P.dma("act", cw[:], convw[:, :, :], s_cw, writes=[cwB])
        P.dma("act", cb[:], convb[:, :], s_cw, writes=[cwB])
        ob = Ring(P, 4, [128, 512], BF16, "ob")
        vst = Ring(P, 2, [128, 1024], BF16, "vst")
        m2b = P.mark()
        rC = P.tile([128, SEQ], F32, "rC"); rS = P.tile([128, SEQ], F32, "rS"); ropB = Buf()
        P.dma("act", rC[:], ropeC[:, :], s_rope, writes=[ropB])
        P.dma("act", rS[:], ropeS[:, :], s_rope, writes=[ropB])
        t1r = Ring(P, 4, [128, 512], F32, "t1", sem=False)
        win_v = w_in.rearrange("(k p) n -> p k n", p=128)
        alt = [0]
        for g in (0, 2, 1, 3, 4, 5, 6, 7):
            if g == 1:
                P.barrier(); P.reset(m2b)
                pbuf = P.tile([128, SEQ + 2], F32, "pbuf"); pbB = Buf()
                ur = Ring(P, 2, [128, SEQ], F32, "ubuf")
                P.op("pool", lambda e: e.memset(pbuf[:, 0:1], 0.0), writes=[pbB])
                P.op("pool", lambda e: e.memset(pbuf[:, SEQ + 1:SEQ + 2], 0.0), writes=[pbB])
            wt, wB, ws = wr.next()
            P.dma("pool", wt[:], win_v[:, :, g * 1024:(g + 1) * 1024], ws, writes=[wB])
            if g in (0, 2):
                wv = wt[:].rearrange("p k (b h j) -> p k b h j", h=2, j=16)
                pv = wperm[:].rearrange("p k (b h j) -> p k b h j", h=2, j=16)
                for k in range(8):
                    P.op("pool", lambda e, wv=wv, k=k: e.tensor_copy(out=pv[:, k, :, 0, :], in_=wv[:, k, :, 1, :]), reads=[wB], writes=[wpB])
                    P.op("pool", lambda e, wv=wv, k=k: e.tensor_copy(out=pv[:, k, :, 1, :], in_=wv[:, k, :, 0, :]), reads=[wB], writes=[wpB])
                dst = KT if g == 0 else QT
                for h in range(8):
                    for i in range(9 if g == 0 else 8):
                        t0 = i * 512; n = 512 if i < 8 else 256
                        pa, paB = ps_next()
                        for k in range(8):
                            P.op("pe", lambda e, pa=pa, wt=wt, k=k, h=h, t0=t0, n=n: e.matmul(pa[:, 0:n], wt[:, k, h * 128:(h + 1) * 128],
                                                                                               hxT[:, k, t0:t0 + n], start=(k == 0), stop=(k == 7)),
                                 reads=[wB, hxB[i]], writes=[paB], sig=(k == 7))
                        o, oB, osem = ob.next()
                        if i < 8:
                            pb_, pbB_ = ps_next()
                            for k in range(8):
                                P.op("pe", lambda e, pb_=pb_, k=k, h=h, t0=t0, n=n: e.matmul(pb_[:, 0:n], wperm[:, k, h * 128:(h + 1) * 128],
                                                                                              hxT[:, k, t0:t0 + n], start=(k == 0), stop=(k == 7)),
                                     reads=[wpB, hxB[i]], writes=[pbB_], sig=(k == 7))
                            ta, taB, _ = t1r.next(); tb, tbB, _ = t1r.next()
                            P.op("dve", lambda e, ta=ta, pa=pa, t0=t0: e.tensor_tensor(out=ta[:], in0=pa[:], in1=rC[:, t0:t0 + 512], op=ALU.mult),
                                 reads=[paB, ropB], writes=[taB])
                            P.op("dve", lambda e, tb=tb, pb_=pb_, t0=t0: e.tensor_tensor(out=tb[:], in0=pb_[:], in1=rS[:, t0:t0 + 512], op=ALU.mult),
                                 reads=[pbB_, ropB], writes=[tbB])
                            P.op("pool", lambda e, o=o, ta=ta, tb=tb: e.tensor_tensor(out=o[:], in0=ta[:], in1=tb[:], op=ALU.add),
                                 reads=[taB, tbB], writes=[oB])
                        else:
                            P.op("act", lambda e, o=o, pa=pa, n=n: e.copy(out=o[:, 0:n], in_=pa[:, 0:n]), reads=[paB], writes=[oB])
                        P.dma("sp", dst[h, :, t0:t0 + n], o[:, 0:n], osem, reads=[oB])
            elif g == 1:
                for j in range(34):
                    vt, vB, vs = vst.next()
                    for hf in range(2):
                        pa, paB = ps_next()
                        for k in range(8):
                            P.op("pe", lambda e, pa=pa, wt=wt, k=k, j=j, hf=hf: e.matmul(pa[:], hxT[:, k, j * 128:(j + 1) * 128],
                                                                                         wt[:, k, hf * 512:(hf + 1) * 512], start=(k == 0), stop=(k == 7)),
                                 reads=[wB, hxB[j // 4]], writes=[paB], sig=(k == 7))
                        if hf == 0:
                            P.op("act", lambda e, vt=vt, pa=pa: e.copy(out=vt[:, 0:512], in_=pa[:]), reads=[paB], writes=[vB])
                        else:
                            P.op("dve", lambda e, vt=vt, pa=pa: e.tensor_copy(out=vt[:, 512:1024], in_=pa[:]), reads=[paB], writes=[vB])
                    P.dma("sp", VV[:, :, j, :].rearrange("h p e -> p h e"), vt[:].rearrange("p (h e) -> p h e", h=NH), vs, reads=[vB])
            elif g in (3, 4, 5):
                for c in range(8):
                    ch = (g - 3) * 8 + c
                    for i in range(8):
                        t0 = i * 512
                        pa, paB = ps_next()
                        for k in range(8):
                            P.op("pe", lambda e, pa=pa, wt=wt, k=k, c=c, t0=t0: e.matmul(pa[:], wt[:, k, c * 128:(c + 1) * 128],
                                                                                         hxT[:, k, t0:t0 + 512], start=(k == 0), stop=(k == 7)),
                                 reads=[wB, hxB[i]], writes=[paB], sig=(k == 7))
                        P.op("act", lambda e, pa=pa, t0=t0: e.copy(out=pbuf[:, 1 + t0:1 + t0 + 512], in_=pa[:]), reads=[paB], writes=[pbB])
                    u, uB, us = ur.next()
                    eng = "dve"
                    P.op(eng, lambda e, u=u, ch=ch: e.tensor_scalar(out=u[:], in0=pbuf[:, 0:SEQ], scalar1=cw[:, 0, ch:ch + 1], scalar2=cb[:, ch:ch + 1],
                                                                    op0=ALU.mult, op1=ALU.add), reads=[pbB, cwB], writes=[uB])
                    P.op(eng, lambda e, u=u, ch=ch: e.scalar_tensor_tensor(out=u[:], in0=pbuf[:, 1:SEQ + 1], scalar=cw[:, 1, ch:ch + 1], in1=u[:],
                                                                           op0=ALU.mult, op1=ALU.add), reads=[pbB, cwB, uB], writes=[uB])
                    P.op(eng, lambda e, u=u, ch=ch: e.scalar_tensor_tensor(out=u[:], in0=pbuf[:, 2:SEQ + 2], scalar=cw[:, 2, ch:ch + 1], in1=u[:],
                                                                           op0=ALU.mult, op1=ALU.add), reads=[pbB, cwB, uB], writes=[uB])
                    P.dma("sp", UU[g - 3, c * 128:(c + 1) * 128, :], u[:], us, reads=[uB])
            else:
                for c in range(8):
                    for i in range(8):
                        t0 = i * 512
                        pa, paB = ps_next()
                        for k in range(8):
                            P.op("pe", lambda e, pa=pa, wt=wt, k=k, c=c, t0=t0: e.matmul(pa[:], wt[:, k, c * 128:(c + 1) * 128],
                                                                                         hxT[:, k, t0:t0 + 512], start=(k == 0), stop=(k == 7)),
                                 reads=[wB, hxB[i]], writes=[paB], sig=(k == 7))
                        o, oB, osem = ob.next()
                        P.op("act", lambda e, o=o, pa=pa: e.activation(out=o[:], in_=pa[:], func=AF.Sigmoid), reads=[paB], writes=[oB])
                        P.dma("sp", GG[(g - 6) * 1024 + c * 128:(g - 6) * 1024 + (c + 1) * 128, t0:t0 + 512], o[:], osem, reads=[oB])
        P.barrier()
        P.reset(m_persist)
        if phases < 3:
            P.emit(); return nc

        s_g = P.dsem("g08")
        g08c = P.tile([128, 1], F32, "g08c")
        P.dma("sp", g08c[:], sublnT[:, :], s_g, writes=[g08B])
        P.op("dve", lambda e: e.tensor_scalar(out=g08c[:], in0=g08c[:], scalar1=0.8, scalar2=None, op0=ALU.mult), reads=[g08B], writes=[g08B])
        qzr = [Ring(P, 2, [128, SEQ], BF16, f"qz{m}") for m in range(2)]
        kr = Ring(P, 2, [128, TOK], BF16, "kT"); vr = Ring(P, 2, [128, 34, 128], BF16, "vh")
        for m in range(2):
            for bi_ in range(2):
                t_ = qzr[m].t[bi_]
                P.op("pool", lambda e, t_=t_, m=m: e.memset(t_[64 * (1 - m):64 * (1 - m) + 64, :], 0.0), writes=[qzr[m].b[bi_]])
        onesb = P.tile([128, 128], BF16, "onesb"); onesbB = Buf("onesb")
        P.op("pool", lambda e: e.memset(onesb[:], 1.0), writes=[onesbB])
        ptr = Ring(P, 3, [128, 34, 512], BF16, "pT", sem=False)
        zfl = P.tile([128, 2048], BF16, "zfl"); ztB = Buf("zfl"); xgB = Buf("XG"); s_z = P.dsem("zfill")
        P.op("pool", lambda e, zfl=zfl: e.memset(zfl[:], 0.0), writes=[ztB])
        XG_z = XG.rearrange("(p a) d -> p (a d)", p=128)
        zwin = [Buf(f"zw{i}") for i in range(4)]; zsem = [s_z] + [P.dsem("zfill") for _ in range(3)]
        for zi in range(NBLK * D // 2048):
            P.dma("pool", XG_z[:, zi * 2048:(zi + 1) * 2048], zfl[:], zsem[zi % 4], reads=[ztB], writes=[zwin[zi % 4]])
        wstg = Ring(P, 1, [128, 6144], BF16, "wstg"); s_wb = P.dsem("wbst"); wbB = Buf("WB")
        def precast(e_):
            st_t, st_B, st_s = wstg.next()
            P.dma("pool", st_t[:, 0:2048], ewg[e_ * 128:(e_ + 1) * 128, :], st_s, writes=[st_B])
            P.dma("pool", st_t[:, 2048:4096], ewu[e_ * 128:(e_ + 1) * 128, :], st_s, reads=[], writes=[])
            P.dma("pool", st_t[:, 4096:6144], ewd[e_ * 128:(e_ + 1) * 128, :], st_s, reads=[], writes=[])
            st_B.w = (st_s, st_s.n)
            P.dma("pool", WB[e_ * 128:(e_ + 1) * 128, :], st_t[:], s_wb, reads=[st_B], writes=([wbB] if e_ == 64 else []))
        tq = P.tile([128, 512], F32, "tq"); tqB = Buf("tq")
        csr = Ring(P, 2, [128, 2, 512], F32, "csum", sem=False)
        rr = Ring(P, 2, [128, 512], F32, "rrec", sem=False)
        oo = P.tile([128, 512], F32, "oo"); ooB = Buf("oo"); o2 = P.tile([128, 512], F32, "o2"); o2B = Buf("o2")
        axo = Ring(P, 2, [128, 512], BF16, "axo")
        SB = [1, 2, 3]; TB = 0; NDV = 28
        OB = {0: (4, 5), 1: (6, 7)}
        sidx = [0]

        def load_head(h):
            k, kB, ks_ = kr.next(); v, vB, vs_ = vr.next()
            qs = []
            for m in range(2):
                q, qB, qs_ = qzr[m].next()
                P.dma("sp" if m == 0 else "act", q[64 * m:64 * m + 64, :], QT[h, 64 * m:64 * m + 64, :], qs_, writes=[qB])
                qs.append((q, qB))
            P.dma("sp", k[:], KT[h, :, :], ks_, writes=[kB])
            P.dma("act", v[:], VV[h, :, :, :], vs_, writes=[vB])
            return (qs, k, kB, v, vB)

        def qk_exp_steps(hd, i, m, pt, ptB):
            qs, k, kB, v, vB = hd
            q, qB = qs[m]
            steps = []
            for j in range(34):
                def st(j=j):
                    b = SB[sidx[0] % 3]; sidx[0] += 1
                    P.op("pe", lambda e: e.matmul(psb[b][:], k[:, j * 128:(j + 1) * 128], q[:, i * 512:(i + 1) * 512], start=True, stop=True),
                         reads=[kB, qB], writes=[psB[b]])
                    P.op("act", lambda e: e.activation(out=pt[:, j, :], in_=psb[b][:], func=AF.Exp, scale=0.125),
                         reads=[psB[b]], writes=[ptB])
                steps.append(st)
            return steps

        def av_steps(hd, m, pt, ptB):
            qs, k, kB, v, vB = hd
            ob_, sb_ = OB[m]
            cs, csB, _ = csr.next()
            steps = []
            for j in range(34):
                def st(j=j):
                    P.op("pe", lambda e: e.matmul(psb[ob_][:], v[:, j, :], pt[:, j, :], start=(j == 0), stop=(j == 33)),
                         reads=[ptB, vB], writes=[psB[ob_]], sig=(j == 33))
                    if j == 1:
                        P.op("dve", lambda e: e.tensor_copy(out=cs[:], in_=pt[:, 0:2, :]), reads=[ptB], writes=[csB])
                    elif j % 2 == 1 and j < NDV:
                        P.op("dve", lambda e: e.tensor_tensor(out=cs[:], in0=pt[:, j - 1:j + 1, :], in1=cs[:], op=ALU.add), reads=[ptB, csB], writes=[csB])
                    if j == NDV - 1:
                        P.op("dve", lambda e: e.tensor_tensor(out=cs[:, 0, :], in0=cs[:, 0, :], in1=cs[:, 1, :], op=ALU.add), reads=[csB], writes=[csB])
                    if j >= NDV:
                        P.op("pe", lambda e: e.matmul(psb[sb_][:], onesb[:], pt[:, j, :], start=(j == NDV), stop=False),
                             reads=[ptB, onesbB], writes=[psB[sb_]], sig=False)
                    if j == 33:
                        P.op("pe", lambda e: e.matmul(psb[sb_][:], ones[:], cs[:, 0, :], start=False, stop=True), reads=[csB, onesB], writes=[psB[sb_]])
                steps.append(st)
            return steps

        def combine_a():
            ob_, sb_ = OB[0]
            r, rB, _ = rr.next()
            P.op("dve", lambda e: e.reciprocal(out=r[:], in_=psb[sb_][:]), reads=[psB[sb_]], writes=[rB])
            P.op("dve", lambda e: e.tensor_tensor(out=tq[:], in0=psb[ob_][:], in1=r[:], op=ALU.mult), reads=[psB[ob_], rB, tqB], writes=[tqB])

        def combine_b(h, i):
            ob_, sb_ = OB[1]
            r, rB, _ = rr.next()
            P.op("dve", lambda e: e.reciprocal(out=r[:], in_=psb[sb_][:]), reads=[psB[sb_]], writes=[rB])
            P.op("dve", lambda e: e.tensor_tensor(out=r[:], in0=psb[ob_][:], in1=r[:], op=ALU.mult), reads=[psB[ob_], rB], writes=[rB])
            P.op("dve", lambda e: e.scalar_tensor_tensor(out=oo[:], in0=r[:], scalar=vec[:, 64:65], in1=tq[:], op0=ALU.mult, op1=ALU.add),
                 reads=[rB, vecB, tqB, ooB], writes=[ooB])
            P.op("pool", lambda e: e.tensor_tensor(out=o2[:], in0=oo[:], in1=oo[:], op=ALU.mult), reads=[ooB, o2B], writes=[o2B])
            P.op("pe", lambda e: e.matmul(psb[TB][:], ones[:], o2[:], start=True, stop=True), reads=[o2B, onesB], writes=[psB[TB]])
            P.op("dve", lambda e: e.tensor_scalar(out=o2[:], in0=psb[TB][:], scalar1=1.0 / 128, scalar2=1e-6, op0=ALU.mult, op1=ALU.add),
                 reads=[psB[TB], o2B], writes=[o2B])
            P.op("act", lambda e: e.activation(out=o2[:], in_=o2[:], func=AF.Ln), reads=[o2B], writes=[o2B])
            P.op("act", lambda e: e.activation(out=o2[:], in_=o2[:], func=AF.Exp, scale=-0.5), reads=[o2B], writes=[o2B])
            ao, aoB, aos = axo.next()
            P.op("dve", lambda e: e.scalar_tensor_tensor(out=ao[:], in0=oo[:], scalar=g08c[:, 0:1], in1=o2[:], op0=ALU.mult, op1=ALU.mult),
                 reads=[ooB, o2B, g08B], writes=[aoB])
            P.dma("sp", AXs[h * 128:(h + 1) * 128, i * 512:(i + 1) * 512], ao[:], aos, reads=[aoB])

        stages = [(h, i, m) for h in range(NH) for i in range(8) for m in range(2)]
        heads = {0: load_head(0)}
        prev = None
        for si, (h, i, m) in enumerate(stages):
            if i == 1 and m == 0 and h + 1 < NH:
                heads[h + 1] = load_head(h + 1)
            if si % 2 == 0:
                precast(si // 2)
            pt, ptB, _ = ptr.next()
            qk = qk_exp_steps(heads[h], i, m, pt, ptB)
            av_prev, post_prev = prev if prev is not None else ([], None)
            for j in range(34):
                qk[j]()
                if av_prev: av_prev[j]()
            if post_prev is not None: post_prev()
            post = (lambda: combine_a()) if m == 0 else (lambda h=h, i=i: combine_b(h, i))
            prev = (av_steps(heads[h], m, pt, ptB), post)
        for st in prev[0]: st()
        prev[1]()
        precast(64)
        P.barrier()
        P.reset(m_persist)
        if phases < 4:
            P.emit(); return nc

        TWO_PI = 2.0 * math.pi
        s_t = P.dsem("tabs"); tabB = Buf("tabs")
        E1 = P.tile([64, 66], BF16, "E1"); TW1 = P.tile([128, 2, 2, 33], F32, "TW1"); E2 = P.tile([128, 3, 128], BF16, "E2")
        Ginv = P.tile([128, 2, 128], BF16, "Ginv"); TW2 = P.tile([66, 2, 2, 128], F32, "TW2"); LAB = P.tile([66, 2, 32], BF16, "LAB")
        hb = P.tile([128, 2048], F32, "hb"); ndl = P.tile([128, 8], F32, "ndl")
        P.dma("pool", E1[:], E1d[:, :], s_t, writes=[tabB]); P.dma("sp", TW1[:], TW1d[:, :, :, :], s_t, writes=[tabB])
        P.dma("pool", E2[:], E2d[:, :, :], s_t, writes=[tabB]); P.dma("pool", Ginv[:], Ginvd[:, :, :], s_t, writes=[tabB])
        P.dma("sp", TW2[:], TW2d[:, :, :, :], s_t, writes=[tabB]); P.dma("pool", LAB[:], LABd[:, :, :], s_t, writes=[tabB])
        P.dma("sp", hb[:], hyb.partition_broadcast(128), s_t, writes=[tabB]); P.dma("sp", ndl[:], negdelta[:, :], s_t, writes=[tabB])
        m4 = P.mark()

        hd2T = P.tile([64, 2 * SEQ], BF16, "hd2T"); hd2B = Buf("hd2")
        w3t = P.tile([64, 4096], BF16, "w3t"); w3B = Buf("w3")
        s_w3 = P.dsem("w3")
        P.dma("pool", w3t[:], fw3[:, :], s_w3, writes=[w3B])
        m4a = P.mark()
        zt = P.tile([33, 2 * SEQ], F32, "zt"); w1t = P.tile([33, 64], F32, "w1t"); w2t = P.tile([64, 64], F32, "w2t"); fv = P.tile([64, 3], F32, "fv")
        fB = Buf("filt_in")
        s_f = P.dsem("filt")
        P.dma("sp", zt[:], zextT[:, :], s_f, writes=[fB]); P.dma("sp", w1t[:], fw1[:, :], s_f, writes=[fB])
        P.dma("sp", w2t[:], fw2[:, :], s_f, writes=[fB]); P.dma("sp", fv[:], fvec[:, :], s_f, writes=[fB])
        ar = Ring(P, 2, [64, 512], F32, "marg", sem=False); kir = Ring(P, 2, [64, 512], I32, "mki", sem=False)
        kfr = Ring(P, 2, [64, 512], F32, "mkf", sem=False); h1r = Ring(P, 2, [64, 512], F32, "mh1", sem=False)

        def sin_layer(ps_, psB_, bcol, out_ap, outB):
            a, aB, _ = ar.next(); ki, kiB, _ = kir.next(); kf_, kfB, _ = kfr.next()
            P.op("dve", lambda e: e.tensor_scalar(out=a[:], in0=ps_[0:64, :], scalar1=fv[:, bcol:bcol + 1], scalar2=fv[:, 2:3], op0=ALU.add, op1=ALU.mult),
                 reads=[psB_, fB], writes=[aB])
            P.op("dve", lambda e: e.tensor_scalar(out=ki[:], in0=a[:], scalar1=1.0 / TWO_PI, scalar2=None, op0=ALU.mult), reads=[aB], writes=[kiB])
            P.op("dve", lambda e: e.tensor_copy(out=kf_[:], in_=ki[:]), reads=[kiB], writes=[kfB])
            P.op("dve", lambda e: e.scalar_tensor_tensor(out=a[:], in0=kf_[:], scalar=-TWO_PI, in1=a[:], op0=ALU.mult, op1=ALU.add),
                 reads=[kfB, aB], writes=[aB])
            P.op("dve", lambda e: e.tensor_scalar(out=a[:], in0=a[:], scalar1=math.pi, scalar2=-math.pi, op0=ALU.min, op1=ALU.max), reads=[aB], writes=[aB])
            P.op("act", lambda e: e.activation(out=out_ap, in_=a[:], func=AF.Sin), reads=[aB], writes=[outB])

        for tt in range(16):
            pa, paB = ps_next()
            P.op("pe", lambda e, pa=pa, tt=tt: e.matmul(pa[0:64, :], w1t[:], zt[:, tt * 512:(tt + 1) * 512], start=True, stop=True),
                 reads=[fB], writes=[paB])
            h1, h1B, _ = h1r.next()
            sin_layer(pa, paB, 0, h1[:], h1B)
            pb_, pbB_ = ps_next()
            P.op("pe", lambda e, pb_=pb_, h1=h1: e.matmul(pb_[0:64, :], w2t[:], h1[:], start=True, stop=True), reads=[fB, h1B], writes=[pbB_])
            sin_layer(pb_, pbB_, 1, hd2T[:, tt * 512:(tt + 1) * 512], hd2B)
        P.barrier(); P.reset(m4a)

        ttb = P.tile([128, 2 * SEQ], F32, "ttb"); ttB = Buf("ttb")
        s_tt = P.dsem("ttb")
        P.dma("sp", ttb[:], ttx.partition_broadcast(128), s_tt, writes=[ttB])
        kur = Ring(P, 2, [128, 2 * SEQ], F32, "ku", sem=False); kbr = Ring(P, 2, [128, 2 * SEQ], BF16, "kub")
        dkr = Ring(P, 3, [128, 512], F32, "dk", sem=False); abr = Ring(P, 3, [128, 512], F32, "kab", sem=False)
        asum = P.tile([128, 32], F32, "asum"); asB = Buf("asum")
        asum2 = P.tile([128, 2, 32], F32, "asum2")
        for cc in range(8):
            kus = [kur.next() for _ in range(2)]
            for tt in range(16):
                dr = 0 if tt < 8 else 1
                dk, dkB, _ = dkr.next()
                P.op("act", lambda e, dk=dk, tt=tt, cc=cc: e.activation(out=dk[:], in_=ttb[:, tt * 512:(tt + 1) * 512], func=AF.Exp, scale=ndl[:, cc:cc + 1]),
                     reads=[ttB, tabB], writes=[dkB])
                for o in range(2):
                    ku, kuB, _ = kus[o]
                    col0 = o * 2048 + dr * 1024 + cc * 128
                    pa, paB = ps_next()
                    P.op("pe", lambda e, pa=pa, col0=col0, tt=tt: e.matmul(pa[:], w3t[:, col0:col0 + 128], hd2T[:, tt * 512:(tt + 1) * 512], start=True, stop=True),
                         reads=[w3B, hd2B], writes=[paB])
                    P.op("dve", lambda e, ku=ku, pa=pa, dk=dk, tt=tt: e.tensor_tensor(out=ku[:, tt * 512:(tt + 1) * 512], in0=pa[:], in1=dk[:], op=ALU.mult),
                         reads=[paB, dkB], writes=[kuB])
                    ab, abB, _ = abr.next()
                    P.op("act", lambda e, ab=ab, ku=ku, tt=tt: e.activation(out=ab[:], in_=ku[:, tt * 512:(tt + 1) * 512], func=AF.Abs), reads=[kuB], writes=[abB])
                    P.op("dve", lambda e, ab=ab, tt=tt, o=o: e.tensor_reduce(out=asum2[:, o, tt:tt + 1], in_=ab[:], axis=AXL.X, op=ALU.add), reads=[abB, asB], writes=[asB])
            for o in range(2):
                ku, kuB, _ = kus[o]
                P.op("dve", lambda e, o=o: e.tensor_reduce(out=asum2[:, o, 16:17], in_=asum2[:, o, 0:16], axis=AXL.X, op=ALU.add), reads=[asB], writes=[asB])
                P.op("dve", lambda e, o=o: e.reciprocal(out=asum2[:, o, 17:18], in_=asum2[:, o, 16:17]), reads=[asB], writes=[asB])
                kb, kbB, kbs = kbr.next()
                P.op("act", lambda e, kb=kb, ku=ku, o=o: e.activation(out=kb[:], in_=ku[:], func=AF.Copy, scale=asum2[:, o, 17:18]),
                     reads=[kuB, asB], writes=[kbB])
                P.dma("sp", KERN[o, cc * 128:(cc + 1) * 128, :], kb[:], kbs, reads=[kbB])
        P.barrier(); P.reset(m4)
        if phases < 5:
            P.emit(); return nc

        tmr = Ring(P, 4, [128, 7, 2, 33], F32, "twtmp", sem=False)

        def fft_fwd(Z, ZB, K, nch, Bt, BtB):
            c = 0
            while c < nch:
                g = min(7, nch - c)
                pa, paB = ps_next()
                for u in range(g):
                    P.op("pe", lambda e, pa=pa, u=u, c=c: e.matmul(pa[:, u * 66:(u + 1) * 66], Z[0:K, c + u, :], E1[0:K, :], start=True, stop=True),
                         reads=[ZB, tabB], writes=[paB], sig=(u == g - 1))
                A = pa[:, 0:g * 66].rearrange("p (g r f) -> p g r f", r=2, f=33)
                t1, t1B, _ = tmr.next(); t2, t2B, _ = tmr.next()
                P.op("dve", lambda e, A=A, t1=t1, g=g: e.tensor_tensor(out=t1[:, 0:g], in0=A, in1=TW1[:, 0].unsqueeze(1).to_broadcast([128, g, 2, 33]), op=ALU.mult),
                     reads=[paB, tabB], writes=[t1B])
                P.op("dve", lambda e, A=A, t2=t2, g=g: e.tensor_tensor(out=t2[:, 0:g], in0=A, in1=TW1[:, 1].unsqueeze(1).to_broadcast([128, g, 2, 33]), op=ALU.mult),
                     reads=[paB, tabB], writes=[t2B])
                P.op("pool", lambda e, t1=t1, g=g, c=c: e.tensor_tensor(out=Bt[:, 0, c:c + g, :], in0=t1[:, 0:g, 0, :], in1=t1[:, 0:g, 1, :], op=ALU.subtract),
                     reads=[t1B], writes=[BtB])
                P.op("pool", lambda e, t2=t2, g=g, c=c: e.tensor_tensor(out=Bt[:, 1, c:c + g, :], in0=t2[:, 0:g, 0, :], in1=t2[:, 0:g, 1, :], op=ALU.add),
                     reads=[t2B], writes=[BtB])
                c += g

        def fft_stage2(Bt, BtB, c0, n):
            xr_, xrB = ps_next(); xi_, xiB = ps_next()
            br = Bt[:, 0, c0:c0 + n, :].rearrange("p c f -> p (c f)"); bi = Bt[:, 1, c0:c0 + n, :].rearrange("p c f -> p (c f)")
            w = n * 33
            P.op("pe", lambda e: e.matmul(xr_[:, 0:w], E2[:, 0, :], br, start=True, stop=False), reads=[BtB, tabB], writes=[xrB], sig=False)
            P.op("pe", lambda e: e.matmul(xr_[:, 0:w], E2[:, 2, :], bi, start=False, stop=True), reads=[BtB, tabB], writes=[xrB])
            P.op("pe", lambda e: e.matmul(xi_[:, 0:w], E2[:, 0, :], bi, start=True, stop=False), reads=[BtB, tabB], writes=[xiB], sig=False)
            P.op("pe", lambda e: e.matmul(xi_[:, 0:w], E2[:, 1, :], br, start=False, stop=True), reads=[BtB, tabB], writes=[xiB])
            return xr_, xrB, xi_, xiB

        def fft_fwd_g(Z, ZB, K, nch, Bt, BtB, tring):
            c = 0
            while c < nch:
                g = min(7, nch - c)
                pa, paB = ps_next()
                for u in range(g):
                    P.op("pe", lambda e, pa=pa, u=u, c=c: e.matmul(pa[:, u * 66:(u + 1) * 66], Z[0:K, c + u, :], E1[0:K, :], start=True, stop=True),
                         reads=[ZB, tabB], writes=[paB], sig=(u == g - 1))
                A = pa[:, 0:g * 66].rearrange("p (g r f) -> p g r f", r=2, f=33)
                t1, t1B, _ = tring.next(); t2, t2B, _ = tring.next()
                P.op("dve", lambda e, A=A, t1=t1, g=g: e.tensor_tensor(out=t1[:, 0:g], in0=A, in1=TW1[:, 0].unsqueeze(1).to_broadcast([128, g, 2, 33]), op=ALU.mult),
                     reads=[paB, tabB], writes=[t1B])
                P.op("dve", lambda e, A=A, t2=t2, g=g: e.tensor_tensor(out=t2[:, 0:g], in0=A, in1=TW1[:, 1].unsqueeze(1).to_broadcast([128, g, 2, 33]), op=ALU.mult),
                     reads=[paB, tabB], writes=[t2B])
                P.op("pool", lambda e, t1=t1, g=g, c=c: e.tensor_tensor(out=Bt[:, 0, c:c + g, :], in0=t1[:, 0:g, 0, :], in1=t1[:, 0:g, 1, :], op=ALU.subtract),
                     reads=[t1B], writes=[BtB])
                P.op("pool", lambda e, t2=t2, g=g, c=c: e.tensor_tensor(out=Bt[:, 1, c:c + g, :], in0=t2[:, 0:g, 0, :], in1=t2[:, 0:g, 1, :], op=ALU.add),
                     reads=[t2B], writes=[BtB])
                c += g
                yield

        zkr = Ring(P, 2, [64, 64, 128], BF16, "zk"); btr = Ring(P, 2, [128, 2, 64, 33], BF16, "bt", sem=False)
        kfo = Ring(P, 2, [128, 64, 2, 33], F32, "kfo")

        def kf_chain(o, hc):
            zk, zkB, zks = zkr.next()
            for q4 in range(2):
                P.dma("sp", zk[:, q4 * 32:(q4 + 1) * 32, :],
                      KERN[o, hc * 64 + q4 * 32:hc * 64 + (q4 + 1) * 32, :].rearrange("c (a b) -> a c b", b=128), zks, writes=[zkB])
            bt, btB, _ = btr.next()
            yield
            yield from fft_fwd_g(zk, zkB, 64, 64, bt, btB, tmr)
            ko, koB, kos = kfo.next()
            c0 = 0
            while c0 < 64:
                n = min(15, 64 - c0)
                xr_, xrB, xi_, xiB = fft_stage2(bt, btB, c0, n)
                P.op("dve", lambda e, ko=ko, xr_=xr_, c0=c0, n=n, o=o, hc=hc: e.tensor_tensor(
                    out=ko[:, c0:c0 + n, 0, :], in0=xr_[:, 0:n * 33].rearrange("p (c f) -> p c f", f=33),
                    in1=hb[:, o * 1024 + hc * 64 + c0:o * 1024 + hc * 64 + c0 + n].unsqueeze(2).to_broadcast([128, n, 33]), op=ALU.add),
                    reads=[xrB, tabB], writes=[koB])
                P.op("act", lambda e, ko=ko, xi_=xi_, c0=c0, n=n: e.copy(out=ko[:, c0:c0 + n, 1, :], in_=xi_[:, 0:n * 33].rearrange("p (c f) -> p c f", f=33)),
                     reads=[xiB], writes=[koB])
                c0 += n
                yield
            P.dma("act", KF[o, :, hc * 64:(hc + 1) * 64, :, :], ko[:], kos, reads=[koB])

        for hc in range(16):
            gens = [kf_chain(0, hc), kf_chain(1, hc)]
            alive = [True, True]
            while any(alive):
                for gi_ in range(2):
                    if alive[gi_]:
                        try:
                            next(gens[gi_])
                        except StopIteration:
                            alive[gi_] = False
        P.barrier(); P.reset(m4)
        if phases < 6:
            P.emit(); return nc

        NCG = 32
        def chain_bufs(p):
            d = {}
            d["zv"] = Ring(P, 1, [32, NCG, 128], BF16, f"zv{p}"); d["x1"] = Ring(P, 1, [32, NCG, 128], BF16, f"x1t{p}"); d["x2"] = Ring(P, 1, [32, NCG, 128], BF16, f"x2t{p}")
            d["k0"] = Ring(P, 1, [128, NCG, 2, 33], F32, f"kf0{p}"); d["k1"] = Ring(P, 1, [128, NCG, 2, 33], F32, f"kf1{p}")
            d["bt"] = Ring(P, 1, [128, 2, NCG, 33], BF16, f"btd{p}", sem=False); d["xp"] = Ring(P, 1, [128, NCG, 2, 33], BF16, f"xpt{p}", sem=False)
            d["zz"] = Ring(P, 1, [32, NCG, 128], BF16, f"zz{p}", sem=False); d["hy"] = Ring(P, 1, [32, NCG, 128], BF16, f"hyo{p}")
            return d
        CB = [chain_bufs(0), chain_bufs(1)]
        Rr = Ring(P, 3, [66, 2, 16, 128], BF16, "Rinv", sem=False)
        pwr = Ring(P, 6, [128, 15, 33], F32, "pwt", sem=False); sar = Ring(P, 6, [66, 2, 2, 128], F32, "sat", sem=False)
        tmr2 = Ring(P, 6, [128, 7, 2, 33], F32, "twtmp2", sem=False)

        def conv_g(cb, Zin, ZinB, kf_, kfB, gate, gateB, out_t, outB):
            bt, btB, _ = cb["bt"].next(); xp, xpB, _ = cb["xp"].next()
            yield from fft_fwd_g(Zin, ZinB, 32, NCG, bt, btB, tmr2)
            c0 = 0
            while c0 < NCG:
                n = min(15, NCG - c0)
                xr_, xrB, xi_, xiB = fft_stage2(bt, btB, c0, n)
                XR = xr_[:, 0:n * 33].rearrange("p (c f) -> p c f", f=33); XI = xi_[:, 0:n * 33].rearrange("p (c f) -> p c f", f=33)
                kr_ = kf_[:, c0:c0 + n, 0, :]; ki_ = kf_[:, c0:c0 + n, 1, :]
                ts = [pwr.next() for _ in range(4)]
                for (tb_, src, kk) in ((ts[0], XR, kr_), (ts[1], XI, ki_), (ts[2], XR, ki_), (ts[3], XI, kr_)):
                    P.op("dve", lambda e, tb_=tb_, src=src, kk=kk, n=n: e.tensor_tensor(out=tb_[0][:, 0:n, :], in0=src, in1=kk, op=ALU.mult),
                         reads=[xrB, xiB, kfB], writes=[tb_[1]])
                P.op("pool", lambda e, xp=xp, c0=c0, n=n, ts=ts: e.tensor_tensor(out=xp[:, c0:c0 + n, 0, :], in0=ts[0][0][:, 0:n, :], in1=ts[1][0][:, 0:n, :], op=ALU.subtract),
                     reads=[ts[0][1], ts[1][1]], writes=[xpB])
                P.op("pool", lambda e, xp=xp, c0=c0, n=n, ts=ts: e.tensor_tensor(out=xp[:, c0:c0 + n, 1, :], in0=ts[2][0][:, 0:n, :], in1=ts[3][0][:, 0:n, :], op=ALU.add),
                     reads=[ts[2][1], ts[3][1]], writes=[xpB])
                c0 += n
                yield
            for c16 in range(NCG // 16):
                R, RB, _ = Rr.next()
                for cp in range(8):
                    c = c16 * 16 + cp * 2
                    pa, paB = ps_next()
                    for u in range(2):
                        P.op("pe", lambda e, pa=pa, u=u, c=c: e.matmul(pa[0:66, u * 256:(u + 1) * 256], xp[:, c + u, :, :].rearrange("p r f -> p (r f)"),
                                                                     Ginv[:].rearrange("p r s -> p (r s)"), start=True, stop=True),
                             reads=[xpB, tabB], writes=[paB], sig=(u == 1))
                    Pv = pa[0:66, :].rearrange("p (g h s) -> p g h s", g=2, h=2)
                    q1 = sar.next(); q2 = sar.next()
                    P.op("dve", lambda e, Pv=Pv, q1=q1: e.tensor_tensor(out=q1[0][:], in0=Pv, in1=TW2[:, 0].unsqueeze(1).to_broadcast([66, 2, 2, 128]), op=ALU.mult),
                         reads=[paB, tabB], writes=[q1[1]])
                    P.op("dve", lambda e, Pv=Pv, q2=q2: e.tensor_tensor(out=q2[0][:], in0=Pv, in1=TW2[:, 1].unsqueeze(1).to_broadcast([66, 2, 2, 128]), op=ALU.mult),
                         reads=[paB, tabB], writes=[q2[1]])
                    P.op("pool", lambda e, R=R, q1=q1, cp=cp: e.tensor_tensor(out=R[:, 0, cp * 2:cp * 2 + 2, :], in0=q1[0][:, :, 0, :], in1=q1[0][:, :, 1, :], op=ALU.add),
                         reads=[q1[1]], writes=[RB])
                    P.op("pool", lambda e, R=R, q2=q2, cp=cp: e.tensor_tensor(out=R[:, 1, cp * 2:cp * 2 + 2, :], in0=q2[0][:, :, 0, :], in1=q2[0][:, :, 1, :], op=ALU.add),
                         reads=[q2[1]], writes=[RB])
                    yield
                for c4 in range(4):
                    c = c16 * 16 + c4 * 4
                    pa, paB = ps_next()
                    P.op("pe", lambda e, pa=pa, R=R, c4=c4: e.matmul(pa[0:32, :], LAB[:, 0, :], R[:, 0, c4 * 4:c4 * 4 + 4, :].rearrange("p c s -> p (c s)"), start=True, stop=False),
                         reads=[RB, tabB], writes=[paB], sig=False)
                    P.op("pe", lambda e, pa=pa, R=R, c4=c4: e.matmul(pa[0:32, :], LAB[:, 1, :], R[:, 1, c4 * 4:c4 * 4 + 4, :].rearrange("p c s -> p (c s)"), start=False, stop=True),
                         reads=[RB, tabB], writes=[paB])
                    P.op("dve", lambda e, pa=pa, c=c: e.tensor_tensor(out=out_t[:, c:c + 4, :], in0=pa[0:32, :].rearrange("p (c s) -> p c s", s=128),
                                                                      in1=gate[:, c:c + 4, :], op=ALU.mult),
                         reads=[paB, gateB], writes=[outB])
                    yield

        def chain_g(p, hc):
            cb = CB[p]; cb0 = hc * NCG
            zv, zvB, zvs = cb["zv"].next(); x1t, x1B, x1s = cb["x1"].next(); x2t, x2B, x2s = cb["x2"].next()
            k0, k0B, k0s = cb["k0"].next(); k1, k1B, k1s = cb["k1"].next()
            for (dst, dB, dsm, src) in ((zv, zvB, zvs, UU[0, cb0:cb0 + NCG, :]), (x1t, x1B, x1s, UU[1, cb0:cb0 + NCG, :]), (x2t, x2B, x2s, UU[2, cb0:cb0 + NCG, :])):
                P.dma("pool", dst[:], src.rearrange("c (a b) -> a c b", b=128), dsm, writes=[dB])
            P.dma("sp", k0[:], KF[0, :, cb0:cb0 + NCG, :, :], k0s, writes=[k0B])
            P.dma("sp", k1[:], KF[1, :, cb0:cb0 + NCG, :, :], k1s, writes=[k1B])
            zz, zzB, _ = cb["zz"].next(); hy, hyB, hys = cb["hy"].next()
            yield
            yield from conv_g(cb, zv, zvB, k0, k0B, x1t, x1B, zz, zzB)
            yield from conv_g(cb, zz, zzB, k1, k1B, x2t, x2B, hy, hyB)
            P.dma("act", HY[cb0:cb0 + NCG, :].rearrange("c (a b) -> a c b", b=128), hy[:], hys, reads=[hyB])

        for pr_ in range(D // NCG // 2):
            gens = [chain_g(0, 2 * pr_), chain_g(1, 2 * pr_ + 1)]
            alive = [True, True]
            while any(alive):
                for gi_ in range(2):
                    if alive[gi_]:
                        try:
                            next(gens[gi_])
                        except StopIteration:
                            alive[gi_] = False
        P.barrier(); P.reset(m_persist)
        if phases < 7:
            P.emit(); return nc

        posA = P.tile([128, 32, 64], F32, "posA"); gatA = P.tile([128, 32, 64], F32, "gatA"); pgB = Buf("posgate")
        slotI = P.tile([128, 256], I32, "slotI"); gkA = P.tile([128, 32, 8], F32, "gkA"); blkI = P.tile([128, NBLK], I32, "blkI"); metaB = Buf("meta")
        m_persist = P.mark()
        s_w = P.dsem("w5"); w5B = Buf("w5")
        wpa = P.tile([128, 8, D], BF16, "wpa"); wph = P.tile([128, 8, D], BF16, "wph"); wo = P.tile([128, 8, D], BF16, "wo")
        rwt = P.tile([128, 8, 64], F32, "rwt"); rbt = P.tile([128, 64], F32, "rbt")
        P.dma("pool", wpa[:], w_pa.rearrange("(k p) n -> p k n", p=128), s_w, writes=[w5B])
        P.dma("pool", wph[:], w_ph.rearrange("(k p) n -> p k n", p=128), s_w, writes=[w5B])
        P.dma("pool", wo[:], w_o.rearrange("(k p) n -> p k n", p=128), s_w, writes=[w5B])
        P.dma("sp", rwt[:], rw.rearrange("(k p) n -> p k n", p=128), s_w, writes=[w5B])
        P.dma("sp", rbt[:], rbias.partition_broadcast(128), s_w, writes=[w5B])
        selb = P.tile([128, 64], BF16, "selb"); ltri = P.tile([128, 128], BF16, "ltri"); onesb5 = P.tile([128, 128], BF16, "onesb5"); triB = Buf("tri")
        carry = P.tile([128, 64], F32, "carry"); carB = Buf("carry")
        P.op("pool", lambda e: e.memset(carry[:], 0.0), writes=[carB])
        P.op("pool", lambda e: e.memset(onesb5[:], 1.0), writes=[triB])
        P.op("pool", lambda e: e.memset(ltri[:], 1.0), writes=[triB])
        P.op("pool", lambda e: e.affine_select(out=ltri[:], in_=ltri[:], pattern=[[1, 128]], compare_op=ALU.is_gt, fill=0.0, base=0, channel_multiplier=-1),
             reads=[triB], writes=[triB])
        m5b = P.mark()
        htkr = Ring(P, 2, [128, D], BF16, "htk")
        axr = Ring(P, 1, [128, 8, 512], BF16, "axT"); hyr5 = Ring(P, 1, [128, 8, 512], BF16, "hyT"); gtr = Ring(P, 1, [128, 16, 512], BF16, "gt")
        xr5 = Ring(P, 1, [128, 8, 512], F32, "xt5")
        yT = P.tile([128, 8, 512], BF16, "yT"); yB = Buf("yT")
        x1r5 = Ring(P, 1, [128, 8, 512], F32, "x1T")
        sq5 = P.tile([128, 8, 512], F32, "sq5"); sq5B = Buf("sq5"); rstd5 = P.tile([128, 512], F32, "rstd5"); rs5B = Buf("rs5")
        hx2f = P.tile([128, 8, 512], F32, "hx2f"); hx2fB = Buf("hx2f")
        hx2b = Ring(P, 1, [128, 8, 512], BF16, "hx2b")
        y1r = Ring(P, 4, [128, 512], F32, "y1", sem=False)
        rt = [P.tile([128, 64], F32, f"rt{i}") for i in range(6)]; rtB = Buf("rt")
        rs_ = P.tile([128, 40], F32, "rsm")
        AX_v = AXs.rearrange("(k p) t -> p k t", p=128); HY_v = HY.rearrange("(k p) t -> p k t", p=128)
        GG_v = GG.rearrange("(k p) t -> p k t", p=128); X1_v = X1.rearrange("(k p) t -> p k t", p=128); HX2_v = HX2.rearrange("(k p) t -> p k t", p=128)
        for i in range(8):
            t0 = i * 512
            ax_, axB, axs = axr.next(); hy_, hyB5, hys5 = hyr5.next(); gt, gtB, gts = gtr.next(); xt5, xB5, xs5 = xr5.next()
            P.dma("sp", ax_[:], AX_v[:, :, t0:t0 + 512], axs, writes=[axB])
            P.dma("act", hy_[:], HY_v[:, :, t0:t0 + 512], hys5, writes=[hyB5])
            P.dma("sp", gt[:, 0:8, :], GG_v[:, 0:8, t0:t0 + 512], gts, writes=[gtB])
            P.dma("act", gt[:, 8:16, :], GG_v[:, 8:16, t0:t0 + 512], gts, writes=[gtB])
            P.dma("sp", xt5[:], xT_v[:, :, t0:t0 + 512], xs5, writes=[xB5])
            for dc in range(8):
                pa, paB = ps_next(); ph_, phB = ps_next()
                for k in range(8):
                    P.op("pe", lambda e, pa=pa, k=k, dc=dc, ax_=ax_: e.matmul(pa[:], wpa[:, k, dc * 128:(dc + 1) * 128], ax_[:, k, :], start=(k == 0), stop=(k == 7)),
                         reads=[w5B, axB], writes=[paB], sig=(k == 7))
                for k in range(8):
                    P.op("pe", lambda e, ph_=ph_, k=k, dc=dc, hy_=hy_: e.matmul(ph_[:], wph[:, k, dc * 128:(dc + 1) * 128], hy_[:, k, :], start=(k == 0), stop=(k == 7)),
                         reads=[w5B, hyB5], writes=[phB], sig=(k == 7))
                ya, yaB, _ = y1r.next(); yb_, ybB, _ = y1r.next()
                P.op("dve", lambda e, ya=ya, pa=pa, gt=gt, dc=dc: e.tensor_tensor(out=ya[:], in0=pa[:], in1=gt[:, dc, :], op=ALU.mult), reads=[paB, gtB], writes=[yaB])
                P.op("dve", lambda e, yb_=yb_, ph_=ph_, gt=gt, dc=dc: e.tensor_tensor(out=yb_[:], in0=ph_[:], in1=gt[:, 8 + dc, :], op=ALU.mult), reads=[phB, gtB], writes=[ybB])
                P.op("pool", lambda e, ya=ya, yb_=yb_, dc=dc: e.tensor_tensor(out=yT[:, dc, :], in0=ya[:], in1=yb_[:], op=ALU.add), reads=[yaB, ybB], writes=[yB])
            x1T, x1B5, x1s5 = x1r5.next()
            for dc in range(8):
                pm, pmB = ps_next()
                for k in range(8):
                    P.op("pe", lambda e, pm=pm, k=k, dc=dc: e.matmul(pm[:], wo[:, k, dc * 128:(dc + 1) * 128], yT[:, k, :], start=(k == 0), stop=(k == 7)),
                         reads=[w5B, yB], writes=[pmB], sig=(k == 7))
                P.op("dve", lambda e, pm=pm, dc=dc, x1T=x1T, xt5=xt5: e.scalar_tensor_tensor(out=x1T[:, dc, :], in0=pm[:], scalar=vec[:, 48 + dc:49 + dc], in1=xt5[:, dc, :],
                                                                                          op0=ALU.mult, op1=ALU.add), reads=[pmB, vecB, xB5], writes=[x1B5])
            P.dma("sp", X1_v[:, :, t0:t0 + 512], x1T[:], x1s5, reads=[x1B5])
            rms_rstd(x1T, x1B5, 512, sq=sq5, sqB=sq5B, rstd=rstd5, rsB=rs5B)
            P.op("dve", lambda e, x1T=x1T: e.tensor_tensor(out=sq5[:], in0=x1T[:], in1=rstd5[:].unsqueeze(1).to_broadcast([128, 8, 512]), op=ALU.mult),
                 reads=[x1B5, rs5B, sq5B], writes=[sq5B])
            hb2, hb2B, hb2s = hx2b.next()
            for k in range(8):
                P.op("act", lambda e, k=k: e.activation(out=hx2f[:, k, :], in_=sq5[:, k, :], func=AF.Identity, scale=vec[:, 32 + k:33 + k], bias=vec[:, 40 + k:41 + k]),
                     reads=[sq5B, vecB], writes=[hx2fB])
            P.op("pool", lambda e, hb2=hb2: e.tensor_copy(out=hb2[:], in_=hx2f[:]), reads=[hx2fB], writes=[hb2B])
            P.dma("act", HX2_v[:, :, t0:t0 + 512], hb2[:], hb2s, reads=[hb2B])
            for sb_ in range(4):
                st_ = i * 4 + sb_
                pr, prB = ps_next()
                for k in range(8):
                    P.op("pe", lambda e, pr=pr, k=k, sb_=sb_: e.matmul(pr[:, 0:64], hx2f[:, k, sb_ * 128:(sb_ + 1) * 128], rwt[:, k, :], start=(k == 0), stop=(k == 7)),
                         reads=[hx2fB, w5B], writes=[prB], sig=(k == 7))
                for hf in range(2):
                    ptk, ptkB = ps_next()
                    for kk in range(4):
                        k = hf * 4 + kk
                        P.op("pe", lambda e, ptk=ptk, k=k, kk=kk, sb_=sb_: e.transpose(out=ptk[:, kk * 128:(kk + 1) * 128], in_=hx2f[:, k, sb_ * 128:(sb_ + 1) * 128], identity=ident[:]),
                             reads=[hx2fB, identB], writes=[ptkB], sig=(kk == 3))
                    if hf == 0:
                        htk, htkB, htks = htkr.next()
                        P.op("act", lambda e, htk=htk, ptk=ptk: e.copy(out=htk[:, 0:512], in_=ptk[:]), reads=[ptkB], writes=[htkB])
                    else:
                        P.op("dve", lambda e, htk=htk, ptk=ptk: e.tensor_copy(out=htk[:, 512:1024], in_=ptk[:]), reads=[ptkB], writes=[htkB])
                P.dma("act", HX2tok[st_ * 128:(st_ + 1) * 128, :], htk[:], htks, reads=[htkB])
                sc_, ch_, c2_, msk, wse, gte = rt
                def R_(fn, eng="dve", rd=(), wr=()):
                    P.op(eng, fn, reads=[rtB] + list(rd), writes=[rtB] + list(wr))
                R_(lambda e, pr=pr: e.activation(out=sc_[:], in_=pr[:, 0:64], func=AF.Sigmoid), eng="act", rd=[prB])
                R_(lambda e: e.tensor_tensor(out=ch_[:], in0=sc_[:], in1=rbt[:], op=ALU.add), rd=[w5B])
                R_(lambda e: e.tensor_reduce(out=rs_[:, 0:8], in_=ch_[:].rearrange("p (g j) -> p g j", j=8), axis=AXL.X, op=ALU.max))
                R_(lambda e: e.tensor_tensor(out=c2_[:].rearrange("p (g j) -> p g j", j=8), in0=ch_[:].rearrange("p (g j) -> p g j", j=8),
                                             in1=rs_[:, 0:8].unsqueeze(2).to_broadcast([128, 8, 8]), op=ALU.is_equal))
                R_(lambda e: e.scalar_tensor_tensor(out=c2_[:], in0=c2_[:], scalar=-1.0e9, in1=ch_[:], op0=ALU.mult, op1=ALU.add))
                R_(lambda e: e.tensor_reduce(out=rs_[:, 8:16], in_=c2_[:].rearrange("p (g j) -> p g j", j=8), axis=AXL.X, op=ALU.max))
                R_(lambda e: e.tensor_tensor(out=rs_[:, 0:8], in0=rs_[:, 0:8], in1=rs_[:, 8:16], op=ALU.add))
                R_(lambda e: e.max(out=rs_[:, 16:24], in_=rs_[:, 0:8]))
                R_(lambda e: e.tensor_scalar(out=rs_[:, 8:16], in0=rs_[:, 0:8], scalar1=rs_[:, 19:20], scalar2=None, op0=ALU.is_ge))
                R_(lambda e: e.tensor_scalar(out=rs_[:, 8:16], in0=rs_[:, 8:16], scalar1=-1.0, scalar2=1.0e9, op0=ALU.add, op1=ALU.mult))
                R_(lambda e: e.tensor_tensor(out=msk[:].rearrange("p (g j) -> p g j", j=8), in0=ch_[:].rearrange("p (g j) -> p g j", j=8),
                                             in1=rs_[:, 8:16].unsqueeze(2).to_broadcast([128, 8, 8]), op=ALU.add))
                R_(lambda e: e.max(out=rs_[:, 24:32], in_=msk[:]))
                R_(lambda e: e.tensor_scalar(out=wse[:], in0=msk[:], scalar1=rs_[:, 31:32], scalar2=None, op0=ALU.is_ge))
                R_(lambda e: e.tensor_copy(out=selb[:], in_=wse[:]))
                R_(lambda e: e.tensor_tensor(out=wse[:], in0=wse[:], in1=sc_[:], op=ALU.mult))
                R_(lambda e: e.tensor_reduce(out=rs_[:, 32:33], in_=wse[:], axis=AXL.X, op=ALU.add))
                R_(lambda e: e.reciprocal(out=rs_[:, 33:34], in_=rs_[:, 32:33]))
                R_(lambda e, st_=st_: e.tensor_scalar(out=gatA[:, st_, :], in0=wse[:], scalar1=rs_[:, 33:34], scalar2=2.5, op0=ALU.mult, op1=ALU.mult), wr=[pgB])
                pp, ppB = ps_next()
                P.op("pe", lambda e, pp=pp: e.matmul(pp[:, 0:64], ltri[:], selb[:], start=True, stop=True), reads=[rtB, triB], writes=[ppB])
                P.op("pe", lambda e, pp=pp: e.matmul(pp[:, 64:128], onesb5[:], selb[:], start=True, stop=True), reads=[rtB, triB], writes=[ppB])
                P.op("dve", lambda e, pp=pp, st_=st_: e.tensor_tensor(out=posA[:, st_, :], in0=pp[:, 0:64], in1=carry[:], op=ALU.add), reads=[ppB, carB, pgB], writes=[pgB])
                P.op("dve", lambda e, pp=pp: e.tensor_tensor(out=carry[:], in0=pp[:, 64:128], in1=carry[:], op=ALU.add), reads=[ppB, carB], writes=[carB])
        P.barrier(); P.reset(m5b)
        W2K = 32 * 64
        slotf = P.tile([128, 32, 64], F32, "slotf"); keyt = P.tile([128, 32, 64], F32, "keyt"); eqt = P.tile([128, 32, 64], F32, "eqt"); m2B = Buf("meta2")
        rowA = P.tile([128, 64], F32, "rowA"); rowB = P.tile([128, 64], F32, "rowB"); rowI = P.tile([128, 64], I32, "rowI"); padd = P.tile([128, 64], F32, "padd")
        k8 = P.tile([128, 32, 8], F32, "k8"); skf = P.tile([128, 32, 8], F32, "skf")
        bvt = P.tile([128, NBLK], F32, "bvt"); blkf = P.tile([128, NBLK], F32, "blkf"); chg = P.tile([128, NBLK], F32, "chg"); pct = P.tile([128, 1], F32, "pct")
        cmpt = P.tile([128, 80, 64], F32, "cmpt")
        s_m = P.dsem("meta")
        P.dma("sp", bvt[:], bvals.partition_broadcast(128), s_m, writes=[m2B]); P.dma("sp", pct[:], pcol[:, :], s_m, writes=[m2B])
        def M_(fn, eng="dve", rd=(), wr=()):
            P.op(eng, fn, reads=[m2B] + list(rd), writes=[m2B] + list(wr))
        M_(lambda e: e.tensor_scalar(out=rowA[:], in0=carry[:], scalar1=1.0 / 128, scalar2=127.0 / 128 - 0.5 + 1.0 / 256, op0=ALU.mult, op1=ALU.add), rd=[carB])
        M_(lambda e: e.tensor_copy(out=rowI[:], in_=rowA[:]))
        M_(lambda e: e.tensor_copy(out=rowA[:], in_=rowI[:]))
        M_(lambda e: e.tensor_scalar(out=padd[:], in0=rowA[:], scalar1=128.0, scalar2=None, op0=ALU.mult))
        M_(lambda e: e.tensor_copy(out=rowA[:], in_=padd[:]))
        cur, oth = rowA, rowB
        for sh in (1, 2, 4, 8, 16, 32):
            M_(lambda e, cur=cur, oth=oth, sh=sh: e.tensor_copy(out=oth[:, 0:sh], in_=cur[:, 0:sh]))
            M_(lambda e, cur=cur, oth=oth, sh=sh: e.tensor_tensor(out=oth[:, sh:64], in0=cur[:, sh:64], in1=cur[:, 0:64 - sh], op=ALU.add))
            cur, oth = oth, cur
        pend = cur; pstart = oth
        M_(lambda e: e.tensor_tensor(out=pstart[:], in0=pend[:], in1=padd[:], op=ALU.subtract))
        M_(lambda e: e.tensor_tensor(out=slotf[:], in0=posA[:], in1=pstart[:].unsqueeze(1).to_broadcast([128, 32, 64]), op=ALU.add), rd=[pgB])
        M_(lambda e: e.tensor_scalar(out=keyt[:], in0=slotf[:], scalar1=-1.0, scalar2=BIGK + 1.0, op0=ALU.mult, op1=ALU.add))
        M_(lambda e: e.tensor_single_scalar(out=eqt[:], in_=gatA[:], scalar=0.0, op=ALU.is_gt), rd=[pgB])
        M_(lambda e: e.tensor_tensor(out=keyt[:], in0=keyt[:], in1=eqt[:], op=ALU.mult))
        M_(lambda e: e.tensor_scalar(out=keyt[:], in0=keyt[:], scalar1=-1.0, scalar2=None, op0=ALU.add))
        for st_ in range(32):
            M_(lambda e, st_=st_: e.max(out=k8[:, st_, :], in_=keyt[:, st_, :]))
        M_(lambda e: e.tensor_scalar(out=skf[:], in0=k8[:], scalar1=-1.0, scalar2=BIGK, op0=ALU.mult, op1=ALU.add))
        M_(lambda e: e.tensor_copy(out=slotI[:], in_=skf[:].rearrange("p a b -> p (a b)")), wr=[metaB])
        for k in range(8):
            M_(lambda e, k=k: e.tensor_tensor(out=eqt[:], in0=slotf[:], in1=skf[:, :, k:k + 1].to_broadcast([128, 32, 64]), op=ALU.is_equal))
            M_(lambda e: e.tensor_tensor(out=eqt[:], in0=eqt[:], in1=gatA[:], op=ALU.mult), rd=[pgB])
            M_(lambda e, k=k: e.tensor_reduce(out=gkA[:, :, k], in_=eqt[:], axis=AXL.X, op=ALU.add), wr=[metaB])
        for cq in range(NBLK // 80):
            M_(lambda e, cq=cq: e.tensor_tensor(out=cmpt[:], in0=pend[:].unsqueeze(1).to_broadcast([128, 80, 64]),
                                                in1=bvt[:, cq * 80:(cq + 1) * 80].unsqueeze(2).to_broadcast([128, 80, 64]), op=ALU.is_le))
            M_(lambda e, cq=cq: e.tensor_reduce(out=blkf[:, cq * 80:(cq + 1) * 80], in_=cmpt[:], axis=AXL.X, op=ALU.add))
        M_(lambda e: e.tensor_scalar(out=blkf[:], in0=blkf[:], scalar1=63.0, scalar2=128.0, op0=ALU.min, op1=ALU.mult))
        M_(lambda e: e.memset(chg[:, 0:NWB], 1.0))
        M_(lambda e: e.tensor_tensor(out=chg[:, NWB:NBLK], in0=blkf[:, NWB:NBLK], in1=blkf[:, 0:NBLK - NWB], op=ALU.not_equal))
        M_(lambda e: e.tensor_scalar(out=blkf[:], in0=blkf[:], scalar1=pct[:, 0:1], scalar2=-BIGI, op0=ALU.add, op1=ALU.add))
        M_(lambda e: e.tensor_tensor(out=blkf[:], in0=blkf[:], in1=chg[:], op=ALU.mult))
        M_(lambda e: e.tensor_scalar(out=blkf[:], in0=blkf[:], scalar1=BIGI, scalar2=None, op0=ALU.add))
        M_(lambda e: e.tensor_copy(out=blkI[:], in_=blkf[:]), wr=[metaB])
        dtr = Ring(P, 3, [128, D], BF16, "dtok")
        dsc = [P.dsem("scat") for _ in range(3)]
        for st_ in range(32):
            dt_, dtB, dts = dtr.next()
            P.dma("sp", dt_[:], HX2tok[st_ * 128:(st_ + 1) * 128, :], dts, writes=[dtB])
            for k in range(8):
                waits = P._deps("pool", [dtB, metaB] + zwin, [])
                ssem = dsc[dtr.i]
                ssem.n += 16
                P.q["pool"].append((waits, (lambda e, dt_=dt_, st_=st_, k=k: e.indirect_dma_start(
                    out=XG[:, :], out_offset=bass.IndirectOffsetOnAxis(ap=slotI[:, st_ * 8 + k:st_ * 8 + k + 1], axis=0), in_=dt_[:, :], in_offset=None,
                    bounds_check=P.reg(e, NSLOT - 1), oob_is_err=False)), (ssem.h, 16)))
                dtB.r.append((ssem, ssem.n))
        xgBs = [Buf(f"xg{i}") for i in range(3)]
        for i_, b_ in enumerate(xgBs):
            b_.w = (dsc[i_], dsc[i_].n)
        if debug:
            s_dbg = P.dsem("dbg")
            P.dma("sp", dbg["slot"].rearrange("p a b -> p (a b)"), slotI[:], s_dbg, reads=[metaB]); P.dma("sp", dbg["gk"][:, :, :], gkA[:], s_dbg, reads=[metaB])
            P.dma("sp", dbg["blk"][:, :], blkI[:], s_dbg, reads=[metaB])
        P.barrier(); P.reset(m_persist)
        if phases < 8:
            P.emit(); return nc

        wbuf = [(P.tile([128, 6144], BF16, f"wall{i}"), Buf(f"bw{i}"), P.dsem("bw")) for i in range(NWB)]
        for (a_, wB_, _) in wbuf:
            P.op("pool", lambda e, a_=a_: e.memset(a_[:], 0.0), writes=[wB_])
        xbr = Ring(P, 4, [128, D], BF16, "xb"); xgtr = Ring(P, 2, [128, 8, 128], BF16, "xgT", sem=False)
        sgr6 = Ring(P, 2, [128, 256], F32, "sg6", sem=False); atkr = Ring(P, 2, [128, 256], BF16, "atk", sem=False)
        atr = Ring(P, 2, [128, 2, 128], BF16, "aT", sem=False); ybr = Ring(P, 3, [128, D], F32, "yb")
        ygB = Buf("YG")
        identb = P.tile([128, 128], BF16, "identb"); identbB = Buf("identb")
        P.op("dve", lambda e: e.tensor_copy(out=identb[:], in_=ident[:]), reads=[identB], writes=[identbB])
        def block_g(b):
            wall, wB_, wsem = wbuf[b % NWB]
            wg_ = wall[:, 0:2048].rearrange("p (k h) -> p k h", k=8); wu_ = wall[:, 2048:4096].rearrange("p (k h) -> p k h", k=8)
            wd_ = wall[:, 4096:6144].rearrange("p (k d) -> p k d", k=2)
            waits = P._deps("pool", [metaB, wbB], [wB_])
            wsem.n += 16
            P.q["pool"].append((waits, (lambda e, wall=wall, b=b: e.indirect_dma_start(
                out=wall[:, :], out_offset=None, in_=WB[:, :], in_offset=bass.IndirectOffsetOnAxis(ap=blkI[:, b:b + 1], axis=0),
                bounds_check=P.reg(e, 65 * 128 - 1), oob_is_err=False)), (wsem.h, 16)))
            wB_.w = (wsem, wsem.n); wB_.r = []
            xb, xbB, xbs = xbr.next()
            P.dma("sp", xb[:], XG[b * 128:(b + 1) * 128, :], xbs, reads=xgBs, writes=[xbB])
            ptb, ptbB = ps_next()
            ptv = ptb[:].bitcast(BF16)
            for k in range(8):
                P.op("pe", lambda e, ptv=ptv, xb=xb, k=k: e.transpose(out=ptv[:, k * 128:(k + 1) * 128], in_=xb[:, k * 128:(k + 1) * 128], identity=identb[:]),
                     reads=[xbB, identbB], writes=[ptbB], sig=(k == 7))
            xgT, xgTB, _ = xgtr.next()
            P.op("dve", lambda e, xgT=xgT, ptv=ptv: e.tensor_copy(out=xgT[:].rearrange("p k s -> p (k s)"), in_=ptv), reads=[ptbB], writes=[xgTB])
            yield
            pg, pgB_ = ps_next(); pu, puB = ps_next()
            for k in range(8):
                P.op("pe", lambda e, pg=pg, wg_=wg_, k=k, xgT=xgT: e.matmul(pg[:, 0:256], xgT[:, k, :], wg_[:, k, :], start=(k == 0), stop=(k == 7)),
                     reads=[wB_, xgTB], writes=[pgB_], sig=(k == 7))
                P.op("pe", lambda e, pu=pu, wu_=wu_, k=k, xgT=xgT: e.matmul(pu[:, 0:256], xgT[:, k, :], wu_[:, k, :], start=(k == 0), stop=(k == 7)),
                     reads=[wB_, xgTB], writes=[puB], sig=(k == 7))
            sg, sgB, _ = sgr6.next(); atk, atkB, _ = atkr.next(); aT, aTB, _ = atr.next()
            P.op("act", lambda e, sg=sg, pg=pg: e.activation(out=sg[:], in_=pg[:, 0:256], func=AF.Silu), reads=[pgB_], writes=[sgB])
            P.op("dve", lambda e, atk=atk, sg=sg, pu=pu: e.tensor_tensor(out=atk[:], in0=pu[:, 0:256], in1=sg[:], op=ALU.mult), reads=[puB, sgB], writes=[atkB])
            yield
            pat, patB = ps_next()
            patv = pat[:].bitcast(BF16)
            for hh in range(2):
                P.op("pe", lambda e, patv=patv, atk=atk, hh=hh: e.transpose(out=patv[:, hh * 128:(hh + 1) * 128], in_=atk[:, hh * 128:(hh + 1) * 128], identity=identb[:]),
                     reads=[atkB, identbB], writes=[patB], sig=(hh == 1))
            P.op("act", lambda e, aT=aT, patv=patv: e.copy(out=aT[:].rearrange("p h s -> p (h s)"), in_=patv[:, 0:256]), reads=[patB], writes=[aTB])
            yield
            yb, ybB, ybs = ybr.next()
            for hf in range(2):
                py, pyB = ps_next()
                for hh in range(2):
                    P.op("pe", lambda e, py=py, aT=aT, wd_=wd_, hh=hh, hf=hf: e.matmul(py[:], aT[:, hh, :], wd_[:, hh, hf * 512:(hf + 1) * 512], start=(hh == 0), stop=(hh == 1)),
                         reads=[aTB, wB_], writes=[pyB], sig=(hh == 1))
                if hf == 0:
                    P.op("act", lambda e, yb=yb, py=py: e.copy(out=yb[:, 0:512], in_=py[:]), reads=[pyB], writes=[ybB])
                else:
                    P.op("dve", lambda e, yb=yb, py=py: e.tensor_copy(out=yb[:, 512:1024], in_=py[:]), reads=[pyB], writes=[ybB])
            P.dma("act", YG[b * 128:(b + 1) * 128, :], yb[:], ybs, reads=[ybB])

        live = []
        for b in range(NBLK + 4):
            if b < NBLK:
                live.append(block_g(b))
            nxt = []
            for g_ in live:
                try:
                    next(g_); nxt.append(g_)
                except StopIteration:
                    pass
            live = nxt
        P.barrier(); P.reset(m_persist)
        if debug:
            pass
        if phases < 9:
            P.emit(); return nc

        swg = P.tile([128, 8, 256], BF16, "swg"); swu = P.tile([128, 8, 256], BF16, "swu"); swd = P.tile([128, 2, D], BF16, "swd"); swB = Buf("sw"); s_sw = P.dsem("sw")
        P.dma("sp", swg[:].rearrange("p a b -> p (a b)"), WB[64 * 128:65 * 128, 0:2048], s_sw, reads=[wbB], writes=[swB])
        P.dma("sp", swu[:].rearrange("p a b -> p (a b)"), WB[64 * 128:65 * 128, 2048:4096], s_sw, reads=[wbB], writes=[swB])
        P.dma("sp", swd[:].rearrange("p a b -> p (a b)"), WB[64 * 128:65 * 128, 4096:6144], s_sw, reads=[wbB], writes=[swB])
        hxr = Ring(P, 2, [128, 8, 512], BF16, "hx6"); sg6 = Ring(P, 2, [128, 512], F32, "sgs", sem=False); aa6 = Ring(P, 2, [128, 512], BF16, "aas", sem=False)
        accS = P.tile([128, 8, 512], F32, "accS"); accSB = Buf("accS")
        ykr = Ring(P, 4, [128, D], F32, "yk"); atok = P.tile([128, D], F32, "atok"); atokB = Buf("atok")
        for t_ in ykr.t:
            P.op("pool", lambda e, t_=t_: e.memset(t_[:], 0.0), writes=[ykr.b[ykr.t.index(t_)]])
        x1l = Ring(P, 1, [128, 8, 512], F32, "x1l"); sq6 = P.tile([128, 8, 512], F32, "sq6"); sq6B = Buf("sq6")
        rstd6 = P.tile([128, 512], F32, "rstd6"); rs6B = Buf("rs6"); otr = Ring(P, 2, [128, 8, 512], F32, "outt")
        outT_v = outT.rearrange("(k p) t -> p k t", p=128)
        for i in range(8):
            t0 = i * 512
            hx_, hxB_, hxs_ = hxr.next()
            P.dma("sp", hx_[:], HX2_v[:, :, t0:t0 + 512], hxs_, writes=[hxB_])
            aas = []
            for hh in range(2):
                pg, pgB_ = ps_next(); pu, puB = ps_next()
                for k in range(8):
                    P.op("pe", lambda e, pg=pg, k=k, hh=hh, hx_=hx_: e.matmul(pg[:], swg[:, k, hh * 128:(hh + 1) * 128], hx_[:, k, :], start=(k == 0), stop=(k == 7)),
                         reads=[swB, hxB_], writes=[pgB_], sig=(k == 7))
                for k in range(8):
                    P.op("pe", lambda e, pu=pu, k=k, hh=hh, hx_=hx_: e.matmul(pu[:], swu[:, k, hh * 128:(hh + 1) * 128], hx_[:, k, :], start=(k == 0), stop=(k == 7)),
                         reads=[swB, hxB_], writes=[puB], sig=(k == 7))
                sg, sgB, _ = sg6.next(); aa, aaB, _ = aa6.next()
                P.op("act", lambda e, sg=sg, pg=pg: e.activation(out=sg[:], in_=pg[:], func=AF.Silu), reads=[pgB_], writes=[sgB])
                P.op("dve", lambda e, aa=aa, pu=pu, sg=sg: e.tensor_tensor(out=aa[:], in0=pu[:], in1=sg[:], op=ALU.mult), reads=[puB, sgB], writes=[aaB])
                aas.append((aa, aaB))
            for dc in range(8):
                pd, pdB = ps_next()
                for hh in range(2):
                    P.op("pe", lambda e, pd=pd, hh=hh, dc=dc, aa=aas[hh][0]: e.matmul(pd[:], swd[:, hh, dc * 128:(dc + 1) * 128], aa[:], start=(hh == 0), stop=(hh == 1)),
                         reads=[swB, aas[hh][1]], writes=[pdB], sig=(hh == 1))
                P.op("act", lambda e, pd=pd, dc=dc: e.copy(out=accS[:, dc, :], in_=pd[:]), reads=[pdB], writes=[accSB])
            for sb_ in range(4):
                st_ = i * 4 + sb_
                for k in range(8):
                    yk, ykB, yks = ykr.next()
                    waits = P._deps("pool", [metaB, ygB], [ykB])
                    yks.n += 16
                    P.q["pool"].append((waits, (lambda e, yk=yk, st_=st_, k=k: e.indirect_dma_start(
                        out=yk[:, :], out_offset=None, in_=YG[:, :], in_offset=bass.IndirectOffsetOnAxis(ap=slotI[:, st_ * 8 + k:st_ * 8 + k + 1], axis=0),
                        bounds_check=P.reg(e, NSLOT - 1), oob_is_err=False)), (yks.h, 16)))
                    ykB.w = (yks, yks.n); ykB.r = []
                    if k == 0:
                        P.op("dve", lambda e, yk=yk, st_=st_: e.tensor_scalar(out=atok[:], in0=yk[:], scalar1=gkA[:, st_, 0:1], scalar2=None, op0=ALU.mult),
                             reads=[ykB, metaB, atokB], writes=[atokB])
                    else:
                        P.op("dve", lambda e, yk=yk, st_=st_, k=k: e.scalar_tensor_tensor(out=atok[:], in0=yk[:], scalar=gkA[:, st_, k:k + 1], in1=atok[:], op0=ALU.mult, op1=ALU.add),
                             reads=[ykB, metaB, atokB], writes=[atokB])
                for kg in range(2):
                    ptk, ptkB = ps_next()
                    for kk in range(4):
                        k = kg * 4 + kk
                        P.op("pe", lambda e, ptk=ptk, k=k, kk=kk: e.transpose(out=ptk[:, kk * 128:(kk + 1) * 128], in_=atok[:, k * 128:(k + 1) * 128], identity=ident[:]),
                             reads=[atokB, identB], writes=[ptkB], sig=(kk == 3))
                    P.op("dve", lambda e, ptk=ptk, kg=kg, sb_=sb_: e.tensor_tensor(out=accS[:, kg * 4:(kg + 1) * 4, sb_ * 128:(sb_ + 1) * 128],
                                                                                in0=ptk[:].rearrange("p (k t) -> p k t", k=4),
                                                                                in1=accS[:, kg * 4:(kg + 1) * 4, sb_ * 128:(sb_ + 1) * 128], op=ALU.add),
                         reads=[ptkB, accSB], writes=[accSB])
            x1t_, x1B_, x1s_ = x1l.next()
            P.dma("sp", x1t_[:], X1_v[:, :, t0:t0 + 512], x1s_, writes=[x1B_])
            for k in range(8):
                P.op("dve", lambda e, k=k, x1t_=x1t_: e.scalar_tensor_tensor(out=x1t_[:, k, :], in0=accS[:, k, :], scalar=vec[:, 56 + k:57 + k], in1=x1t_[:, k, :],
                                                                           op0=ALU.mult, op1=ALU.add), reads=[accSB, vecB, x1B_], writes=[x1B_])
            rms_rstd(x1t_, x1B_, 512, sq=sq6, sqB=sq6B, rstd=rstd6, rsB=rs6B)
            P.op("dve", lambda e, x1t_=x1t_: e.tensor_tensor(out=sq6[:], in0=x1t_[:], in1=rstd6[:].unsqueeze(1).to_broadcast([128, 8, 512]), op=ALU.mult),
                 reads=[x1B_, rs6B, sq6B], writes=[sq6B])
            ot, otB, ots = otr.next()
            for k in range(8):
                P.op("act", lambda e, k=k, ot=ot: e.activation(out=ot[:, k, :], in_=sq6[:, k, :], func=AF.Copy, scale=vec[:, 68 + k:69 + k]),
                     reads=[sq6B, vecB], writes=[otB])
            P.dma("act", outT_v[:, :, t0:t0 + 512], ot[:], ots, reads=[otB])
        P.emit()
        nc._n_dsems = len(P.dsems)
    return nc


def prep_inputs(inputs):
    f = lambda a: np.ascontiguousarray(np.asarray(a, dtype=np.float32))
    x = f(inputs["x"]); ctx = f(inputs["ctx"]); c = f(inputs["c"]); c_ctx = f(inputs["c_ctx"])
    C, S = rope_tables()
    shared = {
        "ada_w": f(inputs["ada_w"][0]),
        "ada_bT": f(inputs["ada_b"][0].reshape(48, 128).T),
        "gvec": f(np.concatenate([np.asarray(inputs[k]).reshape(8, 128).T for k in ("norm1_g", "norm2_g", "final_norm_g")], axis=1)),
        "lamv": f(np.concatenate([np.asarray(inputs[k]).reshape(-1) for k in ("lam_q1", "lam_k1", "lam_q2", "lam_k2")])[None, :]),
        "w_in": f(inputs["w_in"][0]),
        "ropeC": C, "ropeS": S,
        "convw": f(np.asarray(inputs["hy_conv_w"][0]).reshape(3, 24, 128).transpose(2, 0, 1)),
        "convb": f(np.asarray(inputs["hy_conv_b"][0]).reshape(24, 128).T),
        "subln": f(np.asarray(inputs["subln_g"][0]).reshape(1, 128)),
        "sublnT": f(np.asarray(inputs["subln_g"][0]).reshape(128, 1)),
        "fw1": f(inputs["filt_w1"][0]), "fw2": f(inputs["filt_w2"][0]), "fw3": f(inputs["filt_w3"][0]),
        "fvec": f(np.stack([np.asarray(inputs[k][0]) for k in ("filt_b1", "filt_b2", "filt_freq")], axis=1)),
        "hyb": f(np.asarray(inputs["hy_bias"][0]).reshape(1, 2048)),
        "w_pa": f(inputs["w_branch_attn"][0]), "w_ph": f(inputs["w_branch_hyena"][0]), "w_o": f(inputs["w_out"][0]),
        "rw": f(inputs["router_w"][0]), "rbias": f(np.asarray(inputs["router_bias"][0]).reshape(1, 64)),
        "ewg": f(np.concatenate([inputs["exp_w_gate"][0], inputs["shared_w_gate"]], axis=0).reshape(65, 8, 128, 256).transpose(0, 2, 1, 3).reshape(65 * 128, 2048)),
        "ewu": f(np.concatenate([inputs["exp_w_up"][0], inputs["shared_w_up"]], axis=0).reshape(65, 8, 128, 256).transpose(0, 2, 1, 3).reshape(65 * 128, 2048)),
        "ewd": f(np.concatenate([inputs["exp_w_down"][0], inputs["shared_w_down"]], axis=0).reshape(65, 2, 128, 1024).transpose(0, 2, 1, 3).reshape(65 * 128, 2048)),
        "bvals": f((np.arange(NBLK) * 128.0).reshape(1, NBLK)), "pcol": f(np.arange(128.0).reshape(128, 1)),
    }
    shared.update(hyena_tables())
    maps = []
    for b in range(8):
        m = dict(shared)
        m["xT"] = np.ascontiguousarray(np.concatenate([x[b].T, ctx[b].T], axis=1))
        m["cc"] = np.ascontiguousarray(np.stack([c[b].reshape(8, 128).T, c_ctx.reshape(8, 128).T], axis=-1))
        maps.append(m)
    return maps


def kernel(**inputs):
    nc = build(DEBUG, PHASES)
    maps = prep_inputs(inputs)
    res = run_bass_kernel_spmd(nc, maps, core_ids=list(range(8)))
    out = np.stack([np.asarray(r["outT"]).T for r in res.results], axis=0)
    return np.ascontiguousarray(out.astype(np.float32))
```

```python
import math
from contextlib import ExitStack
import numpy as np
import concourse.bass as bass
import concourse.mybir as mybir
from concourse.bass_utils import run_bass_kernel_spmd

F32 = mybir.dt.float32; BF16 = mybir.dt.bfloat16; I32 = mybir.dt.int32
AF = mybir.ActivationFunctionType
ALU = mybir.AluOpType
AXL = mybir.AxisListType
DTSZ = {F32: 4, BF16: 2, I32: 4}

D = 1024; SEQ = 4096; CTX = 256; TOK = SEQ + CTX; NH = 8; INW = 8192
NBLK = 320; NSLOT = NBLK * 128; BIGK = 65536.0; BIGI = 1.0e6; NWB = 4
DEBUG = False
PHASES = 99


class Buf:
    __slots__ = ("name", "w", "r")

    def __init__(self, name=""):
        self.name = name; self.w = None; self.r = []


class Sem:
    def __init__(self, h):
        self.h = h; self.n = 0


class Prog:
    ENG = ("pe", "act", "dve", "pool", "sp")

    def __init__(self, nc, es):
        self.nc = nc; self.es = es
        self.q = {e: [] for e in self.ENG}
        self.esem = {e: Sem(es.enter_context(nc.semaphore("prog_" + e))) for e in self.ENG}
        self.known = {e: {} for e in self.ENG}
        self.dsems = []
        self.off = 16640; self.ntile = 0
        self.cap = nc.SBUF_PARTITION_SIZE_BYTES
        self.ninst = 0

    def tile(self, shape, dtype, name=None):
        nb = int(np.prod(shape[1:])) * DTSZ[dtype]
        nb = (nb + 63) // 64 * 64
        assert self.off + nb <= self.cap, f"SBUF overflow {self.off}+{nb} > {self.cap} ({name})"
        self.ntile += 1
        t = self.nc.alloc_sbuf_tensor_at(f"{name or 't'}_{self.ntile}", list(shape), dtype, offset=self.off)
        self.off += nb
        return t

    def mark(self):
        return self.off

    def reset(self, m):
        self.off = m

    def dsem(self, name="d"):
        s = Sem(self.es.enter_context(self.nc.semaphore(f"{name}_{len(self.dsems)}")))
        self.dsems.append(s); return s

    def _deps(self, eng, reads, writes):
        deps = []
        for b in reads:
            if b.w is not None: deps.append(b.w)
        for b in writes:
            if b.w is not None: deps.append(b.w)
            deps.extend(b.r)
        best = {}
        for (s, v) in deps:
            k = id(s)
            if k not in best or best[k][1] < v: best[k] = (s, v)
        waits = []
        kn = self.known[eng]
        for (s, v) in best.values():
            if eng == "pe" and s is self.esem["pe"]: continue
            if kn.get(id(s), 0) >= v: continue
            kn[id(s)] = v
            waits.append((s, v))
        return waits

    def op(self, eng, fn, reads=(), writes=(), sig=True):
        waits = self._deps(eng, reads, writes)
        S = self.esem[eng]
        if sig: S.n += 1
        tok = (S, S.n if sig else S.n + 1)
        self.q[eng].append((waits, fn, (S.h, 1) if sig else None))
        for b in reads: b.r.append(tok)
        for b in writes:
            b.w = tok; b.r = []
        self.ninst += 1
        return tok

    def dma(self, eng, out, in_, sem, reads=(), writes=(), **kw):
        waits = self._deps(eng, reads, writes)
        sem.n += 16
        tok = (sem, sem.n)
        self.q[eng].append((waits, lambda e: e.dma_start(out=out, in_=in_, **kw), (sem.h, 16)))
        for b in reads: b.r.append(tok)
        for b in writes:
            b.w = tok; b.r = []
        self.ninst += 1
        return tok

    def reg(self, e, v):
        if not hasattr(self, "_regs"): self._regs = {}
        if v not in self._regs: self._regs[v] = e.to_reg(v)
        return self._regs[v]

    def barrier(self):
        allsem = [self.esem[e] for e in self.ENG] + self.dsems
        for e in self.ENG:
            waits = []
            kn = self.known[e]
            for s in allsem:
                if s.n > 0 and kn.get(id(s), 0) < s.n and not (s is self.esem[e]):
                    kn[id(s)] = s.n; waits.append((s, s.n))
            if waits: self.q[e].append((waits, None, None))

    def emit(self):
        nc = self.nc
        self.barrier()
        with nc.Block() as block:
            def run(engname):
                def f(e):
                    for (waits, fn, inc) in self.q[engname]:
                        for (s, v) in waits: e.wait_ge(s.h, v)
                        if fn is None: continue
                        ins = fn(e)
                        if inc is not None: ins.then_inc(inc[0], inc[1])
                return f
            block.tensor(run("pe")); block.scalar(run("act")); block.vector(run("dve"))
            block.gpsimd(run("pool")); block.sync(run("sp"))


class Ring:
    def __init__(self, P, n, shape, dtype, name, sem=True):
        self.t = [P.tile(shape, dtype, f"{name}{i}") for i in range(n)]
        self.b = [Buf(f"{name}{i}") for i in range(n)]
        self.s = [P.dsem(name) for i in range(n)] if sem else [None] * n
        self.i = -1; self.n = n

    def next(self):
        self.i = (self.i + 1) % self.n
        return self.t[self.i], self.b[self.i], self.s[self.i]


def rope_tables():
    p = np.arange(128); d = p % 64
    axis = d // 32; half = (d % 32) // 16; fr = d % 16
    inv = (10000.0 ** (-(np.arange(0, 32, 2, dtype=np.float32)) / 32.0)).astype(np.float32)
    t = np.arange(SEQ)
    row = (t // 64).astype(np.float32); col = (t % 64).astype(np.float32)
    pos = np.where(axis[:, None] == 0, row[None, :], col[None, :]).astype(np.float32)
    ang = (pos * inv[fr][:, None]).astype(np.float32)
    C = np.cos(ang).astype(np.float32)
    S = np.sin(ang).astype(np.float32) * np.where(half == 0, -1.0, 1.0)[:, None].astype(np.float32)
    return np.ascontiguousarray(C), np.ascontiguousarray(S.astype(np.float32))


def hyena_tables():
    n = SEQ; N = 2 * SEQ
    tp = np.arange(N)
    pos = np.where(tp < n, tp, N - tp).astype(np.float32)
    pos[n] = 0.0
    tt = (pos / np.float32(n - 1)).astype(np.float32)
    w = (np.float32(2 * math.pi) * pos / np.float32(n)).astype(np.float32)
    bands = np.linspace(1e-4, 15.0, 16, dtype=np.float32)
    bw = (bands[None, :] * w[:, None]).astype(np.float32)
    z = np.concatenate([tt[:, None], np.cos(bw), -np.sin(bw)], axis=1).astype(np.float32)
    ttx = tt.copy(); ttx[n] = 1.0e4
    deltas = np.abs(np.linspace(math.log(1e-2) / 1.5, math.log(1e-2) / 0.3, D, dtype=np.float32)).astype(np.float32)
    T = {}
    T["zextT"] = np.ascontiguousarray(z.T)
    T["ttx"] = np.ascontiguousarray(ttx[None, :])
    T["negdelta"] = np.ascontiguousarray((-deltas).reshape(8, 128).T)
    s1 = np.arange(64)[:, None].astype(np.float64); f1 = np.arange(33)[None, :].astype(np.float64)
    a = 2 * np.pi * s1 * f1 / 64
    T["E1"] = np.concatenate([np.cos(a), -np.sin(a)], axis=1).astype(np.float32)
    s2 = np.arange(128)[:, None].astype(np.float64)
    a = 2 * np.pi * s2 * f1 / N
    Tr, Ti = np.cos(a), -np.sin(a)
    T["TW1"] = np.stack([np.stack([Tr, Ti], 1), np.stack([Ti, Tr], 1)], 1).astype(np.float32)
    f2 = np.arange(128)[None, :].astype(np.float64)
    a = 2 * np.pi * s2 * f2 / 128
    T["E2"] = np.stack([np.cos(a), -np.sin(a), np.sin(a)], 1).astype(np.float32)
    T["Ginv"] = np.stack([np.cos(a.T), np.sin(a.T)], 1).astype(np.float32)
    f1c = np.arange(33)[:, None].astype(np.float64); s2r = np.arange(128)[None, :].astype(np.float64)
    a = 2 * np.pi * f1c * s2r / N
    Tc, Ts = np.cos(a), np.sin(a)
    W1 = np.concatenate([np.stack([Tc, -Ts], 1), np.stack([-Ts, -Tc], 1)], 0)
    W2 = np.concatenate([np.stack([Ts, Tc], 1), np.stack([Tc, -Ts], 1)], 0)
    T["TW2"] = np.stack([W1, W2], 1).astype(np.float32)
    wt = np.full(33, 2.0); wt[0] = 1.0; wt[32] = 1.0
    s1r = np.arange(32)[None, :].astype(np.float64)
    a = 2 * np.pi * f1c * s1r / 64
    la = wt[:, None] * np.cos(a) / N; lb = -wt[:, None] * np.sin(a) / N
    T["LAB"] = np.stack([np.concatenate([la, la], 0), np.concatenate([lb, lb], 0)], 1).astype(np.float32)
    return {k: np.ascontiguousarray(v) for k, v in T.items()}


def build(debug=False, phases=99):
    nc = bass.Bass("TRN2", target_bir_lowering=False)
    skind = "ExternalOutput" if debug else "Internal"

    def din(name, shape, dt=F32):
        return nc.dram_tensor(name, list(shape), dt, kind="ExternalInput").ap()

    def dscr(name, shape, dt):
        return nc.dram_tensor(name, list(shape), dt, kind=skind).ap()

    xT = din("xT", [D, TOK]); cc = din("cc", [128, 8, 2]); ada_w = din("ada_w", [D, 6 * D])
    ada_bT = din("ada_bT", [128, 48]); gvec = din("gvec", [128, 24]); lamv = din("lamv", [1, 256])
    w_in = din("w_in", [D, INW]); ropeC = din("ropeC", [128, SEQ]); ropeS = din("ropeS", [128, SEQ])
    convw = din("convw", [128, 3, 24]); convb = din("convb", [128, 24]); subln = din("subln", [1, 128]); sublnT = din("sublnT", [128, 1])
    zextT = din("zextT", [33, 2 * SEQ]); ttx = din("ttx", [1, 2 * SEQ]); negdelta = din("negdelta", [128, 8])
    E1d = din("E1", [64, 66]); TW1d = din("TW1", [128, 2, 2, 33]); E2d = din("E2", [128, 3, 128]); Ginvd = din("Ginv", [128, 2, 128])
    TW2d = din("TW2", [66, 2, 2, 128]); LABd = din("LAB", [66, 2, 32])
    w_pa = din("w_pa", [D, D]); w_ph = din("w_ph", [D, D]); w_o = din("w_o", [D, D]); rw = din("rw", [D, 64]); rbias = din("rbias", [1, 64])
    ewg = din("ewg", [65 * 128, 2048]); ewu = din("ewu", [65 * 128, 2048]); ewd = din("ewd", [65 * 128, 2048])
    bvals = din("bvals", [1, NBLK]); pcol = din("pcol", [128, 1])
    fw1 = din("fw1", [33, 64]); fw2 = din("fw2", [64, 64]); fw3 = din("fw3", [64, 4096]); fvec = din("fvec", [64, 3]); hyb = din("hyb", [1, 2048])
    outT = nc.dram_tensor("outT", [D, SEQ], F32, kind="ExternalOutput").ap()

    QT = dscr("QT", [NH, 128, SEQ], BF16); KT = dscr("KT", [NH, 128, TOK], BF16)
    AXs = dscr("AXs", [D, SEQ], BF16); KERN = dscr("KERN", [2, D, 2 * SEQ], BF16)
    KF = dscr("KF", [2, 128, D, 2, 33], F32); HY = dscr("HY", [D, SEQ], BF16)
    X1 = dscr("X1", [D, SEQ], F32); HX2 = dscr("HX2", [D, SEQ], BF16); HX2tok = dscr("HX2tok", [SEQ, D], BF16)
    XG = dscr("XG", [NSLOT, D], BF16); YG = dscr("YG", [NSLOT, D], F32); WB = dscr("WB", [65 * 128, 6144], BF16)
    VV = dscr("VV", [NH, 128, 34, 128], BF16); UU = dscr("UU", [3, D, SEQ], F32); GG = dscr("GG", [2 * D, SEQ], BF16)
    dbg = {}
    if debug:
        dbg["hx"] = nc.dram_tensor("dbg_hx", [128, 8, TOK], BF16, kind="ExternalOutput").ap()
        dbg["vec"] = nc.dram_tensor("dbg_vec", [128, 80], F32, kind="ExternalOutput").ap()
        dbg["slot"] = nc.dram_tensor("dbg_slot", [128, 32, 8], I32, kind="ExternalOutput").ap()
        dbg["gk"] = nc.dram_tensor("dbg_gk", [128, 32, 8], F32, kind="ExternalOutput").ap()
        dbg["blk"] = nc.dram_tensor("dbg_blk", [128, NBLK], I32, kind="ExternalOutput").ap()
        dbg["acc"] = nc.dram_tensor("dbg_acc", [2, 128, 8, 2048], F32, kind="ExternalOutput").ap()

    with ExitStack() as es:
        P = Prog(nc, es)
        psb = [nc.alloc_psum_tensor(f"psb{i}", [128, 512], F32) for i in range(8)]
        psB = [Buf(f"ps{i}") for i in range(8)]
        pi = [0]

        def ps_next():
            pi[0] = (pi[0] + 1) % 8
            return psb[pi[0]], psB[pi[0]]

        vec = P.tile([128, 80], F32, "vec"); vecB = Buf("vec")
        ones = P.tile([128, 128], F32, "ones"); onesB = Buf("ones")
        ident = P.tile([128, 128], F32, "ident"); identB = Buf("ident")
        P.op("pool", lambda e: e.memset(ones[:], 1.0), writes=[onesB])
        P.op("pool", lambda e: e.memset(ident[:], 0.0), writes=[identB])
        P.op("pool", lambda e: e.affine_select(out=ident[:], in_=ident[:], pattern=[[-1, 128]], compare_op=ALU.not_equal,
                                               fill=1.0, base=0, channel_multiplier=1), reads=[identB], writes=[identB])
        g08 = P.tile([128, 128], F32, "g08"); g08B = Buf("g08")
        m_persist = P.mark()
        hxT = P.tile([128, 8, TOK], BF16, "hxT"); hxB = [Buf(f"hx{i}") for i in range(9)]
        m_hx = P.mark()

        cct = P.tile([128, 8, 2], F32, "cct"); sil = P.tile([128, 8, 2], F32, "sil"); cB = Buf(); silB = Buf()
        adab = P.tile([128, 48], F32, "adab"); gv = P.tile([128, 24], F32, "gv"); lamt = P.tile([128, 256], F32, "lamt")
        modT = P.tile([128, 48, 2], F32, "modT"); modB = Buf()
        smB = Buf(); s_small = P.dsem("small"); s_cc = P.dsem("cc")
        P.dma("sp", cct[:], cc[:, :, :], s_cc, writes=[cB])
        P.dma("sp", adab[:], ada_bT[:, :], s_small, writes=[smB])
        P.dma("sp", gv[:], gvec[:, :], s_small, writes=[smB])
        P.dma("sp", lamt[:], lamv.partition_broadcast(128), s_small, writes=[smB])
        P.op("act", lambda e: e.activation(out=sil[:], in_=cct[:], func=AF.Sigmoid), reads=[cB], writes=[silB])
        P.op("dve", lambda e: e.tensor_tensor(out=sil[:], in0=sil[:], in1=cct[:], op=ALU.mult), reads=[cB, silB], writes=[silB])
        aw = Ring(P, 2, [128, 8, 1024], F32, "adaw")
        psm, psmB = ps_next()
        adaw_v = ada_w.rearrange("(k p) f -> p k f", p=128)
        for g in range(6):
            wt, wB, ws = aw.next()
            P.dma("sp" if g % 2 == 0 else "act", wt[:], adaw_v[:, :, g * 1024:(g + 1) * 1024], ws, writes=[wB])
            for fc in range(8):
                f = g * 8 + fc
                for k in range(8):
                    P.op("pe", lambda e, wt=wt, k=k, fc=fc, f=f: e.matmul(psm[:, 2 * f:2 * f + 2], wt[:, k, fc * 128:(fc + 1) * 128],
                                                                         sil[:, k, :], start=(k == 0), stop=(k == 7)),
                         reads=[wB, silB], writes=[psmB], sig=(k == 7))
        P.op("dve", lambda e: e.tensor_tensor(out=modT[:], in0=psm[:, 0:96].rearrange("p (f j) -> p f j", j=2),
                                              in1=adab[:].unsqueeze(2).to_broadcast([128, 48, 2]), op=ALU.add),
             reads=[psmB, smB], writes=[modB])
        def vop(fn, rd=(modB, smB)):
            P.op("dve", fn, reads=list(rd) + [vecB], writes=[vecB])
        vop(lambda e: e.scalar_tensor_tensor(out=vec[:, 0:8], in0=modT[:, 8:16, 0], scalar=1.0, in1=gv[:, 0:8], op0=ALU.add, op1=ALU.mult))
        vop(lambda e: e.tensor_copy(out=vec[:, 8:16], in_=modT[:, 0:8, 0]))
        vop(lambda e: e.scalar_tensor_tensor(out=vec[:, 16:24], in0=modT[:, 8:16, 1], scalar=1.0, in1=gv[:, 0:8], op0=ALU.add, op1=ALU.mult))
        vop(lambda e: e.tensor_copy(out=vec[:, 24:32], in_=modT[:, 0:8, 1]))
        vop(lambda e: e.scalar_tensor_tensor(out=vec[:, 32:40], in0=modT[:, 32:40, 0], scalar=1.0, in1=gv[:, 8:16], op0=ALU.add, op1=ALU.mult))
        vop(lambda e: e.tensor_copy(out=vec[:, 40:48], in_=modT[:, 24:32, 0]))
        vop(lambda e: e.tensor_copy(out=vec[:, 48:56], in_=modT[:, 16:24, 0]))
        vop(lambda e: e.tensor_copy(out=vec[:, 56:64], in_=modT[:, 40:48, 0]))
        vop(lambda e: e.tensor_copy(out=vec[:, 68:76], in_=gv[:, 16:24]))
        lt = P.tile([128, 128], F32, "lt"); ltB = Buf()
        P.op("dve", lambda e: e.tensor_tensor(out=lt[:].rearrange("p (a b) -> p a b", a=2),
                                              in0=lamt[:].rearrange("p (a c b) -> p a c b", a=2, c=2)[:, :, 0, :],
                                              in1=lamt[:].rearrange("p (a c b) -> p a c b", a=2, c=2)[:, :, 1, :], op=ALU.mult),
             reads=[smB], writes=[ltB])
        P.op("dve", lambda e: e.tensor_reduce(out=vec[:, 66:68], in_=lt[:].rearrange("p (a b) -> p a b", a=2), axis=AXL.X, op=ALU.add),
             reads=[ltB, vecB], writes=[vecB])
        P.op("act", lambda e: e.activation(out=vec[:, 66:68], in_=vec[:, 66:68], func=AF.Exp), reads=[vecB], writes=[vecB])
        P.op("dve", lambda e: e.scalar_tensor_tensor(out=vec[:, 64:65], in0=vec[:, 67:68], scalar=-0.2, in1=vec[:, 66:67],
                                                     op0=ALU.add, op1=ALU.subtract), reads=[vecB], writes=[vecB])
        if debug:
            P.dma("sp", dbg["vec"][:, :], vec[:], s_small, reads=[vecB])

        xr = Ring(P, 2, [128, 8, 512], F32, "xt"); sq = P.tile([128, 8, 512], F32, "sq"); sqB = Buf()
        rstd = P.tile([128, 512], F32, "rstd"); rsB = Buf()
        xT_v = xT.rearrange("(k p) t -> p k t", p=128)

        def rms_rstd(src_t, src_b, n, sq=sq, sqB=sqB, rstd=rstd, rsB=rsB, eps=1e-6, dim=1024.0):
            P.op("act", lambda e: e.activation(out=sq[:, :, 0:n], in_=src_t[:, :, 0:n], func=AF.Square), reads=[src_b], writes=[sqB])
            pt, pB = ps_next()
            for k in range(8):
                P.op("pe", lambda e, k=k: e.matmul(pt[:, 0:n], ones[:], sq[:, k, 0:n], start=(k == 0), stop=(k == 7)),
                     reads=[sqB, onesB], writes=[pB], sig=(k == 7))
            P.op("dve", lambda e: e.tensor_scalar(out=rstd[:, 0:n], in0=pt[:, 0:n], scalar1=1.0 / dim, scalar2=eps, op0=ALU.mult, op1=ALU.add),
                 reads=[pB], writes=[rsB])
            P.op("act", lambda e: e.activation(out=rstd[:, 0:n], in_=rstd[:, 0:n], func=AF.Sqrt), reads=[rsB], writes=[rsB])
            P.op("dve", lambda e: e.reciprocal(out=rstd[:, 0:n], in_=rstd[:, 0:n]), reads=[rsB], writes=[rsB])

        for i in range(9):
            t0 = i * 512; n = 512 if i < 8 else 256
            xt, xB, xs = xr.next()
            P.dma("sp", xt[:, :, 0:n], xT_v[:, :, t0:t0 + n], xs, writes=[xB])
            rms_rstd(xt, xB, n)
            P.op("dve", lambda e, xt=xt, n=n: e.tensor_tensor(out=sq[:, :, 0:n], in0=xt[:, :, 0:n],
                                                              in1=rstd[:, 0:n].unsqueeze(1).to_broadcast([128, 8, n]), op=ALU.mult),
                 reads=[xB, rsB, sqB], writes=[sqB])
            ao = 0 if i < 8 else 16
            for k in range(8):
                P.op("act", lambda e, k=k, t0=t0, n=n, ao=ao: e.activation(out=hxT[:, k, t0:t0 + n], in_=sq[:, k, 0:n], func=AF.Identity,
                                                                             scale=vec[:, ao + k:ao + k + 1], bias=vec[:, ao + 8 + k:ao + 9 + k]),
                     reads=[sqB, vecB], writes=[hxB[i]])
        if debug:
            P.dma("sp", dbg["hx"][:, :, :], hxT[:], s_small, reads=hxB)
        P.barrier()
        P.reset(m_hx)
        if phases < 2:
            P.emit(); return nc

        m2 = P.mark()
        wr = Ring(P, 2, [128, 8, 1024], BF16, "wg"); wperm = P.tile([128, 8, 1024], BF16, "wperm"); wpB = Buf()
        cw = P.tile([128, 3, 24], F32, "cw"); cb = P.tile([128, 24], F32, "cb"); cwB = Buf()
        s_cw = P.dsem("cw"); s_rope = P.dsem("rope")
        P.dma("act", cw[:], convw[:, :, :], s_cw, writes=[cwB])
        P.dma("act", cb[:], convb[:, :], s_cw, writes=[cwB])
        ob = Ring(P, 4, [128, 512], BF16, "ob")
        vst = Ring(P, 2, [128, 1024], BF16, "vst")
        m2b = P.mark()
        rC = P.tile([128, SEQ], F32, "rC"); rS = P.tile([128, SEQ], F32, "rS"); ropB = Buf()
        P.dma("act", rC[:], ropeC[:, :], s_rope, writes=[ropB])
        P.dma("act", rS[:], ropeS[:, :], s_rope, writes=[ropB])
        t1r = Ring(P, 4, [128, 512], F32, "t1", sem=False)
        win_v = w_in.rearrange("(k p) n -> p k n", p=128)
        alt = [0]
        for g in (0, 2, 1, 3, 4, 5, 6, 7):
            if g == 1:
                P.barrier(); P.reset(m2b)
                pbuf = P.tile([128, SEQ + 2], F32, "pbuf"); pbB = Buf()
                ur = Ring(P, 2, [128, SEQ], F32, "ubuf")
                P.op("pool", lambda e: e.memset(pbuf[:, 0:1], 0.0), writes=[pbB])
                P.op("pool", lambda e: e.memset(pbuf[:, SEQ + 1:SEQ + 2], 0.0), writes=[pbB])
            wt, wB, ws = wr.next()
            P.dma("pool", wt[:], win_v[:, :, g * 1024:(g + 1) * 1024], ws, writes=[wB])
            if g in (0, 2):
                wv = wt[:].rearrange("p k (b h j) -> p k b h j", h=2, j=16)
                pv = wperm[:].rearrange("p k (b h j) -> p k b h j", h=2, j=16)
                for k in range(8):
                    P.op("pool", lambda e, wv=wv, k=k: e.tensor_copy(out=pv[:, k, :, 0, :], in_=wv[:, k, :, 1, :]), reads=[wB], writes=[wpB])
                    P.op("pool", lambda e, wv=wv, k=k: e.tensor_copy(out=pv[:, k, :, 1, :], in_=wv[:, k, :, 0, :]), reads=[wB], writes=[wpB])
                dst = KT if g == 0 else QT
                for h in range(8):
                    for i in range(9 if g == 0 else 8):
                        t0 = i * 512; n = 512 if i < 8 else 256
                        pa, paB = ps_next()
                        for k in range(8):
                            P.op("pe", lambda e, pa=pa, wt=wt, k=k, h=h, t0=t0, n=n: e.matmul(pa[:, 0:n], wt[:, k, h * 128:(h + 1) * 128],
                                                                                               hxT[:, k, t0:t0 + n], start=(k == 0), stop=(k == 7)),
                                 reads=[wB, hxB[i]], writes=[paB], sig=(k == 7))
                        o, oB, osem = ob.next()
                        if i < 8:
                            pb_, pbB_ = ps_next()
                            for k in range(8):
                                P.op("pe", lambda e, pb_=pb_, k=k, h=h, t0=t0, n=n: e.matmul(pb_[:, 0:n], wperm[:, k, h * 128:(h + 1) * 128],
                                                                                              hxT[:, k, t0:t0 + n], start=(k == 0), stop=(k == 7)),
                                     reads=[wpB, hxB[i]], writes=[pbB_], sig=(k == 7))
                            ta, taB, _ = t1r.next(); tb, tbB, _ = t1r.next()
                            P.op("dve", lambda e, ta=ta, pa=pa, t0=t0: e.tensor_tensor(out=ta[:], in0=pa[:], in1=rC[:, t0:t0 + 512], op=ALU.mult),
                                 reads=[paB, ropB], writes=[taB])
                            P.op("dve", lambda e, tb=tb, pb_=pb_, t0=t0: e.tensor_tensor(out=tb[:], in0=pb_[:], in1=rS[:, t0:t0 + 512], op=ALU.mult),
                                 reads=[pbB_, ropB], writes=[tbB])
                            P.op("pool", lambda e, o=o, ta=ta, tb=tb: e.tensor_tensor(out=o[:], in0=ta[:], in1=tb[:], op=ALU.add),
                                 reads=[taB, tbB], writes=[oB])
                        else:
                            P.op("act", lambda e, o=o, pa=pa, n=n: e.copy(out=o[:, 0:n], in_=pa[:, 0:n]), reads=[paB], writes=[oB])
                        P.dma("sp", dst[h, :, t0:t0 + n], o[:, 0:n], osem, reads=[oB])
            elif g == 1:
                for j in range(34):
                    vt, vB, vs = vst.next()
                    for hf in range(2):
                        pa, paB = ps_next()
                        for k in range(8):
                            P.op("pe", lambda e, pa=pa, wt=wt, k=k, j=j, hf=hf: e.matmul(pa[:], hxT[:, k, j * 128:(j + 1) * 128],
                                                                                         wt[:, k, hf * 512:(hf + 1) * 512], start=(k == 0), stop=(k == 7)),
                                 reads=[wB, hxB[j // 4]], writes=[paB], sig=(k == 7))
                        if hf == 0:
                            P.op("act", lambda e, vt=vt, pa=pa: e.copy(out=vt[:, 0:512], in_=pa[:]), reads=[paB], writes=[vB])
                        else:
                            P.op("dve", lambda e, vt=vt, pa=pa: e.tensor_copy(out=vt[:, 512:1024], in_=pa[:]), reads=[paB], writes=[vB])
                    P.dma("sp", VV[:, :, j, :].rearrange("h p e -> p h e"), vt[:].rearrange("p (h e) -> p h e", h=NH), vs, reads=[vB])
            elif g in (3, 4, 5):
                for c in range(8):
                    ch = (g - 3) * 8 + c
                    for i in range(8):
                        t0 = i * 512
                        pa, paB = ps_next()
                        for k in range(8):
                            P.op("pe", lambda e, pa=pa, wt=wt, k=k, c=c, t0=t0: e.matmul(pa[:], wt[:, k, c * 128:(c + 1) * 128],
                                                                                         hxT[:, k, t0:t0 + 512], start=(k == 0), stop=(k == 7)),
                                 reads=[wB, hxB[i]], writes=[paB], sig=(k == 7))
                        P.op("act", lambda e, pa=pa, t0=t0: e.copy(out=pbuf[:, 1 + t0:1 + t0 + 512], in_=pa[:]), reads=[paB], writes=[pbB])
                    u, uB, us = ur.next()
                    eng = "dve"
                    P.op(eng, lambda e, u=u, ch=ch: e.tensor_scalar(out=u[:], in0=pbuf[:, 0:SEQ], scalar1=cw[:, 0, ch:ch + 1], scalar2=cb[:, ch:ch + 1],
                                                                    op0=ALU.mult, op1=ALU.add), reads=[pbB, cwB], writes=[uB])
                    P.op(eng, lambda e, u=u, ch=ch: e.scalar_tensor_tensor(out=u[:], in0=pbuf[:, 1:SEQ + 1], scalar=cw[:, 1, ch:ch + 1], in1=u[:],
                                                                           op0=ALU.mult, op1=ALU.add), reads=[pbB, cwB, uB], writes=[uB])
                    P.op(eng, lambda e, u=u, ch=ch: e.scalar_tensor_tensor(out=u[:], in0=pbuf[:, 2:SEQ + 2], scalar=cw[:, 2, ch:ch + 1], in1=u[:],
                                                                           op0=ALU.mult, op1=ALU.add), reads=[pbB, cwB, uB], writes=[uB])
                    P.dma("sp", UU[g - 3, c * 128:(c + 1) * 128, :], u[:], us, reads=[uB])
            else:
                for c in range(8):
                    for i in range(8):
                        t0 = i * 512
                        pa, paB = ps_next()
                        for k in range(8):
                            P.op("pe", lambda e, pa=pa, wt=wt, k=k, c=c, t0=t0: e.matmul(pa[:], wt[:, k, c * 128:(c + 1) * 128],
                                                                                         hxT[:, k, t0:t0 + 512], start=(k == 0), stop=(k == 7)),
                                 reads=[wB, hxB[i]], writes=[paB], sig=(k == 7))
                        o, oB, osem = ob.next()
                        P.op("act", lambda e, o=o, pa=pa: e.activation(out=o[:], in_=pa[:], func=AF.Sigmoid), reads=[paB], writes=[oB])
                        P.dma("sp", GG[(g - 6) * 1024 + c * 128:(g - 6) * 1024 + (c + 1) * 128, t0:t0 + 512], o[:], osem, reads=[oB])
        P.barrier()
        P.reset(m_persist)
        if phases < 3:
            P.emit(); return nc

        s_g = P.dsem("g08")
        g08c = P.tile([128, 1], F32, "g08c")
        P.dma("sp", g08c[:], sublnT[:, :], s_g, writes=[g08B])
        P.op("dve", lambda e: e.tensor_scalar(out=g08c[:], in0=g08c[:], scalar1=0.8, scalar2=None, op0=ALU.mult), reads=[g08B], writes=[g08B])
        qzr = [Ring(P, 2, [128, SEQ], BF16, f"qz{m}") for m in range(2)]
        kr = Ring(P, 2, [128, TOK], BF16, "kT"); vr = Ring(P, 2, [128, 34, 128], BF16, "vh")
        for m in range(2):
            for bi_ in range(2):
                t_ = qzr[m].t[bi_]
                P.op("pool", lambda e, t_=t_, m=m: e.memset(t_[64 * (1 - m):64 * (1 - m) + 64, :], 0.0), writes=[qzr[m].b[bi_]])
        onesb = P.tile([128, 128], BF16, "onesb"); onesbB = Buf("onesb")
        P.op("pool", lambda e: e.memset(onesb[:], 1.0), writes=[onesbB])
        ptr = Ring(P, 3, [128, 34, 512], BF16, "pT", sem=False)
        zfl = P.tile([128, 2048], BF16, "zfl"); ztB = Buf("zfl"); xgB = Buf("XG"); s_z = P.dsem("zfill")
        P.op("pool", lambda e, zfl=zfl: e.memset(zfl[:], 0.0), writes=[ztB])
        XG_z = XG.rearrange("(p a) d -> p (a d)", p=128)
        zwin = [Buf(f"zw{i}") for i in range(4)]; zsem = [s_z] + [P.dsem("zfill") for _ in range(3)]
        for zi in range(NBLK * D // 2048):
            P.dma("pool", XG_z[:, zi * 2048:(zi + 1) * 2048], zfl[:], zsem[zi % 4], reads=[ztB], writes=[zwin[zi % 4]])
        wstg = Ring(P, 1, [128, 6144], BF16, "wstg"); s_wb = P.dsem("wbst"); wbB = Buf("WB")
        def precast(e_):
            st_t, st_B, st_s = wstg.next()
            P.dma("pool", st_t[:, 0:2048], ewg[e_ * 128:(e_ + 1) * 128, :], st_s, writes=[st_B])
            P.dma("pool", st_t[:, 2048:4096], ewu[e_ * 128:(e_ + 1) * 128, :], st_s, reads=[], writes=[])
            P.dma("pool", st_t[:, 4096:6144], ewd[e_ * 128:(e_ + 1) * 128, :], st_s, reads=[], writes=[])
            st_B.w = (st_s, st_s.n)
            P.dma("pool", WB[e_ * 128:(e_ + 1) * 128, :], st_t[:], s_wb, reads=[st_B], writes=([wbB] if e_ == 64 else []))
        tq = P.tile([128, 512], F32, "tq"); tqB = Buf("tq")
        csr = Ring(P, 2, [128, 2, 512], F32, "csum", sem=False)
        rr = Ring(P, 2, [128, 512], F32, "rrec", sem=False)
        oo = P.tile([128, 512], F32, "oo"); ooB = Buf("oo"); o2 = P.tile([128, 512], F32, "o2"); o2B = Buf("o2")
        axo = Ring(P, 2, [128, 512], BF16, "axo")
        SB = [1, 2, 3]; TB = 0; NDV = 24
        OB = {0: (4, 5), 1: (6, 7)}
        sidx = [0]

        def load_head(h):
            k, kB, ks_ = kr.next(); v, vB, vs_ = vr.next()
            qs = []
            for m in range(2):
                q, qB, qs_ = qzr[m].next()
                P.dma("sp" if m == 0 else "act", q[64 * m:64 * m + 64, :], QT[h, 64 * m:64 * m + 64, :], qs_, writes=[qB])
                qs.append((q, qB))
            P.dma("sp", k[:], KT[h, :, :], ks_, writes=[kB])
            P.dma("act", v[:], VV[h, :, :, :], vs_, writes=[vB])
            return (qs, k, kB, v, vB)

        def qk_exp_steps(hd, i, m, pt, ptB):
            qs, k, kB, v, vB = hd
            q, qB = qs[m]
            steps = []
            for j in range(34):
                def st(j=j):
                    b = SB[sidx[0] % 3]; sidx[0] += 1
                    P.op("pe", lambda e: e.matmul(psb[b][:], k[:, j * 128:(j + 1) * 128], q[:, i * 512:(i + 1) * 512], start=True, stop=True),
                         reads=[kB, qB], writes=[psB[b]])
                    P.op("act", lambda e: e.activation(out=pt[:, j, :], in_=psb[b][:], func=AF.Exp, scale=0.125),
                         reads=[psB[b]], writes=[ptB])
                steps.append(st)
            return steps

        def av_steps(hd, m, pt, ptB):
            qs, k, kB, v, vB = hd
            ob_, sb_ = OB[m]
            cs, csB, _ = csr.next()
            steps = []
            for j in range(34):
                def st(j=j):
                    P.op("pe", lambda e: e.matmul(psb[ob_][:], v[:, j, :], pt[:, j, :], start=(j == 0), stop=(j == 33)),
                         reads=[ptB, vB], writes=[psB[ob_]], sig=(j == 33))
                    if j == 1:
                        P.op("dve", lambda e: e.tensor_copy(out=cs[:], in_=pt[:, 0:2, :]), reads=[ptB], writes=[csB])
                    elif j % 2 == 1 and j < NDV:
                        P.op("dve", lambda e: e.tensor_tensor(out=cs[:], in0=pt[:, j - 1:j + 1, :], in1=cs[:], op=ALU.add), reads=[ptB, csB], writes=[csB])
                    if j == NDV - 1:
                        P.op("dve", lambda e: e.tensor_tensor(out=cs[:, 0, :], in0=cs[:, 0, :], in1=cs[:, 1, :], op=ALU.add), reads=[csB], writes=[csB])
                    if j >= NDV:
                        P.op("pe", lambda e: e.matmul(psb[sb_][:], onesb[:], pt[:, j, :], start=(j == NDV), stop=False),
                             reads=[ptB, onesbB], writes=[psB[sb_]], sig=False)
                    if j == 33:
                        P.op("pe", lambda e: e.matmul(psb[sb_][:], ones[:], cs[:, 0, :], start=False, stop=True), reads=[csB, onesB], writes=[psB[sb_]])
                steps.append(st)
            return steps

        def combine_a():
            ob_, sb_ = OB[0]
            r, rB, _ = rr.next()
            P.op("dve", lambda e: e.reciprocal(out=r[:], in_=psb[sb_][:]), reads=[psB[sb_]], writes=[rB])
            P.op("dve", lambda e: e.tensor_tensor(out=tq[:], in0=psb[ob_][:], in1=r[:], op=ALU.mult), reads=[psB[ob_], rB, tqB], writes=[tqB])

        def combine_b(h, i):
            ob_, sb_ = OB[1]
            r, rB, _ = rr.next()
            P.op("dve", lambda e: e.reciprocal(out=r[:], in_=psb[sb_][:]), reads=[psB[sb_]], writes=[rB])
            P.op("dve", lambda e: e.tensor_tensor(out=r[:], in0=psb[ob_][:], in1=r[:], op=ALU.mult), reads=[psB[ob_], rB], writes=[rB])
            P.op("dve", lambda e: e.scalar_tensor_tensor(out=oo[:], in0=r[:], scalar=vec[:, 64:65], in1=tq[:], op0=ALU.mult, op1=ALU.add),
                 reads=[rB, vecB, tqB, ooB], writes=[ooB])
            P.op("dve", lambda e: e.tensor_tensor(out=o2[:], in0=oo[:], in1=oo[:], op=ALU.mult), reads=[ooB, o2B], writes=[o2B])
            P.op("pe", lambda e: e.matmul(psb[TB][:], ones[:], o2[:], start=True, stop=True), reads=[o2B, onesB], writes=[psB[TB]])
            P.op("dve", lambda e: e.tensor_scalar(out=o2[:], in0=psb[TB][:], scalar1=1.0 / 128, scalar2=1e-6, op0=ALU.mult, op1=ALU.add),
                 reads=[psB[TB], o2B], writes=[o2B])
            P.op("act", lambda e: e.activation(out=o2[:], in_=o2[:], func=AF.Ln), reads=[o2B], writes=[o2B])
            P.op("act", lambda e: e.activation(out=o2[:], in_=o2[:], func=AF.Exp, scale=-0.5), reads=[o2B], writes=[o2B])
            ao, aoB, aos = axo.next()
            P.op("dve", lambda e: e.scalar_tensor_tensor(out=ao[:], in0=oo[:], scalar=g08c[:, 0:1], in1=o2[:], op0=ALU.mult, op1=ALU.mult),
                 reads=[ooB, o2B, g08B], writes=[aoB])
            P.dma("sp", AXs[h * 128:(h + 1) * 128, i * 512:(i + 1) * 512], ao[:], aos, reads=[aoB])

        stages = [(h, i, m) for h in range(NH) for i in range(8) for m in range(2)]
        heads = {0: load_head(0)}
        prev = None
        for si, (h, i, m) in enumerate(stages):
            if i == 1 and m == 0 and h + 1 < NH:
                heads[h + 1] = load_head(h + 1)
            if si % 2 == 0:
                precast(si // 2)
            pt, ptB, _ = ptr.next()
            qk = qk_exp_steps(heads[h], i, m, pt, ptB)
            av_prev, post_prev = prev if prev is not None else ([], None)
            for j in range(34):
                qk[j]()
                if av_prev: av_prev[j]()
            if post_prev is not None: post_prev()
            post = (lambda: combine_a()) if m == 0 else (lambda h=h, i=i: combine_b(h, i))
            prev = (av_steps(heads[h], m, pt, ptB), post)
        for st in prev[0]: st()
        prev[1]()
        precast(64)
        P.barrier()
        P.reset(m_persist)
        if phases < 4:
            P.emit(); return nc

        TWO_PI = 2.0 * math.pi
        s_t = P.dsem("tabs"); tabB = Buf("tabs")
        E1 = P.tile([64, 66], BF16, "E1"); TW1 = P.tile([128, 2, 2, 33], F32, "TW1"); E2 = P.tile([128, 3, 128], BF16, "E2")
        Ginv = P.tile([128, 2, 128], BF16, "Ginv"); TW2 = P.tile([66, 2, 2, 128], F32, "TW2"); LAB = P.tile([66, 2, 32], BF16, "LAB")
        hb = P.tile([128, 2048], F32, "hb"); ndl = P.tile([128, 8], F32, "ndl")
        P.dma("pool", E1[:], E1d[:, :], s_t, writes=[tabB]); P.dma("sp", TW1[:], TW1d[:, :, :, :], s_t, writes=[tabB])
        P.dma("pool", E2[:], E2d[:, :, :], s_t, writes=[tabB]); P.dma("pool", Ginv[:], Ginvd[:, :, :], s_t, writes=[tabB])
        P.dma("sp", TW2[:], TW2d[:, :, :, :], s_t, writes=[tabB]); P.dma("pool", LAB[:], LABd[:, :, :], s_t, writes=[tabB])
        P.dma("sp", hb[:], hyb.partition_broadcast(128), s_t, writes=[tabB]); P.dma("sp", ndl[:], negdelta[:, :], s_t, writes=[tabB])
        m4 = P.mark()

        hd2T = P.tile([64, 2 * SEQ], BF16, "hd2T"); hd2B = Buf("hd2")
        w3t = P.tile([64, 4096], BF16, "w3t"); w3B = Buf("w3")
        s_w3 = P.dsem("w3")
        P.dma("pool", w3t[:], fw3[:, :], s_w3, writes=[w3B])
        m4a = P.mark()
        zt = P.tile([33, 2 * SEQ], F32, "zt"); w1t = P.tile([33, 64], F32, "w1t"); w2t = P.tile([64, 64], F32, "w2t"); fv = P.tile([64, 3], F32, "fv")
        fB = Buf("filt_in")
        s_f = P.dsem("filt")
        P.dma("sp", zt[:], zextT[:, :], s_f, writes=[fB]); P.dma("sp", w1t[:], fw1[:, :], s_f, writes=[fB])
        P.dma("sp", w2t[:], fw2[:, :], s_f, writes=[fB]); P.dma("sp", fv[:], fvec[:, :], s_f, writes=[fB])
        ar = Ring(P, 2, [64, 512], F32, "marg", sem=False); kir = Ring(P, 2, [64, 512], I32, "mki", sem=False)
        kfr = Ring(P, 2, [64, 512], F32, "mkf", sem=False); h1r = Ring(P, 2, [64, 512], F32, "mh1", sem=False)

        def sin_layer(ps_, psB_, bcol, out_ap, outB):
            a, aB, _ = ar.next(); ki, kiB, _ = kir.next(); kf_, kfB, _ = kfr.next()
            P.op("dve", lambda e: e.tensor_scalar(out=a[:], in0=ps_[0:64, :], scalar1=fv[:, bcol:bcol + 1], scalar2=fv[:, 2:3], op0=ALU.add, op1=ALU.mult),
                 reads=[psB_, fB], writes=[aB])
            P.op("dve", lambda e: e.tensor_scalar(out=ki[:], in0=a[:], scalar1=1.0 / TWO_PI, scalar2=None, op0=ALU.mult), reads=[aB], writes=[kiB])
            P.op("dve", lambda e: e.tensor_copy(out=kf_[:], in_=ki[:]), reads=[kiB], writes=[kfB])
            P.op("dve", lambda e: e.scalar_tensor_tensor(out=a[:], in0=kf_[:], scalar=-TWO_PI, in1=a[:], op0=ALU.mult, op1=ALU.add),
                 reads=[kfB, aB], writes=[aB])
            P.op("dve", lambda e: e.tensor_scalar(out=a[:], in0=a[:], scalar1=math.pi, scalar2=-math.pi, op0=ALU.min, op1=ALU.max), reads=[aB], writes=[aB])
            P.op("act", lambda e: e.activation(out=out_ap, in_=a[:], func=AF.Sin), reads=[aB], writes=[outB])

        for tt in range(16):
            pa, paB = ps_next()
            P.op("pe", lambda e, pa=pa, tt=tt: e.matmul(pa[0:64, :], w1t[:], zt[:, tt * 512:(tt + 1) * 512], start=True, stop=True),
                 reads=[fB], writes=[paB])
            h1, h1B, _ = h1r.next()
            sin_layer(pa, paB, 0, h1[:], h1B)
            pb_, pbB_ = ps_next()
            P.op("pe", lambda e, pb_=pb_, h1=h1: e.matmul(pb_[0:64, :], w2t[:], h1[:], start=True, stop=True), reads=[fB, h1B], writes=[pbB_])
            sin_layer(pb_, pbB_, 1, hd2T[:, tt * 512:(tt + 1) * 512], hd2B)
        P.barrier(); P.reset(m4a)

        ttb = P.tile([128, 2 * SEQ], F32, "ttb"); ttB = Buf("ttb")
        s_tt = P.dsem("ttb")
        P.dma("sp", ttb[:], ttx.partition_broadcast(128), s_tt, writes=[ttB])
        kur = Ring(P, 2, [128, 2 * SEQ], F32, "ku", sem=False); kbr = Ring(P, 2, [128, 2 * SEQ], BF16, "kub")
        dkr = Ring(P, 3, [128, 512], F32, "dk", sem=False); abr = Ring(P, 3, [128, 512], F32, "kab", sem=False)
        asum = P.tile([128, 32], F32, "asum"); asB = Buf("asum")
        asum2 = P.tile([128, 2, 32], F32, "asum2")
        for cc in range(8):
            kus = [kur.next() for _ in range(2)]
            for tt in range(16):
                dr = 0 if tt < 8 else 1
                dk, dkB, _ = dkr.next()
                P.op("act", lambda e, dk=dk, tt=tt, cc=cc: e.activation(out=dk[:], in_=ttb[:, tt * 512:(tt + 1) * 512], func=AF.Exp, scale=ndl[:, cc:cc + 1]),
                     reads=[ttB, tabB], writes=[dkB])
                for o in range(2):
                    ku, kuB, _ = kus[o]
                    col0 = o * 2048 + dr * 1024 + cc * 128
                    pa, paB = ps_next()
                    P.op("pe", lambda e, pa=pa, col0=col0, tt=tt: e.matmul(pa[:], w3t[:, col0:col0 + 128], hd2T[:, tt * 512:(tt + 1) * 512], start=True, stop=True),
                         reads=[w3B, hd2B], writes=[paB])
                    P.op("dve", lambda e, ku=ku, pa=pa, dk=dk, tt=tt: e.tensor_tensor(out=ku[:, tt * 512:(tt + 1) * 512], in0=pa[:], in1=dk[:], op=ALU.mult),
                         reads=[paB, dkB], writes=[kuB])
                    ab, abB, _ = abr.next()
                    P.op("act", lambda e, ab=ab, ku=ku, tt=tt: e.activation(out=ab[:], in_=ku[:, tt * 512:(tt + 1) * 512], func=AF.Abs), reads=[kuB], writes=[abB])
                    P.op("dve", lambda e, ab=ab, tt=tt, o=o: e.tensor_reduce(out=asum2[:, o, tt:tt + 1], in_=ab[:], axis=AXL.X, op=ALU.add), reads=[abB, asB], writes=[asB])
            for o in range(2):
                ku, kuB, _ = kus[o]
                P.op("dve", lambda e, o=o: e.tensor_reduce(out=asum2[:, o, 16:17], in_=asum2[:, o, 0:16], axis=AXL.X, op=ALU.add), reads=[asB], writes=[asB])
                P.op("dve", lambda e, o=o: e.reciprocal(out=asum2[:, o, 17:18], in_=asum2[:, o, 16:17]), reads=[asB], writes=[asB])
                kb, kbB, kbs = kbr.next()
                P.op("act", lambda e, kb=kb, ku=ku, o=o: e.activation(out=kb[:], in_=ku[:], func=AF.Copy, scale=asum2[:, o, 17:18]),
                     reads=[kuB, asB], writes=[kbB])
                P.dma("sp", KERN[o, cc * 128:(cc + 1) * 128, :], kb[:], kbs, reads=[kbB])
        P.barrier(); P.reset(m4)
        if phases < 5:
            P.emit(); return nc

        tmr = Ring(P, 4, [128, 7, 2, 33], F32, "twtmp", sem=False)

        def fft_fwd(Z, ZB, K, nch, Bt, BtB):
            c = 0
            while c < nch:
                g = min(7, nch - c)
                pa, paB = ps_next()
                for u in range(g):
                    P.op("pe", lambda e, pa=pa, u=u, c=c: e.matmul(pa[:, u * 66:(u + 1) * 66], Z[0:K, c + u, :], E1[0:K, :], start=True, stop=True),
                         reads=[ZB, tabB], writes=[paB], sig=(u == g - 1))
                A = pa[:, 0:g * 66].rearrange("p (g r f) -> p g r f", r=2, f=33)
                t1, t1B, _ = tmr.next(); t2, t2B, _ = tmr.next()
                P.op("dve", lambda e, A=A, t1=t1, g=g: e.tensor_tensor(out=t1[:, 0:g], in0=A, in1=TW1[:, 0].unsqueeze(1).to_broadcast([128, g, 2, 33]), op=ALU.mult),
                     reads=[paB, tabB], writes=[t1B])
                P.op("dve", lambda e, A=A, t2=t2, g=g: e.tensor_tensor(out=t2[:, 0:g], in0=A, in1=TW1[:, 1].unsqueeze(1).to_broadcast([128, g, 2, 33]), op=ALU.mult),
                     reads=[paB, tabB], writes=[t2B])
                P.op("pool", lambda e, t1=t1, g=g, c=c: e.tensor_tensor(out=Bt[:, 0, c:c + g, :], in0=t1[:, 0:g, 0, :], in1=t1[:, 0:g, 1, :], op=ALU.subtract),
                     reads=[t1B], writes=[BtB])
                P.op("pool", lambda e, t2=t2, g=g, c=c: e.tensor_tensor(out=Bt[:, 1, c:c + g, :], in0=t2[:, 0:g, 0, :], in1=t2[:, 0:g, 1, :], op=ALU.add),
                     reads=[t2B], writes=[BtB])
                c += g

        def fft_stage2(Bt, BtB, c0, n):
            xr_, xrB = ps_next(); xi_, xiB = ps_next()
            br = Bt[:, 0, c0:c0 + n, :].rearrange("p c f -> p (c f)"); bi = Bt[:, 1, c0:c0 + n, :].rearrange("p c f -> p (c f)")
            w = n * 33
            P.op("pe", lambda e: e.matmul(xr_[:, 0:w], E2[:, 0, :], br, start=True, stop=False), reads=[BtB, tabB], writes=[xrB], sig=False)
            P.op("pe", lambda e: e.matmul(xr_[:, 0:w], E2[:, 2, :], bi, start=False, stop=True), reads=[BtB, tabB], writes=[xrB])
            P.op("pe", lambda e: e.matmul(xi_[:, 0:w], E2[:, 0, :], bi, start=True, stop=False), reads=[BtB, tabB], writes=[xiB], sig=False)
            P.op("pe", lambda e: e.matmul(xi_[:, 0:w], E2[:, 1, :], br, start=False, stop=True), reads=[BtB, tabB], writes=[xiB])
            return xr_, xrB, xi_, xiB

        def fft_fwd_g(Z, ZB, K, nch, Bt, BtB, tring):
            c = 0
            while c < nch:
                g = min(7, nch - c)
                pa, paB = ps_next()
                for u in range(g):
                    P.op("pe", lambda e, pa=pa, u=u, c=c: e.matmul(pa[:, u * 66:(u + 1) * 66], Z[0:K, c + u, :], E1[0:K, :], start=True, stop=True),
                         reads=[ZB, tabB], writes=[paB], sig=(u == g - 1))
                A = pa[:, 0:g * 66].rearrange("p (g r f) -> p g r f", r=2, f=33)
                t1, t1B, _ = tring.next(); t2, t2B, _ = tring.next()
                P.op("dve", lambda e, A=A, t1=t1, g=g: e.tensor_tensor(out=t1[:, 0:g], in0=A, in1=TW1[:, 0].unsqueeze(1).to_broadcast([128, g, 2, 33]), op=ALU.mult),
                     reads=[paB, tabB], writes=[t1B])
                P.op("dve", lambda e, A=A, t2=t2, g=g: e.tensor_tensor(out=t2[:, 0:g], in0=A, in1=TW1[:, 1].unsqueeze(1).to_broadcast([128, g, 2, 33]), op=ALU.mult),
                     reads=[paB, tabB], writes=[t2B])
                P.op("pool", lambda e, t1=t1, g=g, c=c: e.tensor_tensor(out=Bt[:, 0, c:c + g, :], in0=t1[:, 0:g, 0, :], in1=t1[:, 0:g, 1, :], op=ALU.subtract),
                     reads=[t1B], writes=[BtB])
                P.op("pool", lambda e, t2=t2, g=g, c=c: e.tensor_tensor(out=Bt[:, 1, c:c + g, :], in0=t2[:, 0:g, 0, :], in1=t2[:, 0:g, 1, :], op=ALU.add),
                     reads=[t2B], writes=[BtB])
                c += g
                yield

        zkr = Ring(P, 2, [64, 64, 128], BF16, "zk"); btr = Ring(P, 2, [128, 2, 64, 33], BF16, "bt", sem=False)
        kfo = Ring(P, 2, [128, 64, 2, 33], F32, "kfo")

        def kf_chain(o, hc):
            zk, zkB, zks = zkr.next()
            for q4 in range(2):
                P.dma("sp", zk[:, q4 * 32:(q4 + 1) * 32, :],
                      KERN[o, hc * 64 + q4 * 32:hc * 64 + (q4 + 1) * 32, :].rearrange("c (a b) -> a c b", b=128), zks, writes=[zkB])
            bt, btB, _ = btr.next()
            yield
            yield from fft_fwd_g(zk, zkB, 64, 64, bt, btB, tmr)
            ko, koB, kos = kfo.next()
            c0 = 0
            while c0 < 64:
                n = min(15, 64 - c0)
                xr_, xrB, xi_, xiB = fft_stage2(bt, btB, c0, n)
                P.op("dve", lambda e, ko=ko, xr_=xr_, c0=c0, n=n, o=o, hc=hc: e.tensor_tensor(
                    out=ko[:, c0:c0 + n, 0, :], in0=xr_[:, 0:n * 33].rearrange("p (c f) -> p c f", f=33),
                    in1=hb[:, o * 1024 + hc * 64 + c0:o * 1024 + hc * 64 + c0 + n].unsqueeze(2).to_broadcast([128, n, 33]), op=ALU.add),
                    reads=[xrB, tabB], writes=[koB])
                P.op("act", lambda e, ko=ko, xi_=xi_, c0=c0, n=n: e.copy(out=ko[:, c0:c0 + n, 1, :], in_=xi_[:, 0:n * 33].rearrange("p (c f) -> p c f", f=33)),
                     reads=[xiB], writes=[koB])
                c0 += n
                yield
            P.dma("act", KF[o, :, hc * 64:(hc + 1) * 64, :, :], ko[:], kos, reads=[koB])

        for hc in range(16):
            gens = [kf_chain(0, hc), kf_chain(1, hc)]
            alive = [True, True]
            while any(alive):
                for gi_ in range(2):
                    if alive[gi_]:
                        try:
                            next(gens[gi_])
                        except StopIteration:
                            alive[gi_] = False
        P.barrier(); P.reset(m4)
        if phases < 6:
            P.emit(); return nc

        NCG = 32
        def chain_bufs(p):
            d = {}
            d["zv"] = Ring(P, 1, [32, NCG, 128], BF16, f"zv{p}"); d["x1"] = Ring(P, 1, [32, NCG, 128], BF16, f"x1t{p}"); d["x2"] = Ring(P, 1, [32, NCG, 128], BF16, f"x2t{p}")
            d["k0"] = Ring(P, 1, [128, NCG, 2, 33], F32, f"kf0{p}"); d["k1"] = Ring(P, 1, [128, NCG, 2, 33], F32, f"kf1{p}")
            d["bt"] = Ring(P, 1, [128, 2, NCG, 33], BF16, f"btd{p}", sem=False); d["xp"] = Ring(P, 1, [128, NCG, 2, 33], BF16, f"xpt{p}", sem=False)
            d["zz"] = Ring(P, 1, [32, NCG, 128], BF16, f"zz{p}", sem=False); d["hy"] = Ring(P, 1, [32, NCG, 128], BF16, f"hyo{p}")
            return d
        CB = [chain_bufs(0), chain_bufs(1)]
        Rr = Ring(P, 3, [66, 2, 16, 128], BF16, "Rinv", sem=False)
        pwr = Ring(P, 6, [128, 15, 33], F32, "pwt", sem=False); sar = Ring(P, 6, [66, 2, 2, 128], F32, "sat", sem=False)
        tmr2 = Ring(P, 6, [128, 7, 2, 33], F32, "twtmp2", sem=False)

        def conv_g(cb, Zin, ZinB, kf_, kfB, gate, gateB, out_t, outB):
            bt, btB, _ = cb["bt"].next(); xp, xpB, _ = cb["xp"].next()
            yield from fft_fwd_g(Zin, ZinB, 32, NCG, bt, btB, tmr2)
            c0 = 0
            while c0 < NCG:
                n = min(15, NCG - c0)
                xr_, xrB, xi_, xiB = fft_stage2(bt, btB, c0, n)
                XR = xr_[:, 0:n * 33].rearrange("p (c f) -> p c f", f=33); XI = xi_[:, 0:n * 33].rearrange("p (c f) -> p c f", f=33)
                kr_ = kf_[:, c0:c0 + n, 0, :]; ki_ = kf_[:, c0:c0 + n, 1, :]
                ts = [pwr.next() for _ in range(4)]
                for (tb_, src, kk) in ((ts[0], XR, kr_), (ts[1], XI, ki_), (ts[2], XR, ki_), (ts[3], XI, kr_)):
                    P.op("dve", lambda e, tb_=tb_, src=src, kk=kk, n=n: e.tensor_tensor(out=tb_[0][:, 0:n, :], in0=src, in1=kk, op=ALU.mult),
                         reads=[xrB, xiB, kfB], writes=[tb_[1]])
                P.op("pool", lambda e, xp=xp, c0=c0, n=n, ts=ts: e.tensor_tensor(out=xp[:, c0:c0 + n, 0, :], in0=ts[0][0][:, 0:n, :], in1=ts[1][0][:, 0:n, :], op=ALU.subtract),
                     reads=[ts[0][1], ts[1][1]], writes=[xpB])
                P.op("pool", lambda e, xp=xp, c0=c0, n=n, ts=ts: e.tensor_tensor(out=xp[:, c0:c0 + n, 1, :], in0=ts[2][0][:, 0:n, :], in1=ts[3][0][:, 0:n, :], op=ALU.add),
                     reads=[ts[2][1], ts[3][1]], writes=[xpB])
                c0 += n
                yield
            for c16 in range(NCG // 16):
                R, RB, _ = Rr.next()
                for cp in range(8):
                    c = c16 * 16 + cp * 2
                    pa, paB = ps_next()
                    for u in range(2):
                        P.op("pe", lambda e, pa=pa, u=u, c=c: e.matmul(pa[0:66, u * 256:(u + 1) * 256], xp[:, c + u, :, :].rearrange("p r f -> p (r f)"),
                                                                     Ginv[:].rearrange("p r s -> p (r s)"), start=True, stop=True),
                             reads=[xpB, tabB], writes=[paB], sig=(u == 1))
                    Pv = pa[0:66, :].rearrange("p (g h s) -> p g h s", g=2, h=2)
                    q1 = sar.next(); q2 = sar.next()
                    P.op("dve", lambda e, Pv=Pv, q1=q1: e.tensor_tensor(out=q1[0][:], in0=Pv, in1=TW2[:, 0].unsqueeze(1).to_broadcast([66, 2, 2, 128]), op=ALU.mult),
                         reads=[paB, tabB], writes=[q1[1]])
                    P.op("dve", lambda e, Pv=Pv, q2=q2: e.tensor_tensor(out=q2[0][:], in0=Pv, in1=TW2[:, 1].unsqueeze(1).to_broadcast([66, 2, 2, 128]), op=ALU.mult),
                         reads=[paB, tabB], writes=[q2[1]])
                    P.op("pool", lambda e, R=R, q1=q1, cp=cp: e.tensor_tensor(out=R[:, 0, cp * 2:cp * 2 + 2, :], in0=q1[0][:, :, 0, :], in1=q1[0][:, :, 1, :], op=ALU.add),
                         reads=[q1[1]], writes=[RB])
                    P.op("pool", lambda e, R=R, q2=q2, cp=cp: e.tensor_tensor(out=R[:, 1, cp * 2:cp * 2 + 2, :], in0=q2[0][:, :, 0, :], in1=q2[0][:, :, 1, :], op=ALU.add),
                         reads=[q2[1]], writes=[RB])
                    yield
                for c4 in range(4):
                    c = c16 * 16 + c4 * 4
                    pa, paB = ps_next()
                    P.op("pe", lambda e, pa=pa, R=R, c4=c4: e.matmul(pa[0:32, :], LAB[:, 0, :], R[:, 0, c4 * 4:c4 * 4 + 4, :].rearrange("p c s -> p (c s)"), start=True, stop=False),
                         reads=[RB, tabB], writes=[paB], sig=False)
                    P.op("pe", lambda e, pa=pa, R=R, c4=c4: e.matmul(pa[0:32, :], LAB[:, 1, :], R[:, 1, c4 * 4:c4 * 4 + 4, :].rearrange("p c s -> p (c s)"), start=False, stop=True),
                         reads=[RB, tabB], writes=[paB])
                    P.op("dve", lambda e, pa=pa, c=c: e.tensor_tensor(out=out_t[:, c:c + 4, :], in0=pa[0:32, :].rearrange("p (c s) -> p c s", s=128),
                                                                      in1=gate[:, c:c + 4, :], op=ALU.mult),
                         reads=[paB, gateB], writes=[outB])
                    yield

        def chain_g(p, hc):
            cb = CB[p]; cb0 = hc * NCG
            zv, zvB, zvs = cb["zv"].next(); x1t, x1B, x1s = cb["x1"].next(); x2t, x2B, x2s = cb["x2"].next()
            k0, k0B, k0s = cb["k0"].next(); k1, k1B, k1s = cb["k1"].next()
            for (dst, dB, dsm, src) in ((zv, zvB, zvs, UU[0, cb0:cb0 + NCG, :]), (x1t, x1B, x1s, UU[1, cb0:cb0 + NCG, :]), (x2t, x2B, x2s, UU[2, cb0:cb0 + NCG, :])):
                P.dma("pool", dst[:], src.rearrange("c (a b) -> a c b", b=128), dsm, writes=[dB])
            P.dma("sp", k0[:], KF[0, :, cb0:cb0 + NCG, :, :], k0s, writes=[k0B])
            P.dma("sp", k1[:], KF[1, :, cb0:cb0 + NCG, :, :], k1s, writes=[k1B])
            zz, zzB, _ = cb["zz"].next(); hy, hyB, hys = cb["hy"].next()
            yield
            yield from conv_g(cb, zv, zvB, k0, k0B, x1t, x1B, zz, zzB)
            yield from conv_g(cb, zz, zzB, k1, k1B, x2t, x2B, hy, hyB)
            P.dma("act", HY[cb0:cb0 + NCG, :].rearrange("c (a b) -> a c b", b=128), hy[:], hys, reads=[hyB])

        for pr_ in range(D // NCG // 2):
            gens = [chain_g(0, 2 * pr_), chain_g(1, 2 * pr_ + 1)]
            alive = [True, True]
            while any(alive):
                for gi_ in range(2):
                    if alive[gi_]:
                        try:
                            next(gens[gi_])
                        except StopIteration:
                            alive[gi_] = False
        P.barrier(); P.reset(m_persist)
        if phases < 7:
            P.emit(); return nc

        posA = P.tile([128, 32, 64], F32, "posA"); gatA = P.tile([128, 32, 64], F32, "gatA"); pgB = Buf("posgate")
        slotI = P.tile([128, 256], I32, "slotI"); gkA = P.tile([128, 32, 8], F32, "gkA"); blkI = P.tile([128, NBLK], I32, "blkI"); metaB = Buf("meta")
        m_persist = P.mark()
        s_w = P.dsem("w5"); w5B = Buf("w5")
        wpa = P.tile([128, 8, D], BF16, "wpa"); wph = P.tile([128, 8, D], BF16, "wph"); wo = P.tile([128, 8, D], BF16, "wo")
        rwt = P.tile([128, 8, 64], F32, "rwt"); rbt = P.tile([128, 64], F32, "rbt")
        P.dma("pool", wpa[:], w_pa.rearrange("(k p) n -> p k n", p=128), s_w, writes=[w5B])
        P.dma("pool", wph[:], w_ph.rearrange("(k p) n -> p k n", p=128), s_w, writes=[w5B])
        P.dma("pool", wo[:], w_o.rearrange("(k p) n -> p k n", p=128), s_w, writes=[w5B])
        P.dma("sp", rwt[:], rw.rearrange("(k p) n -> p k n", p=128), s_w, writes=[w5B])
        P.dma("sp", rbt[:], rbias.partition_broadcast(128), s_w, writes=[w5B])
        selb = P.tile([128, 64], BF16, "selb"); ltri = P.tile([128, 128], BF16, "ltri"); onesb5 = P.tile([128, 128], BF16, "onesb5"); triB = Buf("tri")
        carry = P.tile([128, 64], F32, "carry"); carB = Buf("carry")
        P.op("pool", lambda e: e.memset(carry[:], 0.0), writes=[carB])
        P.op("pool", lambda e: e.memset(onesb5[:], 1.0), writes=[triB])
        P.op("pool", lambda e: e.memset(ltri[:], 1.0), writes=[triB])
        P.op("pool", lambda e: e.affine_select(out=ltri[:], in_=ltri[:], pattern=[[1, 128]], compare_op=ALU.is_gt, fill=0.0, base=0, channel_multiplier=-1),
             reads=[triB], writes=[triB])
        m5b = P.mark()
        htkr = Ring(P, 2, [128, D], BF16, "htk")
        axr = Ring(P, 1, [128, 8, 512], BF16, "axT"); hyr5 = Ring(P, 1, [128, 8, 512], BF16, "hyT"); gtr = Ring(P, 1, [128, 16, 512], BF16, "gt")
        xr5 = Ring(P, 1, [128, 8, 512], F32, "xt5")
        yT = P.tile([128, 8, 512], BF16, "yT"); yB = Buf("yT")
        x1r5 = Ring(P, 1, [128, 8, 512], F32, "x1T")
        sq5 = P.tile([128, 8, 512], F32, "sq5"); sq5B = Buf("sq5"); rstd5 = P.tile([128, 512], F32, "rstd5"); rs5B = Buf("rs5")
        hx2f = P.tile([128, 8, 512], F32, "hx2f"); hx2fB = Buf("hx2f")
        hx2b = Ring(P, 1, [128, 8, 512], BF16, "hx2b")
        y1r = Ring(P, 4, [128, 512], F32, "y1", sem=False)
        rt = [P.tile([128, 64], F32, f"rt{i}") for i in range(6)]; rtB = Buf("rt")
        rs_ = P.tile([128, 40], F32, "rsm")
        AX_v = AXs.rearrange("(k p) t -> p k t", p=128); HY_v = HY.rearrange("(k p) t -> p k t", p=128)
        GG_v = GG.rearrange("(k p) t -> p k t", p=128); X1_v = X1.rearrange("(k p) t -> p k t", p=128); HX2_v = HX2.rearrange("(k p) t -> p k t", p=128)
        for i in range(8):
            t0 = i * 512
            ax_, axB, axs = axr.next(); hy_, hyB5, hys5 = hyr5.next(); gt, gtB, gts = gtr.next(); xt5, xB5, xs5 = xr5.next()
            P.dma("sp", ax_[:], AX_v[:, :, t0:t0 + 512], axs, writes=[axB])
            P.dma("act", hy_[:], HY_v[:, :, t0:t0 + 512], hys5, writes=[hyB5])
            P.dma("sp", gt[:, 0:8, :], GG_v[:, 0:8, t0:t0 + 512], gts, writes=[gtB])
            P.dma("act", gt[:, 8:16, :], GG_v[:, 8:16, t0:t0 + 512], gts, writes=[gtB])
            P.dma("sp", xt5[:], xT_v[:, :, t0:t0 + 512], xs5, writes=[xB5])
            for dc in range(8):
                pa, paB = ps_next(); ph_, phB = ps_next()
                for k in range(8):
                    P.op("pe", lambda e, pa=pa, k=k, dc=dc, ax_=ax_: e.matmul(pa[:], wpa[:, k, dc * 128:(dc + 1) * 128], ax_[:, k, :], start=(k == 0), stop=(k == 7)),
                         reads=[w5B, axB], writes=[paB], sig=(k == 7))
                for k in range(8):
                    P.op("pe", lambda e, ph_=ph_, k=k, dc=dc, hy_=hy_: e.matmul(ph_[:], wph[:, k, dc * 128:(dc + 1) * 128], hy_[:, k, :], start=(k == 0), stop=(k == 7)),
                         reads=[w5B, hyB5], writes=[phB], sig=(k == 7))
                ya, yaB, _ = y1r.next(); yb_, ybB, _ = y1r.next()
                P.op("dve", lambda e, ya=ya, pa=pa, gt=gt, dc=dc: e.tensor_tensor(out=ya[:], in0=pa[:], in1=gt[:, dc, :], op=ALU.mult), reads=[paB, gtB], writes=[yaB])
                P.op("dve", lambda e, yb_=yb_, ph_=ph_, gt=gt, dc=dc: e.tensor_tensor(out=yb_[:], in0=ph_[:], in1=gt[:, 8 + dc, :], op=ALU.mult), reads=[phB, gtB], writes=[ybB])
                P.op("pool", lambda e, ya=ya, yb_=yb_, dc=dc: e.tensor_tensor(out=yT[:, dc, :], in0=ya[:], in1=yb_[:], op=ALU.add), reads=[yaB, ybB], writes=[yB])
            x1T, x1B5, x1s5 = x1r5.next()
            for dc in range(8):
                pm, pmB = ps_next()
                for k in range(8):
                    P.op("pe", lambda e, pm=pm, k=k, dc=dc: e.matmul(pm[:], wo[:, k, dc * 128:(dc + 1) * 128], yT[:, k, :], start=(k == 0), stop=(k == 7)),
                         reads=[w5B, yB], writes=[pmB], sig=(k == 7))
                P.op("dve", lambda e, pm=pm, dc=dc, x1T=x1T, xt5=xt5: e.scalar_tensor_tensor(out=x1T[:, dc, :], in0=pm[:], scalar=vec[:, 48 + dc:49 + dc], in1=xt5[:, dc, :],
                                                                                          op0=ALU.mult, op1=ALU.add), reads=[pmB, vecB, xB5], writes=[x1B5])
            P.dma("sp", X1_v[:, :, t0:t0 + 512], x1T[:], x1s5, reads=[x1B5])
            rms_rstd(x1T, x1B5, 512, sq=sq5, sqB=sq5B, rstd=rstd5, rsB=rs5B)
            P.op("dve", lambda e, x1T=x1T: e.tensor_tensor(out=sq5[:], in0=x1T[:], in1=rstd5[:].unsqueeze(1).to_broadcast([128, 8, 512]), op=ALU.mult),
                 reads=[x1B5, rs5B, sq5B], writes=[sq5B])
            hb2, hb2B, hb2s = hx2b.next()
            for k in range(8):
                P.op("act", lambda e, k=k: e.activation(out=hx2f[:, k, :], in_=sq5[:, k, :], func=AF.Identity, scale=vec[:, 32 + k:33 + k], bias=vec[:, 40 + k:41 + k]),
                     reads=[sq5B, vecB], writes=[hx2fB])
            P.op("pool", lambda e, hb2=hb2: e.tensor_copy(out=hb2[:], in_=hx2f[:]), reads=[hx2fB], writes=[hb2B])
            P.dma("act", HX2_v[:, :, t0:t0 + 512], hb2[:], hb2s, reads=[hb2B])
            for sb_ in range(4):
                st_ = i * 4 + sb_
                pr, prB = ps_next()
                for k in range(8):
                    P.op("pe", lambda e, pr=pr, k=k, sb_=sb_: e.matmul(pr[:, 0:64], hx2f[:, k, sb_ * 128:(sb_ + 1) * 128], rwt[:, k, :], start=(k == 0), stop=(k == 7)),
                         reads=[hx2fB, w5B], writes=[prB], sig=(k == 7))
                for hf in range(2):
                    ptk, ptkB = ps_next()
                    for kk in range(4):
                        k = hf * 4 + kk
                        P.op("pe", lambda e, ptk=ptk, k=k, kk=kk, sb_=sb_: e.transpose(out=ptk[:, kk * 128:(kk + 1) * 128], in_=hx2f[:, k, sb_ * 128:(sb_ + 1) * 128], identity=ident[:]),
                             reads=[hx2fB, identB], writes=[ptkB], sig=(kk == 3))
                    if hf == 0:
                        htk, htkB, htks = htkr.next()
                        P.op("act", lambda e, htk=htk, ptk=ptk: e.copy(out=htk[:, 0:512], in_=ptk[:]), reads=[ptkB], writes=[htkB])
                    else:
                        P.op("dve", lambda e, htk=htk, ptk=ptk: e.tensor_copy(out=htk[:, 512:1024], in_=ptk[:]), reads=[ptkB], writes=[htkB])
                P.dma("act", HX2tok[st_ * 128:(st_ + 1) * 128, :], htk[:], htks, reads=[htkB])
                sc_, ch_, c2_, msk, wse, gte = rt
                def R_(fn, eng="dve", rd=(), wr=()):
                    P.op(eng, fn, reads=[rtB] + list(rd), writes=[rtB] + list(wr))
                R_(lambda e, pr=pr: e.activation(out=sc_[:], in_=pr[:, 0:64], func=AF.Sigmoid), eng="act", rd=[prB])
                R_(lambda e: e.tensor_tensor(out=ch_[:], in0=sc_[:], in1=rbt[:], op=ALU.add), rd=[w5B])
                R_(lambda e: e.tensor_reduce(out=rs_[:, 0:8], in_=ch_[:].rearrange("p (g j) -> p g j", j=8), axis=AXL.X, op=ALU.max))
                R_(lambda e: e.tensor_tensor(out=c2_[:].rearrange("p (g j) -> p g j", j=8), in0=ch_[:].rearrange("p (g j) -> p g j", j=8),
                                             in1=rs_[:, 0:8].unsqueeze(2).to_broadcast([128, 8, 8]), op=ALU.is_equal))
                R_(lambda e: e.scalar_tensor_tensor(out=c2_[:], in0=c2_[:], scalar=-1.0e9, in1=ch_[:], op0=ALU.mult, op1=ALU.add))
                R_(lambda e: e.tensor_reduce(out=rs_[:, 8:16], in_=c2_[:].rearrange("p (g j) -> p g j", j=8), axis=AXL.X, op=ALU.max))
                R_(lambda e: e.tensor_tensor(out=rs_[:, 0:8], in0=rs_[:, 0:8], in1=rs_[:, 8:16], op=ALU.add))
                R_(lambda e: e.max(out=rs_[:, 16:24], in_=rs_[:, 0:8]))
                R_(lambda e: e.tensor_scalar(out=rs_[:, 8:16], in0=rs_[:, 0:8], scalar1=rs_[:, 19:20], scalar2=None, op0=ALU.is_ge))
                R_(lambda e: e.tensor_scalar(out=rs_[:, 8:16], in0=rs_[:, 8:16], scalar1=-1.0, scalar2=1.0e9, op0=ALU.add, op1=ALU.mult))
                R_(lambda e: e.tensor_tensor(out=msk[:].rearrange("p (g j) -> p g j", j=8), in0=ch_[:].rearrange("p (g j) -> p g j", j=8),
                                             in1=rs_[:, 8:16].unsqueeze(2).to_broadcast([128, 8, 8]), op=ALU.add))
                R_(lambda e: e.max(out=rs_[:, 24:32], in_=msk[:]))
                R_(lambda e: e.tensor_scalar(out=wse[:], in0=msk[:], scalar1=rs_[:, 31:32], scalar2=None, op0=ALU.is_ge))
                R_(lambda e: e.tensor_copy(out=selb[:], in_=wse[:]))
                R_(lambda e: e.tensor_tensor(out=wse[:], in0=wse[:], in1=sc_[:], op=ALU.mult))
                R_(lambda e: e.tensor_reduce(out=rs_[:, 32:33], in_=wse[:], axis=AXL.X, op=ALU.add))
                R_(lambda e: e.reciprocal(out=rs_[:, 33:34], in_=rs_[:, 32:33]))
                R_(lambda e, st_=st_: e.tensor_scalar(out=gatA[:, st_, :], in0=wse[:], scalar1=rs_[:, 33:34], scalar2=2.5, op0=ALU.mult, op1=ALU.mult), wr=[pgB])
                pp, ppB = ps_next()
                P.op("pe", lambda e, pp=pp: e.matmul(pp[:, 0:64], ltri[:], selb[:], start=True, stop=True), reads=[rtB, triB], writes=[ppB])
                P.op("pe", lambda e, pp=pp: e.matmul(pp[:, 64:128], onesb5[:], selb[:], start=True, stop=True), reads=[rtB, triB], writes=[ppB])
                P.op("dve", lambda e, pp=pp, st_=st_: e.tensor_tensor(out=posA[:, st_, :], in0=pp[:, 0:64], in1=carry[:], op=ALU.add), reads=[ppB, carB, pgB], writes=[pgB])
                P.op("dve", lambda e, pp=pp: e.tensor_tensor(out=carry[:], in0=pp[:, 64:128], in1=carry[:], op=ALU.add), reads=[ppB, carB], writes=[carB])
        P.barrier(); P.reset(m5b)
        W2K = 32 * 64
        slotf = P.tile([128, 32, 64], F32, "slotf"); keyt = P.tile([128, 32, 64], F32, "keyt"); eqt = P.tile([128, 32, 64], F32, "eqt"); m2B = Buf("meta2")
        rowA = P.tile([128, 64], F32, "rowA"); rowB = P.tile([128, 64], F32, "rowB"); rowI = P.tile([128, 64], I32, "rowI"); padd = P.tile([128, 64], F32, "padd")
        k8 = P.tile([128, 32, 8], F32, "k8"); skf = P.tile([128, 32, 8], F32, "skf")
        bvt = P.tile([128, NBLK], F32, "bvt"); blkf = P.tile([128, NBLK], F32, "blkf"); chg = P.tile([128, NBLK], F32, "chg"); pct = P.tile([128, 1], F32, "pct")
        cmpt = P.tile([128, 80, 64], F32, "cmpt")
        s_m = P.dsem("meta")
        P.dma("sp", bvt[:], bvals.partition_broadcast(128), s_m, writes=[m2B]); P.dma("sp", pct[:], pcol[:, :], s_m, writes=[m2B])
        def M_(fn, eng="dve", rd=(), wr=()):
            P.op(eng, fn, reads=[m2B] + list(rd), writes=[m2B] + list(wr))
        M_(lambda e: e.tensor_scalar(out=rowA[:], in0=carry[:], scalar1=1.0 / 128, scalar2=127.0 / 128 - 0.5 + 1.0 / 256, op0=ALU.mult, op1=ALU.add), rd=[carB])
        M_(lambda e: e.tensor_copy(out=rowI[:], in_=rowA[:]))
        M_(lambda e: e.tensor_copy(out=rowA[:], in_=rowI[:]))
        M_(lambda e: e.tensor_scalar(out=padd[:], in0=rowA[:], scalar1=128.0, scalar2=None, op0=ALU.mult))
        M_(lambda e: e.tensor_copy(out=rowA[:], in_=padd[:]))
        cur, oth = rowA, rowB
        for sh in (1, 2, 4, 8, 16, 32):
            M_(lambda e, cur=cur, oth=oth, sh=sh: e.tensor_copy(out=oth[:, 0:sh], in_=cur[:, 0:sh]))
            M_(lambda e, cur=cur, oth=oth, sh=sh: e.tensor_tensor(out=oth[:, sh:64], in0=cur[:, sh:64], in1=cur[:, 0:64 - sh], op=ALU.add))
            cur, oth = oth, cur
        pend = cur; pstart = oth
        M_(lambda e: e.tensor_tensor(out=pstart[:], in0=pend[:], in1=padd[:], op=ALU.subtract))
        M_(lambda e: e.tensor_tensor(out=slotf[:], in0=posA[:], in1=pstart[:].unsqueeze(1).to_broadcast([128, 32, 64]), op=ALU.add), rd=[pgB])
        M_(lambda e: e.tensor_scalar(out=keyt[:], in0=slotf[:], scalar1=-1.0, scalar2=BIGK + 1.0, op0=ALU.mult, op1=ALU.add))
        M_(lambda e: e.tensor_single_scalar(out=eqt[:], in_=gatA[:], scalar=0.0, op=ALU.is_gt), rd=[pgB])
        M_(lambda e: e.tensor_tensor(out=keyt[:], in0=keyt[:], in1=eqt[:], op=ALU.mult))
        M_(lambda e: e.tensor_scalar(out=keyt[:], in0=keyt[:], scalar1=-1.0, scalar2=None, op0=ALU.add))
        for st_ in range(32):
            M_(lambda e, st_=st_: e.max(out=k8[:, st_, :], in_=keyt[:, st_, :]))
        M_(lambda e: e.tensor_scalar(out=skf[:], in0=k8[:], scalar1=-1.0, scalar2=BIGK, op0=ALU.mult, op1=ALU.add))
        M_(lambda e: e.tensor_copy(out=slotI[:], in_=skf[:].rearrange("p a b -> p (a b)")), wr=[metaB])
        for k in range(8):
            M_(lambda e, k=k: e.tensor_tensor(out=eqt[:], in0=slotf[:], in1=skf[:, :, k:k + 1].to_broadcast([128, 32, 64]), op=ALU.is_equal))
            M_(lambda e: e.tensor_tensor(out=eqt[:], in0=eqt[:], in1=gatA[:], op=ALU.mult), rd=[pgB])
            M_(lambda e, k=k: e.tensor_reduce(out=gkA[:, :, k], in_=eqt[:], axis=AXL.X, op=ALU.add), wr=[metaB])
        for cq in range(NBLK // 80):
            M_(lambda e, cq=cq: e.tensor_tensor(out=cmpt[:], in0=pend[:].unsqueeze(1).to_broadcast([128, 80, 64]),
                                                in1=bvt[:, cq * 80:(cq + 1) * 80].unsqueeze(2).to_broadcast([128, 80, 64]), op=ALU.is_le))
            M_(lambda e, cq=cq: e.tensor_reduce(out=blkf[:, cq * 80:(cq + 1) * 80], in_=cmpt[:], axis=AXL.X, op=ALU.add))
        M_(lambda e: e.tensor_scalar(out=blkf[:], in0=blkf[:], scalar1=63.0, scalar2=128.0, op0=ALU.min, op1=ALU.mult))
        M_(lambda e: e.memset(chg[:, 0:NWB], 1.0))
        M_(lambda e: e.tensor_tensor(out=chg[:, NWB:NBLK], in0=blkf[:, NWB:NBLK], in1=blkf[:, 0:NBLK - NWB], op=ALU.not_equal))
        M_(lambda e: e.tensor_scalar(out=blkf[:], in0=blkf[:], scalar1=pct[:, 0:1], scalar2=-BIGI, op0=ALU.add, op1=ALU.add))
        M_(lambda e: e.tensor_tensor(out=blkf[:], in0=blkf[:], in1=chg[:], op=ALU.mult))
        M_(lambda e: e.tensor_scalar(out=blkf[:], in0=blkf[:], scalar1=BIGI, scalar2=None, op0=ALU.add))
        M_(lambda e: e.tensor_copy(out=blkI[:], in_=blkf[:]), wr=[metaB])
        dtr = Ring(P, 3, [128, D], BF16, "dtok")
        dsc = [P.dsem("scat") for _ in range(3)]
        for st_ in range(32):
            dt_, dtB, dts = dtr.next()
            P.dma("sp", dt_[:], HX2tok[st_ * 128:(st_ + 1) * 128, :], dts, writes=[dtB])
            for k in range(8):
                waits = P._deps("pool", [dtB, metaB] + zwin, [])
                ssem = dsc[dtr.i]
                ssem.n += 16
                P.q["pool"].append((waits, (lambda e, dt_=dt_, st_=st_, k=k: e.indirect_dma_start(
                    out=XG[:, :], out_offset=bass.IndirectOffsetOnAxis(ap=slotI[:, st_ * 8 + k:st_ * 8 + k + 1], axis=0), in_=dt_[:, :], in_offset=None,
                    bounds_check=P.reg(e, NSLOT - 1), oob_is_err=False)), (ssem.h, 16)))
                dtB.r.append((ssem, ssem.n))
        xgBs = [Buf(f"xg{i}") for i in range(3)]
        for i_, b_ in enumerate(xgBs):
            b_.w = (dsc[i_], dsc[i_].n)
        if debug:
            s_dbg = P.dsem("dbg")
            P.dma("sp", dbg["slot"].rearrange("p a b -> p (a b)"), slotI[:], s_dbg, reads=[metaB]); P.dma("sp", dbg["gk"][:, :, :], gkA[:], s_dbg, reads=[metaB])
            P.dma("sp", dbg["blk"][:, :], blkI[:], s_dbg, reads=[metaB])
        P.barrier(); P.reset(m_persist)
        if phases < 8:
            P.emit(); return nc

        wbuf = [(P.tile([128, 6144], BF16, f"wall{i}"), Buf(f"bw{i}"), P.dsem("bw")) for i in range(NWB)]
        for (a_, wB_, _) in wbuf:
            P.op("pool", lambda e, a_=a_: e.memset(a_[:], 0.0), writes=[wB_])
        xbr = Ring(P, 4, [128, D], BF16, "xb"); xgtr = Ring(P, 2, [128, 8, 128], BF16, "xgT", sem=False)
        sgr6 = Ring(P, 2, [128, 256], F32, "sg6", sem=False); atkr = Ring(P, 2, [128, 256], BF16, "atk", sem=False)
        atr = Ring(P, 2, [128, 2, 128], BF16, "aT", sem=False); ybr = Ring(P, 3, [128, D], F32, "yb")
        ygB = Buf("YG")
        identb = P.tile([128, 128], BF16, "identb"); identbB = Buf("identb")
        P.op("dve", lambda e: e.tensor_copy(out=identb[:], in_=ident[:]), reads=[identB], writes=[identbB])
        def block_g(b):
            wall, wB_, wsem = wbuf[b % NWB]
            wg_ = wall[:, 0:2048].rearrange("p (k h) -> p k h", k=8); wu_ = wall[:, 2048:4096].rearrange("p (k h) -> p k h", k=8)
            wd_ = wall[:, 4096:6144].rearrange("p (k d) -> p k d", k=2)
            waits = P._deps("pool", [metaB, wbB], [wB_])
            wsem.n += 16
            P.q["pool"].append((waits, (lambda e, wall=wall, b=b: e.indirect_dma_start(
                out=wall[:, :], out_offset=None, in_=WB[:, :], in_offset=bass.IndirectOffsetOnAxis(ap=blkI[:, b:b + 1], axis=0),
                bounds_check=P.reg(e, 65 * 128 - 1), oob_is_err=False)), (wsem.h, 16)))
            wB_.w = (wsem, wsem.n); wB_.r = []
            xb, xbB, xbs = xbr.next()
            P.dma("sp", xb[:], XG[b * 128:(b + 1) * 128, :], xbs, reads=xgBs, writes=[xbB])
            ptb, ptbB = ps_next()
            ptv = ptb[:].bitcast(BF16)
            for k in range(8):
                P.op("pe", lambda e, ptv=ptv, xb=xb, k=k: e.transpose(out=ptv[:, k * 128:(k + 1) * 128], in_=xb[:, k * 128:(k + 1) * 128], identity=identb[:]),
                     reads=[xbB, identbB], writes=[ptbB], sig=(k == 7))
            xgT, xgTB, _ = xgtr.next()
            P.op("dve", lambda e, xgT=xgT, ptv=ptv: e.tensor_copy(out=xgT[:].rearrange("p k s -> p (k s)"), in_=ptv), reads=[ptbB], writes=[xgTB])
            yield
            pg, pgB_ = ps_next(); pu, puB = ps_next()
            for k in range(8):
                P.op("pe", lambda e, pg=pg, wg_=wg_, k=k, xgT=xgT: e.matmul(pg[:, 0:256], xgT[:, k, :], wg_[:, k, :], start=(k == 0), stop=(k == 7)),
                     reads=[wB_, xgTB], writes=[pgB_], sig=(k == 7))
                P.op("pe", lambda e, pu=pu, wu_=wu_, k=k, xgT=xgT: e.matmul(pu[:, 0:256], xgT[:, k, :], wu_[:, k, :], start=(k == 0), stop=(k == 7)),
                     reads=[wB_, xgTB], writes=[puB], sig=(k == 7))
            sg, sgB, _ = sgr6.next(); atk, atkB, _ = atkr.next(); aT, aTB, _ = atr.next()
            P.op("act", lambda e, sg=sg, pg=pg: e.activation(out=sg[:], in_=pg[:, 0:256], func=AF.Silu), reads=[pgB_], writes=[sgB])
            P.op("dve", lambda e, atk=atk, sg=sg, pu=pu: e.tensor_tensor(out=atk[:], in0=pu[:, 0:256], in1=sg[:], op=ALU.mult), reads=[puB, sgB], writes=[atkB])
            yield
            pat, patB = ps_next()
            patv = pat[:].bitcast(BF16)
            for hh in range(2):
                P.op("pe", lambda e, patv=patv, atk=atk, hh=hh: e.transpose(out=patv[:, hh * 128:(hh + 1) * 128], in_=atk[:, hh * 128:(hh + 1) * 128], identity=identb[:]),
                     reads=[atkB, identbB], writes=[patB], sig=(hh == 1))
            P.op("act", lambda e, aT=aT, patv=patv: e.copy(out=aT[:].rearrange("p h s -> p (h s)"), in_=patv[:, 0:256]), reads=[patB], writes=[aTB])
            yield
            yb, ybB, ybs = ybr.next()
            for hf in range(2):
                py, pyB = ps_next()
                for hh in range(2):
                    P.op("pe", lambda e, py=py, aT=aT, wd_=wd_, hh=hh, hf=hf: e.matmul(py[:], aT[:, hh, :], wd_[:, hh, hf * 512:(hf + 1) * 512], start=(hh == 0), stop=(hh == 1)),
                         reads=[aTB, wB_], writes=[pyB], sig=(hh == 1))
                if hf == 0:
                    P.op("act", lambda e, yb=yb, py=py: e.copy(out=yb[:, 0:512], in_=py[:]), reads=[pyB], writes=[ybB])
                else:
                    P.op("dve", lambda e, yb=yb, py=py: e.tensor_copy(out=yb[:, 512:1024], in_=py[:]), reads=[pyB], writes=[ybB])
            P.dma("act", YG[b * 128:(b + 1) * 128, :], yb[:], ybs, reads=[ybB])

        live = []
        for b in range(NBLK + 4):
            if b < NBLK:
                live.append(block_g(b))
            nxt = []
            for g_ in live:
                try:
                    next(g_); nxt.append(g_)
                except StopIteration:
                    pass
            live = nxt
        P.barrier(); P.reset(m_persist)
        if debug:
            pass
        if phases < 9:
            P.emit(); return nc

        swg = P.tile([128, 8, 256], BF16, "swg"); swu = P.tile([128, 8, 256], BF16, "swu"); swd = P.tile([128, 2, D], BF16, "swd"); swB = Buf("sw"); s_sw = P.dsem("sw")
        P.dma("sp", swg[:].rearrange("p a b -> p (a b)"), WB[64 * 128:65 * 128, 0:2048], s_sw, reads=[wbB], writes=[swB])
        P.dma("sp", swu[:].rearrange("p a b -> p (a b)"), WB[64 * 128:65 * 128, 2048:4096], s_sw, reads=[wbB], writes=[swB])
        P.dma("sp", swd[:].rearrange("p a b -> p (a b)"), WB[64 * 128:65 * 128, 4096:6144], s_sw, reads=[wbB], writes=[swB])
        hxr = Ring(P, 2, [128, 8, 512], BF16, "hx6"); sg6 = Ring(P, 2, [128, 512], F32, "sgs", sem=False); aa6 = Ring(P, 2, [128, 512], BF16, "aas", sem=False)
        accS = P.tile([128, 8, 512], F32, "accS"); accSB = Buf("accS")
        ykr = Ring(P, 4, [128, D], F32, "yk"); atok = P.tile([128, D], F32, "atok"); atokB = Buf("atok")
        for t_ in ykr.t:
            P.op("pool", lambda e, t_=t_: e.memset(t_[:], 0.0), writes=[ykr.b[ykr.t.index(t_)]])
        x1l = Ring(P, 1, [128, 8, 512], F32, "x1l"); sq6 = P.tile([128, 8, 512], F32, "sq6"); sq6B = Buf("sq6")
        rstd6 = P.tile([128, 512], F32, "rstd6"); rs6B = Buf("rs6"); otr = Ring(P, 2, [128, 8, 512], F32, "outt")
        outT_v = outT.rearrange("(k p) t -> p k t", p=128)
        for i in range(8):
            t0 = i * 512
            hx_, hxB_, hxs_ = hxr.next()
            P.dma("sp", hx_[:], HX2_v[:, :, t0:t0 + 512], hxs_, writes=[hxB_])
            aas = []
            for hh in range(2):
                pg, pgB_ = ps_next(); pu, puB = ps_next()
                for k in range(8):
                    P.op("pe", lambda e, pg=pg, k=k, hh=hh, hx_=hx_: e.matmul(pg[:], swg[:, k, hh * 128:(hh + 1) * 128], hx_[:, k, :], start=(k == 0), stop=(k == 7)),
                         reads=[swB, hxB_], writes=[pgB_], sig=(k == 7))
                for k in range(8):
                    P.op("pe", lambda e, pu=pu, k=k, hh=hh, hx_=hx_: e.matmul(pu[:], swu[:, k, hh * 128:(hh + 1) * 128], hx_[:, k, :], start=(k == 0), stop=(k == 7)),
                         reads=[swB, hxB_], writes=[puB], sig=(k == 7))
                sg, sgB, _ = sg6.next(); aa, aaB, _ = aa6.next()
                P.op("act", lambda e, sg=sg, pg=pg: e.activation(out=sg[:], in_=pg[:], func=AF.Silu), reads=[pgB_], writes=[sgB])
                P.op("dve", lambda e, aa=aa, pu=pu, sg=sg: e.tensor_tensor(out=aa[:], in0=pu[:], in1=sg[:], op=ALU.mult), reads=[puB, sgB], writes=[aaB])
                aas.append((aa, aaB))
            for dc in range(8):
                pd, pdB = ps_next()
                for hh in range(2):
                    P.op("pe", lambda e, pd=pd, hh=hh, dc=dc, aa=aas[hh][0]: e.matmul(pd[:], swd[:, hh, dc * 128:(dc + 1) * 128], aa[:], start=(hh == 0), stop=(hh == 1)),
                         reads=[swB, aas[hh][1]], writes=[pdB], sig=(hh == 1))
                P.op("act", lambda e, pd=pd, dc=dc: e.copy(out=accS[:, dc, :], in_=pd[:]), reads=[pdB], writes=[accSB])
            for sb_ in range(4):
                st_ = i * 4 + sb_
                for k in range(8):
                    yk, ykB, yks = ykr.next()
                    waits = P._deps("pool", [metaB, ygB], [ykB])
                    yks.n += 16
                    P.q["pool"].append((waits, (lambda e, yk=yk, st_=st_, k=k: e.indirect_dma_start(
                        out=yk[:, :], out_offset=None, in_=YG[:, :], in_offset=bass.IndirectOffsetOnAxis(ap=slotI[:, st_ * 8 + k:st_ * 8 + k + 1], axis=0),
                        bounds_check=P.reg(e, NSLOT - 1), oob_is_err=False)), (yks.h, 16)))
                    ykB.w = (yks, yks.n); ykB.r = []
                    if k == 0:
                        P.op("dve", lambda e, yk=yk, st_=st_: e.tensor_scalar(out=atok[:], in0=yk[:], scalar1=gkA[:, st_, 0:1], scalar2=None, op0=ALU.mult),
                             reads=[ykB, metaB, atokB], writes=[atokB])
                    else:
                        P.op("dve", lambda e, yk=yk, st_=st_, k=k: e.scalar_tensor_tensor(out=atok[:], in0=yk[:], scalar=gkA[:, st_, k:k + 1], in1=atok[:], op0=ALU.mult, op1=ALU.add),
                             reads=[ykB, metaB, atokB], writes=[atokB])
                for kg in range(2):
                    ptk, ptkB = ps_next()
                    for kk in range(4):
                        k = kg * 4 + kk
                        P.op("pe", lambda e, ptk=ptk, k=k, kk=kk: e.transpose(out=ptk[:, kk * 128:(kk + 1) * 128], in_=atok[:, k * 128:(k + 1) * 128], identity=ident[:]),
                             reads=[atokB, identB], writes=[ptkB], sig=(kk == 3))
                    P.op("dve", lambda e, ptk=ptk, kg=kg, sb_=sb_: e.tensor_tensor(out=accS[:, kg * 4:(kg + 1) * 4, sb_ * 128:(sb_ + 1) * 128],
                                                                                in0=ptk[:].rearrange("p (k t) -> p k t", k=4),
                                                                                in1=accS[:, kg * 4:(kg + 1) * 4, sb_ * 128:(sb_ + 1) * 128], op=ALU.add),
                         reads=[ptkB, accSB], writes=[accSB])
            x1t_, x1B_, x1s_ = x1l.next()
            P.dma("sp", x1t_[:], X1_v[:, :, t0:t0 + 512], x1s_, writes=[x1B_])
            for k in range(8):
                P.op("dve", lambda e, k=k, x1t_=x1t_: e.scalar_tensor_tensor(out=x1t_[:, k, :], in0=accS[:, k, :], scalar=vec[:, 56 + k:57 + k], in1=x1t_[:, k, :],
                                                                           op0=ALU.mult, op1=ALU.add), reads=[accSB, vecB, x1B_], writes=[x1B_])
            rms_rstd(x1t_, x1B_, 512, sq=sq6, sqB=sq6B, rstd=rstd6, rsB=rs6B)
            P.op("dve", lambda e, x1t_=x1t_: e.tensor_tensor(out=sq6[:], in0=x1t_[:], in1=rstd6[:].unsqueeze(1).to_broadcast([128, 8, 512]), op=ALU.mult),
                 reads=[x1B_, rs6B, sq6B], writes=[sq6B])
            ot, otB, ots = otr.next()
            for k in range(8):
                P.op("act", lambda e, k=k, ot=ot: e.activation(out=ot[:, k, :], in_=sq6[:, k, :], func=AF.Copy, scale=vec[:, 68 + k:69 + k]),
                     reads=[sq6B, vecB], writes=[otB])
            P.dma("act", outT_v[:, :, t0:t0 + 512], ot[:], ots, reads=[otB])
        P.emit()
        nc._n_dsems = len(P.dsems)
    return nc


def prep_inputs(inputs):
    f = lambda a: np.ascontiguousarray(np.asarray(a, dtype=np.float32))
    x = f(inputs["x"]); ctx = f(inputs["ctx"]); c = f(inputs["c"]); c_ctx = f(inputs["c_ctx"])
    C, S = rope_tables()
    shared = {
        "ada_w": f(inputs["ada_w"][0]),
        "ada_bT": f(inputs["ada_b"][0].reshape(48, 128).T),
        "gvec": f(np.concatenate([np.asarray(inputs[k]).reshape(8, 128).T for k in ("norm1_g", "norm2_g", "final_norm_g")], axis=1)),
        "lamv": f(np.concatenate([np.asarray(inputs[k]).reshape(-1) for k in ("lam_q1", "lam_k1", "lam_q2", "lam_k2")])[None, :]),
        "w_in": f(inputs["w_in"][0]),
        "ropeC": C, "ropeS": S,
        "convw": f(np.asarray(inputs["hy_conv_w"][0]).reshape(3, 24, 128).transpose(2, 0, 1)),
        "convb": f(np.asarray(inputs["hy_conv_b"][0]).reshape(24, 128).T),
        "subln": f(np.asarray(inputs["subln_g"][0]).reshape(1, 128)),
        "sublnT": f(np.asarray(inputs["subln_g"][0]).reshape(128, 1)),
        "fw1": f(inputs["filt_w1"][0]), "fw2": f(inputs["filt_w2"][0]), "fw3": f(inputs["filt_w3"][0]),
        "fvec": f(np.stack([np.asarray(inputs[k][0]) for k in ("filt_b1", "filt_b2", "filt_freq")], axis=1)),
        "hyb": f(np.asarray(inputs["hy_bias"][0]).reshape(1, 2048)),
        "w_pa": f(inputs["w_branch_attn"][0]), "w_ph": f(inputs["w_branch_hyena"][0]), "w_o": f(inputs["w_out"][0]),
        "rw": f(inputs["router_w"][0]), "rbias": f(np.asarray(inputs["router_bias"][0]).reshape(1, 64)),
        "ewg": f(np.concatenate([inputs["exp_w_gate"][0], inputs["shared_w_gate"]], axis=0).reshape(65, 8, 128, 256).transpose(0, 2, 1, 3).reshape(65 * 128, 2048)),
        "ewu": f(np.concatenate([inputs["exp_w_up"][0], inputs["shared_w_up"]], axis=0).reshape(65, 8, 128, 256).transpose(0, 2, 1, 3).reshape(65 * 128, 2048)),
        "ewd": f(np.concatenate([inputs["exp_w_down"][0], inputs["shared_w_down"]], axis=0).reshape(65, 2, 128, 1024).transpose(0, 2, 1, 3).reshape(65 * 128, 2048)),
        "bvals": f((np.arange(NBLK) * 128.0).reshape(1, NBLK)), "pcol": f(np.arange(128.0).reshape(128, 1)),
    }
    shared.update(hyena_tables())
    maps = []
    for b in range(8):
        m = dict(shared)
        m["xT"] = np.ascontiguousarray(np.concatenate([x[b].T, ctx[b].T], axis=1))
        m["cc"] = np.ascontiguousarray(np.stack([c[b].reshape(8, 128).T, c_ctx.reshape(8, 128).T], axis=-1))
        maps.append(m)
    return maps


def kernel(**inputs):
    nc = build(DEBUG, PHASES)
    maps = prep_inputs(inputs)
    res = run_bass_kernel_spmd(nc, maps, core_ids=list(range(8)))
    out = np.stack([np.asarray(r["outT"]).T for r in res.results], axis=0)
    return np.ascontiguousarray(out.astype(np.float32))
```

```python
import math
from contextlib import ExitStack
import numpy as np
import concourse.bass as bass
import concourse.mybir as mybir
from concourse.bass_utils import run_bass_kernel_spmd

F32 = mybir.dt.float32; BF16 = mybir.dt.bfloat16; I32 = mybir.dt.int32
AF = mybir.ActivationFunctionType
ALU = mybir.AluOpType
AXL = mybir.AxisListType
DTSZ = {F32: 4, BF16: 2, I32: 4}

D = 1024; SEQ = 4096; CTX = 256; TOK = SEQ + CTX; NH = 8; INW = 8192
NBLK = 320; NSLOT = NBLK * 128; BIGK = 65536.0; BIGI = 1.0e6; NWB = 4
DEBUG = False
PHASES = 99


class Buf:
    __slots__ = ("name", "w", "r")

    def __init__(self, name=""):
        self.name = name; self.w = None; self.r = []


class Sem:
    def __init__(self, h):
        self.h = h; self.n = 0


class Prog:
    ENG = ("pe", "act", "dve", "pool", "sp")

    def __init__(self, nc, es):
        self.nc = nc; self.es = es
        self.q = {e: [] for e in self.ENG}
        self.esem = {e: Sem(es.enter_context(nc.semaphore("prog_" + e))) for e in self.ENG}
        self.known = {e: {} for e in self.ENG}
        self.dsems = []
        self.off = 16640; self.ntile = 0
        self.cap = nc.SBUF_PARTITION_SIZE_BYTES
        self.ninst = 0

    def tile(self, shape, dtype, name=None):
        nb = int(np.prod(shape[1:])) * DTSZ[dtype]
        nb = (nb + 63) // 64 * 64
        assert self.off + nb <= self.cap, f"SBUF overflow {self.off}+{nb} > {self.cap} ({name})"
        self.ntile += 1
        t = self.nc.alloc_sbuf_tensor_at(f"{name or 't'}_{self.ntile}", list(shape), dtype, offset=self.off)
        self.off += nb
        return t

    def mark(self):
        return self.off

    def reset(self, m):
        self.off = m

    def dsem(self, name="d"):
        s = Sem(self.es.enter_context(self.nc.semaphore(f"{name}_{len(self.dsems)}")))
        self.dsems.append(s); return s

    def _deps(self, eng, reads, writes):
        deps = []
        for b in reads:
            if b.w is not None: deps.append(b.w)
        for b in writes:
            if b.w is not None: deps.append(b.w)
            deps.extend(b.r)
        best = {}
        for (s, v) in deps:
            k = id(s)
            if k not in best or best[k][1] < v: best[k] = (s, v)
        waits = []
        kn = self.known[eng]
        for (s, v) in best.values():
            if eng == "pe" and s is self.esem["pe"]: continue
            if kn.get(id(s), 0) >= v: continue
            kn[id(s)] = v
            waits.append((s, v))
        return waits

    def op(self, eng, fn, reads=(), writes=(), sig=True):
        waits = self._deps(eng, reads, writes)
        S = self.esem[eng]
        if sig: S.n += 1
        tok = (S, S.n if sig else S.n + 1)
        self.q[eng].append((waits, fn, (S.h, 1) if sig else None))
        for b in reads: b.r.append(tok)
        for b in writes:
            b.w = tok; b.r = []
        self.ninst += 1
        return tok

    def dma(self, eng, out, in_, sem, reads=(), writes=(), **kw):
        waits = self._deps(eng, reads, writes)
        sem.n += 16
        tok = (sem, sem.n)
        self.q[eng].append((waits, lambda e: e.dma_start(out=out, in_=in_, **kw), (sem.h, 16)))
        for b in reads: b.r.append(tok)
        for b in writes:
            b.w = tok; b.r = []
        self.ninst += 1
        return tok

    def reg(self, e, v):
        if not hasattr(self, "_regs"): self._regs = {}
        if v not in self._regs: self._regs[v] = e.to_reg(v)
        return self._regs[v]

    def barrier(self):
        allsem = [self.esem[e] for e in self.ENG] + self.dsems
        for e in self.ENG:
            waits = []
            kn = self.known[e]
            for s in allsem:
                if s.n > 0 and kn.get(id(s), 0) < s.n and not (s is self.esem[e]):
                    kn[id(s)] = s.n; waits.append((s, s.n))
            if waits: self.q[e].append((waits, None, None))

    def emit(self):
        nc = self.nc
        self.barrier()
        with nc.Block() as block:
            def run(engname):
                def f(e):
                    for (waits, fn, inc) in self.q[engname]:
                        for (s, v) in waits: e.wait_ge(s.h, v)
                        if fn is None: continue
                        ins = fn(e)
                        if inc is not None: ins.then_inc(inc[0], inc[1])
                return f
            block.tensor(run("pe")); block.scalar(run("act")); block.vector(run("dve"))
            block.gpsimd(run("pool")); block.sync(run("sp"))


class Ring:
    def __init__(self, P, n, shape, dtype, name, sem=True):
        self.t = [P.tile(shape, dtype, f"{name}{i}") for i in range(n)]
        self.b = [Buf(f"{name}{i}") for i in range(n)]
        self.s = [P.dsem(name) for i in range(n)] if sem else [None] * n
        self.i = -1; self.n = n

    def next(self):
        self.i = (self.i + 1) % self.n
        return self.t[self.i], self.b[self.i], self.s[self.i]


def rope_tables():
    p = np.arange(128); d = p % 64
    axis = d // 32; half = (d % 32) // 16; fr = d % 16
    inv = (10000.0 ** (-(np.arange(0, 32, 2, dtype=np.float32)) / 32.0)).astype(np.float32)
    t = np.arange(SEQ)
    row = (t // 64).astype(np.float32); col = (t % 64).astype(np.float32)
    pos = np.where(axis[:, None] == 0, row[None, :], col[None, :]).astype(np.float32)
    ang = (pos * inv[fr][:, None]).astype(np.float32)
    C = np.cos(ang).astype(np.float32)
    S = np.sin(ang).astype(np.float32) * np.where(half == 0, -1.0, 1.0)[:, None].astype(np.float32)
    return np.ascontiguousarray(C), np.ascontiguousarray(S.astype(np.float32))


def hyena_tables():
    n = SEQ; N = 2 * SEQ
    tp = np.arange(N)
    pos = np.where(tp < n, tp, N - tp).astype(np.float32)
    pos[n] = 0.0
    tt = (pos / np.float32(n - 1)).astype(np.float32)
    w = (np.float32(2 * math.pi) * pos / np.float32(n)).astype(np.float32)
    bands = np.linspace(1e-4, 15.0, 16, dtype=np.float32)
    bw = (bands[None, :] * w[:, None]).astype(np.float32)
    z = np.concatenate([tt[:, None], np.cos(bw), -np.sin(bw)], axis=1).astype(np.float32)
    ttx = tt.copy(); ttx[n] = 1.0e4
    deltas = np.abs(np.linspace(math.log(1e-2) / 1.5, math.log(1e-2) / 0.3, D, dtype=np.float32)).astype(np.float32)
    T = {}
    T["zextT"] = np.ascontiguousarray(z.T)
    T["ttx"] = np.ascontiguousarray(ttx[None, :])
    T["negdelta"] = np.ascontiguousarray((-deltas).reshape(8, 128).T)
    s1 = np.arange(64)[:, None].astype(np.float64); f1 = np.arange(33)[None, :].astype(np.float64)
    a = 2 * np.pi * s1 * f1 / 64
    T["E1"] = np.concatenate([np.cos(a), -np.sin(a)], axis=1).astype(np.float32)
    s2 = np.arange(128)[:, None].astype(np.float64)
    a = 2 * np.pi * s2 * f1 / N
    Tr, Ti = np.cos(a), -np.sin(a)
    T["TW1"] = np.stack([np.stack([Tr, Ti], 1), np.stack([Ti, Tr], 1)], 1).astype(np.float32)
    f2 = np.arange(128)[None, :].astype(np.float64)
    a = 2 * np.pi * s2 * f2 / 128
    T["E2"] = np.stack([np.cos(a), -np.sin(a), np.sin(a)], 1).astype(np.float32)
    T["Ginv"] = np.stack([np.cos(a.T), np.sin(a.T)], 1).astype(np.float32)
    f1c = np.arange(33)[:, None].astype(np.float64); s2r = np.arange(128)[None, :].astype(np.float64)
    a = 2 * np.pi * f1c * s2r / N
    Tc, Ts = np.cos(a), np.sin(a)
    W1 = np.concatenate([np.stack([Tc, -Ts], 1), np.stack([-Ts, -Tc], 1)], 0)
    W2 = np.concatenate([np.stack([Ts, Tc], 1), np.stack([Tc, -Ts], 1)], 0)
    T["TW2"] = np.stack([W1, W2], 1).astype(np.float32)
    wt = np.full(33, 2.0); wt[0] = 1.0; wt[32] = 1.0
    s1r = np.arange(32)[None, :].astype(np.float64)
    a = 2 * np.pi * f1c * s1r / 64
    la = wt[:, None] * np.cos(a) / N; lb = -wt[:, None] * np.sin(a) / N
    T["LAB"] = np.stack([np.concatenate([la, la], 0), np.concatenate([lb, lb], 0)], 1).astype(np.float32)
    return {k: np.ascontiguousarray(v) for k, v in T.items()}


def build(debug=False, phases=99):
    nc = bass.Bass("TRN2", target_bir_lowering=False)
    skind = "ExternalOutput" if debug else "Internal"

    def din(name, shape, dt=F32):
        return nc.dram_tensor(name, list(shape), dt, kind="ExternalInput").ap()

    def dscr(name, shape, dt):
        return nc.dram_tensor(name, list(shape), dt, kind=skind).ap()

    xT = din("xT", [D, TOK]); cc = din("cc", [128, 8, 2]); ada_w = din("ada_w", [D, 6 * D])
    ada_bT = din("ada_bT", [128, 48]); gvec = din("gvec", [128, 24]); lamv = din("lamv", [1, 256])
    w_in = din("w_in", [D, INW]); ropeC = din("ropeC", [128, SEQ]); ropeS = din("ropeS", [128, SEQ])
    convw = din("convw", [128, 3, 24]); convb = din("convb", [128, 24]); subln = din("subln", [1, 128]); sublnT = din("sublnT", [128, 1])
    zextT = din("zextT", [33, 2 * SEQ]); ttx = din("ttx", [1, 2 * SEQ]); negdelta = din("negdelta", [128, 8])
    E1d = din("E1", [64, 66]); TW1d = din("TW1", [128, 2, 2, 33]); E2d = din("E2", [128, 3, 128]); Ginvd = din("Ginv", [128, 2, 128])
    TW2d = din("TW2", [66, 2, 2, 128]); LABd = din("LAB", [66, 2, 32])
    w_pa = din("w_pa", [D, D]); w_ph = din("w_ph", [D, D]); w_o = din("w_o", [D, D]); rw = din("rw", [D, 64]); rbias = din("rbias", [1, 64])
    ewg = din("ewg", [65 * 128, 2048]); ewu = din("ewu", [65 * 128, 2048]); ewd = din("ewd", [65 * 128, 2048])
    bvals = din("bvals", [1, NBLK]); pcol = din("pcol", [128, 1])
    fw1 = din("fw1", [33, 64]); fw2 = din("fw2", [64, 64]); fw3 = din("fw3", [64, 4096]); fvec = din("fvec", [64, 3]); hyb = din("hyb", [1, 2048])
    outT = nc.dram_tensor("outT", [D, SEQ], F32, kind="ExternalOutput").ap()

    QT = dscr("QT", [NH, 128, SEQ], BF16); KT = dscr("KT", [NH, 128, TOK], BF16)
    AXs = dscr("AXs", [D, SEQ], BF16); KERN = dscr("KERN", [2, D, 2 * SEQ], BF16)
    KF = dscr("KF", [2, 128, D, 2, 33], F32); HY = dscr("HY", [D, SEQ], BF16)
    X1 = dscr("X1", [D, SEQ], F32); HX2 = dscr("HX2", [D, SEQ], BF16); HX2tok = dscr("HX2tok", [SEQ, D], BF16)
    XG = dscr("XG", [NSLOT, D], BF16); YG = dscr("YG", [NSLOT, D], F32); WB = dscr("WB", [65 * 128, 6144], BF16)
    VV = dscr("VV", [NH, 128, 34, 128], BF16); UU = dscr("UU", [3, D, SEQ], F32); GG = dscr("GG", [2 * D, SEQ], BF16)
    dbg = {}
    if debug:
        dbg["hx"] = nc.dram_tensor("dbg_hx", [128, 8, TOK], BF16, kind="ExternalOutput").ap()
        dbg["vec"] = nc.dram_tensor("dbg_vec", [128, 80], F32, kind="ExternalOutput").ap()
        dbg["slot"] = nc.dram_tensor("dbg_slot", [128, 32, 8], I32, kind="ExternalOutput").ap()
        dbg["gk"] = nc.dram_tensor("dbg_gk", [128, 32, 8], F32, kind="ExternalOutput").ap()
        dbg["blk"] = nc.dram_tensor("dbg_blk", [128, NBLK], I32, kind="ExternalOutput").ap()
        dbg["acc"] = nc.dram_tensor("dbg_acc", [2, 128, 8, 2048], F32, kind="ExternalOutput").ap()

    with ExitStack() as es:
        P = Prog(nc, es)
        psb = [nc.alloc_psum_tensor(f"psb{i}", [128, 512], F32) for i in range(8)]
        psB = [Buf(f"ps{i}") for i in range(8)]
        pi = [0]

        def ps_next():
            pi[0] = (pi[0] + 1) % 8
            return psb[pi[0]], psB[pi[0]]

        vec = P.tile([128, 80], F32, "vec"); vecB = Buf("vec")
        ones = P.tile([128, 128], F32, "ones"); onesB = Buf("ones")
        ident = P.tile([128, 128], F32, "ident"); identB = Buf("ident")
        P.op("pool", lambda e: e.memset(ones[:], 1.0), writes=[onesB])
        P.op("pool", lambda e: e.memset(ident[:], 0.0), writes=[identB])
        P.op("pool", lambda e: e.affine_select(out=ident[:], in_=ident[:], pattern=[[-1, 128]], compare_op=ALU.not_equal,
                                               fill=1.0, base=0, channel_multiplier=1), reads=[identB], writes=[identB])
        g08 = P.tile([128, 128], F32, "g08"); g08B = Buf("g08")
        m_persist = P.mark()
        hxT = P.tile([128, 8, TOK], BF16, "hxT"); hxB = [Buf(f"hx{i}") for i in range(9)]
        m_hx = P.mark()

        cct = P.tile([128, 8, 2], F32, "cct"); sil = P.tile([128, 8, 2], F32, "sil"); cB = Buf(); silB = Buf()
        adab = P.tile([128, 48], F32, "adab"); gv = P.tile([128, 24], F32, "gv"); lamt = P.tile([128, 256], F32, "lamt")
        modT = P.tile([128, 48, 2], F32, "modT"); modB = Buf()
        smB = Buf(); s_small = P.dsem("small"); s_cc = P.dsem("cc")
        P.dma("sp", cct[:], cc[:, :, :], s_cc, writes=[cB])
        P.dma("sp", adab[:], ada_bT[:, :], s_small, writes=[smB])
        P.dma("sp", gv[:], gvec[:, :], s_small, writes=[smB])
        P.dma("sp", lamt[:], lamv.partition_broadcast(128), s_small, writes=[smB])
        P.op("act", lambda e: e.activation(out=sil[:], in_=cct[:], func=AF.Sigmoid), reads=[cB], writes=[silB])
        P.op("dve", lambda e: e.tensor_tensor(out=sil[:], in0=sil[:], in1=cct[:], op=ALU.mult), reads=[cB, silB], writes=[silB])
        aw = Ring(P, 2, [128, 8, 1024], F32, "adaw")
        psm, psmB = ps_next()
        adaw_v = ada_w.rearrange("(k p) f -> p k f", p=128)
        for g in range(6):
            wt, wB, ws = aw.next()
            P.dma("sp" if g % 2 == 0 else "act", wt[:], adaw_v[:, :, g * 1024:(g + 1) * 1024], ws, writes=[wB])
            for fc in range(8):
                f = g * 8 + fc
                for k in range(8):
                    P.op("pe", lambda e, wt=wt, k=k, fc=fc, f=f: e.matmul(psm[:, 2 * f:2 * f + 2], wt[:, k, fc * 128:(fc + 1) * 128],
                                                                         sil[:, k, :], start=(k == 0), stop=(k == 7)),
                         reads=[wB, silB], writes=[psmB], sig=(k == 7))
        P.op("dve", lambda e: e.tensor_tensor(out=modT[:], in0=psm[:, 0:96].rearrange("p (f j) -> p f j", j=2),
                                              in1=adab[:].unsqueeze(2).to_broadcast([128, 48, 2]), op=ALU.add),
             reads=[psmB, smB], writes=[modB])
        def vop(fn, rd=(modB, smB)):
            P.op("dve", fn, reads=list(rd) + [vecB], writes=[vecB])
        vop(lambda e: e.scalar_tensor_tensor(out=vec[:, 0:8], in0=modT[:, 8:16, 0], scalar=1.0, in1=gv[:, 0:8], op0=ALU.add, op1=ALU.mult))
        vop(lambda e: e.tensor_copy(out=vec[:, 8:16], in_=modT[:, 0:8, 0]))
        vop(lambda e: e.scalar_tensor_tensor(out=vec[:, 16:24], in0=modT[:, 8:16, 1], scalar=1.0, in1=gv[:, 0:8], op0=ALU.add, op1=ALU.mult))
        vop(lambda e: e.tensor_copy(out=vec[:, 24:32], in_=modT[:, 0:8, 1]))
        vop(lambda e: e.scalar_tensor_tensor(out=vec[:, 32:40], in0=modT[:, 32:40, 0], scalar=1.0, in1=gv[:, 8:16], op0=ALU.add, op1=ALU.mult))
        vop(lambda e: e.tensor_copy(out=vec[:, 40:48], in_=modT[:, 24:32, 0]))
        vop(lambda e: e.tensor_copy(out=vec[:, 48:56], in_=modT[:, 16:24, 0]))
        vop(lambda e: e.tensor_copy(out=vec[:, 56:64], in_=modT[:, 40:48, 0]))
        vop(lambda e: e.tensor_copy(out=vec[:, 68:76], in_=gv[:, 16:24]))
        lt = P.tile([128, 128], F32, "lt"); ltB = Buf()
        P.op("dve", lambda e: e.tensor_tensor(out=lt[:].rearrange("p (a b) -> p a b", a=2),
                                              in0=lamt[:].rearrange("p (a c b) -> p a c b", a=2, c=2)[:, :, 0, :],
                                              in1=lamt[:].rearrange("p (a c b) -> p a c b", a=2, c=2)[:, :, 1, :], op=ALU.mult),
             reads=[smB], writes=[ltB])
        P.op("dve", lambda e: e.tensor_reduce(out=vec[:, 66:68], in_=lt[:].rearrange("p (a b) -> p a b", a=2), axis=AXL.X, op=ALU.add),
             reads=[ltB, vecB], writes=[vecB])
        P.op("act", lambda e: e.activation(out=vec[:, 66:68], in_=vec[:, 66:68], func=AF.Exp), reads=[vecB], writes=[vecB])
        P.op("dve", lambda e: e.scalar_tensor_tensor(out=vec[:, 64:65], in0=vec[:, 67:68], scalar=-0.2, in1=vec[:, 66:67],
                                                     op0=ALU.add, op1=ALU.subtract), reads=[vecB], writes=[vecB])
        if debug:
            P.dma("sp", dbg["vec"][:, :], vec[:], s_small, reads=[vecB])

        xr = Ring(P, 2, [128, 8, 512], F32, "xt"); sq = P.tile([128, 8, 512], F32, "sq"); sqB = Buf()
        rstd = P.tile([128, 512], F32, "rstd"); rsB = Buf()
        xT_v = xT.rearrange("(k p) t -> p k t", p=128)

        def rms_rstd(src_t, src_b, n, sq=sq, sqB=sqB, rstd=rstd, rsB=rsB, eps=1e-6, dim=1024.0):
            P.op("act", lambda e: e.activation(out=sq[:, :, 0:n], in_=src_t[:, :, 0:n], func=AF.Square), reads=[src_b], writes=[sqB])
            pt, pB = ps_next()
            for k in range(8):
                P.op("pe", lambda e, k=k: e.matmul(pt[:, 0:n], ones[:], sq[:, k, 0:n], start=(k == 0), stop=(k == 7)),
                     reads=[sqB, onesB], writes=[pB], sig=(k == 7))
            P.op("dve", lambda e: e.tensor_scalar(out=rstd[:, 0:n], in0=pt[:, 0:n], scalar1=1.0 / dim, scalar2=eps, op0=ALU.mult, op1=ALU.add),
                 reads=[pB], writes=[rsB])
            P.op("act", lambda e: e.activation(out=rstd[:, 0:n], in_=rstd[:, 0:n], func=AF.Sqrt), reads=[rsB], writes=[rsB])
            P.op("dve", lambda e: e.reciprocal(out=rstd[:, 0:n], in_=rstd[:, 0:n]), reads=[rsB], writes=[rsB])

        for i in range(9):
            t0 = i * 512; n = 512 if i < 8 else 256
            xt, xB, xs = xr.next()
            P.dma("sp", xt[:, :, 0:n], xT_v[:, :, t0:t0 + n], xs, writes=[xB])
            rms_rstd(xt, xB, n)
            P.op("dve", lambda e, xt=xt, n=n: e.tensor_tensor(out=sq[:, :, 0:n], in0=xt[:, :, 0:n],
                                                              in1=rstd[:, 0:n].unsqueeze(1).to_broadcast([128, 8, n]), op=ALU.mult),
                 reads=[xB, rsB, sqB], writes=[sqB])
            ao = 0 if i < 8 else 16
            for k in range(8):
                P.op("act", lambda e, k=k, t0=t0, n=n, ao=ao: e.activation(out=hxT[:, k, t0:t0 + n], in_=sq[:, k, 0:n], func=AF.Identity,
                                                                             scale=vec[:, ao + k:ao + k + 1], bias=vec[:, ao + 8 + k:ao + 9 + k]),
                     reads=[sqB, vecB], writes=[hxB[i]])
        if debug:
            P.dma("sp", dbg["hx"][:, :, :], hxT[:], s_small, reads=hxB)
        P.barrier()
        P.reset(m_hx)
        if phases < 2:
            P.emit(); return nc

        m2 = P.mark()
        wr = Ring(P, 2, [128, 8, 1024], BF16, "wg"); wperm = P.tile([128, 8, 1024], BF16, "wperm"); wpB = Buf()
        cw = P.tile([128, 3, 24], F32, "cw"); cb = P.tile([128, 24], F32, "cb"); cwB = Buf()
        s_cw = P.dsem("cw"); s_rope = P.dsem("rope")
        P.dma("act", cw[:], convw[:, :, :], s_cw, writes=[cwB])
        P.dma("act", cb[:], convb[:, :], s_cw, writes=[cwB])
        ob = Ring(P, 4, [128, 512], BF16, "ob")
        vst = Ring(P, 2, [128, 1024], BF16, "vst")
        m2b = P.mark()
        rC = P.tile([128, SEQ], F32, "rC"); rS = P.tile([128, SEQ], F32, "rS"); ropB = Buf()
        P.dma("act", rC[:], ropeC[:, :], s_rope, writes=[ropB])
        P.dma("act", rS[:], ropeS[:, :], s_rope, writes=[ropB])
        t1r = Ring(P, 4, [128, 512], F32, "t1", sem=False)
        win_v = w_in.rearrange("(k p) n -> p k n", p=128)
        alt = [0]
        g_order = (0, 2, 1, 3, 4, 5, 6, 7); g_loaded = {}
        def issue_w(g_):
            wt_, wB_, ws_ = wr.next()
            P.dma("pool", wt_[:], win_v[:, :, g_ * 1024:(g_ + 1) * 1024], ws_, writes=[wB_])
            g_loaded[g_] = (wt_, wB_)
        issue_w(g_order[0])
        for gidx, g in enumerate(g_order):
            if g == 1:
                P.barrier(); P.reset(m2b)
                pbuf = P.tile([128, SEQ + 2], F32, "pbuf"); pbB = Buf()
                ur = Ring(P, 2, [128, SEQ], F32, "ubuf")
                P.op("pool", lambda e: e.memset(pbuf[:, 0:1], 0.0), writes=[pbB])
                P.op("pool", lambda e: e.memset(pbuf[:, SEQ + 1:SEQ + 2], 0.0), writes=[pbB])
            if gidx + 1 < len(g_order):
                issue_w(g_order[gidx + 1])
            wt, wB = g_loaded[g]
            if g in (0, 2):
                wv = wt[:].rearrange("p k (b h j) -> p k b h j", h=2, j=16)
                pv = wperm[:].rearrange("p k (b h j) -> p k b h j", h=2, j=16)
                for k in range(8):
                    P.op("pool", lambda e, wv=wv, k=k: e.tensor_copy(out=pv[:, k, :, 0, :], in_=wv[:, k, :, 1, :]), reads=[wB], writes=[wpB])
                    P.op("pool", lambda e, wv=wv, k=k: e.tensor_copy(out=pv[:, k, :, 1, :], in_=wv[:, k, :, 0, :]), reads=[wB], writes=[wpB])
                dst = KT if g == 0 else QT
                for h in range(8):
                    for i in range(9 if g == 0 else 8):
                        t0 = i * 512; n = 512 if i < 8 else 256
                        pa, paB = ps_next()
                        for k in range(8):
                            P.op("pe", lambda e, pa=pa, wt=wt, k=k, h=h, t0=t0, n=n: e.matmul(pa[:, 0:n], wt[:, k, h * 128:(h + 1) * 128],
                                                                                               hxT[:, k, t0:t0 + n], start=(k == 0), stop=(k == 7)),
                                 reads=[wB, hxB[i]], writes=[paB], sig=(k == 7))
                        o, oB, osem = ob.next()
                        if i < 8:
                            pb_, pbB_ = ps_next()
                            for k in range(8):
                                P.op("pe", lambda e, pb_=pb_, k=k, h=h, t0=t0, n=n: e.matmul(pb_[:, 0:n], wperm[:, k, h * 128:(h + 1) * 128],
                                                                                              hxT[:, k, t0:t0 + n], start=(k == 0), stop=(k == 7)),
                                     reads=[wpB, hxB[i]], writes=[pbB_], sig=(k == 7))
                            ta, taB, _ = t1r.next(); tb, tbB, _ = t1r.next()
                            P.op("dve", lambda e, ta=ta, pa=pa, t0=t0: e.tensor_tensor(out=ta[:], in0=pa[:], in1=rC[:, t0:t0 + 512], op=ALU.mult),
                                 reads=[paB, ropB], writes=[taB])
                            P.op("dve", lambda e, tb=tb, pb_=pb_, t0=t0: e.tensor_tensor(out=tb[:], in0=pb_[:], in1=rS[:, t0:t0 + 512], op=ALU.mult),
                                 reads=[pbB_, ropB], writes=[tbB])
                            P.op("pool", lambda e, o=o, ta=ta, tb=tb: e.tensor_tensor(out=o[:], in0=ta[:], in1=tb[:], op=ALU.add),
                                 reads=[taB, tbB], writes=[oB])
                        else:
                            P.op("act", lambda e, o=o, pa=pa, n=n: e.copy(out=o[:, 0:n], in_=pa[:, 0:n]), reads=[paB], writes=[oB])
                        P.dma("sp", dst[h, :, t0:t0 + n], o[:, 0:n], osem, reads=[oB])
            elif g == 1:
                for j in range(34):
                    vt, vB, vs = vst.next()
                    for hf in range(2):
                        pa, paB = ps_next()
                        for k in range(8):
                            P.op("pe", lambda e, pa=pa, wt=wt, k=k, j=j, hf=hf: e.matmul(pa[:], hxT[:, k, j * 128:(j + 1) * 128],
                                                                                         wt[:, k, hf * 512:(hf + 1) * 512], start=(k == 0), stop=(k == 7)),
                                 reads=[wB, hxB[j // 4]], writes=[paB], sig=(k == 7))
                        if hf == 0:
                            P.op("act", lambda e, vt=vt, pa=pa: e.copy(out=vt[:, 0:512], in_=pa[:]), reads=[paB], writes=[vB])
                        else:
                            P.op("dve", lambda e, vt=vt, pa=pa: e.tensor_copy(out=vt[:, 512:1024], in_=pa[:]), reads=[paB], writes=[vB])
                    P.dma("sp", VV[:, :, j, :].rearrange("h p e -> p h e"), vt[:].rearrange("p (h e) -> p h e", h=NH), vs, reads=[vB])
            elif g in (3, 4, 5):
                for c in range(8):
                    ch = (g - 3) * 8 + c
                    for i in range(8):
                        t0 = i * 512
                        pa, paB = ps_next()
                        for k in range(8):
                            P.op("pe", lambda e, pa=pa, wt=wt, k=k, c=c, t0=t0: e.matmul(pa[:], wt[:, k, c * 128:(c + 1) * 128],
                                                                                         hxT[:, k, t0:t0 + 512], start=(k == 0), stop=(k == 7)),
                                 reads=[wB, hxB[i]], writes=[paB], sig=(k == 7))
                        P.op("act", lambda e, pa=pa, t0=t0: e.copy(out=pbuf[:, 1 + t0:1 + t0 + 512], in_=pa[:]), reads=[paB], writes=[pbB])
                    u, uB, us = ur.next()
                    eng = "dve"
                    P.op(eng, lambda e, u=u, ch=ch: e.tensor_scalar(out=u[:], in0=pbuf[:, 0:SEQ], scalar1=cw[:, 0, ch:ch + 1], scalar2=cb[:, ch:ch + 1],
                                                                    op0=ALU.mult, op1=ALU.add), reads=[pbB, cwB], writes=[uB])
                    P.op(eng, lambda e, u=u, ch=ch: e.scalar_tensor_tensor(out=u[:], in0=pbuf[:, 1:SEQ + 1], scalar=cw[:, 1, ch:ch + 1], in1=u[:],
                                                                           op0=ALU.mult, op1=ALU.add), reads=[pbB, cwB, uB], writes=[uB])
                    P.op(eng, lambda e, u=u, ch=ch: e.scalar_tensor_tensor(out=u[:], in0=pbuf[:, 2:SEQ + 2], scalar=cw[:, 2, ch:ch + 1], in1=u[:],
                                                                           op0=ALU.mult, op1=ALU.add), reads=[pbB, cwB, uB], writes=[uB])
                    P.dma("sp", UU[g - 3, c * 128:(c + 1) * 128, :], u[:], us, reads=[uB])
            else:
                for c in range(8):
                    for i in range(8):
                        t0 = i * 512
                        pa, paB = ps_next()
                        for k in range(8):
                            P.op("pe", lambda e, pa=pa, wt=wt, k=k, c=c, t0=t0: e.matmul(pa[:], wt[:, k, c * 128:(c + 1) * 128],
                                                                                         hxT[:, k, t0:t0 + 512], start=(k == 0), stop=(k == 7)),
                                 reads=[wB, hxB[i]], writes=[paB], sig=(k == 7))
                        o, oB, osem = ob.next()
                        P.op("act", lambda e, o=o, pa=pa: e.activation(out=o[:], in_=pa[:], func=AF.Sigmoid), reads=[paB], writes=[oB])
                        P.dma("sp", GG[(g - 6) * 1024 + c * 128:(g - 6) * 1024 + (c + 1) * 128, t0:t0 + 512], o[:], osem, reads=[oB])
        P.barrier()
        P.reset(m_persist)
        if phases < 3:
            P.emit(); return nc

        s_g = P.dsem("g08")
        g08c = P.tile([128, 1], F32, "g08c")
        P.dma("sp", g08c[:], sublnT[:, :], s_g, writes=[g08B])
        P.op("dve", lambda e: e.tensor_scalar(out=g08c[:], in0=g08c[:], scalar1=0.8, scalar2=None, op0=ALU.mult), reads=[g08B], writes=[g08B])
        qzr = [Ring(P, 2, [128, SEQ], BF16, f"qz{m}") for m in range(2)]
        kr = Ring(P, 2, [128, TOK], BF16, "kT"); vr = Ring(P, 2, [128, 34, 128], BF16, "vh")
        for m in range(2):
            for bi_ in range(2):
                t_ = qzr[m].t[bi_]
                P.op("pool", lambda e, t_=t_, m=m: e.memset(t_[64 * (1 - m):64 * (1 - m) + 64, :], 0.0), writes=[qzr[m].b[bi_]])
        onesb = P.tile([128, 128], BF16, "onesb"); onesbB = Buf("onesb")
        P.op("pool", lambda e: e.memset(onesb[:], 1.0), writes=[onesbB])
        ptr = Ring(P, 3, [128, 34, 512], BF16, "pT", sem=False)
        zfl = P.tile([128, 2048], BF16, "zfl"); ztB = Buf("zfl"); xgB = Buf("XG"); s_z = P.dsem("zfill")
        P.op("pool", lambda e, zfl=zfl: e.memset(zfl[:], 0.0), writes=[ztB])
        XG_z = XG.rearrange("(p a) d -> p (a d)", p=128)
        zwin = [Buf(f"zw{i}") for i in range(4)]; zsem = [s_z] + [P.dsem("zfill") for _ in range(3)]
        for zi in range(NBLK * D // 2048):
            P.dma("pool", XG_z[:, zi * 2048:(zi + 1) * 2048], zfl[:], zsem[zi % 4], reads=[ztB], writes=[zwin[zi % 4]])
        wstg = Ring(P, 1, [128, 6144], BF16, "wstg"); s_wb = P.dsem("wbst"); wbB = Buf("WB")
        def precast(e_):
            st_t, st_B, st_s = wstg.next()
            P.dma("pool", st_t[:, 0:2048], ewg[e_ * 128:(e_ + 1) * 128, :], st_s, writes=[st_B])
            P.dma("pool", st_t[:, 2048:4096], ewu[e_ * 128:(e_ + 1) * 128, :], st_s, reads=[], writes=[])
            P.dma("pool", st_t[:, 4096:6144], ewd[e_ * 128:(e_ + 1) * 128, :], st_s, reads=[], writes=[])
            st_B.w = (st_s, st_s.n)
            P.dma("pool", WB[e_ * 128:(e_ + 1) * 128, :], st_t[:], s_wb, reads=[st_B], writes=([wbB] if e_ == 64 else []))
        tq = P.tile([128, 512], F32, "tq"); tqB = Buf("tq")
        csr = Ring(P, 2, [128, 2, 512], F32, "csum", sem=False)
        rr = Ring(P, 2, [128, 512], F32, "rrec", sem=False)
        oo = P.tile([128, 512], F32, "oo"); ooB = Buf("oo"); o2 = P.tile([128, 512], F32, "o2"); o2B = Buf("o2")
        axo = Ring(P, 2, [128, 512], BF16, "axo")
        SB = [1, 2, 3]; TB = 0; NDV = 24
        OB = {0: (4, 5), 1: (6, 7)}
        sidx = [0]

        def load_head(h):
            k, kB, ks_ = kr.next(); v, vB, vs_ = vr.next()
            qs = []
            for m in range(2):
                q, qB, qs_ = qzr[m].next()
                P.dma("sp" if m == 0 else "act", q[64 * m:64 * m + 64, :], QT[h, 64 * m:64 * m + 64, :], qs_, writes=[qB])
                qs.append((q, qB))
            P.dma("sp", k[:], KT[h, :, :], ks_, writes=[kB])
            P.dma("act", v[:], VV[h, :, :, :], vs_, writes=[vB])
            return (qs, k, kB, v, vB)

        def qk_exp_steps(hd, i, m, pt, ptB):
            qs, k, kB, v, vB = hd
            q, qB = qs[m]
            steps = []
            for j in range(34):
                def st(j=j):
                    b = SB[sidx[0] % 3]; sidx[0] += 1
                    P.op("pe", lambda e: e.matmul(psb[b][:], k[:, j * 128:(j + 1) * 128], q[:, i * 512:(i + 1) * 512], start=True, stop=True),
                         reads=[kB, qB], writes=[psB[b]])
                    P.op("act", lambda e: e.activation(out=pt[:, j, :], in_=psb[b][:], func=AF.Exp, scale=0.125),
                         reads=[psB[b]], writes=[ptB])
                steps.append(st)
            return steps

        def av_steps(hd, m, pt, ptB):
            qs, k, kB, v, vB = hd
            ob_, sb_ = OB[m]
            cs, csB, _ = csr.next()
            steps = []
            for j in range(34):
                def st(j=j):
                    P.op("pe", lambda e: e.matmul(psb[ob_][:], v[:, j, :], pt[:, j, :], start=(j == 0), stop=(j == 33)),
                         reads=[ptB, vB], writes=[psB[ob_]], sig=(j == 33))
                    if j == 1:
                        P.op("dve", lambda e: e.tensor_copy(out=cs[:], in_=pt[:, 0:2, :]), reads=[ptB], writes=[csB])
                    elif j % 2 == 1 and j < NDV:
                        P.op("dve", lambda e: e.tensor_tensor(out=cs[:], in0=pt[:, j - 1:j + 1, :], in1=cs[:], op=ALU.add), reads=[ptB, csB], writes=[csB])
                    if j == NDV - 1:
                        P.op("dve", lambda e: e.tensor_tensor(out=cs[:, 0, :], in0=cs[:, 0, :], in1=cs[:, 1, :], op=ALU.add), reads=[csB], writes=[csB])
                    if j >= NDV:
                        P.op("pe", lambda e: e.matmul(psb[sb_][:], onesb[:], pt[:, j, :], start=(j == NDV), stop=False),
                             reads=[ptB, onesbB], writes=[psB[sb_]], sig=False)
                    if j == 33:
                        P.op("pe", lambda e: e.matmul(psb[sb_][:], ones[:], cs[:, 0, :], start=False, stop=True), reads=[csB, onesB], writes=[psB[sb_]])
                steps.append(st)
            return steps

        def combine_a():
            ob_, sb_ = OB[0]
            r, rB, _ = rr.next()
            P.op("dve", lambda e: e.reciprocal(out=r[:], in_=psb[sb_][:]), reads=[psB[sb_]], writes=[rB])
            P.op("dve", lambda e: e.tensor_tensor(out=tq[:], in0=psb[ob_][:], in1=r[:], op=ALU.mult), reads=[psB[ob_], rB, tqB], writes=[tqB])

        def combine_b(h, i):
            ob_, sb_ = OB[1]
            r, rB, _ = rr.next()
            P.op("dve", lambda e: e.reciprocal(out=r[:], in_=psb[sb_][:]), reads=[psB[sb_]], writes=[rB])
            P.op("dve", lambda e: e.tensor_tensor(out=r[:], in0=psb[ob_][:], in1=r[:], op=ALU.mult), reads=[psB[ob_], rB], writes=[rB])
            P.op("dve", lambda e: e.scalar_tensor_tensor(out=oo[:], in0=r[:], scalar=vec[:, 64:65], in1=tq[:], op0=ALU.mult, op1=ALU.add),
                 reads=[rB, vecB, tqB, ooB], writes=[ooB])
            P.op("dve", lambda e: e.tensor_tensor(out=o2[:], in0=oo[:], in1=oo[:], op=ALU.mult), reads=[ooB, o2B], writes=[o2B])
            P.op("pe", lambda e: e.matmul(psb[TB][:], ones[:], o2[:], start=True, stop=True), reads=[o2B, onesB], writes=[psB[TB]])
            P.op("dve", lambda e: e.tensor_scalar(out=o2[:], in0=psb[TB][:], scalar1=1.0 / 128, scalar2=1e-6, op0=ALU.mult, op1=ALU.add),
                 reads=[psB[TB], o2B], writes=[o2B])
            P.op("act", lambda e: e.activation(out=o2[:], in_=o2[:], func=AF.Ln), reads=[o2B], writes=[o2B])
            P.op("act", lambda e: e.activation(out=o2[:], in_=o2[:], func=AF.Exp, scale=-0.5), reads=[o2B], writes=[o2B])
            ao, aoB, aos = axo.next()
            P.op("dve", lambda e: e.scalar_tensor_tensor(out=ao[:], in0=oo[:], scalar=g08c[:, 0:1], in1=o2[:], op0=ALU.mult, op1=ALU.mult),
                 reads=[ooB, o2B, g08B], writes=[aoB])
            P.dma("sp", AXs[h * 128:(h + 1) * 128, i * 512:(i + 1) * 512], ao[:], aos, reads=[aoB])

        stages = [(h, i, m) for h in range(NH) for i in range(8) for m in range(2)]
        heads = {0: load_head(0)}
        prev = None
        for si, (h, i, m) in enumerate(stages):
            if i == 1 and m == 0 and h + 1 < NH:
                heads[h + 1] = load_head(h + 1)
            if si % 2 == 0:
                precast(si // 2)
            pt, ptB, _ = ptr.next()
            qk = qk_exp_steps(heads[h], i, m, pt, ptB)
            av_prev, post_prev = prev if prev is not None else ([], None)
            for j in range(34):
                qk[j]()
                if av_prev: av_prev[j]()
            if post_prev is not None: post_prev()
            post = (lambda: combine_a()) if m == 0 else (lambda h=h, i=i: combine_b(h, i))
            prev = (av_steps(heads[h], m, pt, ptB), post)
        for st in prev[0]: st()
        prev[1]()
        precast(64)
        P.barrier()
        P.reset(m_persist)
        if phases < 4:
            P.emit(); return nc

        TWO_PI = 2.0 * math.pi
        s_t = P.dsem("tabs"); tabB = Buf("tabs")
        E1 = P.tile([64, 66], BF16, "E1"); TW1 = P.tile([128, 2, 2, 33], F32, "TW1"); E2 = P.tile([128, 3, 128], BF16, "E2")
        Ginv = P.tile([128, 2, 128], BF16, "Ginv"); TW2 = P.tile([66, 2, 2, 128], F32, "TW2"); LAB = P.tile([66, 2, 32], BF16, "LAB")
        hb = P.tile([128, 2048], F32, "hb"); ndl = P.tile([128, 8], F32, "ndl")
        P.dma("pool", E1[:], E1d[:, :], s_t, writes=[tabB]); P.dma("sp", TW1[:], TW1d[:, :, :, :], s_t, writes=[tabB])
        P.dma("pool", E2[:], E2d[:, :, :], s_t, writes=[tabB]); P.dma("pool", Ginv[:], Ginvd[:, :, :], s_t, writes=[tabB])
        P.dma("sp", TW2[:], TW2d[:, :, :, :], s_t, writes=[tabB]); P.dma("pool", LAB[:], LABd[:, :, :], s_t, writes=[tabB])
        P.dma("sp", hb[:], hyb.partition_broadcast(128), s_t, writes=[tabB]); P.dma("sp", ndl[:], negdelta[:, :], s_t, writes=[tabB])
        m4 = P.mark()

        hd2T = P.tile([64, 2 * SEQ], BF16, "hd2T"); hd2B = Buf("hd2")
        w3t = P.tile([64, 4096], BF16, "w3t"); w3B = Buf("w3")
        s_w3 = P.dsem("w3")
        P.dma("pool", w3t[:], fw3[:, :], s_w3, writes=[w3B])
        m4a = P.mark()
        zt = P.tile([33, 2 * SEQ], F32, "zt"); w1t = P.tile([33, 64], F32, "w1t"); w2t = P.tile([64, 64], F32, "w2t"); fv = P.tile([64, 3], F32, "fv")
        fB = Buf("filt_in")
        s_f = P.dsem("filt")
        P.dma("sp", zt[:], zextT[:, :], s_f, writes=[fB]); P.dma("sp", w1t[:], fw1[:, :], s_f, writes=[fB])
        P.dma("sp", w2t[:], fw2[:, :], s_f, writes=[fB]); P.dma("sp", fv[:], fvec[:, :], s_f, writes=[fB])
        ar = Ring(P, 2, [64, 512], F32, "marg", sem=False); kir = Ring(P, 2, [64, 512], I32, "mki", sem=False)
        kfr = Ring(P, 2, [64, 512], F32, "mkf", sem=False); h1r = Ring(P, 2, [64, 512], F32, "mh1", sem=False)

        def sin_layer(ps_, psB_, bcol, out_ap, outB):
            a, aB, _ = ar.next(); ki, kiB, _ = kir.next(); kf_, kfB, _ = kfr.next()
            P.op("dve", lambda e: e.tensor_scalar(out=a[:], in0=ps_[0:64, :], scalar1=fv[:, bcol:bcol + 1], scalar2=fv[:, 2:3], op0=ALU.add, op1=ALU.mult),
                 reads=[psB_, fB], writes=[aB])
            P.op("dve", lambda e: e.tensor_scalar(out=ki[:], in0=a[:], scalar1=1.0 / TWO_PI, scalar2=None, op0=ALU.mult), reads=[aB], writes=[kiB])
            P.op("dve", lambda e: e.tensor_copy(out=kf_[:], in_=ki[:]), reads=[kiB], writes=[kfB])
            P.op("dve", lambda e: e.scalar_tensor_tensor(out=a[:], in0=kf_[:], scalar=-TWO_PI, in1=a[:], op0=ALU.mult, op1=ALU.add),
                 reads=[kfB, aB], writes=[aB])
            P.op("dve", lambda e: e.tensor_scalar(out=a[:], in0=a[:], scalar1=math.pi, scalar2=-math.pi, op0=ALU.min, op1=ALU.max), reads=[aB], writes=[aB])
            P.op("act", lambda e: e.activation(out=out_ap, in_=a[:], func=AF.Sin), reads=[aB], writes=[outB])

        for tt in range(16):
            pa, paB = ps_next()
            P.op("pe", lambda e, pa=pa, tt=tt: e.matmul(pa[0:64, :], w1t[:], zt[:, tt * 512:(tt + 1) * 512], start=True, stop=True),
                 reads=[fB], writes=[paB])
            h1, h1B, _ = h1r.next()
            sin_layer(pa, paB, 0, h1[:], h1B)
            pb_, pbB_ = ps_next()
            P.op("pe", lambda e, pb_=pb_, h1=h1: e.matmul(pb_[0:64, :], w2t[:], h1[:], start=True, stop=True), reads=[fB, h1B], writes=[pbB_])
            sin_layer(pb_, pbB_, 1, hd2T[:, tt * 512:(tt + 1) * 512], hd2B)
        P.barrier(); P.reset(m4a)

        ttb = P.tile([128, 2 * SEQ], F32, "ttb"); ttB = Buf("ttb")
        s_tt = P.dsem("ttb")
        P.dma("sp", ttb[:], ttx.partition_broadcast(128), s_tt, writes=[ttB])
        kur = Ring(P, 2, [128, 2 * SEQ], F32, "ku", sem=False); kbr = Ring(P, 2, [128, 2 * SEQ], BF16, "kub")
        dkr = Ring(P, 3, [128, 512], F32, "dk", sem=False); abr = Ring(P, 3, [128, 512], F32, "kab", sem=False)
        asum = P.tile([128, 32], F32, "asum"); asB = Buf("asum")
        asum2 = P.tile([128, 2, 32], F32, "asum2")
        for cc in range(8):
            kus = [kur.next() for _ in range(2)]
            for tt in range(16):
                dr = 0 if tt < 8 else 1
                dk, dkB, _ = dkr.next()
                P.op("act", lambda e, dk=dk, tt=tt, cc=cc: e.activation(out=dk[:], in_=ttb[:, tt * 512:(tt + 1) * 512], func=AF.Exp, scale=ndl[:, cc:cc + 1]),
                     reads=[ttB, tabB], writes=[dkB])
                for o in range(2):
                    ku, kuB, _ = kus[o]
                    col0 = o * 2048 + dr * 1024 + cc * 128
                    pa, paB = ps_next()
                    P.op("pe", lambda e, pa=pa, col0=col0, tt=tt: e.matmul(pa[:], w3t[:, col0:col0 + 128], hd2T[:, tt * 512:(tt + 1) * 512], start=True, stop=True),
                         reads=[w3B, hd2B], writes=[paB])
                    P.op("dve", lambda e, ku=ku, pa=pa, dk=dk, tt=tt: e.tensor_tensor(out=ku[:, tt * 512:(tt + 1) * 512], in0=pa[:], in1=dk[:], op=ALU.mult),
                         reads=[paB, dkB], writes=[kuB])
                    ab, abB, _ = abr.next()
                    P.op("act", lambda e, ab=ab, ku=ku, tt=tt: e.activation(out=ab[:], in_=ku[:, tt * 512:(tt + 1) * 512], func=AF.Abs), reads=[kuB], writes=[abB])
                    P.op("dve", lambda e, ab=ab, tt=tt, o=o: e.tensor_reduce(out=asum2[:, o, tt:tt + 1], in_=ab[:], axis=AXL.X, op=ALU.add), reads=[abB, asB], writes=[asB])
            for o in range(2):
                ku, kuB, _ = kus[o]
                P.op("dve", lambda e, o=o: e.tensor_reduce(out=asum2[:, o, 16:17], in_=asum2[:, o, 0:16], axis=AXL.X, op=ALU.add), reads=[asB], writes=[asB])
                P.op("dve", lambda e, o=o: e.reciprocal(out=asum2[:, o, 17:18], in_=asum2[:, o, 16:17]), reads=[asB], writes=[asB])
                kb, kbB, kbs = kbr.next()
                P.op("act", lambda e, kb=kb, ku=ku, o=o: e.activation(out=kb[:], in_=ku[:], func=AF.Copy, scale=asum2[:, o, 17:18]),
                     reads=[kuB, asB], writes=[kbB])
                P.dma("sp", KERN[o, cc * 128:(cc + 1) * 128, :], kb[:], kbs, reads=[kbB])
        P.barrier(); P.reset(m4)
        if phases < 5:
            P.emit(); return nc

        tmr = Ring(P, 4, [128, 7, 2, 33], F32, "twtmp", sem=False)

        def fft_fwd(Z, ZB, K, nch, Bt, BtB):
            c = 0
            while c < nch:
                g = min(7, nch - c)
                pa, paB = ps_next()
                for u in range(g):
                    P.op("pe", lambda e, pa=pa, u=u, c=c: e.matmul(pa[:, u * 66:(u + 1) * 66], Z[0:K, c + u, :], E1[0:K, :], start=True, stop=True),
                         reads=[ZB, tabB], writes=[paB], sig=(u == g - 1))
                A = pa[:, 0:g * 66].rearrange("p (g r f) -> p g r f", r=2, f=33)
                t1, t1B, _ = tmr.next(); t2, t2B, _ = tmr.next()
                P.op("dve", lambda e, A=A, t1=t1, g=g: e.tensor_tensor(out=t1[:, 0:g], in0=A, in1=TW1[:, 0].unsqueeze(1).to_broadcast([128, g, 2, 33]), op=ALU.mult),
                     reads=[paB, tabB], writes=[t1B])
                P.op("dve", lambda e, A=A, t2=t2, g=g: e.tensor_tensor(out=t2[:, 0:g], in0=A, in1=TW1[:, 1].unsqueeze(1).to_broadcast([128, g, 2, 33]), op=ALU.mult),
                     reads=[paB, tabB], writes=[t2B])
                P.op("pool", lambda e, t1=t1, g=g, c=c: e.tensor_tensor(out=Bt[:, 0, c:c + g, :], in0=t1[:, 0:g, 0, :], in1=t1[:, 0:g, 1, :], op=ALU.subtract),
                     reads=[t1B], writes=[BtB])
                P.op("pool", lambda e, t2=t2, g=g, c=c: e.tensor_tensor(out=Bt[:, 1, c:c + g, :], in0=t2[:, 0:g, 0, :], in1=t2[:, 0:g, 1, :], op=ALU.add),
                     reads=[t2B], writes=[BtB])
                c += g

        def fft_stage2(Bt, BtB, c0, n):
            xr_, xrB = ps_next(); xi_, xiB = ps_next()
            br = Bt[:, 0, c0:c0 + n, :].rearrange("p c f -> p (c f)"); bi = Bt[:, 1, c0:c0 + n, :].rearrange("p c f -> p (c f)")
            w = n * 33
            P.op("pe", lambda e: e.matmul(xr_[:, 0:w], E2[:, 0, :], br, start=True, stop=False), reads=[BtB, tabB], writes=[xrB], sig=False)
            P.op("pe", lambda e: e.matmul(xr_[:, 0:w], E2[:, 2, :], bi, start=False, stop=True), reads=[BtB, tabB], writes=[xrB])
            P.op("pe", lambda e: e.matmul(xi_[:, 0:w], E2[:, 0, :], bi, start=True, stop=False), reads=[BtB, tabB], writes=[xiB], sig=False)
            P.op("pe", lambda e: e.matmul(xi_[:, 0:w], E2[:, 1, :], br, start=False, stop=True), reads=[BtB, tabB], writes=[xiB])
            return xr_, xrB, xi_, xiB

        def fft_fwd_g(Z, ZB, K, nch, Bt, BtB, tring):
            c = 0
            while c < nch:
                g = min(7, nch - c)
                pa, paB = ps_next()
                for u in range(g):
                    P.op("pe", lambda e, pa=pa, u=u, c=c: e.matmul(pa[:, u * 66:(u + 1) * 66], Z[0:K, c + u, :], E1[0:K, :], start=True, stop=True),
                         reads=[ZB, tabB], writes=[paB], sig=(u == g - 1))
                A = pa[:, 0:g * 66].rearrange("p (g r f) -> p g r f", r=2, f=33)
                t1, t1B, _ = tring.next(); t2, t2B, _ = tring.next()
                P.op("dve", lambda e, A=A, t1=t1, g=g: e.tensor_tensor(out=t1[:, 0:g], in0=A, in1=TW1[:, 0].unsqueeze(1).to_broadcast([128, g, 2, 33]), op=ALU.mult),
                     reads=[paB, tabB], writes=[t1B])
                P.op("dve", lambda e, A=A, t2=t2, g=g: e.tensor_tensor(out=t2[:, 0:g], in0=A, in1=TW1[:, 1].unsqueeze(1).to_broadcast([128, g, 2, 33]), op=ALU.mult),
                     reads=[paB, tabB], writes=[t2B])
                P.op("pool", lambda e, t1=t1, g=g, c=c: e.tensor_tensor(out=Bt[:, 0, c:c + g, :], in0=t1[:, 0:g, 0, :], in1=t1[:, 0:g, 1, :], op=ALU.subtract),
                     reads=[t1B], writes=[BtB])
                P.op("pool", lambda e, t2=t2, g=g, c=c: e.tensor_tensor(out=Bt[:, 1, c:c + g, :], in0=t2[:, 0:g, 0, :], in1=t2[:, 0:g, 1, :], op=ALU.add),
                     reads=[t2B], writes=[BtB])
                c += g
                yield

        zkr = Ring(P, 2, [64, 64, 128], BF16, "zk"); btr = Ring(P, 2, [128, 2, 64, 33], BF16, "bt", sem=False)
        kfo = Ring(P, 2, [128, 64, 2, 33], F32, "kfo")

        def kf_chain(o, hc):
            zk, zkB, zks = zkr.next()
            for q4 in range(2):
                P.dma("sp", zk[:, q4 * 32:(q4 + 1) * 32, :],
                      KERN[o, hc * 64 + q4 * 32:hc * 64 + (q4 + 1) * 32, :].rearrange("c (a b) -> a c b", b=128), zks, writes=[zkB])
            bt, btB, _ = btr.next()
            yield
            yield from fft_fwd_g(zk, zkB, 64, 64, bt, btB, tmr)
            ko, koB, kos = kfo.next()
            c0 = 0
            while c0 < 64:
                n = min(15, 64 - c0)
                xr_, xrB, xi_, xiB = fft_stage2(bt, btB, c0, n)
                P.op("dve", lambda e, ko=ko, xr_=xr_, c0=c0, n=n, o=o, hc=hc: e.tensor_tensor(
                    out=ko[:, c0:c0 + n, 0, :], in0=xr_[:, 0:n * 33].rearrange("p (c f) -> p c f", f=33),
                    in1=hb[:, o * 1024 + hc * 64 + c0:o * 1024 + hc * 64 + c0 + n].unsqueeze(2).to_broadcast([128, n, 33]), op=ALU.add),
                    reads=[xrB, tabB], writes=[koB])
                P.op("act", lambda e, ko=ko, xi_=xi_, c0=c0, n=n: e.copy(out=ko[:, c0:c0 + n, 1, :], in_=xi_[:, 0:n * 33].rearrange("p (c f) -> p c f", f=33)),
                     reads=[xiB], writes=[koB])
                c0 += n
                yield
            P.dma("act", KF[o, :, hc * 64:(hc + 1) * 64, :, :], ko[:], kos, reads=[koB])

        for hc in range(16):
            gens = [kf_chain(0, hc), kf_chain(1, hc)]
            alive = [True, True]
            while any(alive):
                for gi_ in range(2):
                    if alive[gi_]:
                        try:
                            next(gens[gi_])
                        except StopIteration:
                            alive[gi_] = False
        P.barrier(); P.reset(m4)
        if phases < 6:
            P.emit(); return nc

        NCG = 32
        def chain_bufs(p):
            d = {}
            d["zv"] = Ring(P, 1, [32, NCG, 128], BF16, f"zv{p}"); d["x1"] = Ring(P, 1, [32, NCG, 128], BF16, f"x1t{p}"); d["x2"] = Ring(P, 1, [32, NCG, 128], BF16, f"x2t{p}")
            d["k0"] = Ring(P, 1, [128, NCG, 2, 33], F32, f"kf0{p}"); d["k1"] = Ring(P, 1, [128, NCG, 2, 33], F32, f"kf1{p}")
            d["bt"] = Ring(P, 1, [128, 2, NCG, 33], BF16, f"btd{p}", sem=False); d["xp"] = Ring(P, 1, [128, NCG, 2, 33], BF16, f"xpt{p}", sem=False)
            d["zz"] = Ring(P, 1, [32, NCG, 128], BF16, f"zz{p}", sem=False); d["hy"] = Ring(P, 1, [32, NCG, 128], BF16, f"hyo{p}")
            return d
        CB = [chain_bufs(0), chain_bufs(1)]
        Rr = Ring(P, 3, [66, 2, 16, 128], BF16, "Rinv", sem=False)
        pwr = Ring(P, 6, [128, 15, 33], F32, "pwt", sem=False); sar = Ring(P, 6, [66, 2, 2, 128], F32, "sat", sem=False)
        tmr2 = Ring(P, 6, [128, 7, 2, 33], F32, "twtmp2", sem=False)

        def conv_g(cb, Zin, ZinB, kf_, kfB, gate, gateB, out_t, outB):
            bt, btB, _ = cb["bt"].next(); xp, xpB, _ = cb["xp"].next()
            yield from fft_fwd_g(Zin, ZinB, 32, NCG, bt, btB, tmr2)
            c0 = 0
            while c0 < NCG:
                n = min(15, NCG - c0)
                xr_, xrB, xi_, xiB = fft_stage2(bt, btB, c0, n)
                XR = xr_[:, 0:n * 33].rearrange("p (c f) -> p c f", f=33); XI = xi_[:, 0:n * 33].rearrange("p (c f) -> p c f", f=33)
                kr_ = kf_[:, c0:c0 + n, 0, :]; ki_ = kf_[:, c0:c0 + n, 1, :]
                ts = [pwr.next() for _ in range(4)]
                for (tb_, src, kk) in ((ts[0], XR, kr_), (ts[1], XI, ki_), (ts[2], XR, ki_), (ts[3], XI, kr_)):
                    P.op("dve", lambda e, tb_=tb_, src=src, kk=kk, n=n: e.tensor_tensor(out=tb_[0][:, 0:n, :], in0=src, in1=kk, op=ALU.mult),
                         reads=[xrB, xiB, kfB], writes=[tb_[1]])
                P.op("pool", lambda e, xp=xp, c0=c0, n=n, ts=ts: e.tensor_tensor(out=xp[:, c0:c0 + n, 0, :], in0=ts[0][0][:, 0:n, :], in1=ts[1][0][:, 0:n, :], op=ALU.subtract),
                     reads=[ts[0][1], ts[1][1]], writes=[xpB])
                P.op("pool", lambda e, xp=xp, c0=c0, n=n, ts=ts: e.tensor_tensor(out=xp[:, c0:c0 + n, 1, :], in0=ts[2][0][:, 0:n, :], in1=ts[3][0][:, 0:n, :], op=ALU.add),
                     reads=[ts[2][1], ts[3][1]], writes=[xpB])
                c0 += n
                yield
            for c16 in range(NCG // 16):
                R, RB, _ = Rr.next()
                for cp in range(8):
                    c = c16 * 16 + cp * 2
                    pa, paB = ps_next()
                    for u in range(2):
                        P.op("pe", lambda e, pa=pa, u=u, c=c: e.matmul(pa[0:66, u * 256:(u + 1) * 256], xp[:, c + u, :, :].rearrange("p r f -> p (r f)"),
                                                                     Ginv[:].rearrange("p r s -> p (r s)"), start=True, stop=True),
                             reads=[xpB, tabB], writes=[paB], sig=(u == 1))
                    Pv = pa[0:66, :].rearrange("p (g h s) -> p g h s", g=2, h=2)
                    q1 = sar.next(); q2 = sar.next()
                    P.op("dve", lambda e, Pv=Pv, q1=q1: e.tensor_tensor(out=q1[0][:], in0=Pv, in1=TW2[:, 0].unsqueeze(1).to_broadcast([66, 2, 2, 128]), op=ALU.mult),
                         reads=[paB, tabB], writes=[q1[1]])
                    P.op("dve", lambda e, Pv=Pv, q2=q2: e.tensor_tensor(out=q2[0][:], in0=Pv, in1=TW2[:, 1].unsqueeze(1).to_broadcast([66, 2, 2, 128]), op=ALU.mult),
                         reads=[paB, tabB], writes=[q2[1]])
                    P.op("pool", lambda e, R=R, q1=q1, cp=cp: e.tensor_tensor(out=R[:, 0, cp * 2:cp * 2 + 2, :], in0=q1[0][:, :, 0, :], in1=q1[0][:, :, 1, :], op=ALU.add),
                         reads=[q1[1]], writes=[RB])
                    P.op("pool", lambda e, R=R, q2=q2, cp=cp: e.tensor_tensor(out=R[:, 1, cp * 2:cp * 2 + 2, :], in0=q2[0][:, :, 0, :], in1=q2[0][:, :, 1, :], op=ALU.add),
                         reads=[q2[1]], writes=[RB])
                    yield
                for c4 in range(4):
                    c = c16 * 16 + c4 * 4
                    pa, paB = ps_next()
                    P.op("pe", lambda e, pa=pa, R=R, c4=c4: e.matmul(pa[0:32, :], LAB[:, 0, :], R[:, 0, c4 * 4:c4 * 4 + 4, :].rearrange("p c s -> p (c s)"), start=True, stop=False),
                         reads=[RB, tabB], writes=[paB], sig=False)
                    P.op("pe", lambda e, pa=pa, R=R, c4=c4: e.matmul(pa[0:32, :], LAB[:, 1, :], R[:, 1, c4 * 4:c4 * 4 + 4, :].rearrange("p c s -> p (c s)"), start=False, stop=True),
                         reads=[RB, tabB], writes=[paB])
                    P.op("dve", lambda e, pa=pa, c=c: e.tensor_tensor(out=out_t[:, c:c + 4, :], in0=pa[0:32, :].rearrange("p (c s) -> p c s", s=128),
                                                                      in1=gate[:, c:c + 4, :], op=ALU.mult),
                         reads=[paB, gateB], writes=[outB])
                    yield

        def chain_g(p, hc):
            cb = CB[p]; cb0 = hc * NCG
            zv, zvB, zvs = cb["zv"].next(); x1t, x1B, x1s = cb["x1"].next(); x2t, x2B, x2s = cb["x2"].next()
            k0, k0B, k0s = cb["k0"].next(); k1, k1B, k1s = cb["k1"].next()
            for (dst, dB, dsm, src) in ((zv, zvB, zvs, UU[0, cb0:cb0 + NCG, :]), (x1t, x1B, x1s, UU[1, cb0:cb0 + NCG, :]), (x2t, x2B, x2s, UU[2, cb0:cb0 + NCG, :])):
                P.dma("pool", dst[:], src.rearrange("c (a b) -> a c b", b=128), dsm, writes=[dB])
            P.dma("sp", k0[:], KF[0, :, cb0:cb0 + NCG, :, :], k0s, writes=[k0B])
            P.dma("sp", k1[:], KF[1, :, cb0:cb0 + NCG, :, :], k1s, writes=[k1B])
            zz, zzB, _ = cb["zz"].next(); hy, hyB, hys = cb["hy"].next()
            yield
            yield from conv_g(cb, zv, zvB, k0, k0B, x1t, x1B, zz, zzB)
            yield from conv_g(cb, zz, zzB, k1, k1B, x2t, x2B, hy, hyB)
            P.dma("act", HY[cb0:cb0 + NCG, :].rearrange("c (a b) -> a c b", b=128), hy[:], hys, reads=[hyB])

        for pr_ in range(D // NCG // 2):
            gens = [chain_g(0, 2 * pr_), chain_g(1, 2 * pr_ + 1)]
            alive = [True, True]
            while any(alive):
                for gi_ in range(2):
                    if alive[gi_]:
                        try:
                            next(gens[gi_])
                        except StopIteration:
                            alive[gi_] = False
        P.barrier(); P.reset(m_persist)
        if phases < 7:
            P.emit(); return nc

        posA = P.tile([128, 32, 64], F32, "posA"); gatA = P.tile([128, 32, 64], F32, "gatA"); pgB = Buf("posgate")
        slotI = P.tile([128, 256], I32, "slotI"); gkA = P.tile([128, 32, 8], F32, "gkA"); blkI = P.tile([128, NBLK], I32, "blkI"); metaB = Buf("meta")
        m_persist = P.mark()
        s_w = P.dsem("w5"); w5B = Buf("w5")
        wpa = P.tile([128, 8, D], BF16, "wpa"); wph = P.tile([128, 8, D], BF16, "wph"); wo = P.tile([128, 8, D], BF16, "wo")
        rwt = P.tile([128, 8, 64], F32, "rwt"); rbt = P.tile([128, 64], F32, "rbt")
        P.dma("pool", wpa[:], w_pa.rearrange("(k p) n -> p k n", p=128), s_w, writes=[w5B])
        P.dma("pool", wph[:], w_ph.rearrange("(k p) n -> p k n", p=128), s_w, writes=[w5B])
        P.dma("pool", wo[:], w_o.rearrange("(k p) n -> p k n", p=128), s_w, writes=[w5B])
        P.dma("sp", rwt[:], rw.rearrange("(k p) n -> p k n", p=128), s_w, writes=[w5B])
        P.dma("sp", rbt[:], rbias.partition_broadcast(128), s_w, writes=[w5B])
        selb = P.tile([128, 64], BF16, "selb"); ltri = P.tile([128, 128], BF16, "ltri"); onesb5 = P.tile([128, 128], BF16, "onesb5"); triB = Buf("tri")
        carry = P.tile([128, 64], F32, "carry"); carB = Buf("carry")
        P.op("pool", lambda e: e.memset(carry[:], 0.0), writes=[carB])
        P.op("pool", lambda e: e.memset(onesb5[:], 1.0), writes=[triB])
        P.op("pool", lambda e: e.memset(ltri[:], 1.0), writes=[triB])
        P.op("pool", lambda e: e.affine_select(out=ltri[:], in_=ltri[:], pattern=[[1, 128]], compare_op=ALU.is_gt, fill=0.0, base=0, channel_multiplier=-1),
             reads=[triB], writes=[triB])
        m5b = P.mark()
        htkr = Ring(P, 2, [128, D], BF16, "htk")
        axr = Ring(P, 1, [128, 8, 512], BF16, "axT"); hyr5 = Ring(P, 1, [128, 8, 512], BF16, "hyT"); gtr = Ring(P, 1, [128, 16, 512], BF16, "gt")
        xr5 = Ring(P, 1, [128, 8, 512], F32, "xt5")
        yT = P.tile([128, 8, 512], BF16, "yT"); yB = Buf("yT")
        x1r5 = Ring(P, 1, [128, 8, 512], F32, "x1T")
        sq5 = P.tile([128, 8, 512], F32, "sq5"); sq5B = Buf("sq5"); rstd5 = P.tile([128, 512], F32, "rstd5"); rs5B = Buf("rs5")
        hx2f = P.tile([128, 8, 512], F32, "hx2f"); hx2fB = Buf("hx2f")
        hx2b = Ring(P, 1, [128, 8, 512], BF16, "hx2b")
        y1r = Ring(P, 4, [128, 512], F32, "y1", sem=False)
        rt = [P.tile([128, 64], F32, f"rt{i}") for i in range(6)]; rtB = Buf("rt")
        rs_ = P.tile([128, 40], F32, "rsm")
        AX_v = AXs.rearrange("(k p) t -> p k t", p=128); HY_v = HY.rearrange("(k p) t -> p k t", p=128)
        GG_v = GG.rearrange("(k p) t -> p k t", p=128); X1_v = X1.rearrange("(k p) t -> p k t", p=128); HX2_v = HX2.rearrange("(k p) t -> p k t", p=128)
        for i in range(8):
            t0 = i * 512
            ax_, axB, axs = axr.next(); hy_, hyB5, hys5 = hyr5.next(); gt, gtB, gts = gtr.next(); xt5, xB5, xs5 = xr5.next()
            P.dma("sp", ax_[:], AX_v[:, :, t0:t0 + 512], axs, writes=[axB])
            P.dma("act", hy_[:], HY_v[:, :, t0:t0 + 512], hys5, writes=[hyB5])
            P.dma("sp", gt[:, 0:8, :], GG_v[:, 0:8, t0:t0 + 512], gts, writes=[gtB])
            P.dma("act", gt[:, 8:16, :], GG_v[:, 8:16, t0:t0 + 512], gts, writes=[gtB])
            P.dma("sp", xt5[:], xT_v[:, :, t0:t0 + 512], xs5, writes=[xB5])
            for dc in range(8):
                pa, paB = ps_next(); ph_, phB = ps_next()
                for k in range(8):
                    P.op("pe", lambda e, pa=pa, k=k, dc=dc, ax_=ax_: e.matmul(pa[:], wpa[:, k, dc * 128:(dc + 1) * 128], ax_[:, k, :], start=(k == 0), stop=(k == 7)),
                         reads=[w5B, axB], writes=[paB], sig=(k == 7))
                for k in range(8):
                    P.op("pe", lambda e, ph_=ph_, k=k, dc=dc, hy_=hy_: e.matmul(ph_[:], wph[:, k, dc * 128:(dc + 1) * 128], hy_[:, k, :], start=(k == 0), stop=(k == 7)),
                         reads=[w5B, hyB5], writes=[phB], sig=(k == 7))
                ya, yaB, _ = y1r.next(); yb_, ybB, _ = y1r.next()
                P.op("dve", lambda e, ya=ya, pa=pa, gt=gt, dc=dc: e.tensor_tensor(out=ya[:], in0=pa[:], in1=gt[:, dc, :], op=ALU.mult), reads=[paB, gtB], writes=[yaB])
                P.op("dve", lambda e, yb_=yb_, ph_=ph_, gt=gt, dc=dc: e.tensor_tensor(out=yb_[:], in0=ph_[:], in1=gt[:, 8 + dc, :], op=ALU.mult), reads=[phB, gtB], writes=[ybB])
                P.op("pool", lambda e, ya=ya, yb_=yb_, dc=dc: e.tensor_tensor(out=yT[:, dc, :], in0=ya[:], in1=yb_[:], op=ALU.add), reads=[yaB, ybB], writes=[yB])
            x1T, x1B5, x1s5 = x1r5.next()
            for dc in range(8):
                pm, pmB = ps_next()
                for k in range(8):
                    P.op("pe", lambda e, pm=pm, k=k, dc=dc: e.matmul(pm[:], wo[:, k, dc * 128:(dc + 1) * 128], yT[:, k, :], start=(k == 0), stop=(k == 7)),
                         reads=[w5B, yB], writes=[pmB], sig=(k == 7))
                P.op("dve", lambda e, pm=pm, dc=dc, x1T=x1T, xt5=xt5: e.scalar_tensor_tensor(out=x1T[:, dc, :], in0=pm[:], scalar=vec[:, 48 + dc:49 + dc], in1=xt5[:, dc, :],
                                                                                          op0=ALU.mult, op1=ALU.add), reads=[pmB, vecB, xB5], writes=[x1B5])
            P.dma("sp", X1_v[:, :, t0:t0 + 512], x1T[:], x1s5, reads=[x1B5])
            rms_rstd(x1T, x1B5, 512, sq=sq5, sqB=sq5B, rstd=rstd5, rsB=rs5B)
            P.op("dve", lambda e, x1T=x1T: e.tensor_tensor(out=sq5[:], in0=x1T[:], in1=rstd5[:].unsqueeze(1).to_broadcast([128, 8, 512]), op=ALU.mult),
                 reads=[x1B5, rs5B, sq5B], writes=[sq5B])
            hb2, hb2B, hb2s = hx2b.next()
            for k in range(8):
                P.op("act", lambda e, k=k: e.activation(out=hx2f[:, k, :], in_=sq5[:, k, :], func=AF.Identity, scale=vec[:, 32 + k:33 + k], bias=vec[:, 40 + k:41 + k]),
                     reads=[sq5B, vecB], writes=[hx2fB])
            P.op("pool", lambda e, hb2=hb2: e.tensor_copy(out=hb2[:], in_=hx2f[:]), reads=[hx2fB], writes=[hb2B])
            P.dma("act", HX2_v[:, :, t0:t0 + 512], hb2[:], hb2s, reads=[hb2B])
            for sb_ in range(4):
                st_ = i * 4 + sb_
                pr, prB = ps_next()
                for k in range(8):
                    P.op("pe", lambda e, pr=pr, k=k, sb_=sb_: e.matmul(pr[:, 0:64], hx2f[:, k, sb_ * 128:(sb_ + 1) * 128], rwt[:, k, :], start=(k == 0), stop=(k == 7)),
                         reads=[hx2fB, w5B], writes=[prB], sig=(k == 7))
                for hf in range(2):
                    ptk, ptkB = ps_next()
                    for kk in range(4):
                        k = hf * 4 + kk
                        P.op("pe", lambda e, ptk=ptk, k=k, kk=kk, sb_=sb_: e.transpose(out=ptk[:, kk * 128:(kk + 1) * 128], in_=hx2f[:, k, sb_ * 128:(sb_ + 1) * 128], identity=ident[:]),
                             reads=[hx2fB, identB], writes=[ptkB], sig=(kk == 3))
                    if hf == 0:
                        htk, htkB, htks = htkr.next()
                        P.op("act", lambda e, htk=htk, ptk=ptk: e.copy(out=htk[:, 0:512], in_=ptk[:]), reads=[ptkB], writes=[htkB])
                    else:
                        P.op("dve", lambda e, htk=htk, ptk=ptk: e.tensor_copy(out=htk[:, 512:1024], in_=ptk[:]), reads=[ptkB], writes=[htkB])
                P.dma("act", HX2tok[st_ * 128:(st_ + 1) * 128, :], htk[:], htks, reads=[htkB])
                sc_, ch_, c2_, msk, wse, gte = rt
                def R_(fn, eng="dve", rd=(), wr=()):
                    P.op(eng, fn, reads=[rtB] + list(rd), writes=[rtB] + list(wr))
                R_(lambda e, pr=pr: e.activation(out=sc_[:], in_=pr[:, 0:64], func=AF.Sigmoid), eng="act", rd=[prB])
                R_(lambda e: e.tensor_tensor(out=ch_[:], in0=sc_[:], in1=rbt[:], op=ALU.add), rd=[w5B])
                R_(lambda e: e.tensor_reduce(out=rs_[:, 0:8], in_=ch_[:].rearrange("p (g j) -> p g j", j=8), axis=AXL.X, op=ALU.max))
                R_(lambda e: e.tensor_tensor(out=c2_[:].rearrange("p (g j) -> p g j", j=8), in0=ch_[:].rearrange("p (g j) -> p g j", j=8),
                                             in1=rs_[:, 0:8].unsqueeze(2).to_broadcast([128, 8, 8]), op=ALU.is_equal))
                R_(lambda e: e.scalar_tensor_tensor(out=c2_[:], in0=c2_[:], scalar=-1.0e9, in1=ch_[:], op0=ALU.mult, op1=ALU.add))
                R_(lambda e: e.tensor_reduce(out=rs_[:, 8:16], in_=c2_[:].rearrange("p (g j) -> p g j", j=8), axis=AXL.X, op=ALU.max))
                R_(lambda e: e.tensor_tensor(out=rs_[:, 0:8], in0=rs_[:, 0:8], in1=rs_[:, 8:16], op=ALU.add))
                R_(lambda e: e.max(out=rs_[:, 16:24], in_=rs_[:, 0:8]))
                R_(lambda e: e.tensor_scalar(out=rs_[:, 8:16], in0=rs_[:, 0:8], scalar1=rs_[:, 19:20], scalar2=None, op0=ALU.is_ge))
                R_(lambda e: e.tensor_scalar(out=rs_[:, 8:16], in0=rs_[:, 8:16], scalar1=-1.0, scalar2=1.0e9, op0=ALU.add, op1=ALU.mult))
                R_(lambda e: e.tensor_tensor(out=msk[:].rearrange("p (g j) -> p g j", j=8), in0=ch_[:].rearrange("p (g j) -> p g j", j=8),
                                             in1=rs_[:, 8:16].unsqueeze(2).to_broadcast([128, 8, 8]), op=ALU.add))
                R_(lambda e: e.max(out=rs_[:, 24:32], in_=msk[:]))
                R_(lambda e: e.tensor_scalar(out=wse[:], in0=msk[:], scalar1=rs_[:, 31:32], scalar2=None, op0=ALU.is_ge))
                R_(lambda e: e.tensor_copy(out=selb[:], in_=wse[:]))
                R_(lambda e: e.tensor_tensor(out=wse[:], in0=wse[:], in1=sc_[:], op=ALU.mult))
                R_(lambda e: e.tensor_reduce(out=rs_[:, 32:33], in_=wse[:], axis=AXL.X, op=ALU.add))
                R_(lambda e: e.reciprocal(out=rs_[:, 33:34], in_=rs_[:, 32:33]))
                R_(lambda e, st_=st_: e.tensor_scalar(out=gatA[:, st_, :], in0=wse[:], scalar1=rs_[:, 33:34], scalar2=2.5, op0=ALU.mult, op1=ALU.mult), wr=[pgB])
                pp, ppB = ps_next()
                P.op("pe", lambda e, pp=pp: e.matmul(pp[:, 0:64], ltri[:], selb[:], start=True, stop=True), reads=[rtB, triB], writes=[ppB])
                P.op("pe", lambda e, pp=pp: e.matmul(pp[:, 64:128], onesb5[:], selb[:], start=True, stop=True), reads=[rtB, triB], writes=[ppB])
                P.op("dve", lambda e, pp=pp, st_=st_: e.tensor_tensor(out=posA[:, st_, :], in0=pp[:, 0:64], in1=carry[:], op=ALU.add), reads=[ppB, carB, pgB], writes=[pgB])
                P.op("dve", lambda e, pp=pp: e.tensor_tensor(out=carry[:], in0=pp[:, 64:128], in1=carry[:], op=ALU.add), reads=[ppB, carB], writes=[carB])
        P.barrier(); P.reset(m5b)
        W2K = 32 * 64
        slotf = P.tile([128, 32, 64], F32, "slotf"); keyt = P.tile([128, 32, 64], F32, "keyt"); eqt = P.tile([128, 32, 64], F32, "eqt"); m2B = Buf("meta2")
        rowA = P.tile([128, 64], F32, "rowA"); rowB = P.tile([128, 64], F32, "rowB"); rowI = P.tile([128, 64], I32, "rowI"); padd = P.tile([128, 64], F32, "padd")
        k8 = P.tile([128, 32, 8], F32, "k8"); skf = P.tile([128, 32, 8], F32, "skf")
        bvt = P.tile([128, NBLK], F32, "bvt"); blkf = P.tile([128, NBLK], F32, "blkf"); chg = P.tile([128, NBLK], F32, "chg"); pct = P.tile([128, 1], F32, "pct")
        cmpt = P.tile([128, 80, 64], F32, "cmpt")
        s_m = P.dsem("meta")
        P.dma("sp", bvt[:], bvals.partition_broadcast(128), s_m, writes=[m2B]); P.dma("sp", pct[:], pcol[:, :], s_m, writes=[m2B])
        def M_(fn, eng="dve", rd=(), wr=()):
            P.op(eng, fn, reads=[m2B] + list(rd), writes=[m2B] + list(wr))
        M_(lambda e: e.tensor_scalar(out=rowA[:], in0=carry[:], scalar1=1.0 / 128, scalar2=127.0 / 128 - 0.5 + 1.0 / 256, op0=ALU.mult, op1=ALU.add), rd=[carB])
        M_(lambda e: e.tensor_copy(out=rowI[:], in_=rowA[:]))
        M_(lambda e: e.tensor_copy(out=rowA[:], in_=rowI[:]))
        M_(lambda e: e.tensor_scalar(out=padd[:], in0=rowA[:], scalar1=128.0, scalar2=None, op0=ALU.mult))
        M_(lambda e: e.tensor_copy(out=rowA[:], in_=padd[:]))
        cur, oth = rowA, rowB
        for sh in (1, 2, 4, 8, 16, 32):
            M_(lambda e, cur=cur, oth=oth, sh=sh: e.tensor_copy(out=oth[:, 0:sh], in_=cur[:, 0:sh]))
            M_(lambda e, cur=cur, oth=oth, sh=sh: e.tensor_tensor(out=oth[:, sh:64], in0=cur[:, sh:64], in1=cur[:, 0:64 - sh], op=ALU.add))
            cur, oth = oth, cur
        pend = cur; pstart = oth
        M_(lambda e: e.tensor_tensor(out=pstart[:], in0=pend[:], in1=padd[:], op=ALU.subtract))
        M_(lambda e: e.tensor_tensor(out=slotf[:], in0=posA[:], in1=pstart[:].unsqueeze(1).to_broadcast([128, 32, 64]), op=ALU.add), rd=[pgB])
        M_(lambda e: e.tensor_scalar(out=keyt[:], in0=slotf[:], scalar1=-1.0, scalar2=BIGK + 1.0, op0=ALU.mult, op1=ALU.add))
        M_(lambda e: e.tensor_single_scalar(out=eqt[:], in_=gatA[:], scalar=0.0, op=ALU.is_gt), rd=[pgB])
        M_(lambda e: e.tensor_tensor(out=keyt[:], in0=keyt[:], in1=eqt[:], op=ALU.mult))
        M_(lambda e: e.tensor_scalar(out=keyt[:], in0=keyt[:], scalar1=-1.0, scalar2=None, op0=ALU.add))
        for st_ in range(32):
            M_(lambda e, st_=st_: e.max(out=k8[:, st_, :], in_=keyt[:, st_, :]))
        M_(lambda e: e.tensor_scalar(out=skf[:], in0=k8[:], scalar1=-1.0, scalar2=BIGK, op0=ALU.mult, op1=ALU.add))
        M_(lambda e: e.tensor_copy(out=slotI[:], in_=skf[:].rearrange("p a b -> p (a b)")), wr=[metaB])
        for k in range(8):
            M_(lambda e, k=k: e.tensor_tensor(out=eqt[:], in0=slotf[:], in1=skf[:, :, k:k + 1].to_broadcast([128, 32, 64]), op=ALU.is_equal))
            M_(lambda e: e.tensor_tensor(out=eqt[:], in0=eqt[:], in1=gatA[:], op=ALU.mult), rd=[pgB])
            M_(lambda e, k=k: e.tensor_reduce(out=gkA[:, :, k], in_=eqt[:], axis=AXL.X, op=ALU.add), wr=[metaB])
        for cq in range(NBLK // 80):
            M_(lambda e, cq=cq: e.tensor_tensor(out=cmpt[:], in0=pend[:].unsqueeze(1).to_broadcast([128, 80, 64]),
                                                in1=bvt[:, cq * 80:(cq + 1) * 80].unsqueeze(2).to_broadcast([128, 80, 64]), op=ALU.is_le))
            M_(lambda e, cq=cq: e.tensor_reduce(out=blkf[:, cq * 80:(cq + 1) * 80], in_=cmpt[:], axis=AXL.X, op=ALU.add))
        M_(lambda e: e.tensor_scalar(out=blkf[:], in0=blkf[:], scalar1=63.0, scalar2=128.0, op0=ALU.min, op1=ALU.mult))
        M_(lambda e: e.memset(chg[:, 0:NWB], 1.0))
        M_(lambda e: e.tensor_tensor(out=chg[:, NWB:NBLK], in0=blkf[:, NWB:NBLK], in1=blkf[:, 0:NBLK - NWB], op=ALU.not_equal))
        M_(lambda e: e.tensor_scalar(out=blkf[:], in0=blkf[:], scalar1=pct[:, 0:1], scalar2=-BIGI, op0=ALU.add, op1=ALU.add))
        M_(lambda e: e.tensor_tensor(out=blkf[:], in0=blkf[:], in1=chg[:], op=ALU.mult))
        M_(lambda e: e.tensor_scalar(out=blkf[:], in0=blkf[:], scalar1=BIGI, scalar2=None, op0=ALU.add))
        M_(lambda e: e.tensor_copy(out=blkI[:], in_=blkf[:]), wr=[metaB])
        dtr = Ring(P, 3, [128, D], BF16, "dtok")
        dsc = [P.dsem("scat") for _ in range(3)]
        for st_ in range(32):
            dt_, dtB, dts = dtr.next()
            P.dma("sp", dt_[:], HX2tok[st_ * 128:(st_ + 1) * 128, :], dts, writes=[dtB])
            for k in range(8):
                waits = P._deps("pool", [dtB, metaB] + zwin, [])
                ssem = dsc[dtr.i]
                ssem.n += 16
                P.q["pool"].append((waits, (lambda e, dt_=dt_, st_=st_, k=k: e.indirect_dma_start(
                    out=XG[:, :], out_offset=bass.IndirectOffsetOnAxis(ap=slotI[:, st_ * 8 + k:st_ * 8 + k + 1], axis=0), in_=dt_[:, :], in_offset=None,
                    bounds_check=P.reg(e, NSLOT - 1), oob_is_err=False)), (ssem.h, 16)))
                dtB.r.append((ssem, ssem.n))
        xgBs = [Buf(f"xg{i}") for i in range(3)]
        for i_, b_ in enumerate(xgBs):
            b_.w = (dsc[i_], dsc[i_].n)
        if debug:
            s_dbg = P.dsem("dbg")
            P.dma("sp", dbg["slot"].rearrange("p a b -> p (a b)"), slotI[:], s_dbg, reads=[metaB]); P.dma("sp", dbg["gk"][:, :, :], gkA[:], s_dbg, reads=[metaB])
            P.dma("sp", dbg["blk"][:, :], blkI[:], s_dbg, reads=[metaB])
        P.barrier(); P.reset(m_persist)
        if phases < 8:
            P.emit(); return nc

        wbuf = [(P.tile([128, 6144], BF16, f"wall{i}"), Buf(f"bw{i}"), P.dsem("bw")) for i in range(NWB)]
        for (a_, wB_, _) in wbuf:
            P.op("pool", lambda e, a_=a_: e.memset(a_[:], 0.0), writes=[wB_])
        xbr = Ring(P, 4, [128, D], BF16, "xb"); xgtr = Ring(P, 2, [128, 8, 128], BF16, "xgT", sem=False)
        sgr6 = Ring(P, 2, [128, 256], F32, "sg6", sem=False); atkr = Ring(P, 2, [128, 256], BF16, "atk", sem=False)
        atr = Ring(P, 2, [128, 2, 128], BF16, "aT", sem=False); ybr = Ring(P, 3, [128, D], F32, "yb")
        ygB = Buf("YG")
        identb = P.tile([128, 128], BF16, "identb"); identbB = Buf("identb")
        P.op("dve", lambda e: e.tensor_copy(out=identb[:], in_=ident[:]), reads=[identB], writes=[identbB])
        def block_g(b):
            wall, wB_, wsem = wbuf[b % NWB]
            wg_ = wall[:, 0:2048].rearrange("p (k h) -> p k h", k=8); wu_ = wall[:, 2048:4096].rearrange("p (k h) -> p k h", k=8)
            wd_ = wall[:, 4096:6144].rearrange("p (k d) -> p k d", k=2)
            waits = P._deps("pool", [metaB, wbB], [wB_])
            wsem.n += 16
            P.q["pool"].append((waits, (lambda e, wall=wall, b=b: e.indirect_dma_start(
                out=wall[:, :], out_offset=None, in_=WB[:, :], in_offset=bass.IndirectOffsetOnAxis(ap=blkI[:, b:b + 1], axis=0),
                bounds_check=P.reg(e, 65 * 128 - 1), oob_is_err=False)), (wsem.h, 16)))
            wB_.w = (wsem, wsem.n); wB_.r = []
            xb, xbB, xbs = xbr.next()
            P.dma("sp", xb[:], XG[b * 128:(b + 1) * 128, :], xbs, reads=xgBs, writes=[xbB])
            ptb, ptbB = ps_next()
            ptv = ptb[:].bitcast(BF16)
            for k in range(8):
                P.op("pe", lambda e, ptv=ptv, xb=xb, k=k: e.transpose(out=ptv[:, k * 128:(k + 1) * 128], in_=xb[:, k * 128:(k + 1) * 128], identity=identb[:]),
                     reads=[xbB, identbB], writes=[ptbB], sig=(k == 7))
            xgT, xgTB, _ = xgtr.next()
            P.op("dve", lambda e, xgT=xgT, ptv=ptv: e.tensor_copy(out=xgT[:].rearrange("p k s -> p (k s)"), in_=ptv), reads=[ptbB], writes=[xgTB])
            yield
            pg, pgB_ = ps_next(); pu, puB = ps_next()
            for k in range(8):
                P.op("pe", lambda e, pg=pg, wg_=wg_, k=k, xgT=xgT: e.matmul(pg[:, 0:256], xgT[:, k, :], wg_[:, k, :], start=(k == 0), stop=(k == 7)),
                     reads=[wB_, xgTB], writes=[pgB_], sig=(k == 7))
                P.op("pe", lambda e, pu=pu, wu_=wu_, k=k, xgT=xgT: e.matmul(pu[:, 0:256], xgT[:, k, :], wu_[:, k, :], start=(k == 0), stop=(k == 7)),
                     reads=[wB_, xgTB], writes=[puB], sig=(k == 7))
            sg, sgB, _ = sgr6.next(); atk, atkB, _ = atkr.next(); aT, aTB, _ = atr.next()
            P.op("act", lambda e, sg=sg, pg=pg: e.activation(out=sg[:], in_=pg[:, 0:256], func=AF.Silu), reads=[pgB_], writes=[sgB])
            P.op("dve", lambda e, atk=atk, sg=sg, pu=pu: e.tensor_tensor(out=atk[:], in0=pu[:, 0:256], in1=sg[:], op=ALU.mult), reads=[puB, sgB], writes=[atkB])
            yield
            pat, patB = ps_next()
            patv = pat[:].bitcast(BF16)
            for hh in range(2):
                P.op("pe", lambda e, patv=patv, atk=atk, hh=hh: e.transpose(out=patv[:, hh * 128:(hh + 1) * 128], in_=atk[:, hh * 128:(hh + 1) * 128], identity=identb[:]),
                     reads=[atkB, identbB], writes=[patB], sig=(hh == 1))
            P.op("act", lambda e, aT=aT, patv=patv: e.copy(out=aT[:].rearrange("p h s -> p (h s)"), in_=patv[:, 0:256]), reads=[patB], writes=[aTB])
            yield
            yb, ybB, ybs = ybr.next()
            for hf in range(2):
                py, pyB = ps_next()
                for hh in range(2):
                    P.op("pe", lambda e, py=py, aT=aT, wd_=wd_, hh=hh, hf=hf: e.matmul(py[:], aT[:, hh, :], wd_[:, hh, hf * 512:(hf + 1) * 512], start=(hh == 0), stop=(hh == 1)),
                         reads=[aTB, wB_], writes=[pyB], sig=(hh == 1))
                if hf == 0:
                    P.op("act", lambda e, yb=yb, py=py: e.copy(out=yb[:, 0:512], in_=py[:]), reads=[pyB], writes=[ybB])
                else:
                    P.op("dve", lambda e, yb=yb, py=py: e.tensor_copy(out=yb[:, 512:1024], in_=py[:]), reads=[pyB], writes=[ybB])
            P.dma("act", YG[b * 128:(b + 1) * 128, :], yb[:], ybs, reads=[ybB])

        live = []
        for b in range(NBLK + 4):
            if b < NBLK:
                live.append(block_g(b))
            nxt = []
            for g_ in live:
                try:
                    next(g_); nxt.append(g_)
                except StopIteration:
                    pass
            live = nxt
        P.barrier(); P.reset(m_persist)
        if debug:
            pass
        if phases < 9:
            P.emit(); return nc

        swg = P.tile([128, 8, 256], BF16, "swg"); swu = P.tile([128, 8, 256], BF16, "swu"); swd = P.tile([128, 2, D], BF16, "swd"); swB = Buf("sw"); s_sw = P.dsem("sw")
        P.dma("sp", swg[:].rearrange("p a b -> p (a b)"), WB[64 * 128:65 * 128, 0:2048], s_sw, reads=[wbB], writes=[swB])
        P.dma("sp", swu[:].rearrange("p a b -> p (a b)"), WB[64 * 128:65 * 128, 2048:4096], s_sw, reads=[wbB], writes=[swB])
        P.dma("sp", swd[:].rearrange("p a b -> p (a b)"), WB[64 * 128:65 * 128, 4096:6144], s_sw, reads=[wbB], writes=[swB])
        hxr = Ring(P, 2, [128, 8, 512], BF16, "hx6"); sg6 = Ring(P, 2, [128, 512], F32, "sgs", sem=False); aa6 = Ring(P, 2, [128, 512], BF16, "aas", sem=False)
        accS = P.tile([128, 8, 512], F32, "accS"); accSB = Buf("accS")
        ykr = Ring(P, 4, [128, D], F32, "yk"); atok = P.tile([128, D], F32, "atok"); atokB = Buf("atok")
        for t_ in ykr.t:
            P.op("pool", lambda e, t_=t_: e.memset(t_[:], 0.0), writes=[ykr.b[ykr.t.index(t_)]])
        x1l = Ring(P, 1, [128, 8, 512], F32, "x1l"); sq6 = P.tile([128, 8, 512], F32, "sq6"); sq6B = Buf("sq6")
        rstd6 = P.tile([128, 512], F32, "rstd6"); rs6B = Buf("rs6"); otr = Ring(P, 2, [128, 8, 512], F32, "outt")
        outT_v = outT.rearrange("(k p) t -> p k t", p=128)
        for i in range(8):
            t0 = i * 512
            hx_, hxB_, hxs_ = hxr.next()
            P.dma("sp", hx_[:], HX2_v[:, :, t0:t0 + 512], hxs_, writes=[hxB_])
            aas = []
            for hh in range(2):
                pg, pgB_ = ps_next(); pu, puB = ps_next()
                for k in range(8):
                    P.op("pe", lambda e, pg=pg, k=k, hh=hh, hx_=hx_: e.matmul(pg[:], swg[:, k, hh * 128:(hh + 1) * 128], hx_[:, k, :], start=(k == 0), stop=(k == 7)),
                         reads=[swB, hxB_], writes=[pgB_], sig=(k == 7))
                for k in range(8):
                    P.op("pe", lambda e, pu=pu, k=k, hh=hh, hx_=hx_: e.matmul(pu[:], swu[:, k, hh * 128:(hh + 1) * 128], hx_[:, k, :], start=(k == 0), stop=(k == 7)),
                         reads=[swB, hxB_], writes=[puB], sig=(k == 7))
                sg, sgB, _ = sg6.next(); aa, aaB, _ = aa6.next()
                P.op("act", lambda e, sg=sg, pg=pg: e.activation(out=sg[:], in_=pg[:], func=AF.Silu), reads=[pgB_], writes=[sgB])
                P.op("dve", lambda e, aa=aa, pu=pu, sg=sg: e.tensor_tensor(out=aa[:], in0=pu[:], in1=sg[:], op=ALU.mult), reads=[puB, sgB], writes=[aaB])
                aas.append((aa, aaB))
            for dc in range(8):
                pd, pdB = ps_next()
                for hh in range(2):
                    P.op("pe", lambda e, pd=pd, hh=hh, dc=dc, aa=aas[hh][0]: e.matmul(pd[:], swd[:, hh, dc * 128:(dc + 1) * 128], aa[:], start=(hh == 0), stop=(hh == 1)),
                         reads=[swB, aas[hh][1]], writes=[pdB], sig=(hh == 1))
                P.op("act", lambda e, pd=pd, dc=dc: e.copy(out=accS[:, dc, :], in_=pd[:]), reads=[pdB], writes=[accSB])
            for sb_ in range(4):
                st_ = i * 4 + sb_
                for k in range(8):
                    yk, ykB, yks = ykr.next()
                    waits = P._deps("pool", [metaB, ygB], [ykB])
                    yks.n += 16
                    P.q["pool"].append((waits, (lambda e, yk=yk, st_=st_, k=k: e.indirect_dma_start(
                        out=yk[:, :], out_offset=None, in_=YG[:, :], in_offset=bass.IndirectOffsetOnAxis(ap=slotI[:, st_ * 8 + k:st_ * 8 + k + 1], axis=0),
                        bounds_check=P.reg(e, NSLOT - 1), oob_is_err=False)), (yks.h, 16)))
                    ykB.w = (yks, yks.n); ykB.r = []
                    if k == 0:
                        P.op("dve", lambda e, yk=yk, st_=st_: e.tensor_scalar(out=atok[:], in0=yk[:], scalar1=gkA[:, st_, 0:1], scalar2=None, op0=ALU.mult),
                             reads=[ykB, metaB, atokB], writes=[atokB])
                    else:
                        P.op("dve", lambda e, yk=yk, st_=st_, k=k: e.scalar_tensor_tensor(out=atok[:], in0=yk[:], scalar=gkA[:, st_, k:k + 1], in1=atok[:], op0=ALU.mult, op1=ALU.add),
                             reads=[ykB, metaB, atokB], writes=[atokB])
                for kg in range(2):
                    ptk, ptkB = ps_next()
                    for kk in range(4):
                        k = kg * 4 + kk
                        P.op("pe", lambda e, ptk=ptk, k=k, kk=kk: e.transpose(out=ptk[:, kk * 128:(kk + 1) * 128], in_=atok[:, k * 128:(k + 1) * 128], identity=ident[:]),
                             reads=[atokB, identB], writes=[ptkB], sig=(kk == 3))
                    P.op("dve", lambda e, ptk=ptk, kg=kg, sb_=sb_: e.tensor_tensor(out=accS[:, kg * 4:(kg + 1) * 4, sb_ * 128:(sb_ + 1) * 128],
                                                                                in0=ptk[:].rearrange("p (k t) -> p k t", k=4),
                                                                                in1=accS[:, kg * 4:(kg + 1) * 4, sb_ * 128:(sb_ + 1) * 128], op=ALU.add),
                         reads=[ptkB, accSB], writes=[accSB])
            x1t_, x1B_, x1s_ = x1l.next()
            P.dma("sp", x1t_[:], X1_v[:, :, t0:t0 + 512], x1s_, writes=[x1B_])
            for k in range(8):
                P.op("dve", lambda e, k=k, x1t_=x1t_: e.scalar_tensor_tensor(out=x1t_[:, k, :], in0=accS[:, k, :], scalar=vec[:, 56 + k:57 + k], in1=x1t_[:, k, :],
                                                                           op0=ALU.mult, op1=ALU.add), reads=[accSB, vecB, x1B_], writes=[x1B_])
            rms_rstd(x1t_, x1B_, 512, sq=sq6, sqB=sq6B, rstd=rstd6, rsB=rs6B)
            P.op("dve", lambda e, x1t_=x1t_: e.tensor_tensor(out=sq6[:], in0=x1t_[:], in1=rstd6[:].unsqueeze(1).to_broadcast([128, 8, 512]), op=ALU.mult),
                 reads=[x1B_, rs6B, sq6B], writes=[sq6B])
            ot, otB, ots = otr.next()
            for k in range(8):
                P.op("act", lambda e, k=k, ot=ot: e.activation(out=ot[:, k, :], in_=sq6[:, k, :], func=AF.Copy, scale=vec[:, 68 + k:69 + k]),
                     reads=[sq6B, vecB], writes=[otB])
            P.dma("act", outT_v[:, :, t0:t0 + 512], ot[:], ots, reads=[otB])
        P.emit()
        nc._n_dsems = len(P.dsems)
    return nc


def prep_inputs(inputs):
    f = lambda a: np.ascontiguousarray(np.asarray(a, dtype=np.float32))
    x = f(inputs["x"]); ctx = f(inputs["ctx"]); c = f(inputs["c"]); c_ctx = f(inputs["c_ctx"])
    C, S = rope_tables()
    shared = {
        "ada_w": f(inputs["ada_w"][0]),
        "ada_bT": f(inputs["ada_b"][0].reshape(48, 128).T),
        "gvec": f(np.concatenate([np.asarray(inputs[k]).reshape(8, 128).T for k in ("norm1_g", "norm2_g", "final_norm_g")], axis=1)),
        "lamv": f(np.concatenate([np.asarray(inputs[k]).reshape(-1) for k in ("lam_q1", "lam_k1", "lam_q2", "lam_k2")])[None, :]),
        "w_in": f(inputs["w_in"][0]),
        "ropeC": C, "ropeS": S,
        "convw": f(np.asarray(inputs["hy_conv_w"][0]).reshape(3, 24, 128).transpose(2, 0, 1)),
        "convb": f(np.asarray(inputs["hy_conv_b"][0]).reshape(24, 128).T),
        "subln": f(np.asarray(inputs["subln_g"][0]).reshape(1, 128)),
        "sublnT": f(np.asarray(inputs["subln_g"][0]).reshape(128, 1)),
        "fw1": f(inputs["filt_w1"][0]), "fw2": f(inputs["filt_w2"][0]), "fw3": f(inputs["filt_w3"][0]),
        "fvec": f(np.stack([np.asarray(inputs[k][0]) for k in ("filt_b1", "filt_b2", "filt_freq")], axis=1)),
        "hyb": f(np.asarray(inputs["hy_bias"][0]).reshape(1, 2048)),
        "w_pa": f(inputs["w_branch_attn"][0]), "w_ph": f(inputs["w_branch_hyena"][0]), "w_o": f(inputs["w_out"][0]),
        "rw": f(inputs["router_w"][0]), "rbias": f(np.asarray(inputs["router_bias"][0]).reshape(1, 64)),
        "ewg": f(np.concatenate([inputs["exp_w_gate"][0], inputs["shared_w_gate"]], axis=0).reshape(65, 8, 128, 256).transpose(0, 2, 1, 3).reshape(65 * 128, 2048)),
        "ewu": f(np.concatenate([inputs["exp_w_up"][0], inputs["shared_w_up"]], axis=0).reshape(65, 8, 128, 256).transpose(0, 2, 1, 3).reshape(65 * 128, 2048)),
        "ewd": f(np.concatenate([inputs["exp_w_down"][0], inputs["shared_w_down"]], axis=0).reshape(65, 2, 128, 1024).transpose(0, 2, 1, 3).reshape(65 * 128, 2048)),
        "bvals": f((np.arange(NBLK) * 128.0).reshape(1, NBLK)), "pcol": f(np.arange(128.0).reshape(128, 1)),
    }
    shared.update(hyena_tables())
    maps = []
    for b in range(8):
        m = dict(shared)
        m["xT"] = np.ascontiguousarray(np.concatenate([x[b].T, ctx[b].T], axis=1))
        m["cc"] = np.ascontiguousarray(np.stack([c[b].reshape(8, 128).T, c_ctx.reshape(8, 128).T], axis=-1))
        maps.append(m)
    return maps


def kernel(**inputs):
    nc = build(DEBUG, PHASES)
    maps = prep_inputs(inputs)
    res = run_bass_kernel_spmd(nc, maps, core_ids=list(range(8)))
    out = np.stack([np.asarray(r["outT"]).T for r in res.results], axis=0)
    return np.ascontiguousarray(out.astype(np.float32))
```
